# Optimizing a Trainium2 kernel written in Bass

```python
import jax, jax.numpy as jnp
from jax import lax
import numpy as np


D_MODEL = 1024
BATCH = 8
SEQ = 2048
DEPTH = 1

HG_HEADS = 4
HG_DK = 128
HG_DV = 128
HG_WIDTH = HG_HEADS * HG_DV
HG_CHUNK = 64

MLA_HEADS = 4
MLA_Q_LORA = 384
MLA_KV_LORA = 256
MLA_NOPE = 128
MLA_ROPE = 64
MLA_QK = MLA_NOPE + MLA_ROPE
MLA_V = 128
MLA_WIDTH = MLA_HEADS * MLA_V
ATTN_BLOCK = 128
ROPE_THETA = 10000.0

MIX_WIDTH = HG_WIDTH + MLA_WIDTH
IN_SPLITS = (HG_HEADS * HG_DK, HG_HEADS * HG_DK, HG_HEADS * HG_DK, HG_WIDTH, HG_WIDTH,
             MLA_Q_LORA, MLA_KV_LORA, MLA_ROPE)
IN_WIDTH = 3 * HG_HEADS * HG_DK + 2 * HG_WIDTH + MLA_Q_LORA + MLA_KV_LORA + MLA_ROPE

PEER_HEADS = 8
PEER_NKEYS = 128
PEER_EXPERTS = PEER_NKEYS * PEER_NKEYS
PEER_TOPK = 16
PEER_DHALF = 128
PEER_DKEY = 2 * PEER_DHALF
PEER_BLOCK = 128

EPS = 1e-6

kernel_name = 'hybrid_hgrn2_mla_peer_encoder'


def rms_norm(x, gain):
    x32 = x.astype(jnp.float32)
    y = x32 * lax.rsqrt(jnp.mean(x32 * x32, axis=-1, keepdims=True) + EPS)
    return (y * gain.astype(jnp.float32)).astype(x.dtype)


def rope_tables(positions):
    inv_freq = 1.0 / (ROPE_THETA ** (jnp.arange(0, MLA_ROPE, 2, dtype=jnp.float32) / MLA_ROPE))
    ang = positions.astype(jnp.float32)[..., None] * inv_freq
    return jnp.cos(ang)[:, :, None, :], jnp.sin(ang)[:, :, None, :]


def apply_rope(t, cos, sin):
    half = MLA_ROPE // 2
    c = cos.astype(t.dtype)
    s = sin.astype(t.dtype)
    t1, t2 = t[..., :half], t[..., half:]
    return jnp.concatenate([t1 * c - t2 * s, t2 * c + t1 * s], axis=-1)


def hgrn2_chunk_scan(q, k, v, log_f):
    B, S, H, DK = q.shape
    DV = v.shape[-1]
    n = S // HG_CHUNK

    def to_chunks(t):
        return t.reshape(B, n, HG_CHUNK, H, t.shape[-1]).transpose(1, 0, 3, 2, 4)

    mask = jnp.tril(jnp.ones((HG_CHUNK, HG_CHUNK), dtype=bool))[:, :, None]

    def step(state, inp):
        qb, kb, vb, gb = inp
        b = jnp.cumsum(gb, axis=2)
        diff = b[:, :, :, None, :] - b[:, :, None, :, :]
        decay = jnp.exp(jnp.where(mask, diff, -jnp.inf))
        scores = jnp.einsum('bhtk,bhsk,bhtsk->bhts', qb, kb, decay)
        o = jnp.einsum('bhts,bhsv->bhtv', scores, vb) \
            + jnp.einsum('bhtk,bhkv->bhtv', qb * jnp.exp(b), state)
        b_last = b[:, :, -1:, :]
        new_state = jnp.exp(b_last[:, :, 0, :])[..., None] * state \
            + jnp.einsum('bhsk,bhsv->bhkv', kb * jnp.exp(b_last - b), vb)
        return new_state, o

    state0 = jnp.zeros((B, H, DK, DV), jnp.float32)
    _, o = lax.scan(step, state0, (to_chunks(q), to_chunks(k), to_chunks(v), to_chunks(log_f)))
    return o.transpose(1, 0, 3, 2, 4).reshape(B, S, H, DV)


def hgrn2_mixer(q_raw, f_fwd_raw, f_bwd_raw, i_raw, g_raw, lb, o_gain):
    B, S, _ = q_raw.shape
    q = jax.nn.silu(q_raw.reshape(B, S, HG_HEADS, HG_DK).astype(jnp.float32))
    v = i_raw.reshape(B, S, HG_HEADS, HG_DV).astype(jnp.float32)

    def direction(f_raw, lb_dir, reverse):
        lbh = lb_dir.reshape(HG_HEADS, HG_DK)
        f = lbh + (1.0 - lbh) * jax.nn.sigmoid(f_raw.reshape(B, S, HG_HEADS, HG_DK).astype(jnp.float32))
        k = 1.0 - f
        log_f = jnp.log(f)
        if reverse:
            o = hgrn2_chunk_scan(q[:, ::-1], k[:, ::-1], v[:, ::-1], log_f[:, ::-1])
            return o[:, ::-1]
        return hgrn2_chunk_scan(q, k, v, log_f)

    o = direction(f_fwd_raw, lb[0], False) + direction(f_bwd_raw, lb[1], True)
    gate = jax.nn.silu(g_raw.reshape(B, S, HG_HEADS, HG_DV).astype(jnp.float32))
    o = rms_norm(o, o_gain).astype(jnp.float32) * gate
    return o.reshape(B, S, HG_WIDTH).astype(q_raw.dtype)


def bidirectional_attention(q, k, v):
    B, S, H, DQ = q.shape
    nb = S // ATTN_BLOCK
    scale = DQ ** -0.5
    qb = q.reshape(B, nb, ATTN_BLOCK, H, DQ).transpose(1, 0, 2, 3, 4)

    def one_block(q_blk):
        s = jnp.einsum('bqhd,bkhd->bhqk', q_blk, k).astype(jnp.float32) * scale
        p = jax.nn.softmax(s, axis=-1).astype(v.dtype)
        return jnp.einsum('bhqk,bkhd->bqhd', p, v)

    o = lax.map(one_block, qb)
    return o.transpose(1, 0, 2, 3, 4).reshape(B, S, H, v.shape[-1])


def mla_mixer(cq_raw, ckv_raw, kr_raw, cos, sin, q_a_gain, w_q_up, kv_a_gain, w_kv_up,
              q_gain, k_gain, o_gain):
    B, S, _ = cq_raw.shape
    q = (rms_norm(cq_raw, q_a_gain) @ w_q_up).reshape(B, S, MLA_HEADS, MLA_QK)
    kv = (rms_norm(ckv_raw, kv_a_gain) @ w_kv_up).reshape(B, S, MLA_HEADS, MLA_NOPE + MLA_V)
    k_nope, v = kv[..., :MLA_NOPE], kv[..., MLA_NOPE:]
    q_nope = rms_norm(q[..., :MLA_NOPE], q_gain[:MLA_NOPE])
    q_rope = apply_rope(rms_norm(q[..., MLA_NOPE:], q_gain[MLA_NOPE:]), cos, sin)
    k_nope = rms_norm(k_nope, k_gain[:MLA_NOPE])
    k_rope = apply_rope(rms_norm(kr_raw.reshape(B, S, 1, MLA_ROPE), k_gain[MLA_NOPE:]), cos, sin)
    k_rope = jnp.broadcast_to(k_rope, (B, S, MLA_HEADS, MLA_ROPE))
    qf = jnp.concatenate([q_nope, q_rope], axis=-1)
    kf = jnp.concatenate([k_nope, k_rope], axis=-1)
    o = bidirectional_attention(qf, kf, v)
    o = rms_norm(o, o_gain)
    return o.reshape(B, S, MLA_WIDTH)


def peer_ffn(h, w_q, sub_keys, u_tab, v_tab):
    B, S, D = h.shape
    q = (h @ w_q).reshape(B, S, PEER_HEADS, 2, PEER_DHALF)
    scores = jnp.einsum('bspcd,pcnd->bspcn', q, sub_keys).astype(jnp.float32)
    s_top, i_top = lax.top_k(scores, PEER_TOPK)
    cand_s = (s_top[..., 0, :, None] + s_top[..., 1, None, :]).reshape(B, S, PEER_HEADS, PEER_TOPK * PEER_TOPK)
    cand_i = (i_top[..., 0, :, None] * PEER_NKEYS + i_top[..., 1, None, :]).reshape(B, S, PEER_HEADS, PEER_TOPK * PEER_TOPK)
    best_s, best_pos = lax.top_k(cand_s, PEER_TOPK)
    idx = jnp.take_along_axis(cand_i, best_pos, axis=-1)
    gates = jax.nn.softmax(best_s, axis=-1).astype(h.dtype)

    nb = (B * S) // PEER_BLOCK
    hb = h.reshape(nb, PEER_BLOCK, D)
    ib = idx.reshape(nb, PEER_BLOCK, PEER_HEADS, PEER_TOPK)
    gb = gates.reshape(nb, PEER_BLOCK, PEER_HEADS, PEER_TOPK)

    def one_block(args):
        hx, ix, gx = args
        act = jax.nn.gelu(jnp.einsum('td,tpkd->tpk', hx, u_tab[ix]), approximate=False)
        return jnp.einsum('tpk,tpkd->td', gx * act, v_tab[ix])

    y = lax.map(one_block, (hb, ib, gb))
    return y.reshape(B, S, D)


def setup_inputs(seed: int = 0) -> dict:
    key = jax.random.key(seed)
    ks = jax.random.split(key, 24)
    f32 = jnp.float32
    L = DEPTH

    def nrm(k, shape, scale):
        return jax.random.normal(k, shape, f32) * scale

    def gain(k, shape):
        return 1.0 + 0.02 * jax.random.normal(k, shape, f32)

    x = nrm(ks[0], (BATCH, SEQ, D_MODEL), 1.0)
    positions = (jnp.arange(SEQ, dtype=jnp.int32)[None, :]
                 + jax.random.randint(ks[1], (BATCH, 1), 0, 1024, dtype=jnp.int32))
    return {
        'x': x,
        'positions': positions,
        'attn_norm': gain(ks[2], (L, D_MODEL)),
        'w_in': nrm(ks[3], (L, D_MODEL, IN_WIDTH), D_MODEL ** -0.5),
        'hg_lb_logits': nrm(ks[4], (DEPTH + 1, 2, HG_HEADS * HG_DK), 0.5),
        'hg_o_norm': gain(ks[5], (L, HG_HEADS, HG_DV)),
        'q_a_norm': gain(ks[6], (L, MLA_Q_LORA)),
        'w_q_up': nrm(ks[7], (L, MLA_Q_LORA, MLA_HEADS * MLA_QK), MLA_Q_LORA ** -0.5),
        'kv_a_norm': gain(ks[8], (L, MLA_KV_LORA)),
        'w_kv_up': nrm(ks[9], (L, MLA_KV_LORA, MLA_HEADS * (MLA_NOPE + MLA_V)), MLA_KV_LORA ** -0.5),
        'q_norm': gain(ks[10], (L, MLA_QK)),
        'k_norm': gain(ks[11], (L, MLA_QK)),
        'mla_o_norm': gain(ks[12], (L, MLA_HEADS, MLA_V)),
        'w_out': nrm(ks[13], (L, MIX_WIDTH, D_MODEL), MIX_WIDTH ** -0.5),
        'ffn_norm': gain(ks[14], (L, D_MODEL)),
        'peer_w_q': nrm(ks[15], (L, D_MODEL, PEER_HEADS * PEER_DKEY), D_MODEL ** -0.5),
        'peer_sub_keys': nrm(ks[16], (L, PEER_HEADS, 2, PEER_NKEYS, PEER_DHALF), PEER_DHALF ** -0.5),
        'peer_u': nrm(ks[17], (L, PEER_EXPERTS, D_MODEL), D_MODEL ** -0.5),
        'peer_v': nrm(ks[18], (L, PEER_EXPERTS, D_MODEL), 0.3),
    }


def reference(x, positions, attn_norm, w_in, hg_lb_logits, hg_o_norm, q_a_norm, w_q_up,
              kv_a_norm, w_kv_up, q_norm, k_norm, mla_o_norm, w_out, ffn_norm,
              peer_w_q, peer_sub_keys, peer_u, peer_v):
    lower_bounds = jnp.cumsum(jax.nn.softmax(hg_lb_logits.astype(jnp.float32), axis=0), axis=0)
    cos, sin = rope_tables(positions)
    offsets = []
    acc = 0
    for w in IN_SPLITS[:-1]:
        acc += w
        offsets.append(acc)
    for l in range(DEPTH):
        h = rms_norm(x, attn_norm[l])
        proj = h @ w_in[l]
        hq, hf_fwd, hf_bwd, hi, hg, cq, ckv, kr = jnp.split(proj, offsets, axis=-1)
        y_hg = hgrn2_mixer(hq, hf_fwd, hf_bwd, hi, hg, lower_bounds[l], hg_o_norm[l])
        y_mla = mla_mixer(cq, ckv, kr, cos, sin, q_a_norm[l], w_q_up[l], kv_a_norm[l],
                          w_kv_up[l], q_norm[l], k_norm[l], mla_o_norm[l])
        x = x + jnp.concatenate([y_hg, y_mla], axis=-1) @ w_out[l]
        x = x + peer_ffn(rms_norm(x, ffn_norm[l]), peer_w_q[l], peer_sub_keys[l],
                         peer_u[l], peer_v[l])
    return x
```

```python
import numpy as np
from contextlib import ExitStack
import concourse.bass as bass
import concourse.mybir as mybir
from concourse.bass_utils import run_bass_kernel_spmd

F32 = mybir.dt.float32
BF = mybir.dt.bfloat16
I32 = mybir.dt.int32
AF = mybir.ActivationFunctionType
ALU = mybir.AluOpType
AX = mybir.AxisListType

P = 128
T = 2048
NT = 16
D = 1024
EPS = 1e-6
NEXP = 16384
EG = 512
NG = NEXP // EG
IC = 16
NIC = 128 // IC
PI = float(np.pi)


class Buf:
    def __init__(self, name):
        self.name = name
        self.writer = None
        self.readers = []
        self.dsem = None
        self.dcnt = 0


class Sch:
    def __init__(self, nc):
        self.nc = nc
        self.eng = dict(pe=nc.tensor, dve=nc.vector, act=nc.scalar, pool=nc.gpsimd, sp=nc.sync)
        self.sem = {e: nc.alloc_semaphore("sem_" + e) for e in ("pe", "dve", "act", "pool")}
        self.cnt = {e: 0 for e in self.sem}
        self.seen = {e: {} for e in self.eng}
        self.dbufs = []

    def _wait(self, e, dep):
        key, h, v = dep
        if self.seen[e].get(key, 0) >= v:
            return
        self.eng[e].wait_ge(h, v)
        self.seen[e][key] = v

    def _deps(self, e, reads, writes):
        deps = []
        for b in reads:
            if b.writer is not None:
                deps.append(b.writer)
        for b in writes:
            if b.writer is not None:
                deps.append(b.writer)
            deps.extend(b.readers)
        for d in deps:
            if e == "pe" and d[0] == "pe":
                continue
            self._wait(e, d)

    def _mark(self, tok, reads, writes):
        for b in reads:
            b.readers.append(tok)
        for b in writes:
            b.writer = tok
            b.readers = []

    def op(self, e, fn, reads=(), writes=()):
        self._deps(e, reads, writes)
        ins = fn()
        self.cnt[e] += 1
        ins.then_inc(self.sem[e], 1)
        self.seen[e][e] = max(self.seen[e].get(e, 0), 0)
        self._mark((e, self.sem[e], self.cnt[e]), reads, writes)

    def dma(self, q, out, in_, sb, reads=(), writes=()):
        self._deps(q, reads, writes)
        if sb.dsem is None:
            sb.dsem = self.nc.alloc_semaphore("dsem_" + sb.name)
            self.dbufs.append(sb)
        ins = self.eng[q].dma_start(out=out, in_=in_)
        sb.dcnt += 16
        ins.then_inc(sb.dsem, 16)
        self._mark((("d", sb.name), sb.dsem, sb.dcnt), reads, writes)

    def barrier(self):
        for e in self.eng:
            for f in self.sem:
                if f != e and self.cnt[f] > 0:
                    self._wait(e, (f, self.sem[f], self.cnt[f]))
            for b in self.dbufs:
                if b.dcnt > 0:
                    self._wait(e, (("d", b.name), b.dsem, b.dcnt))


class Mem:
    def __init__(self, lo, hi):
        self.free = [(lo, hi)]

    def alloc(self, n):
        n = (n + 63) // 64 * 64
        for k, (a, b) in enumerate(self.free):
            if b - a >= n:
                self.free[k] = (a + n, b)
                return a, n
        raise MemoryError("SBUF arena exhausted (%d bytes) free=%s" % (n, self.free))

    def release(self, a, n):
        fl = sorted(self.free + [(a, a + n)])
        out = []
        for lo, hi in fl:
            if out and out[-1][1] >= lo:
                out[-1] = (out[-1][0], max(out[-1][1], hi))
            elif hi > lo:
                out.append((lo, hi))
        self.free = out


class Scope:
    def __init__(self, mem):
        self.mem = mem
        self.items = []

    def __enter__(self):
        return self

    def __exit__(self, *a):
        self.close()
        return False

    def close(self):
        for a, n in self.items:
            self.mem.release(a, n)
        self.items = []


DT_BYTES = {}


def vap(base, dims, off=0):
    return bass.AP(base.tensor, base.offset + off, [list(base.ap[0])] + [list(d) for d in dims])


def build_program(debug=None, upto=None):
    debug = debug or {}
    nc = bass.Bass("TRN2", target_bir_lowering=False)
    S = Sch(nc)

    def din(name, shape, dt=F32):
        return nc.dram_tensor(name, list(shape), dt, kind="ExternalInput").ap()

    x_d = din("x", [T, D])
    pos_d = din("posT", [P, NT], I32)
    invf_d = din("invf", [P, 32])
    ident_d = din("ident", [P, P])
    maskf_d = din("maskf", [P, P])
    maskb_d = din("maskb", [P, P])
    reset_d = din("resetm", [P, T])
    gattn_d = din("g_attn", [P, 8])
    gffn_d = din("g_ffn", [P, 8])
    lbl_d = din("lbl", [P, 16])
    ghgo_d = din("g_hgo", [P, 4])
    gqa_d = din("g_qa", [P, 3])
    gkva_d = din("g_kva", [P, 2])
    gq_d = din("g_q", [P, 192])
    gk_d = din("g_k", [P, 192])
    gmo_d = din("g_mo", [P, 512])
    iota_d = din("iota", [P, 160])
    win_d = din("w_in", [P, 8, 3264])
    wqup_d = din("w_qup", [P, 3, 768])
    wkvup_d = din("w_kvup", [P, 2, 1024])
    wout_d = din("w_out", [P, 8, 1024])
    wqT_d = din("wqT", [P, 16, 1024])
    keysT_d = din("keysT", [P, 16, 128])
    UT_d = din("UT", [P, 8, NEXP])
    V_d = din("V", [NEXP, D])
    out_d = nc.dram_tensor("out", [T, D], F32, kind="ExternalOutput").ap()
    Wd = nc.dram_tensor("Wd", [NT, P, NEXP], BF).ap()
    dbg_out = {}
    for k, (shp, dt_) in debug.items():
        dbg_out[k] = nc.dram_tensor("dbg_" + k, list(shp), dt_, kind="ExternalOutput").ap()

    es = ExitStack()
    mem = Mem(16512 + 64, 229344 - 64)
    root = Scope(mem)

    def sb(name, shape, dt=F32, stack=None):
        nbytes = int(np.prod(shape[1:])) * (4 if dt in (F32, I32, mybir.dt.uint32) else 2)
        a, n = mem.alloc(nbytes)
        (stack or root).items.append((a, n))
        addr_of[name] = a
        return nc.alloc_sbuf_tensor_at(name, list(shape), dt, offset=a)

    addr_of = {}

    def sb_alias(name, shape, dt, like):
        return nc.alloc_sbuf_tensor_at(name, list(shape), dt, offset=addr_of[like])

    psf_all = es.enter_context(nc.psum_tensor("psf_all", [P, 6, 512], F32))
    psf = [psf_all[:, i, :] for i in range(6)]
    psb = [es.enter_context(nc.psum_tensor("psb%d" % i, [P, 1024], BF)) for i in range(2)]
    psf_b = [Buf("psf%d" % i) for i in range(6)]
    psb_b = [Buf("psb%d" % i) for i in range(2)]

    cb = Buf("consts")
    pEarly = Scope(mem)
    ident_f = sb("ident_f", [P, P], F32, pEarly)
    ident = sb("ident", [P, P], BF)
    maskf = sb("maskf", [P, P], F32, pEarly)
    maskb = sb("maskb", [P, P], F32, pEarly)
    resetm = sb("resetm", [P, T], F32, pEarly)
    invf = sb("invf", [P, 32])
    posT = sb("posT", [P, NT], I32)
    g_attn = sb("g_attn", [P, 8])
    g_ffn = sb("g_ffn", [P, 8])
    lbl = sb("lbl", [P, 16])
    g_hgo = sb("g_hgo", [P, 4])
    g_qa = sb("g_qa", [P, 3])
    g_kva = sb("g_kva", [P, 2])
    g_q = sb("g_q", [P, 192], F32, pEarly)
    g_k = sb("g_k", [P, 192], F32, pEarly)
    g_mo = sb("g_mo", [P, 512], F32, pEarly)
    ones_bf = sb("ones_bf", [P, P], BF)
    iota_f = sb("iota_f", [P, 160])
    iota128 = sb("iota128", [P, P], BF)
    for dst, src in ((ident_f, ident_d), (maskf, maskf_d), (maskb, maskb_d), (resetm, reset_d),
                     (invf, invf_d), (posT, pos_d), (g_attn, gattn_d), (g_ffn, gffn_d), (lbl, lbl_d),
                     (g_hgo, ghgo_d), (g_qa, gqa_d), (g_kva, gkva_d), (g_q, gq_d), (g_k, gk_d),
                     (g_mo, gmo_d), (iota_f, iota_d)):
        S.dma("sp", dst[:], src, cb, writes=[cb])
    cc = Buf("consts2")
    S.op("dve", lambda: nc.vector.tensor_copy(ident[:], ident_f[:]), [cb], [cc])
    S.op("dve", lambda: nc.vector.memset(ones_bf[:], 1.0), [], [cc])
    S.op("dve", lambda: nc.vector.tensor_copy(iota128[:], iota_f[:, 0:128]), [cb], [cc])
    lb = sb("lb", [P, 8])
    oml = sb("oml", [P, 8])
    noml = sb("noml", [P, 8])
    S.op("dve", lambda: nc.vector.tensor_sub(lb[:], lbl[:, 0:8], lbl[:, 8:16]), [cb], [cc])
    S.op("act", lambda: nc.scalar.activation(lb[:], lb[:], AF.Sigmoid), [cc], [cc])
    S.op("dve", lambda: nc.vector.tensor_scalar(oml[:], lb[:], -1.0, 1.0, ALU.mult, ALU.add), [cc], [cc])
    S.op("dve", lambda: nc.vector.tensor_scalar(noml[:], oml[:], -1.0, None, ALU.mult), [cc], [cc])
    cosT = sb("cosT", [P, NT, 32], F32, pEarly)
    sinT = sb("sinT", [P, NT, 32], F32, pEarly)
    with Scope(mem) as st:
        posf = sb("posf", [P, NT], F32, st)
        ang = sb("ang", [P, NT, 32], F32, st)
        ang2 = sb("ang2", [P, NT, 32], F32, st)
        S.op("dve", lambda: nc.vector.tensor_copy(posf[:], posT[:]), [cb], [cc])
        S.op("dve", lambda: nc.vector.tensor_tensor(
            ang[:], vap(posf[:], [[1, NT], [0, 32]]), vap(invf[:], [[0, NT], [1, 32]]), ALU.mult), [cc, cb], [cc])
        ri = sb("ri", [P, NT, 32], I32, st)
        rf = sb("rf", [P, NT, 32], F32, st)
        hi = sb("hi", [P, NT, 32], F32, st)
        S.op("dve", lambda: nc.vector.tensor_scalar(ang[:], ang[:], 1.0 / (2 * PI), None, ALU.mult), [cc], [cc])
        S.op("dve", lambda: nc.vector.tensor_scalar(ang2[:], ang[:], 0.25, None, ALU.add), [cc], [cc])
        for src, dst in ((ang, sinT), (ang2, cosT)):
            S.op("dve", lambda: nc.vector.tensor_copy(ri[:], src[:]), [cc], [cc])
            S.op("dve", lambda: nc.vector.tensor_copy(rf[:], ri[:]), [cc], [cc])
            S.op("dve", lambda: nc.vector.tensor_sub(src[:], src[:], rf[:]), [cc], [cc])
            S.op("dve", lambda: nc.vector.tensor_scalar(hi[:], src[:], 0.5, None, ALU.is_gt), [cc], [cc])
            S.op("dve", lambda: nc.vector.tensor_sub(src[:], src[:], hi[:]), [cc], [cc])
            S.op("dve", lambda: nc.vector.tensor_scalar(hi[:], src[:], -0.5, None, ALU.is_lt), [cc], [cc])
            S.op("dve", lambda: nc.vector.tensor_add(src[:], src[:], hi[:]), [cc], [cc])
            S.op("act", lambda: nc.scalar.activation(dst[:], src[:], AF.Sin, scale=2 * PI), [cc], [cc])
        S.barrier()
    epsb = sb("epsb", [P, 1])
    S.op("dve", lambda: nc.vector.memset(epsb[:], EPS), [], [cc])
    if upto == "0":
        S.barrier()
        return nc

    def dump(name, src_ap, rbufs):
        if name in dbg_out:
            S.barrier()
            tb = Buf("dbg_" + name)
            S.dma("sp", dbg_out[name], src_ap, tb, reads=rbufs)
            S.barrier()

    def rstd_from_ss(dst, ss, n, bufs):
        S.op("act", lambda: nc.scalar.activation(dst, ss, AF.Sqrt, bias=epsb[:dst.shape[0]], scale=1.0 / n), bufs + [cc], bufs)
        S.op("dve", lambda: nc.vector.reciprocal(dst, dst), bufs, bufs)

    pM = Scope(mem)
    mixT = sb("mixT", [P, 8, T], BF, pM)
    mix_b = Buf("mixT")
    ph1 = Scope(mem)
    xnT = sb("xnT", [P, 8, T], BF, ph1)
    xnT_b = Buf("xnT")
    stg = [sb("stg0", [P, 8, 512], F32, ph1)] * 2
    stg_b = [Buf("stg0")] * 2
    with Scope(mem) as st:
        xt = [sb("xt%d" % i, [P, D], F32, st) for i in range(2)]
        xt_b = [Buf("xt%d" % i) for i in range(2)]
        xn = [sb("xn%d" % i, [P, D], BF, st) for i in range(2)]
        xn_b = [Buf("xn%d" % i) for i in range(2)]
        junk = sb("junkA", [P, D], BF, st)
        junk_b = Buf("junkA")
        ssA = sb("ssA", [P, NT], F32, st)
        ssA_b = [Buf("ssA%d" % i) for i in range(NT)]
        for i in range(NT):
            j = i % 2
            S.dma("sp", xt[j][:], x_d[i * P:(i + 1) * P, :], xt_b[j], writes=[xt_b[j]])
            S.op("act", lambda: nc.scalar.activation(junk[:], xt[j][:], AF.Square, accum_out=ssA[:, i:i + 1]),
                 [xt_b[j]], [junk_b, ssA_b[i]])
            rstd_from_ss(ssA[:, i:i + 1], ssA[:, i:i + 1], D, [ssA_b[i]])
            S.op("dve", lambda: nc.vector.tensor_scalar(xn[j][:], xt[j][:], ssA[:, i:i + 1], None, ALU.mult),
                 [xt_b[j], ssA_b[i]], [xn_b[j]])
            pb = psb_b[i % 2]
            for c in range(8):
                S.op("pe", lambda: nc.tensor.transpose(psb[i % 2][:, c * P:(c + 1) * P], xn[j][:, c * P:(c + 1) * P], ident[:]),
                     [xn_b[j], cc], [pb])
            S.op("act", lambda: nc.scalar.copy(
                xnT[:, :, i * P:(i + 1) * P], vap(psb[i % 2][:], [[P, 8], [1, P]])), [pb], [xnT_b])
        S.barrier()

    if upto == "A":
        return nc
    def load_wslice(dst, dst_b, src_d, nchunks, cols, gain, slot):
        sg, sgb = stg[slot], stg_b[slot]
        o = 0
        for (c0, w) in cols:
            S.dma("sp", sg[:, 0:nchunks, o:o + w], src_d[:, :, c0:c0 + w], sgb, writes=[sgb])
            o += w
        for c in range(nchunks):
            if gain is not None:
                S.op("dve", lambda: nc.vector.tensor_scalar(dst[:, c, 0:o], sg[:, c, 0:o], gain[:, c:c + 1], None, ALU.mult),
                     [sgb, cb], [dst_b])
            else:
                S.op("dve", lambda: nc.vector.tensor_copy(dst[:, c, 0:o], sg[:, c, 0:o]), [sgb], [dst_b])

    def proj_fm(ps_ap, ps_b, w, w_b, col0, width, t0, n, src=None, src_b=None, nch=8):
        src = xnT if src is None else src
        src_b = xnT_b if src_b is None else src_b
        for c in range(nch):
            S.op("pe", lambda: nc.tensor.matmul(ps_ap, w[:, c, col0:col0 + width], src[:, c, t0:t0 + n],
                                                start=(c == 0), stop=(c == nch - 1), skip_group_check=True),
                 [w_b, src_b], [ps_b])

    with Scope(mem) as st:
        vtok = sb("vtok", [P, NT, 512], BF, st)
        vtok_b = Buf("vtok")
        wv = sb("wv", [P, 8, 512], BF, st)
        wv_b = Buf("wv")
        load_wslice(wv, wv_b, win_d, 8, [(1536, 512)], g_attn, 0)
        for i in range(NT):
            k = i % 2
            for c in range(8):
                S.op("pe", lambda: nc.tensor.matmul(psf[k][:], xnT[:, c, i * P:(i + 1) * P], wv[:, c, :],
                                                    start=(c == 0), stop=(c == 7), skip_group_check=True),
                     [xnT_b, wv_b], [psf_b[k]])
            S.op("act", lambda: nc.scalar.copy(vtok[:, i, :], psf[k][:]), [psf_b[k]], [vtok_b])
        wh = [wv] * 2
        wh_b = [wv_b] * 2
        if upto == "B1":
            S.barrier()
            return nc
        H = 1024
        qs = sb("qs", [P, T], F32, st)
        gsl = sb("gsl", [P, T], BF, st)
        glog = sb("glog", [P, H], F32, st)
        bcum = sb("bcum", [P, H], F32, st)
        kk = sb("kk", [P, H], F32, st)
        e1 = sb("e1", [P, H], F32, st)
        e2 = glog
        Qt = sb("Qt", [P, 2, T], BF, st)
        Kt = sb("Kt", [P, 2, T], BF, st)
        Ktok = sb("Ktok", [P, 2, NT, P], BF, st)
        Sbf = sb("Sbf", [P, 2, 32, P], BF, st)
        vm = sb("vm", [P, NT, 2, P], BF, st)
        vm_b = Buf("vm")
        Sm = [sb("Sm%d" % i, [P, 2, P], F32, st) for i in range(2)]
        dSd = sb("dSd", [P, 2, P], F32, st)
        dch = sb("dch", [P, 2, 32], F32, st)
        Pm = [sb("Pm%d" % i, [P, 2, P], BF, st) for i in range(2)]
        osq = sb("osq", [P, 512], BF, st)
        rbc = kk
        otmp = e1
        hb = Buf("hg_elem")
        qk_b = Buf("QtKt")
        ktok_b = Buf("Ktok")
        sbf_b = Buf("Sbf")
        sm_b = Buf("Sm")
        pm_b = [Buf("Pm0"), Buf("Pm1")]
        ob = Buf("onorm")
        for h in range(4):
            w = wh[h % 2]
            wb = wh_b[h % 2]
            load_wslice(w, wb, win_d, 8, [(h * P, P), (512 + h * P, P), (1024 + h * P, P), (2048 + h * P, P)],
                        g_attn, (h + 1) % 2)
            for tb in range(4):
                k = tb % 2
                proj_fm(psf[k][:], psf_b[k], w, wb, 0, P, tb * 512, 512)
                S.op("act", lambda: nc.scalar.activation(qs[:, tb * 512:(tb + 1) * 512], psf[k][:], AF.Silu),
                     [psf_b[k]], [hb])
            for tb in range(4):
                k = tb % 2
                proj_fm(psf[k][:], psf_b[k], w, wb, 384, P, tb * 512, 512)
                S.op("act", lambda: nc.scalar.activation(gsl[:, tb * 512:(tb + 1) * 512], psf[k][:], AF.Silu),
                     [psf_b[k]], [hb])
            dch_b = [Buf("dch0"), Buf("dch1")]
            ktk_b = [Buf("ktok0"), Buf("ktok1")]
            sbf_bb = [[Buf("sbf%d_%d" % (d_, c_)) for c_ in range(32)] for d_ in range(2)]
            ch_b = [[Buf("ch%d_%d" % (d_, q_)) for q_ in range(2)] for d_ in range(2)]
            dsd_b = [Buf("dsd0"), Buf("dsd1")]
            smd_b = [[Buf("smd%d_%d" % (d_, q_)) for q_ in range(2)] for d_ in range(2)]

            def elem(d):
                col = d * 4 + h
                for hf in range(2):
                    t0 = hf * H
                    for tb in range(2):
                        k = tb % 2
                        proj_fm(psf[k][:], psf_b[k], w, wb, (1 + d) * P, P, t0 + tb * 512, 512)
                        S.op("act", lambda: nc.scalar.activation(e1[:, tb * 512:(tb + 1) * 512], psf[k][:], AF.Sigmoid),
                             [psf_b[k]], [hb])
                    yield
                    S.op("act", lambda: nc.scalar.activation(glog[:], e1[:], AF.Ln, bias=lb[:, col:col + 1],
                                                             scale=oml[:, col:col + 1]), [hb, cc], [hb])
                    S.op("dve", lambda: nc.vector.tensor_scalar(kk[:], e1[:], noml[:, col:col + 1], oml[:, col:col + 1],
                                                                ALU.mult, ALU.add), [hb, cc], [hb])
                    S.op("dve", lambda: nc.vector.tensor_tensor_scan(bcum[:], resetm[:, t0:t0 + H], glog[:], 0.0,
                                                                      ALU.mult, ALU.add), [hb, cb], [hb])
                    S.op("act", lambda: nc.scalar.activation(dch[:, d, hf * 16:(hf + 1) * 16],
                                                             vap(bcum[:], [[64, 16]], off=63), AF.Exp), [hb], [dch_b[d]])
                    yield
                    if d == 1:
                        S.op("dve", lambda: nc.vector.tensor_sub(glog[:], glog[:], bcum[:]), [hb], [hb])
                        S.op("dve", lambda: nc.vector.tensor_tensor(
                            vap(glog[:], [[64, H // 64], [1, 64]]), vap(glog[:], [[64, H // 64], [1, 64]]),
                            vap(bcum[:], [[64, H // 64], [0, 64]], off=63), ALU.add), [hb], [hb])
                        cur_ap = glog[:]
                    else:
                        cur_ap = bcum[:]
                    S.op("act", lambda: nc.scalar.activation(e1[:], cur_ap, AF.Exp), [hb], [hb])
                    S.op("act", lambda: nc.scalar.activation(e2[:], cur_ap, AF.Exp, scale=-1.0), [hb], [hb])
                    yield
                    S.op("dve", lambda: nc.vector.tensor_tensor(Qt[:, d, t0:t0 + H], qs[:, t0:t0 + H], e1[:], ALU.mult),
                         [hb], [qk_b])
                    S.op("dve", lambda: nc.vector.tensor_tensor(Kt[:, d, t0:t0 + H], kk[:], e2[:], ALU.mult),
                         [hb], [qk_b])
                    yield

            def ktok_transposes(d):
                for g8 in range(2):
                    pbk = (d * 2 + g8) % 2
                    for u in range(8):
                        i = g8 * 8 + u
                        S.op("pe", lambda: nc.tensor.transpose(psb[pbk][:, u * P:(u + 1) * P], Kt[:, d, i * P:(i + 1) * P], ident[:]),
                             [qk_b, cc], [psb_b[pbk]])
                    S.op("act", lambda: nc.scalar.copy(Ktok[:, d, g8 * 8:(g8 + 1) * 8, :], vap(psb[pbk][:], [[P, 8], [1, P]])),
                         [psb_b[pbk]], [ktk_b[d]])

            def chain(d):
                c0 = 0 if d == 0 else 31
                S.op("pool", lambda: nc.gpsimd.memset(Sm[0][:, d, :], 0.0), [], [smd_b[d][0]])
                S.op("pool", lambda: nc.gpsimd.memset(Sbf[:, d, c0, :], 0.0), [], [sbf_bb[d][c0]])
                for step in range(31):
                    pp = step % 2
                    cur, nxt = Sm[pp], Sm[1 - pp]
                    c = step if d == 0 else 31 - step
                    col0 = (2 * d + pp) * P
                    i, jj = c // 2, c % 2
                    S.op("pe", lambda: nc.tensor.matmul(psf[2][:, col0:col0 + P], Ktok[:, d, i, :], vm[:, i, jj, :],
                                                        start=True, stop=True, skip_group_check=True),
                         [ktk_b[d], vm_b], [ch_b[d][pp]])
                    S.op("act", lambda: nc.scalar.mul(dSd[:, d, :], psf[2][:, col0:col0 + P], dch[:, d, c:c + 1]),
                         [ch_b[d][pp], dch_b[d]], [dsd_b[d]])
                    S.op("dve", lambda: nc.vector.scalar_tensor_tensor(nxt[:, d, :], cur[:, d, :], dch[:, d, c:c + 1],
                                                                        dSd[:, d, :], ALU.mult, ALU.add),
                         [smd_b[d][pp], dsd_b[d], dch_b[d]], [smd_b[d][1 - pp]])
                    cn = c + 1 if d == 0 else c - 1
                    S.op("pool", lambda: nc.gpsimd.tensor_copy(Sbf[:, d, cn, :], nxt[:, d, :]), [smd_b[d][1 - pp]],
                         [sbf_bb[d][cn]])
                    yield

            def emit_scores(i):
                ks = i % 2
                for d in range(2):
                    S.op("pe", lambda: nc.tensor.matmul(psf[ks][:, d * P:(d + 1) * P], Kt[:, d, i * P:(i + 1) * P],
                                                        Qt[:, d, i * P:(i + 1) * P], start=True, stop=True,
                                                        skip_group_check=True), [qk_b], [psf_b[ks]])
                S.op("dve", lambda: nc.vector.tensor_tensor(Pm[ks][:, 0, :], psf[ks][:, 0:P], maskf[:], ALU.mult),
                     [psf_b[ks], cb], [pm_b[ks]])
                S.op("dve", lambda: nc.vector.tensor_tensor(Pm[ks][:, 1, :], psf[ks][:, P:2 * P], maskb[:], ALU.mult),
                     [psf_b[ks], cb], [pm_b[ks]])

            def out_tile(i):
                g4, u = divmod(i, 4)
                po = 4 + (g4 % 2)
                ks = i % 2
                oap = psf[po][:, u * P:(u + 1) * P]
                S.op("pe", lambda: nc.tensor.matmul(oap, vtok[:, i, h * P:(h + 1) * P], Pm[ks][:, 0, :], start=True,
                                                    stop=False, skip_group_check=True), [vtok_b, pm_b[ks]], [psf_b[po]])
                S.op("pe", lambda: nc.tensor.matmul(oap, vtok[:, i, h * P:(h + 1) * P], Pm[ks][:, 1, :], start=False,
                                                    stop=False, skip_group_check=True), [vtok_b, pm_b[ks]], [psf_b[po]])
                for d in range(2):
                    for jj in range(2):
                        c = 2 * i + jj
                        S.op("pe", lambda: nc.tensor.matmul(
                            psf[po][:, u * P + jj * 64:u * P + jj * 64 + 64], Sbf[:, d, c, :],
                            Qt[:, d, c * 64:(c + 1) * 64], start=False, stop=(d == 1 and jj == 1),
                            skip_group_check=True), [sbf_bb[d][c], qk_b], [psf_b[po]])

            def out_norm(g4):
                po = 4 + (g4 % 2)
                S.op("act", lambda: nc.scalar.activation(osq[:], psf[po][:], AF.Square), [psf_b[po]], [ob])
                S.op("pe", lambda: nc.tensor.matmul(psf[3][:], ones_bf[:], osq[:], start=True, stop=True,
                                                    skip_group_check=True), [ob, cc], [psf_b[3]])
                S.op("act", lambda: nc.scalar.activation(rbc[:, 0:512], psf[3][:], AF.Sqrt, bias=epsb[:], scale=1.0 / 128),
                     [psf_b[3], cc], [ob, hb])
                S.op("dve", lambda: nc.vector.reciprocal(rbc[:, 0:512], rbc[:, 0:512]), [ob, hb], [ob, hb])
                S.op("dve", lambda: nc.vector.scalar_tensor_tensor(otmp[:, 0:512], psf[po][:], g_hgo[:, h:h + 1], rbc[:, 0:512],
                                                                    ALU.mult, ALU.mult), [psf_b[po], ob, cb, hb], [ob, hb])
                S.op("dve", lambda: nc.vector.tensor_tensor(mixT[:, h, g4 * 512:(g4 + 1) * 512], otmp[:, 0:512],
                                                            gsl[:, g4 * 512:(g4 + 1) * 512], ALU.mult), [ob, hb], [mix_b])

            for jj in range(2):
                S.op("dve", lambda: nc.vector.tensor_scalar(vm[:, :, jj, :], vtok[:, :, h * P:(h + 1) * P],
                                                            maskb[:, jj * 64:jj * 64 + 1], None, ALU.mult),
                     [vtok_b, cb], [vm_b])
            for _ in elem(0):
                pass
            ktok_transposes(0)
            g0 = chain(0)
            for _ in elem(1):
                for _q in range(4):
                    next(g0, None)
            for _ in g0:
                pass
            ktok_transposes(1)
            g1 = chain(1)
            steps_done = 0
            emit_scores(NT - 1)
            for i in range(NT - 1, -1, -1):
                need = min(31, 31 - 2 * i)
                while steps_done < need:
                    next(g1)
                    steps_done += 1
                if i - 1 >= 0:
                    emit_scores(i - 1)
                out_tile(i)
                if i % 4 == 0:
                    out_norm(i // 4)
            for _ in g1:
                pass
        S.barrier()
    dump("mix_hg", mixT[:, 0:4, :], [mix_b])
    if upto == "B":
        return nc

    SCALE = 192.0 ** -0.5
    pc = Scope(mem)
    cT = sb("cT", [P, 5, T], BF, pc)
    cT_b = Buf("cT")
    krraw = sb("krraw", [P, NT, 64], F32, pc)
    kr_b = Buf("krraw")
    rs2 = sb("rs2", [P, NT, 2], F32, pc)
    rs2_b = Buf("rs2")
    wqu = sb("wqu", [P, 3, 768], BF, pc)
    wkvu = sb("wkvu", [P, 2, 1024], BF, pc)
    wu_b = Buf("wup")
    with Scope(mem) as st:
        wm = sb("wm", [P, 8, 704], BF, st)
        wm_b = Buf("wm")
        load_wslice(wm[:, :, 0:512], wm_b, win_d, 8, [(2560, 512)], g_attn, 0)
        load_wslice(wm[:, :, 512:704], wm_b, win_d, 8, [(3072, 192)], g_attn, 1)
        csq = sb("csq", [P, 5, 512], BF, st)
        csq_b = Buf("csq")
        for tb in range(4):
            for j in range(5):
                k = j % 2
                proj_fm(psf[k][:], psf_b[k], wm, wm_b, j * P, P, tb * 512, 512)
                S.op("act", lambda: nc.scalar.copy(cT[:, j, tb * 512:(tb + 1) * 512], psf[k][:]), [psf_b[k]], [cT_b])
                S.op("act", lambda: nc.scalar.activation(csq[:, j, :], psf[k][:], AF.Square), [psf_b[k]], [csq_b])
            for u in range(4):
                i = tb * 4 + u
                for j in range(3):
                    S.op("pe", lambda: nc.tensor.matmul(psf[2][:, i * 2:i * 2 + 1], csq[:, j, u * P:(u + 1) * P],
                                                        ones_bf[:, 0:1], start=(j == 0), stop=(j == 2),
                                                        skip_group_check=True), [csq_b, cc], [psf_b[2]])
                for j in range(2):
                    S.op("pe", lambda: nc.tensor.matmul(psf[2][:, i * 2 + 1:i * 2 + 2], csq[:, 3 + j, u * P:(u + 1) * P],
                                                        ones_bf[:, 0:1], start=(j == 0), stop=(j == 1),
                                                        skip_group_check=True), [csq_b, cc], [psf_b[2]])
        S.op("act", lambda: nc.scalar.copy(rs2[:].rearrange("p a b -> p (a b)"), psf[2][:, 0:2 * NT]), [psf_b[2]], [rs2_b])
        rstd_from_ss(rs2[:, :, 0], rs2[:, :, 0], 384, [rs2_b])
        rstd_from_ss(rs2[:, :, 1], rs2[:, :, 1], 256, [rs2_b])
        for i in range(NT):
            k = 3 + i % 2
            for c in range(8):
                S.op("pe", lambda: nc.tensor.matmul(psf[k][:, 0:64], xnT[:, c, i * P:(i + 1) * P], wm[:, c, 640:704],
                                                    start=(c == 0), stop=(c == 7), skip_group_check=True),
                     [xnT_b, wm_b], [psf_b[k]])
            S.op("act", lambda: nc.scalar.copy(krraw[:, i, :], psf[k][:, 0:64]), [psf_b[k]], [kr_b])
        S.barrier()
    if upto == "C1":
        return nc
    sg0 = stg[0]
    S.dma("sp", sg0[:, 0:3, 0:512], wqup_d[:, :, 0:512], stg_b[0], writes=[stg_b[0]])
    for c in range(3):
        S.op("dve", lambda: nc.vector.tensor_scalar(wqu[:, c, 0:512], sg0[:, c, 0:512], g_qa[:, c:c + 1], None, ALU.mult),
             [stg_b[0], cb], [wu_b])
    S.dma("sp", sg0[:, 0:3, 0:256], wqup_d[:, :, 512:768], stg_b[0], writes=[stg_b[0]])
    for c in range(3):
        S.op("dve", lambda: nc.vector.tensor_scalar(wqu[:, c, 512:768], sg0[:, c, 0:256], g_qa[:, c:c + 1], None, ALU.mult),
             [stg_b[0], cb], [wu_b])
    for half in range(2):
        S.dma("sp", sg0[:, 0:2, 0:512], wkvup_d[:, :, half * 512:(half + 1) * 512], stg_b[0], writes=[stg_b[0]])
        for c in range(2):
            S.op("dve", lambda: nc.vector.tensor_scalar(wkvu[:, c, half * 512:(half + 1) * 512], sg0[:, c, 0:512],
                                                        g_kva[:, c:c + 1], None, ALU.mult), [stg_b[0], cb], [wu_b])
    S.barrier()
    ph1.close()
    qnT = sb("qnT", [P, 4, T], BF, pc)
    knT = sb("knT", [P, 4, T], BF, pc)
    qrT = sb("qrT", [P, 4, T], BF, pc)
    krT = sb("krT", [P, T], BF, pc)
    vaug = sb("vaug", [P, NT, 4, 132], BF, pc)
    qk2_b = Buf("qkT")
    vaug_b = Buf("vaug")
    S.op("pool", lambda: nc.gpsimd.memset(vaug[:], 1.0), [], [vaug_b])
    S.op("pool", lambda: nc.gpsimd.memset(qrT[:], 0.0), [], [qk2_b])
    S.op("pool", lambda: nc.gpsimd.memset(krT[:], 0.0), [], [qk2_b])
    with Scope(mem) as st:
        Qs2 = [sb("Qs%d" % i, [P, 768], F32, st) for i in range(2)]
        KVs2 = [sb("KVs%d" % i, [P, 1024], F32, st) for i in range(2)]
        in_b = [Buf("mla_in0"), Buf("mla_in1")]
        sq = sb("sqm", [P, 1024], F32, st)
        ssn = sb("ssn", [P, 16], F32, st)
        invn = sb("invn", [P, 16], F32, st)
        qn_s = sb("qn_s", [P, 4, P], BF, st)
        kn_s = sb("kn_s", [P, 4, P], BF, st)
        qr_f = sb("qr_f", [P, 4, 64], F32, st)
        kr_f = sb("kr_f", [P, 64], F32, st)
        qr_s = sb("qr_s", [P, 4, 64], BF, st)
        kr_s = sb("kr_s", [P, 64], BF, st)
        ra = sb("ra", [P, 4, 32], F32, st)
        rb_ = sb("rb_", [P, 4, 32], F32, st)
        dv = Buf("mla_dve")
        out_b = Buf("mla_out")
        S.op("dve", lambda: nc.vector.memset(invn[:, 0:4], 1.0 / 128), [], [dv])
        S.op("dve", lambda: nc.vector.memset(invn[:, 4:8], 1.0 / 64), [], [dv])
        S.op("dve", lambda: nc.vector.memset(invn[:, 8:12], 1.0 / 128), [], [dv])
        S.op("dve", lambda: nc.vector.memset(invn[:, 12:16], 1.0 / 64), [], [dv])

        def mla_front(i):
            Qs, KVs, ib = Qs2[i % 2], KVs2[i % 2], in_b[i % 2]
            for half in range(2):
                k = half
                for j in range(3):
                    S.op("pe", lambda: nc.tensor.matmul(psf[k][:, 0:384], cT[:, j, i * P:(i + 1) * P],
                                                        wqu[:, j, half * 384:(half + 1) * 384], start=(j == 0), stop=(j == 2),
                                                        skip_group_check=True), [cT_b, wu_b], [psf_b[k]])
                S.op("act", lambda: nc.scalar.mul(Qs[:, half * 384:(half + 1) * 384], psf[k][:, 0:384],
                                                  rs2[:, i, 0:1]), [psf_b[k], rs2_b], [ib])
            for half in range(2):
                k = 2 + half
                for j in range(2):
                    S.op("pe", lambda: nc.tensor.matmul(psf[k][:], cT[:, 3 + j, i * P:(i + 1) * P],
                                                        wkvu[:, j, half * 512:(half + 1) * 512], start=(j == 0), stop=(j == 1),
                                                        skip_group_check=True), [cT_b, wu_b], [psf_b[k]])
                S.op("act", lambda: nc.scalar.mul(KVs[:, half * 512:(half + 1) * 512], psf[k][:],
                                                  rs2[:, i, 1:2]), [psf_b[k], rs2_b], [ib])

        def mla_chain(i):
            Qs, KVs, ib = Qs2[i % 2], KVs2[i % 2], in_b[i % 2]
            Q3 = Qs[:].rearrange("p (h d) -> p h d", h=4)
            KV3 = KVs[:].rearrange("p (h d) -> p h d", h=4)
            sq3q = sq[:, 0:768].rearrange("p (h d) -> p h d", h=4)
            sq3k = sq[:, 0:512].rearrange("p (h d) -> p h d", h=4)
            S.op("dve", lambda: nc.vector.tensor_tensor(sq[:, 0:768], Qs[:], Qs[:], ALU.mult), [ib, dv], [dv])
            S.op("dve", lambda: nc.vector.tensor_reduce(ssn[:, 0:4], sq3q[:, :, 0:128], AX.X, ALU.add), [dv], [dv])
            S.op("dve", lambda: nc.vector.tensor_reduce(ssn[:, 4:8], sq3q[:, :, 128:192], AX.X, ALU.add), [dv], [dv])
            S.op("dve", lambda: nc.vector.tensor_tensor(sq3k, KV3[:, :, 0:128], KV3[:, :, 0:128], ALU.mult), [ib, dv], [dv])
            S.op("dve", lambda: nc.vector.tensor_reduce(ssn[:, 8:12], sq3k, AX.X, ALU.add), [dv], [dv])
            S.op("dve", lambda: nc.vector.tensor_tensor(sq[:, 0:64], krraw[:, i, :], krraw[:, i, :], ALU.mult), [kr_b, dv], [dv])
            S.op("dve", lambda: nc.vector.tensor_reduce(ssn[:, 12:13], sq[:, 0:64], AX.X, ALU.add), [dv], [dv])
            S.op("dve", lambda: nc.vector.tensor_tensor(ssn[:, 0:13], ssn[:, 0:13], invn[:, 0:13], ALU.mult), [dv], [dv])
            S.op("act", lambda: nc.scalar.activation(ssn[:, 0:13], ssn[:, 0:13], AF.Sqrt, bias=epsb[:], scale=1.0),
                 [dv, cc], [dv])
            S.op("dve", lambda: nc.vector.reciprocal(ssn[:, 0:13], ssn[:, 0:13]), [dv], [dv])
            S.op("dve", lambda: nc.vector.tensor_tensor(sq3q[:, :, 0:128], Q3[:, :, 0:128],
                                                        vap(ssn[:], [[1, 4], [0, 128]]), ALU.mult), [ib, dv], [dv])
            S.op("dve", lambda: nc.vector.tensor_tensor(qn_s[:], sq3q[:, :, 0:128], vap(g_q[:], [[0, 4], [1, 128]]),
                                                        ALU.mult), [dv, cb, out_b], [out_b])
            S.op("dve", lambda: nc.vector.tensor_tensor(sq3k, KV3[:, :, 0:128], vap(ssn[:], [[1, 4], [0, 128]], off=8),
                                                        ALU.mult), [ib, dv], [dv])
            S.op("dve", lambda: nc.vector.tensor_tensor(kn_s[:], sq3k, vap(g_k[:], [[0, 4], [1, 128]]), ALU.mult),
                 [dv, cb, out_b], [out_b])
            S.op("dve", lambda: nc.vector.tensor_tensor(qr_f[:], Q3[:, :, 128:192], vap(ssn[:], [[1, 4], [0, 64]], off=4),
                                                        ALU.mult), [ib, dv], [dv])
            S.op("dve", lambda: nc.vector.tensor_tensor(qr_f[:], qr_f[:], vap(g_q[:], [[0, 4], [1, 64]], off=128),
                                                        ALU.mult), [dv, cb], [dv])
            S.op("dve", lambda: nc.vector.tensor_scalar(kr_f[:], krraw[:, i, :], ssn[:, 12:13], None, ALU.mult),
                 [kr_b, dv], [dv])
            S.op("dve", lambda: nc.vector.tensor_tensor(kr_f[:], kr_f[:], g_k[:, 128:192], ALU.mult), [dv, cb], [dv])
            cos4 = vap(cosT[:], [[0, 4], [1, 32]], off=i * 32)
            sin4 = vap(sinT[:], [[0, 4], [1, 32]], off=i * 32)
            S.op("dve", lambda: nc.vector.tensor_tensor(ra[:], qr_f[:, :, 0:32], cos4, ALU.mult), [dv, cc], [dv])
            S.op("dve", lambda: nc.vector.tensor_tensor(rb_[:], qr_f[:, :, 32:64], sin4, ALU.mult), [dv, cc], [dv])
            S.op("dve", lambda: nc.vector.tensor_sub(qr_s[:, :, 0:32], ra[:], rb_[:]), [dv, out_b], [out_b])
            S.op("dve", lambda: nc.vector.tensor_tensor(ra[:], qr_f[:, :, 32:64], cos4, ALU.mult), [dv, cc, out_b], [dv])
            S.op("dve", lambda: nc.vector.tensor_tensor(rb_[:], qr_f[:, :, 0:32], sin4, ALU.mult), [dv, cc], [dv])
            S.op("dve", lambda: nc.vector.tensor_add(qr_s[:, :, 32:64], ra[:], rb_[:]), [dv, out_b], [out_b])
            c1 = cosT[:, i, :]
            s1 = sinT[:, i, :]
            S.op("dve", lambda: nc.vector.tensor_tensor(ra[:, 0, :], kr_f[:, 0:32], c1, ALU.mult), [dv, cc, out_b], [dv])
            S.op("dve", lambda: nc.vector.tensor_tensor(rb_[:, 0, :], kr_f[:, 32:64], s1, ALU.mult), [dv, cc], [dv])
            S.op("dve", lambda: nc.vector.tensor_sub(kr_s[:, 0:32], ra[:, 0, :], rb_[:, 0, :]), [dv, out_b], [out_b])
            S.op("dve", lambda: nc.vector.tensor_tensor(ra[:, 0, :], kr_f[:, 32:64], c1, ALU.mult), [dv, cc, out_b], [dv])
            S.op("dve", lambda: nc.vector.tensor_tensor(rb_[:, 0, :], kr_f[:, 0:32], s1, ALU.mult), [dv, cc], [dv])
            S.op("dve", lambda: nc.vector.tensor_add(kr_s[:, 32:64], ra[:, 0, :], rb_[:, 0, :]), [dv, out_b], [out_b])
            S.op("pool", lambda: nc.gpsimd.tensor_copy(vaug[:, i, :, 0:128], KV3[:, :, 128:256]), [ib, vaug_b], [vaug_b])

        def mla_tail(i):
            for hh in range(4):
                S.op("pe", lambda: nc.tensor.transpose(psb[0][:, hh * P:(hh + 1) * P], qn_s[:, hh, :], ident[:]),
                     [out_b, cc], [psb_b[0]])
                S.op("pe", lambda: nc.tensor.transpose(psb[0][:, (4 + hh) * P:(5 + hh) * P], kn_s[:, hh, :], ident[:]),
                     [out_b, cc], [psb_b[0]])
                S.op("pe", lambda: nc.tensor.transpose(psb[1][0:64, hh * P:(hh + 1) * P], qr_s[:, hh, :], ident[:]),
                     [out_b, cc], [psb_b[1]])
            S.op("pe", lambda: nc.tensor.transpose(psb[1][0:64, 4 * P:5 * P], kr_s[:], ident[:]), [out_b, cc], [psb_b[1]])
            S.op("act", lambda: nc.scalar.copy(qnT[:, :, i * P:(i + 1) * P], vap(psb[0][:], [[P, 4], [1, P]])),
                 [psb_b[0]], [qk2_b])
            S.op("act", lambda: nc.scalar.copy(knT[:, :, i * P:(i + 1) * P], vap(psb[0][:], [[P, 4], [1, P]], off=4 * P)),
                 [psb_b[0]], [qk2_b])
            S.op("act", lambda: nc.scalar.copy(qrT[0:64, :, i * P:(i + 1) * P], vap(psb[1][0:64, :], [[P, 4], [1, P]])),
                 [psb_b[1]], [qk2_b])
            S.op("act", lambda: nc.scalar.copy(krT[0:64, i * P:(i + 1) * P], psb[1][0:64, 4 * P:5 * P]), [psb_b[1]], [qk2_b])

        mla_front(0)
        for i in range(NT):
            if i + 1 < NT:
                mla_front(i + 1)
            mla_chain(i)
            mla_tail(i)
        S.barrier()
    if upto == "C2":
        return nc
    with Scope(mem) as st:
        PT = [sb("PT%d" % i, [P, 512], BF, st) for i in range(2)]
        PT_b = [Buf("PT0"), Buf("PT1")]
        on4 = sb("on4", [P, 4, P], F32, st)
        onb4 = sb("onb4", [P, 4, P], BF, st)
        junk4 = sb("junk4", [P, 4, P], BF, st)
        rden = sb("rden", [P, 4], F32, st)
        ss4 = sb("ss4", [P, 4], F32, st)
        r_b = [Buf("rden%d" % q) for q in range(4)]
        on_b = [Buf("on%d" % q) for q in range(4)]
        j_b = [Buf("junk%d" % q) for q in range(4)]
        s_b = Buf("ss4")
        onb_b = [Buf("onb%d" % q) for q in range(4)]
        acc = (psf[2], psf[3], psf[4], psf[5])
        acc_b = (psf_b[2], psf_b[3], psf_b[4], psf_b[5])
        it = 0

        def tail_part1(hh, qb):
            for qt in range(4):
                S.op("dve", lambda: nc.vector.reciprocal(rden[:, qt:qt + 1], acc[qt][:, 128:129]), [acc_b[qt]], [r_b[qt]])
            for qt in range(4):
                S.op("act", lambda: nc.scalar.mul(on4[:, qt, :], acc[qt][:, 0:128], rden[:, qt:qt + 1]),
                     [acc_b[qt], r_b[qt]], [on_b[qt]])
            for qt in range(4):
                S.op("act", lambda: nc.scalar.activation(junk4[:, qt, :], on4[:, qt, :], AF.Square,
                                                         accum_out=ss4[:, qt:qt + 1]), [on_b[qt]], [j_b[qt], s_b])
            S.op("act", lambda: nc.scalar.activation(ss4[:], ss4[:], AF.Sqrt, bias=epsb[:], scale=1.0 / 128),
                 [s_b, cc], [s_b])
            S.op("dve", lambda: nc.vector.reciprocal(ss4[:], ss4[:]), [s_b], [s_b])
            for qt in range(4):
                S.op("dve", lambda: nc.vector.scalar_tensor_tensor(onb4[:, qt, :], on4[:, qt, :], ss4[:, qt:qt + 1],
                                                                    g_mo[:, hh * P:(hh + 1) * P], ALU.mult, ALU.mult),
                     [on_b[qt], s_b, cb], [onb_b[qt]])

        def tail_part2(hh, qb):
            for qt in range(4):
                S.op("pe", lambda: nc.tensor.transpose(psb[0][:, qt * P:(qt + 1) * P], onb4[:, qt, :], ident[:]),
                     [onb_b[qt], cc], [psb_b[0]])
            S.op("act", lambda: nc.scalar.copy(mixT[:, 4 + hh, qb * 512:(qb + 1) * 512], psb[0][:, 0:512]),
                 [psb_b[0]], [mix_b])

        blocks = [(hh, qb) for hh in range(4) for qb in range(4)]
        pending = None
        for (hh, qb) in blocks:
            def emit_S(kt, k):
                S.op("pe", lambda: nc.tensor.matmul(psf[k][:], knT[:, hh, kt * P:(kt + 1) * P],
                                                    qnT[:, hh, qb * 512:(qb + 1) * 512], start=True, stop=False,
                                                    skip_group_check=True), [qk2_b], [psf_b[k]])
                S.op("pe", lambda: nc.tensor.matmul(psf[k][:], krT[:, kt * P:(kt + 1) * P],
                                                    qrT[:, hh, qb * 512:(qb + 1) * 512], start=False, stop=True,
                                                    skip_group_check=True), [qk2_b], [psf_b[k]])

            emit_S(0, it % 2)
            for kt in range(NT):
                k = it % 2
                it += 1
                if kt + 1 < NT:
                    emit_S(kt + 1, it % 2)
                S.op("act", lambda: nc.scalar.activation(PT[k][:], psf[k][:], AF.Exp, scale=SCALE),
                     [psf_b[k]], [PT_b[k]])
                for qt in range(4):
                    a = acc[qt]
                    S.op("pe", lambda: nc.tensor.matmul(a[:, 0:129],
                                                        PT[k][:, qt * P:(qt + 1) * P], vaug[:, kt, hh, 0:129],
                                                        start=(kt == 0), stop=(kt == NT - 1), skip_group_check=True),
                         [PT_b[k], vaug_b], [acc_b[qt]])
                if kt == 2 and pending is not None:
                    tail_part2(*pending)
                    pending = None
            tail_part1(hh, qb)
            pending = (hh, qb)
        tail_part2(*pending)
        S.barrier()
    pc.close()
    pEarly.close()
    dump("mix_mla", mixT[:, 4:8, :], [mix_b])
    if upto == "C":
        return nc

    pD = Scope(mem)
    y_acc = sb("y_acc", [P, NT, D], F32, pD)
    y_b = [Buf("y%d" % i) for i in range(NT)]
    h2T = sb("h2T", [P, 8, T], BF, pD)
    h2T_b = Buf("h2T")
    with Scope(mem) as st:
        wo = sb("wo", [P, 8, D], BF, st)
        wo_b = Buf("wo")
        sg = [sb("sgD0", [P, 8, 512], F32, st)] * 2
        sg_b = [Buf("sgD0")] * 2
        for half in range(2):
            S.dma("sp", sg[half][:], wout_d[:, :, half * 512:(half + 1) * 512], sg_b[half], writes=[sg_b[half]])
            for c in range(8):
                S.op("dve", lambda: nc.vector.tensor_copy(wo[:, c, half * 512:(half + 1) * 512], sg[half][:, c, :]),
                     [sg_b[half]], [wo_b])
        xt = [sb("xtD%d" % i, [P, D], F32, st) for i in range(2)]
        xt_b = [Buf("xtD0"), Buf("xtD1")]
        h2 = [sb("h2_%d" % i, [P, D], BF, st) for i in range(2)]
        h2_b = [Buf("h2_0"), Buf("h2_1")]
        junk = sb("junkD", [P, D], BF, st)
        junk_b = Buf("junkD")
        ssD = sb("ssD", [P, NT], F32, st)
        ssD_b = [Buf("ssD%d" % i) for i in range(NT)]
        for i in range(NT):
            j = i % 2
            S.dma("sp", xt[j][:], x_d[i * P:(i + 1) * P, :], xt_b[j], writes=[xt_b[j]])
            for half in range(2):
                k = 2 * j + half
                for c in range(8):
                    S.op("pe", lambda: nc.tensor.matmul(psf[k][:], mixT[:, c, i * P:(i + 1) * P],
                                                        wo[:, c, half * 512:(half + 1) * 512], start=(c == 0), stop=(c == 7),
                                                        skip_group_check=True), [mix_b, wo_b], [psf_b[k]])
                S.op("dve", lambda: nc.vector.tensor_tensor(y_acc[:, i, half * 512:(half + 1) * 512], psf[k][:],
                                                            xt[j][:, half * 512:(half + 1) * 512], ALU.add),
                     [psf_b[k], xt_b[j]], [y_b[i]])
            S.op("act", lambda: nc.scalar.activation(junk[:], y_acc[:, i, :], AF.Square, accum_out=ssD[:, i:i + 1]),
                 [y_b[i]], [junk_b, ssD_b[i]])
            rstd_from_ss(ssD[:, i:i + 1], ssD[:, i:i + 1], D, [ssD_b[i]])
            S.op("dve", lambda: nc.vector.tensor_scalar(h2[j][:], y_acc[:, i, :], ssD[:, i:i + 1], None, ALU.mult),
                 [y_b[i], ssD_b[i]], [h2_b[j]])
            for c in range(8):
                S.op("pe", lambda: nc.tensor.transpose(psb[j][:, c * P:(c + 1) * P], h2[j][:, c * P:(c + 1) * P], ident[:]),
                     [h2_b[j], cc], [psb_b[j]])
            S.op("act", lambda: nc.scalar.copy(h2T[:, :, i * P:(i + 1) * P], vap(psb[j][:], [[P, 8], [1, P]])),
                 [psb_b[j]], [h2T_b])
        S.barrier()
    dump("x1", y_acc[:], y_b)
    pM.close()
    if upto == "D":
        return nc

    U32 = mybir.dt.uint32
    pE = Scope(mem)
    iota16 = iota_f[:, 128:144]
    thr16 = iota_f[:, 144:160]
    with Scope(mem) as st:
        weff = sb("weff", [P, 8, 2048], BF, st)
        weff_b = Buf("weff")
        with Scope(mem) as st2:
            wqT = sb("wqT_s", [P, 8, 1024], BF, st2)
            kT = sb("kT_s", [P, 16, P], BF, st2)
            wq_b = Buf("wqT")
            sg = sb("sgE", [P, 4, 1024], F32, st2)
            sg_b = Buf("sgE")
            S.dma("sp", sg[:, 0:2, :].rearrange("p a b -> p (a b)"), keysT_d.rearrange("p a b -> p (a b)"), sg_b, writes=[sg_b])
            S.op("dve", lambda: nc.vector.tensor_copy(kT[:].rearrange("p a b -> p (a b)"),
                                                      sg[:, 0:2, :].rearrange("p a b -> p (a b)")), [sg_b], [wq_b])
            for hf in range(2):
                for q2 in range(2):
                    q4 = hf * 2 + q2
                    S.dma("sp", sg[:], wqT_d[:, q4 * 4:(q4 + 1) * 4, :], sg_b, writes=[sg_b])
                    S.op("dve", lambda: nc.vector.tensor_copy(wqT[:, q2 * 4:(q2 + 1) * 4, :], sg[:]), [sg_b], [wq_b])
                for c in range(8):
                    for q2 in range(2):
                        q4 = hf * 2 + q2
                        k = q4 % 2
                        for u in range(4):
                            pcx = q4 * 4 + u
                            S.op("pe", lambda: nc.tensor.matmul(psf[k][:, u * P:(u + 1) * P],
                                                                wqT[:, q2 * 4 + u, c * P:(c + 1) * P],
                                                                kT[:, pcx, :], start=True, stop=True, skip_group_check=True),
                                 [wq_b], [psf_b[k]])
                        S.op("act", lambda: nc.scalar.mul(weff[:, c, q4 * 512:(q4 + 1) * 512], psf[k][:],
                                                          g_ffn[:, c:c + 1]), [psf_b[k], cb], [weff_b])
            S.barrier()
        sci = sb("sc0", [P, 16, P], F32, st)
        scb = Buf("sc0")
        GI = 4
        sc2 = [sb("sc2_%d" % j, [P, P], F32, st) for j in range(GI)]
        sc2_b = [Buf("sc2_%d" % j) for j in range(GI)]
        t1_b = [Buf("t1_%d" % j) for j in range(16)]
        i1_b = [Buf("i1_%d" % j) for j in range(16)]
        top = sb("top", [P, 16, 16], F32, st)
        idxu = sb("idxu", [P, 16, 16], U32, st)
        idxf = sb("idxf", [P, 16, 16], F32, st)
        cand = [sb("cand_%d" % j, [P, 256], F32, st) for j in range(GI)]
        cand2 = [sb("cand2_%d" % j, [P, 256], F32, st) for j in range(GI)]
        cd_b = [Buf("cd%d" % j) for j in range(GI)]
        cd2_b = [Buf("cd2_%d" % j) for j in range(GI)]
        sel_b = [Buf("sel%d" % j) for j in range(8)]
        pos_b = [Buf("pos%d" % j) for j in range(8)]
        idxf_b = Buf("idxf")
        posf_b = Buf("posf")
        af_b = Buf("af")
        bfb_b = Buf("bfb")
        g16_b = [Buf("g16a"), Buf("g16b")]
        t16_b = [Buf("t16a"), Buf("t16b")]
        abf_b = [Buf("abf0"), Buf("abf1"), Buf("abf2")]
        es_b = Buf("esel")
        zs_b = Buf("zs")
        sel = sb("sel", [P, 8, 16], F32, st)
        posu = sb("posu", [P, 8, 16], U32, st)
        posf = sb("posf2", [P, P], F32, st)
        ge16 = sb("ge16", [P, P, 16], BF, st)
        af = sb("af", [P, P], F32, st)
        bf_ = sb("bf_", [P, P], F32, st)
        esel = sb("esel", [P, 8, 16], F32, st)
        zs = sb("zs", [P, 8], F32, st)
        ab = sb("ab", [P, 3, P], BF, st)
        abf = sb("abf", [P, 3, P], F32, st)
        abT2 = sb("abT2", [P, 2, 3, P], BF, st)
        abT2_b = [Buf("abT2_0"), Buf("abT2_1")]
        tk = Buf("topk")
        ab_b = Buf("ab")
        SUB = 8
        WT = sb("WT", [P, P, P], BF, st)
        WT_b = Buf("WT")
        NAB = 3
        A1 = [sb("A1_%d" % i, [P, SUB, P], BF, st) for i in range(NAB)]
        A2 = [sb("A2_%d" % i, [P, SUB, P], BF, st) for i in range(NAB)]
        A1_b = [Buf("A1_%d" % i) for i in range(NAB)]
        A2_b = [Buf("A2_%d" % i) for i in range(NAB)]
        wd_b = [Buf("Wd%d" % i) for i in range(NT)]
        cnt = {"it": 0, "bk": 0}

        def e1_front(i):
            for q4 in range(4):
                k = q4
                for c in range(8):
                    S.op("pe", lambda: nc.tensor.matmul(psf[k][:], h2T[:, c, i * P:(i + 1) * P],
                                                        weff[:, c, q4 * 512:(q4 + 1) * 512], start=(c == 0), stop=(c == 7),
                                                        skip_group_check=True), [h2T_b, weff_b], [psf_b[k]])
                S.op("act", lambda: nc.scalar.copy(sci[:, q4 * 4:(q4 + 1) * 4, :].rearrange("p a b -> p (a b)"), psf[k][:]),
                     [psf_b[k]], [scb])


        def e1_chain(i):
            for g2_ in range(16 // GI):
                pcs = tuple(GI * g2_ + q_ for q_ in range(GI))
                for pcx in pcs:
                    S.op("dve", lambda: nc.vector.max(out=top[:, pcx, 0:8], in_=sci[:, pcx, :]), [scb], [t1_b[pcx]])
                for pcx in pcs:
                    S.op("dve", lambda: nc.vector.max_index(out=idxu[:, pcx, 0:8], in_max=top[:, pcx, 0:8],
                                                            in_values=sci[:, pcx, :]), [scb, t1_b[pcx]], [i1_b[pcx]])
                for pcx in pcs:
                    j = pcx % GI
                    S.op("dve", lambda: nc.vector.match_replace(out=sc2[j][:], in_to_replace=top[:, pcx, 0:8],
                                                                in_values=sci[:, pcx, :], imm_value=-1e30),
                         [scb, t1_b[pcx]], [sc2_b[j]])
                for pcx in pcs:
                    j = pcx % GI
                    S.op("dve", lambda: nc.vector.max(out=top[:, pcx, 8:16], in_=sc2[j][:]), [sc2_b[j]], [t1_b[pcx]])
                for pcx in pcs:
                    j = pcx % GI
                    S.op("dve", lambda: nc.vector.max_index(out=idxu[:, pcx, 8:16], in_max=top[:, pcx, 8:16],
                                                            in_values=sc2[j][:]), [sc2_b[j], t1_b[pcx]], [i1_b[pcx]])
                for _y in range(GI):
                    yield
            for h2_ in range(8 // GI):
                ps_ = tuple(GI * h2_ + q_ for q_ in range(GI))
                for p_ in ps_:
                    j = p_ % GI
                    S.op("dve", lambda: nc.vector.tensor_tensor(
                        cand[j][:].rearrange("p (a b) -> p a b", a=16),
                        vap(top[:], [[1, 16], [0, 16]], off=32 * p_), vap(top[:], [[0, 16], [1, 16]], off=32 * p_ + 16), ALU.add),
                        [t1_b[2 * p_], t1_b[2 * p_ + 1]], [cd_b[j]])
                for p_ in ps_:
                    j = p_ % GI
                    S.op("dve", lambda: nc.vector.max(out=sel[:, p_, 0:8], in_=cand[j][:]), [cd_b[j]], [sel_b[p_]])
                for p_ in ps_:
                    j = p_ % GI
                    S.op("dve", lambda: nc.vector.max_index(out=posu[:, p_, 0:8], in_max=sel[:, p_, 0:8],
                                                            in_values=cand[j][:]), [cd_b[j], sel_b[p_]], [pos_b[p_]])
                for p_ in ps_:
                    j = p_ % GI
                    S.op("dve", lambda: nc.vector.match_replace(out=cand2[j][:], in_to_replace=sel[:, p_, 0:8],
                                                                in_values=cand[j][:], imm_value=-1e30),
                         [cd_b[j], sel_b[p_]], [cd2_b[j]])
                for p_ in ps_:
                    j = p_ % GI
                    S.op("dve", lambda: nc.vector.max(out=sel[:, p_, 8:16], in_=cand2[j][:]), [cd2_b[j]], [sel_b[p_]])
                for p_ in ps_:
                    j = p_ % GI
                    S.op("dve", lambda: nc.vector.max_index(out=posu[:, p_, 8:16], in_max=sel[:, p_, 8:16],
                                                            in_values=cand2[j][:]), [cd2_b[j], sel_b[p_]], [pos_b[p_]])
                for _y in range(GI):
                    yield
            S.op("dve", lambda: nc.vector.tensor_copy(posf[:], posu[:].rearrange("p a b -> p (a b)")), pos_b, [posf_b])
            S.op("dve", lambda: nc.vector.tensor_tensor(esel[:], sel[:], vap(sel[:], [[16, 8], [0, 16]]), ALU.subtract),
                 sel_b, [es_b])
            S.op("dve", lambda: nc.vector.tensor_copy(idxf[:], idxu[:]), i1_b, [idxf_b])
            S.op("act", lambda: nc.scalar.activation(esel[:], esel[:], AF.Exp), [es_b], [es_b])
            gA = ge16[:].rearrange("p j a -> p (j a)")
            S.op("dve", lambda: nc.vector.tensor_tensor(ge16[:], vap(posf[:], [[1, P], [0, 16]]),
                                                        vap(thr16, [[0, P], [1, 16]]), ALU.is_ge),
                 [posf_b, cb] + g16_b, g16_b)
            S.op("dve", lambda: nc.vector.tensor_reduce(zs[:], esel[:], AX.X, ALU.add), [es_b], [zs_b])
            S.op("dve", lambda: nc.vector.tensor_reduce(af[:], ge16[:], AX.X, ALU.add), g16_b, [af_b])
            S.op("dve", lambda: nc.vector.reciprocal(zs[:], zs[:]), [zs_b], [zs_b])
            S.op("dve", lambda: nc.vector.tensor_scalar(af[:], af[:], -1.0, None, ALU.add), [af_b], [af_b])
            S.op("dve", lambda: nc.vector.tensor_tensor(abf[:, 2, :].rearrange("p (h k) -> p h k", h=8), esel[:],
                                                        vap(zs[:], [[1, 8], [0, 16]]), ALU.mult), [es_b, zs_b], [abf_b[2]])
            S.op("dve", lambda: nc.vector.scalar_tensor_tensor(bf_[:], af[:], -16.0, posf[:], ALU.mult, ALU.add),
                 [af_b, posf_b], [bfb_b])
            yield
            H_ = P // 2
            for which, src, srcb, o_ in ((0, af, af_b, 0), (1, bf_, bfb_b, 16)):
                for hv in range(2):
                    S.op("dve", lambda: nc.vector.tensor_tensor(
                        ge16[:, hv * H_:(hv + 1) * H_, :], vap(src[:, hv * H_:(hv + 1) * H_], [[1, H_], [0, 16]]),
                        vap(iota16, [[0, H_], [1, 16]]), ALU.is_equal), [srcb, cb, g16_b[hv]], [g16_b[hv]])
                for hv in range(2):
                    S.op("dve", lambda: nc.vector.tensor_tensor(
                        ge16[:, hv * H_:(hv + 1) * H_, :].rearrange("p (h k) a -> p h k a", h=4),
                        ge16[:, hv * H_:(hv + 1) * H_, :].rearrange("p (h k) a -> p h k a", h=4),
                        vap(idxf[:], [[32, 4], [0, 16], [1, 16]], off=o_ + hv * 128), ALU.mult),
                        [g16_b[hv], idxf_b], [g16_b[hv]])
                for hv in range(2):
                    S.op("dve", lambda: nc.vector.tensor_reduce(abf[:, which, hv * H_:(hv + 1) * H_],
                                                                ge16[:, hv * H_:(hv + 1) * H_, :], AX.X, ALU.add),
                         [g16_b[hv]], [abf_b[which]])
                yield
            S.op("dve", lambda: nc.vector.tensor_copy(ab[:], abf[:]), abf_b + [ab_b], [ab_b])

        def e1_tail(i):
            kb = i % 2
            for j in range(3):
                S.op("pe", lambda: nc.tensor.transpose(psb[kb][:, j * P:(j + 1) * P], ab[:, j, :], ident[:]),
                     [ab_b, cc], [psb_b[kb]])
            S.op("act", lambda: nc.scalar.copy(abT2[:, kb, :, :].rearrange("p a b -> p (a b)"), psb[kb][:, 0:3 * P]),
                 [psb_b[kb]], [abT2_b[kb]])

        def e2(i):
            kb = i % 2
            for sub in range(P // SUB):
                s_ = cnt["it"] % NAB
                cnt["it"] += 1
                t0 = sub * SUB
                io_bc = vap(iota128[:], [[0, SUB], [1, P]])
                S.op("dve", lambda: nc.vector.tensor_tensor(A2[s_][:], io_bc, vap(abT2[:, kb, 1, t0:t0 + SUB], [[1, SUB], [0, P]]),
                                                            ALU.is_equal), [abT2_b[kb], cc], [A2_b[s_]])
                S.op("dve", lambda: nc.vector.tensor_tensor(A1[s_][:], io_bc, vap(abT2[:, kb, 0, t0:t0 + SUB], [[1, SUB], [0, P]]),
                                                            ALU.is_equal), [abT2_b[kb], cc], [A1_b[s_]])
                S.op("pool", lambda: nc.gpsimd.tensor_tensor(A1[s_][:], A1[s_][:], vap(abT2[:, kb, 2, t0:t0 + SUB], [[1, SUB], [0, P]]),
                                                             ALU.mult), [abT2_b[kb], A1_b[s_]], [A1_b[s_]])
                for t8 in range(SUB // 8):
                    k0 = (cnt["bk"] % 3) * 2
                    cnt["bk"] += 1
                    for u8 in range(8):
                        tt = t8 * 8 + u8
                        kk_ = k0 + u8 // 4
                        S.op("pe", lambda: nc.tensor.matmul(vap(psf[kk_], [[4, P]], off=u8 % 4), A2[s_][:, tt, :], A1[s_][:, tt, :],
                                                            start=True, stop=True, skip_group_check=True),
                             [A1_b[s_], A2_b[s_]], [psf_b[kk_]])
                    tok = t0 + t8 * 8
                    S.op("act", lambda: nc.scalar.copy(vap(WT[:], [[P, P], [4, 2], [1, 4]], off=tok),
                                                       vap(psf[k0], [[4, P], [512, 2], [1, 4]])),
                         [psf_b[k0], psf_b[k0 + 1]], [WT_b])
                yield
            S.dma("sp", Wd[i], WT[:].rearrange("p a b -> p (a b)"), WT_b, reads=[WT_b], writes=[wd_b[i]])

        e1_front(0)
        for i in range(NT):
            g2 = e2(i - 1) if i >= 1 else iter(())
            kk2 = 0
            for _ in e1_chain(i):
                kk2 += 1
                if kk2 == 16 and i + 1 < NT:
                    e1_front(i + 1)
                if (kk2 * 16) // 27 > ((kk2 - 1) * 16) // 27:
                    next(g2, None)
            for _ in g2:
                pass
            e1_tail(i)
        for _ in e2(NT - 1):
            pass
        S.barrier()
    pE.close()
    if upto == "E":
        return nc

    NB = EG // P
    with Scope(mem) as st:
        ustg = sb("ustg0", [P, 8, EG], F32, st)
        vstg = sb("vstg0", [P, NB, D], F32, st)
        ustg_b, vstg_b = Buf("ustg0"), Buf("vstg0")
        ubf = [sb("ubf%d" % i, [P, 8, EG], BF, st) for i in range(2)]
        vbf = [sb("vbf%d" % i, [P, NB, D], BF, st) for i in range(2)]
        ubf_b = [Buf("ubf0"), Buf("ubf1")]
        vbf_b = [Buf("vbf0"), Buf("vbf1")]
        WTg = [sb("WTg%d" % i, [P, 4, NB * P], BF, st) for i in range(3)]
        WTg_b = [Buf("WTg%d" % i) for i in range(3)]
        ge = [sb("ge%d" % i, [P, 512], BF, st) for i in range(2)]
        ge_b = [Buf("ge0"), Buf("ge1")]
        GT = [sb("GT%d" % i, [P, NB, 512], BF, st) for i in range(2)]
        GT_b = [Buf("GT0"), Buf("GT1")]

        def load_group(g):
            S.dma("sp", ustg[:], UT_d[:, :, g * EG:(g + 1) * EG], ustg_b, writes=[ustg_b])
            S.dma("sp", vstg[:], V_d[g * EG:(g + 1) * EG, :].rearrange("(b p) d -> p b d", p=P), vstg_b,
                  writes=[vstg_b])

        def cast_group(g):
            s_ = g % 2
            S.op("pool", lambda: nc.gpsimd.tensor_tensor(ubf[s_][:], ustg[:], vap(g_ffn[:], [[1, 8], [0, EG]]), ALU.mult),
                 [ustg_b, cb], [ubf_b[s_]])
            S.op("pool", lambda: nc.gpsimd.tensor_copy(vbf[s_][:], vstg[:]), [vstg_b], [vbf_b[s_]])

        seq = [(g, q) for g in range(NG) for q in range(4)]
        NTOT = len(seq)

        def load_w(n):
            g, q = seq[n]
            S.dma("sp", WTg[n % 3][:], Wd[4 * q:4 * q + 4, :, g * EG:(g + 1) * EG].rearrange("a p f -> p a f"),
                  WTg_b[n % 3], reads=wd_b[4 * q:4 * q + 4], writes=[WTg_b[n % 3]])

        abank = [0]

        def st_AG(n):
            g, q = seq[n]
            s_, gt = g % 2, n % 2
            for b_ in range(NB):
                pa = abank[0] % 2
                abank[0] += 1
                for c in range(8):
                    S.op("pe", lambda: nc.tensor.matmul(psf[pa][:], ubf[s_][:, c, b_ * P:(b_ + 1) * P],
                                                        h2T[:, c, q * 512:(q + 1) * 512], start=(c == 0), stop=(c == 7),
                                                        skip_group_check=True), [h2T_b, ubf_b[s_]], [psf_b[pa]])
                S.op("act", lambda: nc.scalar.activation(ge[pa][:], psf[pa][:], AF.Gelu), [psf_b[pa]], [ge_b[pa]])
                S.op("dve", lambda: nc.vector.tensor_tensor(
                    GT[gt][:, b_, :].rearrange("p (a t) -> p a t", a=4), ge[pa][:].rearrange("p (a t) -> p a t", a=4),
                    vap(WTg[n % 3][:], [[NB * P, 4], [1, P]], off=b_ * P), ALU.mult),
                    [ge_b[pa], WTg_b[n % 3]], [GT_b[gt]])

        ybank = [0]

        def st_Y(n):
            g, q = seq[n]
            s_, gt = g % 2, n % 2
            for u in range(4):
                i = 4 * q + u
                for half in range(2):
                    py = 2 + ybank[0] % 4
                    ybank[0] += 1
                    for b_ in range(NB):
                        S.op("pe", lambda: nc.tensor.matmul(psf[py][:], GT[gt][:, b_, u * P:(u + 1) * P],
                                                            vbf[s_][:, b_, half * 512:(half + 1) * 512], start=(b_ == 0),
                                                            stop=(b_ == NB - 1), skip_group_check=True),
                             [GT_b[gt], vbf_b[s_]], [psf_b[py]])
                    S.op("dve", lambda: nc.vector.tensor_tensor(y_acc[:, i, half * 512:(half + 1) * 512],
                                                                psf[py][:], y_acc[:, i, half * 512:(half + 1) * 512],
                                                                ALU.add), [psf_b[py], y_b[i]], [y_b[i]])

        load_group(0)
        cast_group(0)
        if NG > 1:
            load_group(1)
        load_w(0)
        load_w(1)
        for n in range(NTOT):
            g, q = seq[n]
            if n + 2 < NTOT:
                load_w(n + 2)
            if q == 2 and g + 1 < NG:
                cast_group(g + 1)
                if g + 2 < NG:
                    load_group(g + 2)
            st_AG(n)
            if n >= 1:
                st_Y(n - 1)
        st_Y(NTOT - 1)
        ob = Buf("outst")
        for i in range(NT):
            S.dma("sp", out_d[i * P:(i + 1) * P, :], y_acc[:, i, :], ob, reads=[y_b[i]])
        S.barrier()
    pD.close()
    es.close()
    return nc


_HOST_CACHE = {}


def _prep_shared(inp):
    f = np.float32
    sh = {}
    sh["invf"] = np.ascontiguousarray(np.broadcast_to(
        (1.0 / (10000.0 ** (np.arange(0, 64, 2, dtype=f) / f(64)))).astype(f)[None, :], (P, 32)))
    sh["ident"] = np.eye(P, dtype=f)
    s = np.arange(P)[:, None]
    t = np.arange(P)[None, :]
    same = (s // 64) == (t // 64)
    sh["maskf"] = (same & (s <= t)).astype(f)
    sh["maskb"] = (same & (s >= t)).astype(f)
    rm = np.ones((P, T), f)
    rm[:, ::64] = 0.0
    sh["resetm"] = rm
    io = np.zeros((P, 160), f)
    io[:, 0:128] = np.arange(128, dtype=f)[None, :]
    io[:, 128:144] = np.arange(16, dtype=f)[None, :]
    io[:, 144:160] = 16.0 * np.arange(16, dtype=f)[None, :]
    sh["iota"] = io

    def pc(v):
        return np.ascontiguousarray(np.asarray(v, f).reshape(-1, P).T)

    def rep(v):
        v = np.asarray(v, f).reshape(1, -1)
        return np.ascontiguousarray(np.broadcast_to(v, (P, v.shape[1])))

    def kc(w):
        w = np.asarray(w, f)
        return np.ascontiguousarray(w.reshape(-1, P, w.shape[1]).transpose(1, 0, 2))

    sh["g_attn"] = pc(inp["attn_norm"][0])
    sh["g_ffn"] = pc(inp["ffn_norm"][0])
    lbl = np.asarray(inp["hg_lb_logits"], f)
    sh["lbl"] = np.ascontiguousarray(lbl.reshape(2, 2, 4, P).transpose(3, 0, 1, 2).reshape(P, 16))
    sh["g_hgo"] = np.ascontiguousarray(np.asarray(inp["hg_o_norm"][0], f).T)
    sh["g_qa"] = pc(inp["q_a_norm"][0])
    sh["g_kva"] = pc(inp["kv_a_norm"][0])
    sh["g_q"] = rep(inp["q_norm"][0])
    sh["g_k"] = rep(inp["k_norm"][0])
    sh["g_mo"] = rep(inp["mla_o_norm"][0])
    sh["w_in"] = kc(inp["w_in"][0])
    sh["w_qup"] = kc(inp["w_q_up"][0])
    sh["w_kvup"] = kc(inp["w_kv_up"][0])
    sh["w_out"] = kc(inp["w_out"][0])
    wq = np.asarray(inp["peer_w_q"][0], f)
    sh["wqT"] = np.ascontiguousarray(wq.reshape(D, 16, P).transpose(2, 1, 0))
    keys = np.asarray(inp["peer_sub_keys"][0], f)
    sh["keysT"] = np.ascontiguousarray(keys.reshape(16, P, P).transpose(2, 0, 1))
    u = np.asarray(inp["peer_u"][0], f)
    sh["UT"] = np.ascontiguousarray(u.reshape(NEXP, 8, P).transpose(2, 1, 0))
    sh["V"] = np.ascontiguousarray(np.asarray(inp["peer_v"][0], f))
    return sh


def make_in_maps(inputs, cores):
    sh = _prep_shared(inputs)
    x = np.asarray(inputs["x"], np.float32)
    pos = np.asarray(inputs["positions"], np.int32)
    maps = []
    for b in cores:
        m = dict(sh)
        m["x"] = np.ascontiguousarray(x[b])
        m["posT"] = np.ascontiguousarray(pos[b].reshape(NT, P).T)
        maps.append(m)
    return maps


def kernel(**inputs):
    nc = build_program()
    in_maps = make_in_maps(inputs, list(range(8)))
    res = run_bass_kernel_spmd(nc, in_maps, core_ids=list(range(8)))
    out = np.stack([np.asarray(r["out"], np.float32) for r in res.results], axis=0)
    return out
```

```python
import numpy as np
from contextlib import ExitStack
import concourse.bass as bass
import concourse.mybir as mybir
from concourse.bass_utils import run_bass_kernel_spmd

F32 = mybir.dt.float32
BF = mybir.dt.bfloat16
I32 = mybir.dt.int32
AF = mybir.ActivationFunctionType
ALU = mybir.AluOpType
AX = mybir.AxisListType

P = 128
T = 2048
NT = 16
D = 1024
EPS = 1e-6
NEXP = 16384
EG = 512
NG = NEXP // EG
IC = 16
NIC = 128 // IC
PI = float(np.pi)


class Buf:
    def __init__(self, name):
        self.name = name
        self.writer = None
        self.readers = []
        self.dsem = None
        self.dcnt = 0


class Sch:
    def __init__(self, nc):
        self.nc = nc
        self.eng = dict(pe=nc.tensor, dve=nc.vector, act=nc.scalar, pool=nc.gpsimd, sp=nc.sync)
        self.sem = {e: nc.alloc_semaphore("sem_" + e) for e in ("pe", "dve", "act", "pool")}
        self.cnt = {e: 0 for e in self.sem}
        self.seen = {e: {} for e in self.eng}
        self.dbufs = []

    def _wait(self, e, dep):
        key, h, v = dep
        if self.seen[e].get(key, 0) >= v:
            return
        self.eng[e].wait_ge(h, v)
        self.seen[e][key] = v

    def _deps(self, e, reads, writes):
        deps = []
        for b in reads:
            if b.writer is not None:
                deps.append(b.writer)
        for b in writes:
            if b.writer is not None:
                deps.append(b.writer)
            deps.extend(b.readers)
        for d in deps:
            if e == "pe" and d[0] == "pe":
                continue
            self._wait(e, d)

    def _mark(self, tok, reads, writes):
        for b in reads:
            b.readers.append(tok)
        for b in writes:
            b.writer = tok
            b.readers = []

    def op(self, e, fn, reads=(), writes=()):
        self._deps(e, reads, writes)
        ins = fn()
        self.cnt[e] += 1
        ins.then_inc(self.sem[e], 1)
        self.seen[e][e] = max(self.seen[e].get(e, 0), 0)
        self._mark((e, self.sem[e], self.cnt[e]), reads, writes)

    def dma(self, q, out, in_, sb, reads=(), writes=()):
        self._deps(q, reads, writes)
        if sb.dsem is None:
            sb.dsem = self.nc.alloc_semaphore("dsem_" + sb.name)
            self.dbufs.append(sb)
        ins = self.eng[q].dma_start(out=out, in_=in_)
        sb.dcnt += 16
        ins.then_inc(sb.dsem, 16)
        self._mark((("d", sb.name), sb.dsem, sb.dcnt), reads, writes)

    def barrier(self):
        for e in self.eng:
            for f in self.sem:
                if f != e and self.cnt[f] > 0:
                    self._wait(e, (f, self.sem[f], self.cnt[f]))
            for b in self.dbufs:
                if b.dcnt > 0:
                    self._wait(e, (("d", b.name), b.dsem, b.dcnt))


class Mem:
    def __init__(self, lo, hi):
        self.free = [(lo, hi)]

    def alloc(self, n):
        n = (n + 63) // 64 * 64
        for k, (a, b) in enumerate(self.free):
            if b - a >= n:
                self.free[k] = (a + n, b)
                return a, n
        raise MemoryError("SBUF arena exhausted (%d bytes) free=%s" % (n, self.free))

    def release(self, a, n):
        fl = sorted(self.free + [(a, a + n)])
        out = []
        for lo, hi in fl:
            if out and out[-1][1] >= lo:
                out[-1] = (out[-1][0], max(out[-1][1], hi))
            elif hi > lo:
                out.append((lo, hi))
        self.free = out


class Scope:
    def __init__(self, mem):
        self.mem = mem
        self.items = []

    def __enter__(self):
        return self

    def __exit__(self, *a):
        self.close()
        return False

    def close(self):
        for a, n in self.items:
            self.mem.release(a, n)
        self.items = []


DT_BYTES = {}


def vap(base, dims, off=0):
    return bass.AP(base.tensor, base.offset + off, [list(base.ap[0])] + [list(d) for d in dims])


def build_program(debug=None, upto=None):
    debug = debug or {}
    nc = bass.Bass("TRN2", target_bir_lowering=False)
    S = Sch(nc)

    def din(name, shape, dt=F32):
        return nc.dram_tensor(name, list(shape), dt, kind="ExternalInput").ap()

    x_d = din("x", [T, D])
    pos_d = din("posT", [P, NT], I32)
    invf_d = din("invf", [P, 32])
    ident_d = din("ident", [P, P])
    maskf_d = din("maskf", [P, P])
    maskb_d = din("maskb", [P, P])
    reset_d = din("resetm", [P, T])
    gattn_d = din("g_attn", [P, 8])
    gffn_d = din("g_ffn", [P, 8])
    lbl_d = din("lbl", [P, 16])
    ghgo_d = din("g_hgo", [P, 4])
    gqa_d = din("g_qa", [P, 3])
    gkva_d = din("g_kva", [P, 2])
    gq_d = din("g_q", [P, 192])
    gk_d = din("g_k", [P, 192])
    gmo_d = din("g_mo", [P, 512])
    iota_d = din("iota", [P, 160])
    win_d = din("w_in", [P, 8, 3264])
    wqup_d = din("w_qup", [P, 3, 768])
    wkvup_d = din("w_kvup", [P, 2, 1024])
    wout_d = din("w_out", [P, 8, 1024])
    wqT_d = din("wqT", [P, 16, 1024])
    keysT_d = din("keysT", [P, 16, 128])
    UT_d = din("UT", [P, 8, NEXP])
    V_d = din("V", [NEXP, D])
    out_d = nc.dram_tensor("out", [T, D], F32, kind="ExternalOutput").ap()
    Wd = nc.dram_tensor("Wd", [NT, P, NEXP], BF).ap()
    dbg_out = {}
    for k, (shp, dt_) in debug.items():
        dbg_out[k] = nc.dram_tensor("dbg_" + k, list(shp), dt_, kind="ExternalOutput").ap()

    es = ExitStack()
    mem = Mem(16512 + 64, 229344 - 64)
    root = Scope(mem)

    def sb(name, shape, dt=F32, stack=None):
        nbytes = int(np.prod(shape[1:])) * (4 if dt in (F32, I32, mybir.dt.uint32) else 2)
        a, n = mem.alloc(nbytes)
        (stack or root).items.append((a, n))
        addr_of[name] = a
        return nc.alloc_sbuf_tensor_at(name, list(shape), dt, offset=a)

    addr_of = {}

    def sb_alias(name, shape, dt, like):
        return nc.alloc_sbuf_tensor_at(name, list(shape), dt, offset=addr_of[like])

    psf_all = es.enter_context(nc.psum_tensor("psf_all", [P, 6, 512], F32))
    psf = [psf_all[:, i, :] for i in range(6)]
    psb = [es.enter_context(nc.psum_tensor("psb%d" % i, [P, 1024], BF)) for i in range(2)]
    psf_b = [Buf("psf%d" % i) for i in range(6)]
    psb_b = [Buf("psb%d" % i) for i in range(2)]

    cb = Buf("consts")
    pEarly = Scope(mem)
    ident_f = sb("ident_f", [P, P], F32, pEarly)
    ident = sb("ident", [P, P], BF)
    maskf = sb("maskf", [P, P], F32, pEarly)
    maskb = sb("maskb", [P, P], F32, pEarly)
    resetm = sb("resetm", [P, T], F32, pEarly)
    invf = sb("invf", [P, 32])
    posT = sb("posT", [P, NT], I32)
    g_attn = sb("g_attn", [P, 8])
    g_ffn = sb("g_ffn", [P, 8])
    lbl = sb("lbl", [P, 16])
    g_hgo = sb("g_hgo", [P, 4])
    g_qa = sb("g_qa", [P, 3])
    g_kva = sb("g_kva", [P, 2])
    g_q = sb("g_q", [P, 192], F32, pEarly)
    g_k = sb("g_k", [P, 192], F32, pEarly)
    g_mo = sb("g_mo", [P, 512], F32, pEarly)
    ones_bf = sb("ones_bf", [P, P], BF)
    iota_f = sb("iota_f", [P, 160])
    iota128 = sb("iota128", [P, P], BF)
    for dst, src in ((ident_f, ident_d), (maskf, maskf_d), (maskb, maskb_d), (resetm, reset_d),
                     (invf, invf_d), (posT, pos_d), (g_attn, gattn_d), (g_ffn, gffn_d), (lbl, lbl_d),
                     (g_hgo, ghgo_d), (g_qa, gqa_d), (g_kva, gkva_d), (g_q, gq_d), (g_k, gk_d),
                     (g_mo, gmo_d), (iota_f, iota_d)):
        S.dma("sp", dst[:], src, cb, writes=[cb])
    cc = Buf("consts2")
    S.op("dve", lambda: nc.vector.tensor_copy(ident[:], ident_f[:]), [cb], [cc])
    S.op("dve", lambda: nc.vector.memset(ones_bf[:], 1.0), [], [cc])
    S.op("dve", lambda: nc.vector.tensor_copy(iota128[:], iota_f[:, 0:128]), [cb], [cc])
    lb = sb("lb", [P, 8])
    oml = sb("oml", [P, 8])
    noml = sb("noml", [P, 8])
    S.op("dve", lambda: nc.vector.tensor_sub(lb[:], lbl[:, 0:8], lbl[:, 8:16]), [cb], [cc])
    S.op("act", lambda: nc.scalar.activation(lb[:], lb[:], AF.Sigmoid), [cc], [cc])
    S.op("dve", lambda: nc.vector.tensor_scalar(oml[:], lb[:], -1.0, 1.0, ALU.mult, ALU.add), [cc], [cc])
    S.op("dve", lambda: nc.vector.tensor_scalar(noml[:], oml[:], -1.0, None, ALU.mult), [cc], [cc])
    cosT = sb("cosT", [P, NT, 32], F32, pEarly)
    sinT = sb("sinT", [P, NT, 32], F32, pEarly)
    with Scope(mem) as st:
        posf = sb("posf", [P, NT], F32, st)
        ang = sb("ang", [P, NT, 32], F32, st)
        ang2 = sb("ang2", [P, NT, 32], F32, st)
        S.op("dve", lambda: nc.vector.tensor_copy(posf[:], posT[:]), [cb], [cc])
        S.op("dve", lambda: nc.vector.tensor_tensor(
            ang[:], vap(posf[:], [[1, NT], [0, 32]]), vap(invf[:], [[0, NT], [1, 32]]), ALU.mult), [cc, cb], [cc])
        ri = sb("ri", [P, NT, 32], I32, st)
        rf = sb("rf", [P, NT, 32], F32, st)
        hi = sb("hi", [P, NT, 32], F32, st)
        S.op("dve", lambda: nc.vector.tensor_scalar(ang[:], ang[:], 1.0 / (2 * PI), None, ALU.mult), [cc], [cc])
        S.op("dve", lambda: nc.vector.tensor_scalar(ang2[:], ang[:], 0.25, None, ALU.add), [cc], [cc])
        for src, dst in ((ang, sinT), (ang2, cosT)):
            S.op("dve", lambda: nc.vector.tensor_copy(ri[:], src[:]), [cc], [cc])
            S.op("dve", lambda: nc.vector.tensor_copy(rf[:], ri[:]), [cc], [cc])
            S.op("dve", lambda: nc.vector.tensor_sub(src[:], src[:], rf[:]), [cc], [cc])
            S.op("dve", lambda: nc.vector.tensor_scalar(hi[:], src[:], 0.5, None, ALU.is_gt), [cc], [cc])
            S.op("dve", lambda: nc.vector.tensor_sub(src[:], src[:], hi[:]), [cc], [cc])
            S.op("dve", lambda: nc.vector.tensor_scalar(hi[:], src[:], -0.5, None, ALU.is_lt), [cc], [cc])
            S.op("dve", lambda: nc.vector.tensor_add(src[:], src[:], hi[:]), [cc], [cc])
            S.op("act", lambda: nc.scalar.activation(dst[:], src[:], AF.Sin, scale=2 * PI), [cc], [cc])
        S.barrier()
    epsb = sb("epsb", [P, 1])
    S.op("dve", lambda: nc.vector.memset(epsb[:], EPS), [], [cc])
    if upto == "0":
        S.barrier()
        return nc

    def dump(name, src_ap, rbufs):
        if name in dbg_out:
            S.barrier()
            tb = Buf("dbg_" + name)
            S.dma("sp", dbg_out[name], src_ap, tb, reads=rbufs)
            S.barrier()

    def rstd_from_ss(dst, ss, n, bufs):
        S.op("act", lambda: nc.scalar.activation(dst, ss, AF.Sqrt, bias=epsb[:dst.shape[0]], scale=1.0 / n), bufs + [cc], bufs)
        S.op("dve", lambda: nc.vector.reciprocal(dst, dst), bufs, bufs)

    pM = Scope(mem)
    mixT = sb("mixT", [P, 8, T], BF, pM)
    mix_b = Buf("mixT")
    ph1 = Scope(mem)
    xnT = sb("xnT", [P, 8, T], BF, ph1)
    xnT_b = Buf("xnT")
    stg = [sb("stg0", [P, 8, 512], F32, ph1)] * 2
    stg_b = [Buf("stg0")] * 2
    with Scope(mem) as st:
        xt = [sb("xt%d" % i, [P, D], F32, st) for i in range(2)]
        xt_b = [Buf("xt%d" % i) for i in range(2)]
        xn = [sb("xn%d" % i, [P, D], BF, st) for i in range(2)]
        xn_b = [Buf("xn%d" % i) for i in range(2)]
        junk = sb("junkA", [P, D], BF, st)
        junk_b = Buf("junkA")
        ssA = sb("ssA", [P, NT], F32, st)
        ssA_b = [Buf("ssA%d" % i) for i in range(NT)]
        def a_front(i):
            j = i % 2
            S.dma("sp", xt[j][:], x_d[i * P:(i + 1) * P, :], xt_b[j], writes=[xt_b[j]])
            S.op("act", lambda: nc.scalar.activation(junk[:], xt[j][:], AF.Square, accum_out=ssA[:, i:i + 1]),
                 [xt_b[j]], [junk_b, ssA_b[i]])
            rstd_from_ss(ssA[:, i:i + 1], ssA[:, i:i + 1], D, [ssA_b[i]])
            S.op("dve", lambda: nc.vector.tensor_scalar(xn[j][:], xt[j][:], ssA[:, i:i + 1], None, ALU.mult),
                 [xt_b[j], ssA_b[i]], [xn_b[j]])

        def a_tail(i):
            j = i % 2
            pb = psb_b[i % 2]
            for c in range(8):
                S.op("pe", lambda: nc.tensor.transpose(psb[i % 2][:, c * P:(c + 1) * P], xn[j][:, c * P:(c + 1) * P], ident[:]),
                     [xn_b[j], cc], [pb])
            S.op("act", lambda: nc.scalar.copy(
                xnT[:, :, i * P:(i + 1) * P], vap(psb[i % 2][:], [[P, 8], [1, P]])), [pb], [xnT_b])

        a_front(0)
        for i in range(NT):
            if i + 1 < NT:
                a_front(i + 1)
            a_tail(i)
        S.barrier()

    if upto == "A":
        return nc
    def load_wslice(dst, dst_b, src_d, nchunks, cols, gain, slot):
        sg, sgb = stg[slot], stg_b[slot]
        o = 0
        for (c0, w) in cols:
            S.dma("sp", sg[:, 0:nchunks, o:o + w], src_d[:, :, c0:c0 + w], sgb, writes=[sgb])
            o += w
        for c in range(nchunks):
            if gain is not None:
                S.op("dve", lambda: nc.vector.tensor_scalar(dst[:, c, 0:o], sg[:, c, 0:o], gain[:, c:c + 1], None, ALU.mult),
                     [sgb, cb], [dst_b])
            else:
                S.op("dve", lambda: nc.vector.tensor_copy(dst[:, c, 0:o], sg[:, c, 0:o]), [sgb], [dst_b])

    def proj_fm(ps_ap, ps_b, w, w_b, col0, width, t0, n, src=None, src_b=None, nch=8):
        src = xnT if src is None else src
        src_b = xnT_b if src_b is None else src_b
        for c in range(nch):
            S.op("pe", lambda: nc.tensor.matmul(ps_ap, w[:, c, col0:col0 + width], src[:, c, t0:t0 + n],
                                                start=(c == 0), stop=(c == nch - 1), skip_group_check=True),
                 [w_b, src_b], [ps_b])

    with Scope(mem) as st:
        vtok = sb("vtok", [P, NT, 512], BF, st)
        vtok_b = Buf("vtok")
        wv = sb("wv", [P, 8, 512], BF, st)
        wv_b = Buf("wv")
        load_wslice(wv, wv_b, win_d, 8, [(1536, 512)], g_attn, 0)
        for i in range(NT):
            k = i % 2
            for c in range(8):
                S.op("pe", lambda: nc.tensor.matmul(psf[k][:], xnT[:, c, i * P:(i + 1) * P], wv[:, c, :],
                                                    start=(c == 0), stop=(c == 7), skip_group_check=True),
                     [xnT_b, wv_b], [psf_b[k]])
            S.op("act", lambda: nc.scalar.copy(vtok[:, i, :], psf[k][:]), [psf_b[k]], [vtok_b])
        wh = [wv] * 2
        wh_b = [wv_b] * 2
        if upto == "B1":
            S.barrier()
            return nc
        H = 1024
        qs = sb("qs", [P, T], F32, st)
        gsl = sb("gsl", [P, T], BF, st)
        glog = sb("glog", [P, H], F32, st)
        bcum = sb("bcum", [P, H], F32, st)
        kk = sb("kk", [P, H], F32, st)
        e1 = sb("e1", [P, H], F32, st)
        e2 = glog
        Qt = sb("Qt", [P, 2, T], BF, st)
        Kt = sb("Kt", [P, 2, T], BF, st)
        Ktok = sb("Ktok", [P, 2, NT, P], BF, st)
        Sbf = sb("Sbf", [P, 2, 32, P], BF, st)
        vm = sb("vm", [P, NT, 2, P], BF, st)
        vm_b = Buf("vm")
        Sm = [sb("Sm%d" % i, [P, 2, P], F32, st) for i in range(2)]
        dSd = sb("dSd", [P, 2, P], F32, st)
        dch = sb("dch", [P, 2, 32], F32, st)
        Pm = [sb("Pm%d" % i, [P, 2, P], BF, st) for i in range(2)]
        osq = sb("osq", [P, 512], BF, st)
        rbc = kk
        otmp = e1
        hb = Buf("hg_elem")
        qk_b = Buf("QtKt")
        ktok_b = Buf("Ktok")
        sbf_b = Buf("Sbf")
        sm_b = Buf("Sm")
        pm_b = [Buf("Pm0"), Buf("Pm1")]
        ob = Buf("onorm")
        for h in range(4):
            w = wh[h % 2]
            wb = wh_b[h % 2]
            load_wslice(w, wb, win_d, 8, [(h * P, P), (512 + h * P, P), (1024 + h * P, P), (2048 + h * P, P)],
                        g_attn, (h + 1) % 2)
            for tb in range(4):
                k = tb % 2
                proj_fm(psf[k][:], psf_b[k], w, wb, 0, P, tb * 512, 512)
                S.op("act", lambda: nc.scalar.activation(qs[:, tb * 512:(tb + 1) * 512], psf[k][:], AF.Silu),
                     [psf_b[k]], [hb])
            for tb in range(4):
                k = tb % 2
                proj_fm(psf[k][:], psf_b[k], w, wb, 384, P, tb * 512, 512)
                S.op("act", lambda: nc.scalar.activation(gsl[:, tb * 512:(tb + 1) * 512], psf[k][:], AF.Silu),
                     [psf_b[k]], [hb])
            for d in range(2):
                col = d * 4 + h
                for hf in range(2):
                    t0 = hf * H
                    for tb in range(2):
                        k = tb % 2
                        proj_fm(psf[k][:], psf_b[k], w, wb, (1 + d) * P, P, t0 + tb * 512, 512)
                        S.op("act", lambda: nc.scalar.activation(e1[:, tb * 512:(tb + 1) * 512], psf[k][:], AF.Sigmoid),
                             [psf_b[k]], [hb])
                    S.op("act", lambda: nc.scalar.activation(glog[:], e1[:], AF.Ln, bias=lb[:, col:col + 1],
                                                             scale=oml[:, col:col + 1]), [hb, cc], [hb])
                    S.op("dve", lambda: nc.vector.tensor_scalar(kk[:], e1[:], noml[:, col:col + 1], oml[:, col:col + 1],
                                                                ALU.mult, ALU.add), [hb, cc], [hb])
                    S.op("dve", lambda: nc.vector.tensor_tensor_scan(bcum[:], resetm[:, t0:t0 + H], glog[:], 0.0,
                                                                      ALU.mult, ALU.add), [hb, cb], [hb])
                    S.op("act", lambda: nc.scalar.activation(dch[:, d, hf * 16:(hf + 1) * 16],
                                                             vap(bcum[:], [[64, 16]], off=63), AF.Exp), [hb], [hb])
                    if d == 1:
                        S.op("dve", lambda: nc.vector.tensor_sub(glog[:], glog[:], bcum[:]), [hb], [hb])
                        S.op("dve", lambda: nc.vector.tensor_tensor(
                            vap(glog[:], [[64, H // 64], [1, 64]]), vap(glog[:], [[64, H // 64], [1, 64]]),
                            vap(bcum[:], [[64, H // 64], [0, 64]], off=63), ALU.add), [hb], [hb])
                        cur = glog
                        cur_ap = glog[:]
                    else:
                        cur_ap = bcum[:]
                    S.op("act", lambda: nc.scalar.activation(e1[:], cur_ap, AF.Exp), [hb], [hb])
                    S.op("act", lambda: nc.scalar.activation(e2[:], cur_ap, AF.Exp, scale=-1.0), [hb], [hb])
                    S.op("dve", lambda: nc.vector.tensor_tensor(Qt[:, d, t0:t0 + H], qs[:, t0:t0 + H], e1[:], ALU.mult),
                         [hb], [qk_b])
                    S.op("dve", lambda: nc.vector.tensor_tensor(Kt[:, d, t0:t0 + H], kk[:], e2[:], ALU.mult),
                         [hb], [qk_b])
            if upto == "B2":
                S.barrier()
                return nc
            for d in range(2):
                for g8 in range(2):
                    pbk = (d * 2 + g8) % 2
                    for u in range(8):
                        i = g8 * 8 + u
                        S.op("pe", lambda: nc.tensor.transpose(psb[pbk][:, u * P:(u + 1) * P], Kt[:, d, i * P:(i + 1) * P], ident[:]),
                             [qk_b, cc], [psb_b[pbk]])
                    S.op("act", lambda: nc.scalar.copy(Ktok[:, d, g8 * 8:(g8 + 1) * 8, :], vap(psb[pbk][:], [[P, 8], [1, P]])),
                         [psb_b[pbk]], [ktok_b])
            if upto == "B3":
                S.barrier()
                return nc
            for jj in range(2):
                S.op("dve", lambda: nc.vector.tensor_scalar(vm[:, :, jj, :], vtok[:, :, h * P:(h + 1) * P],
                                                            maskb[:, jj * 64:jj * 64 + 1], None, ALU.mult),
                     [vtok_b, cb], [vm_b])
            dsd_b = [Buf("dsd0"), Buf("dsd1")]
            smd_b = [Buf("smd0"), Buf("smd1")]
            S.op("pool", lambda: nc.gpsimd.memset(Sm[0][:], 0.0), [], [smd_b[0], smd_b[1]])
            S.op("pool", lambda: nc.gpsimd.memset(Sbf[:, 0, 0, :], 0.0), [], [sbf_b])
            S.op("pool", lambda: nc.gpsimd.memset(Sbf[:, 1, 31, :], 0.0), [], [sbf_b])
            for step in range(31):
                cur, nxt = Sm[step % 2], Sm[(step + 1) % 2]
                cf, cbk = step, 31 - step
                for d, c in ((0, cf), (1, cbk)):
                    kd = 2 + 2 * d + (step % 2)
                    i, jj = c // 2, c % 2
                    S.op("pe", lambda: nc.tensor.matmul(psf[kd][:, 0:P], Ktok[:, d, i, :],
                                                        vm[:, i, jj, :], start=True, stop=True,
                                                        skip_group_check=True), [ktok_b, vm_b], [psf_b[kd]])
                for d, c in ((0, cf), (1, cbk)):
                    kd = 2 + 2 * d + (step % 2)
                    S.op("act", lambda: nc.scalar.mul(dSd[:, d, :], psf[kd][:, 0:P],
                                                      dch[:, d, c:c + 1]), [psf_b[kd], hb], [dsd_b[d]])
                for d, c in ((0, cf), (1, cbk)):
                    S.op("dve", lambda: nc.vector.scalar_tensor_tensor(nxt[:, d, :], cur[:, d, :], dch[:, d, c:c + 1],
                                                                        dSd[:, d, :], ALU.mult, ALU.add),
                         [smd_b[d], dsd_b[d], hb], [smd_b[d]])
                for d, c in ((0, cf), (1, cbk)):
                    cn = c + 1 if d == 0 else c - 1
                    S.op("pool", lambda: nc.gpsimd.tensor_copy(Sbf[:, d, cn, :], nxt[:, d, :]), [smd_b[d]], [sbf_b])
            if upto == "B4":
                S.barrier()
                return nc
            def emit_scores(i):
                ks = i % 2
                for d in range(2):
                    S.op("pe", lambda: nc.tensor.matmul(psf[ks][:, d * P:(d + 1) * P], Kt[:, d, i * P:(i + 1) * P],
                                                        Qt[:, d, i * P:(i + 1) * P], start=True, stop=True,
                                                        skip_group_check=True), [qk_b], [psf_b[ks]])
                S.op("dve", lambda: nc.vector.tensor_tensor(Pm[ks][:, 0, :], psf[ks][:, 0:P], maskf[:], ALU.mult),
                     [psf_b[ks], cb], [pm_b[ks]])
                S.op("dve", lambda: nc.vector.tensor_tensor(Pm[ks][:, 1, :], psf[ks][:, P:2 * P], maskb[:], ALU.mult),
                     [psf_b[ks], cb], [pm_b[ks]])

            emit_scores(0)
            for i in range(NT):
                g4, u = divmod(i, 4)
                po = 4 + (g4 % 2)
                ks = i % 2
                if i + 1 < NT:
                    emit_scores(i + 1)
                oap = psf[po][:, u * P:(u + 1) * P]
                S.op("pe", lambda: nc.tensor.matmul(oap, vtok[:, i, h * P:(h + 1) * P], Pm[ks][:, 0, :], start=True,
                                                    stop=False, skip_group_check=True), [vtok_b, pm_b[ks]], [psf_b[po]])
                S.op("pe", lambda: nc.tensor.matmul(oap, vtok[:, i, h * P:(h + 1) * P], Pm[ks][:, 1, :], start=False,
                                                    stop=False, skip_group_check=True), [vtok_b, pm_b[ks]], [psf_b[po]])
                for d in range(2):
                    for jj in range(2):
                        c = 2 * i + jj
                        S.op("pe", lambda: nc.tensor.matmul(
                            psf[po][:, u * P + jj * 64:u * P + jj * 64 + 64], Sbf[:, d, c, :],
                            Qt[:, d, c * 64:(c + 1) * 64], start=False, stop=(d == 1 and jj == 1),
                            skip_group_check=True), [sbf_b, qk_b], [psf_b[po]])
                if u != 3:
                    continue
                S.op("act", lambda: nc.scalar.activation(osq[:], psf[po][:], AF.Square), [psf_b[po]], [ob])
                S.op("pe", lambda: nc.tensor.matmul(psf[3][:], ones_bf[:], osq[:], start=True, stop=True,
                                                    skip_group_check=True), [ob, cc], [psf_b[3]])
                S.op("act", lambda: nc.scalar.activation(rbc[:, 0:512], psf[3][:], AF.Sqrt, bias=epsb[:], scale=1.0 / 128),
                     [psf_b[3], cc], [ob, hb])
                S.op("dve", lambda: nc.vector.reciprocal(rbc[:, 0:512], rbc[:, 0:512]), [ob, hb], [ob, hb])
                S.op("dve", lambda: nc.vector.scalar_tensor_tensor(otmp[:, 0:512], psf[po][:], g_hgo[:, h:h + 1], rbc[:, 0:512],
                                                                    ALU.mult, ALU.mult), [psf_b[po], ob, cb, hb], [ob, hb])
                S.op("dve", lambda: nc.vector.tensor_tensor(mixT[:, h, g4 * 512:(g4 + 1) * 512], otmp[:, 0:512],
                                                            gsl[:, g4 * 512:(g4 + 1) * 512], ALU.mult), [ob, hb], [mix_b])
        S.barrier()
    dump("mix_hg", mixT[:, 0:4, :], [mix_b])
    if upto == "B":
        return nc

    SCALE = 192.0 ** -0.5
    pc = Scope(mem)
    cT = sb("cT", [P, 5, T], BF, pc)
    cT_b = Buf("cT")
    krraw = sb("krraw", [P, NT, 64], F32, pc)
    kr_b = Buf("krraw")
    rs2 = sb("rs2", [P, NT, 2], F32, pc)
    rs2_b = Buf("rs2")
    wqu = sb("wqu", [P, 3, 768], BF, pc)
    wkvu = sb("wkvu", [P, 2, 1024], BF, pc)
    wu_b = Buf("wup")
    with Scope(mem) as st:
        wm = sb("wm", [P, 8, 704], BF, st)
        wm_b = Buf("wm")
        load_wslice(wm[:, :, 0:512], wm_b, win_d, 8, [(2560, 512)], g_attn, 0)
        load_wslice(wm[:, :, 512:704], wm_b, win_d, 8, [(3072, 192)], g_attn, 1)
        csq = sb("csq", [P, 5, 512], BF, st)
        csq_b = Buf("csq")
        for tb in range(4):
            for j in range(5):
                k = j % 2
                proj_fm(psf[k][:], psf_b[k], wm, wm_b, j * P, P, tb * 512, 512)
                S.op("act", lambda: nc.scalar.copy(cT[:, j, tb * 512:(tb + 1) * 512], psf[k][:]), [psf_b[k]], [cT_b])
                S.op("act", lambda: nc.scalar.activation(csq[:, j, :], psf[k][:], AF.Square), [psf_b[k]], [csq_b])
            for u in range(4):
                i = tb * 4 + u
                for j in range(3):
                    S.op("pe", lambda: nc.tensor.matmul(psf[2][:, i * 2:i * 2 + 1], csq[:, j, u * P:(u + 1) * P],
                                                        ones_bf[:, 0:1], start=(j == 0), stop=(j == 2),
                                                        skip_group_check=True), [csq_b, cc], [psf_b[2]])
                for j in range(2):
                    S.op("pe", lambda: nc.tensor.matmul(psf[2][:, i * 2 + 1:i * 2 + 2], csq[:, 3 + j, u * P:(u + 1) * P],
                                                        ones_bf[:, 0:1], start=(j == 0), stop=(j == 1),
                                                        skip_group_check=True), [csq_b, cc], [psf_b[2]])
        S.op("act", lambda: nc.scalar.copy(rs2[:].rearrange("p a b -> p (a b)"), psf[2][:, 0:2 * NT]), [psf_b[2]], [rs2_b])
        rstd_from_ss(rs2[:, :, 0], rs2[:, :, 0], 384, [rs2_b])
        rstd_from_ss(rs2[:, :, 1], rs2[:, :, 1], 256, [rs2_b])
        for i in range(NT):
            k = 3 + i % 2
            for c in range(8):
                S.op("pe", lambda: nc.tensor.matmul(psf[k][:, 0:64], xnT[:, c, i * P:(i + 1) * P], wm[:, c, 640:704],
                                                    start=(c == 0), stop=(c == 7), skip_group_check=True),
                     [xnT_b, wm_b], [psf_b[k]])
            S.op("act", lambda: nc.scalar.copy(krraw[:, i, :], psf[k][:, 0:64]), [psf_b[k]], [kr_b])
        S.barrier()
    if upto == "C1":
        return nc
    sg0 = stg[0]
    S.dma("sp", sg0[:, 0:3, 0:512], wqup_d[:, :, 0:512], stg_b[0], writes=[stg_b[0]])
    for c in range(3):
        S.op("dve", lambda: nc.vector.tensor_scalar(wqu[:, c, 0:512], sg0[:, c, 0:512], g_qa[:, c:c + 1], None, ALU.mult),
             [stg_b[0], cb], [wu_b])
    S.dma("sp", sg0[:, 0:3, 0:256], wqup_d[:, :, 512:768], stg_b[0], writes=[stg_b[0]])
    for c in range(3):
        S.op("dve", lambda: nc.vector.tensor_scalar(wqu[:, c, 512:768], sg0[:, c, 0:256], g_qa[:, c:c + 1], None, ALU.mult),
             [stg_b[0], cb], [wu_b])
    for half in range(2):
        S.dma("sp", sg0[:, 0:2, 0:512], wkvup_d[:, :, half * 512:(half + 1) * 512], stg_b[0], writes=[stg_b[0]])
        for c in range(2):
            S.op("dve", lambda: nc.vector.tensor_scalar(wkvu[:, c, half * 512:(half + 1) * 512], sg0[:, c, 0:512],
                                                        g_kva[:, c:c + 1], None, ALU.mult), [stg_b[0], cb], [wu_b])
    S.barrier()
    ph1.close()
    qnT = sb("qnT", [P, 4, T], BF, pc)
    knT = sb("knT", [P, 4, T], BF, pc)
    qrT = sb("qrT", [P, 4, T], BF, pc)
    krT = sb("krT", [P, T], BF, pc)
    vaug = sb("vaug", [P, NT, 4, 132], BF, pc)
    qk2_b = Buf("qkT")
    vaug_b = Buf("vaug")
    S.op("pool", lambda: nc.gpsimd.memset(vaug[:], 1.0), [], [vaug_b])
    S.op("pool", lambda: nc.gpsimd.memset(qrT[:], 0.0), [], [qk2_b])
    S.op("pool", lambda: nc.gpsimd.memset(krT[:], 0.0), [], [qk2_b])
    with Scope(mem) as st:
        Qs2 = [sb("Qs%d" % i, [P, 768], F32, st) for i in range(2)]
        KVs2 = [sb("KVs%d" % i, [P, 1024], F32, st) for i in range(2)]
        in_b = [Buf("mla_in0"), Buf("mla_in1")]
        sq = sb("sqm", [P, 1024], F32, st)
        ssn = sb("ssn", [P, 16], F32, st)
        invn = sb("invn", [P, 16], F32, st)
        qn_s = sb("qn_s", [P, 4, P], BF, st)
        kn_s = sb("kn_s", [P, 4, P], BF, st)
        qr_f = sb("qr_f", [P, 4, 64], F32, st)
        kr_f = sb("kr_f", [P, 64], F32, st)
        qr_s = sb("qr_s", [P, 4, 64], BF, st)
        kr_s = sb("kr_s", [P, 64], BF, st)
        ra = sb("ra", [P, 4, 32], F32, st)
        rb_ = sb("rb_", [P, 4, 32], F32, st)
        dv = Buf("mla_dve")
        out_b = Buf("mla_out")
        S.op("dve", lambda: nc.vector.memset(invn[:, 0:4], 1.0 / 128), [], [dv])
        S.op("dve", lambda: nc.vector.memset(invn[:, 4:8], 1.0 / 64), [], [dv])
        S.op("dve", lambda: nc.vector.memset(invn[:, 8:12], 1.0 / 128), [], [dv])
        S.op("dve", lambda: nc.vector.memset(invn[:, 12:16], 1.0 / 64), [], [dv])

        def mla_front(i):
            Qs, KVs, ib = Qs2[i % 2], KVs2[i % 2], in_b[i % 2]
            for half in range(2):
                k = half
                for j in range(3):
                    S.op("pe", lambda: nc.tensor.matmul(psf[k][:, 0:384], cT[:, j, i * P:(i + 1) * P],
                                                        wqu[:, j, half * 384:(half + 1) * 384], start=(j == 0), stop=(j == 2),
                                                        skip_group_check=True), [cT_b, wu_b], [psf_b[k]])
                S.op("act", lambda: nc.scalar.mul(Qs[:, half * 384:(half + 1) * 384], psf[k][:, 0:384],
                                                  rs2[:, i, 0:1]), [psf_b[k], rs2_b], [ib])
            for half in range(2):
                k = 2 + half
                for j in range(2):
                    S.op("pe", lambda: nc.tensor.matmul(psf[k][:], cT[:, 3 + j, i * P:(i + 1) * P],
                                                        wkvu[:, j, half * 512:(half + 1) * 512], start=(j == 0), stop=(j == 1),
                                                        skip_group_check=True), [cT_b, wu_b], [psf_b[k]])
                S.op("act", lambda: nc.scalar.mul(KVs[:, half * 512:(half + 1) * 512], psf[k][:],
                                                  rs2[:, i, 1:2]), [psf_b[k], rs2_b], [ib])

        sqk_t = sb("sqk_t", [P, 512], F32, st)
        sqr_t = sb("sqr_t", [P, 64], F32, st)
        rak = sb("rak", [P, 32], F32, st)
        rbk = sb("rbk", [P, 32], F32, st)
        Bq, Bk, Br = Buf("m_sqq"), Buf("m_sqk"), Buf("m_sqr")
        Bs = [Buf("m_ss%d" % q_) for q_ in range(4)]
        Bqr, Bkr = Buf("m_qrf"), Buf("m_krf")
        Bra, Brb, Brak, Brbk = Buf("m_ra"), Buf("m_rb"), Buf("m_rak"), Buf("m_rbk")
        o_qn, o_kn, o_qr, o_kr = Buf("o_qn"), Buf("o_kn"), Buf("o_qr"), Buf("o_kr")

        def mla_chain(i):
            Qs, KVs, ib = Qs2[i % 2], KVs2[i % 2], in_b[i % 2]
            Q3 = Qs[:].rearrange("p (h d) -> p h d", h=4)
            KV3 = KVs[:].rearrange("p (h d) -> p h d", h=4)
            sq3q = sq[:, 0:768].rearrange("p (h d) -> p h d", h=4)
            sq3k = sqk_t[:].rearrange("p (h d) -> p h d", h=4)
            V = nc.vector
            S.op("dve", lambda: V.tensor_tensor(sq[:, 0:768], Qs[:], Qs[:], ALU.mult), [ib], [Bq])
            S.op("dve", lambda: V.tensor_tensor(sq3k, KV3[:, :, 0:128], KV3[:, :, 0:128], ALU.mult), [ib], [Bk])
            S.op("dve", lambda: V.tensor_tensor(sqr_t[:], krraw[:, i, :], krraw[:, i, :], ALU.mult), [kr_b], [Br])
            S.op("dve", lambda: V.tensor_reduce(ssn[:, 0:4], sq3q[:, :, 0:128], AX.X, ALU.add), [Bq], [Bs[0]])
            S.op("dve", lambda: V.tensor_reduce(ssn[:, 8:12], sq3k, AX.X, ALU.add), [Bk], [Bs[2]])
            S.op("dve", lambda: V.tensor_reduce(ssn[:, 12:13], sqr_t[:], AX.X, ALU.add), [Br], [Bs[3]])
            S.op("dve", lambda: V.tensor_reduce(ssn[:, 4:8], sq3q[:, :, 128:192], AX.X, ALU.add), [Bq], [Bs[1]])
            S.op("dve", lambda: V.tensor_tensor(ssn[:, 0:13], ssn[:, 0:13], invn[:, 0:13], ALU.mult), Bs + [dv], Bs)
            S.op("act", lambda: nc.scalar.activation(ssn[:, 0:13], ssn[:, 0:13], AF.Sqrt, bias=epsb[:], scale=1.0),
                 Bs + [cc], Bs)
            S.op("dve", lambda: V.reciprocal(ssn[:, 0:13], ssn[:, 0:13]), Bs, Bs)
            S.op("dve", lambda: V.tensor_tensor(sq3q[:, :, 0:128], Q3[:, :, 0:128], vap(ssn[:], [[1, 4], [0, 128]]), ALU.mult),
                 [ib] + Bs, [Bq])
            S.op("dve", lambda: V.tensor_tensor(sq3k, KV3[:, :, 0:128], vap(ssn[:], [[1, 4], [0, 128]], off=8), ALU.mult),
                 [ib] + Bs, [Bk])
            S.op("dve", lambda: V.tensor_tensor(qr_f[:], Q3[:, :, 128:192], vap(ssn[:], [[1, 4], [0, 64]], off=4), ALU.mult),
                 [ib] + Bs, [Bqr])
            S.op("dve", lambda: V.tensor_scalar(kr_f[:], krraw[:, i, :], ssn[:, 12:13], None, ALU.mult), [kr_b] + Bs, [Bkr])
            S.op("dve", lambda: V.tensor_tensor(qn_s[:], sq3q[:, :, 0:128], vap(g_q[:], [[0, 4], [1, 128]]), ALU.mult),
                 [Bq, cb], [o_qn])
            S.op("dve", lambda: V.tensor_tensor(kn_s[:], sq3k, vap(g_k[:], [[0, 4], [1, 128]]), ALU.mult), [Bk, cb], [o_kn])
            S.op("dve", lambda: V.tensor_tensor(qr_f[:], qr_f[:], vap(g_q[:], [[0, 4], [1, 64]], off=128), ALU.mult),
                 [Bqr, cb], [Bqr])
            S.op("dve", lambda: V.tensor_tensor(kr_f[:], kr_f[:], g_k[:, 128:192], ALU.mult), [Bkr, cb], [Bkr])
            cos4 = vap(cosT[:], [[0, 4], [1, 32]], off=i * 32)
            sin4 = vap(sinT[:], [[0, 4], [1, 32]], off=i * 32)
            c1 = cosT[:, i, :]
            s1 = sinT[:, i, :]
            S.op("dve", lambda: V.tensor_tensor(ra[:], qr_f[:, :, 0:32], cos4, ALU.mult), [Bqr, cc], [Bra])
            S.op("dve", lambda: V.tensor_tensor(rak[:], kr_f[:, 0:32], c1, ALU.mult), [Bkr, cc], [Brak])
            S.op("dve", lambda: V.tensor_tensor(rb_[:], qr_f[:, :, 32:64], sin4, ALU.mult), [Bqr, cc], [Brb])
            S.op("dve", lambda: V.tensor_tensor(rbk[:], kr_f[:, 32:64], s1, ALU.mult), [Bkr, cc], [Brbk])
            S.op("dve", lambda: V.tensor_sub(qr_s[:, :, 0:32], ra[:], rb_[:]), [Bra, Brb], [o_qr])
            S.op("dve", lambda: V.tensor_sub(kr_s[:, 0:32], rak[:], rbk[:]), [Brak, Brbk], [o_kr])
            S.op("dve", lambda: V.tensor_tensor(ra[:], qr_f[:, :, 32:64], cos4, ALU.mult), [Bqr, cc], [Bra])
            S.op("dve", lambda: V.tensor_tensor(rak[:], kr_f[:, 32:64], c1, ALU.mult), [Bkr, cc], [Brak])
            S.op("dve", lambda: V.tensor_tensor(rb_[:], qr_f[:, :, 0:32], sin4, ALU.mult), [Bqr, cc], [Brb])
            S.op("dve", lambda: V.tensor_tensor(rbk[:], kr_f[:, 0:32], s1, ALU.mult), [Bkr, cc], [Brbk])
            S.op("dve", lambda: V.tensor_add(qr_s[:, :, 32:64], ra[:], rb_[:]), [Bra, Brb], [o_qr])
            S.op("dve", lambda: V.tensor_add(kr_s[:, 32:64], rak[:], rbk[:]), [Brak, Brbk], [o_kr])
            S.op("pool", lambda: nc.gpsimd.tensor_copy(vaug[:, i, :, 0:128], KV3[:, :, 128:256]), [ib, vaug_b], [vaug_b])

        def mla_tail(i):
            for hh in range(4):
                S.op("pe", lambda: nc.tensor.transpose(psb[0][:, hh * P:(hh + 1) * P], qn_s[:, hh, :], ident[:]),
                     [o_qn, cc], [psb_b[0]])
                S.op("pe", lambda: nc.tensor.transpose(psb[0][:, (4 + hh) * P:(5 + hh) * P], kn_s[:, hh, :], ident[:]),
                     [o_kn, cc], [psb_b[0]])
                S.op("pe", lambda: nc.tensor.transpose(psb[1][0:64, hh * P:(hh + 1) * P], qr_s[:, hh, :], ident[:]),
                     [o_qr, cc], [psb_b[1]])
            S.op("pe", lambda: nc.tensor.transpose(psb[1][0:64, 4 * P:5 * P], kr_s[:], ident[:]), [o_kr, cc], [psb_b[1]])
            S.op("act", lambda: nc.scalar.copy(qnT[:, :, i * P:(i + 1) * P], vap(psb[0][:], [[P, 4], [1, P]])),
                 [psb_b[0]], [qk2_b])
            S.op("act", lambda: nc.scalar.copy(knT[:, :, i * P:(i + 1) * P], vap(psb[0][:], [[P, 4], [1, P]], off=4 * P)),
                 [psb_b[0]], [qk2_b])
            S.op("act", lambda: nc.scalar.copy(qrT[0:64, :, i * P:(i + 1) * P], vap(psb[1][0:64, :], [[P, 4], [1, P]])),
                 [psb_b[1]], [qk2_b])
            S.op("act", lambda: nc.scalar.copy(krT[0:64, i * P:(i + 1) * P], psb[1][0:64, 4 * P:5 * P]), [psb_b[1]], [qk2_b])

        mla_front(0)
        for i in range(NT):
            if i + 1 < NT:
                mla_front(i + 1)
            mla_chain(i)
            mla_tail(i)
        S.barrier()
    if upto == "C2":
        return nc
    with Scope(mem) as st:
        PT = [sb("PT%d" % i, [P, 512], BF, st) for i in range(2)]
        PT_b = [Buf("PT0"), Buf("PT1")]
        on4 = sb("on4", [P, 4, P], F32, st)
        onb4 = sb("onb4", [P, 4, P], BF, st)
        junk4 = sb("junk4", [P, 4, P], BF, st)
        rden = sb("rden", [P, 4], F32, st)
        ss4 = sb("ss4", [P, 4], F32, st)
        r_b = [Buf("rden%d" % q) for q in range(4)]
        on_b = [Buf("on%d" % q) for q in range(4)]
        j_b = [Buf("junk%d" % q) for q in range(4)]
        s_b = Buf("ss4")
        onb_b = [Buf("onb%d" % q) for q in range(4)]
        acc = (psf[2], psf[3], psf[4], psf[5])
        acc_b = (psf_b[2], psf_b[3], psf_b[4], psf_b[5])
        it = 0

        def tail_part1(hh, qb):
            for qt in range(4):
                S.op("dve", lambda: nc.vector.reciprocal(rden[:, qt:qt + 1], acc[qt][:, 128:129]), [acc_b[qt]], [r_b[qt]])
            for qt in range(4):
                S.op("act", lambda: nc.scalar.mul(on4[:, qt, :], acc[qt][:, 0:128], rden[:, qt:qt + 1]),
                     [acc_b[qt], r_b[qt]], [on_b[qt]])
            for qt in range(4):
                S.op("act", lambda: nc.scalar.activation(junk4[:, qt, :], on4[:, qt, :], AF.Square,
                                                         accum_out=ss4[:, qt:qt + 1]), [on_b[qt]], [j_b[qt], s_b])
            S.op("act", lambda: nc.scalar.activation(ss4[:], ss4[:], AF.Sqrt, bias=epsb[:], scale=1.0 / 128),
                 [s_b, cc], [s_b])
            S.op("dve", lambda: nc.vector.reciprocal(ss4[:], ss4[:]), [s_b], [s_b])
            for qt in range(4):
                S.op("dve", lambda: nc.vector.scalar_tensor_tensor(onb4[:, qt, :], on4[:, qt, :], ss4[:, qt:qt + 1],
                                                                    g_mo[:, hh * P:(hh + 1) * P], ALU.mult, ALU.mult),
                     [on_b[qt], s_b, cb], [onb_b[qt]])

        def tail_part2(hh, qb):
            for qt in range(4):
                S.op("pe", lambda: nc.tensor.transpose(psb[0][:, qt * P:(qt + 1) * P], onb4[:, qt, :], ident[:]),
                     [onb_b[qt], cc], [psb_b[0]])
            S.op("act", lambda: nc.scalar.copy(mixT[:, 4 + hh, qb * 512:(qb + 1) * 512], psb[0][:, 0:512]),
                 [psb_b[0]], [mix_b])

        blocks = [(hh, qb) for hh in range(4) for qb in range(4)]
        pending = None
        for (hh, qb) in blocks:
            def emit_S(kt, k):
                S.op("pe", lambda: nc.tensor.matmul(psf[k][:], knT[:, hh, kt * P:(kt + 1) * P],
                                                    qnT[:, hh, qb * 512:(qb + 1) * 512], start=True, stop=False,
                                                    skip_group_check=True), [qk2_b], [psf_b[k]])
                S.op("pe", lambda: nc.tensor.matmul(psf[k][:], krT[:, kt * P:(kt + 1) * P],
                                                    qrT[:, hh, qb * 512:(qb + 1) * 512], start=False, stop=True,
                                                    skip_group_check=True), [qk2_b], [psf_b[k]])

            emit_S(0, it % 2)
            for kt in range(NT):
                k = it % 2
                it += 1
                if kt + 1 < NT:
                    emit_S(kt + 1, it % 2)
                S.op("act", lambda: nc.scalar.activation(PT[k][:], psf[k][:], AF.Exp, scale=SCALE),
                     [psf_b[k]], [PT_b[k]])
                for qt in range(4):
                    a = acc[qt]
                    S.op("pe", lambda: nc.tensor.matmul(a[:, 0:129],
                                                        PT[k][:, qt * P:(qt + 1) * P], vaug[:, kt, hh, 0:129],
                                                        start=(kt == 0), stop=(kt == NT - 1), skip_group_check=True),
                         [PT_b[k], vaug_b], [acc_b[qt]])
                if kt == 2 and pending is not None:
                    tail_part2(*pending)
                    pending = None
            tail_part1(hh, qb)
            pending = (hh, qb)
        tail_part2(*pending)
        S.barrier()
    pc.close()
    pEarly.close()
    dump("mix_mla", mixT[:, 4:8, :], [mix_b])
    if upto == "C":
        return nc

    pD = Scope(mem)
    y_acc = sb("y_acc", [P, NT, D], F32, pD)
    y_b = [Buf("y%d" % i) for i in range(NT)]
    h2T = sb("h2T", [P, 8, T], BF, pD)
    h2T_b = Buf("h2T")
    with Scope(mem) as st:
        wo = sb("wo", [P, 8, D], BF, st)
        wo_b = Buf("wo")
        sg = [sb("sgD0", [P, 8, 512], F32, st)] * 2
        sg_b = [Buf("sgD0")] * 2
        for half in range(2):
            S.dma("sp", sg[half][:], wout_d[:, :, half * 512:(half + 1) * 512], sg_b[half], writes=[sg_b[half]])
            for c in range(8):
                S.op("dve", lambda: nc.vector.tensor_copy(wo[:, c, half * 512:(half + 1) * 512], sg[half][:, c, :]),
                     [sg_b[half]], [wo_b])
        xt = [sb("xtD%d" % i, [P, D], F32, st) for i in range(2)]
        xt_b = [Buf("xtD0"), Buf("xtD1")]
        h2 = [sb("h2_%d" % i, [P, D], BF, st) for i in range(2)]
        h2_b = [Buf("h2_0"), Buf("h2_1")]
        junk = sb("junkD", [P, D], BF, st)
        junk_b = Buf("junkD")
        ssD = sb("ssD", [P, NT], F32, st)
        ssD_b = [Buf("ssD%d" % i) for i in range(NT)]
        def d_front(i):
            j = i % 2
            S.dma("sp", xt[j][:], x_d[i * P:(i + 1) * P, :], xt_b[j], writes=[xt_b[j]])
            for half in range(2):
                k = 2 * j + half
                for c in range(8):
                    S.op("pe", lambda: nc.tensor.matmul(psf[k][:], mixT[:, c, i * P:(i + 1) * P],
                                                        wo[:, c, half * 512:(half + 1) * 512], start=(c == 0), stop=(c == 7),
                                                        skip_group_check=True), [mix_b, wo_b], [psf_b[k]])
                S.op("dve", lambda: nc.vector.tensor_tensor(y_acc[:, i, half * 512:(half + 1) * 512], psf[k][:],
                                                            xt[j][:, half * 512:(half + 1) * 512], ALU.add),
                     [psf_b[k], xt_b[j]], [y_b[i]])
            S.op("act", lambda: nc.scalar.activation(junk[:], y_acc[:, i, :], AF.Square, accum_out=ssD[:, i:i + 1]),
                 [y_b[i]], [junk_b, ssD_b[i]])
            rstd_from_ss(ssD[:, i:i + 1], ssD[:, i:i + 1], D, [ssD_b[i]])
            S.op("dve", lambda: nc.vector.tensor_scalar(h2[j][:], y_acc[:, i, :], ssD[:, i:i + 1], None, ALU.mult),
                 [y_b[i], ssD_b[i]], [h2_b[j]])

        def d_tail(i):
            j = i % 2
            for c in range(8):
                S.op("pe", lambda: nc.tensor.transpose(psb[j][:, c * P:(c + 1) * P], h2[j][:, c * P:(c + 1) * P], ident[:]),
                     [h2_b[j], cc], [psb_b[j]])
            S.op("act", lambda: nc.scalar.copy(h2T[:, :, i * P:(i + 1) * P], vap(psb[j][:], [[P, 8], [1, P]])),
                 [psb_b[j]], [h2T_b])

        d_front(0)
        for i in range(NT):
            if i + 1 < NT:
                d_front(i + 1)
            d_tail(i)
        S.barrier()
    dump("x1", y_acc[:], y_b)
    pM.close()
    if upto == "D":
        return nc

    U32 = mybir.dt.uint32
    pE = Scope(mem)
    iota16 = iota_f[:, 128:144]
    thr16 = iota_f[:, 144:160]
    with Scope(mem) as st:
        weff = sb("weff", [P, 8, 2048], BF, st)
        weff_b = Buf("weff")
        with Scope(mem) as st2:
            wqT = sb("wqT_s", [P, 8, 1024], BF, st2)
            kT = sb("kT_s", [P, 16, P], BF, st2)
            wq_b = Buf("wqT")
            sg = sb("sgE", [P, 4, 1024], F32, st2)
            sg_b = Buf("sgE")
            S.dma("sp", sg[:, 0:2, :].rearrange("p a b -> p (a b)"), keysT_d.rearrange("p a b -> p (a b)"), sg_b, writes=[sg_b])
            S.op("dve", lambda: nc.vector.tensor_copy(kT[:].rearrange("p a b -> p (a b)"),
                                                      sg[:, 0:2, :].rearrange("p a b -> p (a b)")), [sg_b], [wq_b])
            for hf in range(2):
                for q2 in range(2):
                    q4 = hf * 2 + q2
                    S.dma("sp", sg[:], wqT_d[:, q4 * 4:(q4 + 1) * 4, :], sg_b, writes=[sg_b])
                    S.op("dve", lambda: nc.vector.tensor_copy(wqT[:, q2 * 4:(q2 + 1) * 4, :], sg[:]), [sg_b], [wq_b])
                for c in range(8):
                    for q2 in range(2):
                        q4 = hf * 2 + q2
                        k = q4 % 2
                        for u in range(4):
                            pcx = q4 * 4 + u
                            S.op("pe", lambda: nc.tensor.matmul(psf[k][:, u * P:(u + 1) * P],
                                                                wqT[:, q2 * 4 + u, c * P:(c + 1) * P],
                                                                kT[:, pcx, :], start=True, stop=True, skip_group_check=True),
                                 [wq_b], [psf_b[k]])
                        S.op("act", lambda: nc.scalar.mul(weff[:, c, q4 * 512:(q4 + 1) * 512], psf[k][:],
                                                          g_ffn[:, c:c + 1]), [psf_b[k], cb], [weff_b])
            S.barrier()
        sci = sb("sc0", [P, 16, P], F32, st)
        scb = Buf("sc0")
        GI = 4
        sc2 = [sb("sc2_%d" % j, [P, P], F32, st) for j in range(GI)]
        sc2_b = [Buf("sc2_%d" % j) for j in range(GI)]
        t1_b = [Buf("t1_%d" % j) for j in range(16)]
        i1_b = [Buf("i1_%d" % j) for j in range(16)]
        top = sb("top", [P, 16, 16], F32, st)
        idxu = sb("idxu", [P, 16, 16], U32, st)
        idxf = sb("idxf", [P, 16, 16], F32, st)
        cand = [sb("cand_%d" % j, [P, 256], F32, st) for j in range(GI)]
        cand2 = [sb("cand2_%d" % j, [P, 256], F32, st) for j in range(GI)]
        cd_b = [Buf("cd%d" % j) for j in range(GI)]
        cd2_b = [Buf("cd2_%d" % j) for j in range(GI)]
        sel_b = [Buf("sel%d" % j) for j in range(8)]
        pos_b = [Buf("pos%d" % j) for j in range(8)]
        idxf_b = Buf("idxf")
        posf_b = Buf("posf")
        af_b = Buf("af")
        bfb_b = Buf("bfb")
        g16_b = [Buf("g16a"), Buf("g16b")]
        t16_b = [Buf("t16a"), Buf("t16b")]
        abf_b = [Buf("abf0"), Buf("abf1"), Buf("abf2")]
        es_b = Buf("esel")
        zs_b = Buf("zs")
        sel = sb("sel", [P, 8, 16], F32, st)
        posu = sb("posu", [P, 8, 16], U32, st)
        posf = sb("posf2", [P, P], F32, st)
        ge16 = sb("ge16", [P, P, 16], BF, st)
        af = sb("af", [P, P], F32, st)
        bf_ = sb("bf_", [P, P], F32, st)
        esel = sb("esel", [P, 8, 16], F32, st)
        zs = sb("zs", [P, 8], F32, st)
        ab = sb("ab", [P, 3, P], BF, st)
        abf = sb("abf", [P, 3, P], F32, st)
        abT2 = sb("abT2", [P, 2, 3, P], BF, st)
        abT2_b = [Buf("abT2_0"), Buf("abT2_1")]
        tk = Buf("topk")
        ab_b = Buf("ab")
        SUB = 8
        WT = sb("WT", [P, P, P], BF, st)
        WT_b = Buf("WT")
        NAB = 3
        A1 = [sb("A1_%d" % i, [P, SUB, P], BF, st) for i in range(NAB)]
        A2 = [sb("A2_%d" % i, [P, SUB, P], BF, st) for i in range(NAB)]
        A1_b = [Buf("A1_%d" % i) for i in range(NAB)]
        A2_b = [Buf("A2_%d" % i) for i in range(NAB)]
        wd_b = [Buf("Wd%d" % i) for i in range(NT)]
        cnt = {"it": 0, "bk": 0}

        def e1_front(i):
            for q4 in range(4):
                k = q4
                for c in range(8):
                    S.op("pe", lambda: nc.tensor.matmul(psf[k][:], h2T[:, c, i * P:(i + 1) * P],
                                                        weff[:, c, q4 * 512:(q4 + 1) * 512], start=(c == 0), stop=(c == 7),
                                                        skip_group_check=True), [h2T_b, weff_b], [psf_b[k]])
                S.op("act", lambda: nc.scalar.copy(sci[:, q4 * 4:(q4 + 1) * 4, :].rearrange("p a b -> p (a b)"), psf[k][:]),
                     [psf_b[k]], [scb])


        def e1_chain(i):
            for g2_ in range(16 // GI):
                pcs = tuple(GI * g2_ + q_ for q_ in range(GI))
                for pcx in pcs:
                    S.op("dve", lambda: nc.vector.max(out=top[:, pcx, 0:8], in_=sci[:, pcx, :]), [scb], [t1_b[pcx]])
                for pcx in pcs:
                    S.op("dve", lambda: nc.vector.max_index(out=idxu[:, pcx, 0:8], in_max=top[:, pcx, 0:8],
                                                            in_values=sci[:, pcx, :]), [scb, t1_b[pcx]], [i1_b[pcx]])
                for pcx in pcs:
                    j = pcx % GI
                    S.op("dve", lambda: nc.vector.match_replace(out=sc2[j][:], in_to_replace=top[:, pcx, 0:8],
                                                                in_values=sci[:, pcx, :], imm_value=-1e30),
                         [scb, t1_b[pcx]], [sc2_b[j]])
                for pcx in pcs:
                    j = pcx % GI
                    S.op("dve", lambda: nc.vector.max(out=top[:, pcx, 8:16], in_=sc2[j][:]), [sc2_b[j]], [t1_b[pcx]])
                for pcx in pcs:
                    j = pcx % GI
                    S.op("dve", lambda: nc.vector.max_index(out=idxu[:, pcx, 8:16], in_max=top[:, pcx, 8:16],
                                                            in_values=sc2[j][:]), [sc2_b[j], t1_b[pcx]], [i1_b[pcx]])
                for _y in range(GI):
                    yield
            for h2_ in range(8 // GI):
                ps_ = tuple(GI * h2_ + q_ for q_ in range(GI))
                for p_ in ps_:
                    j = p_ % GI
                    S.op("dve", lambda: nc.vector.tensor_tensor(
                        cand[j][:].rearrange("p (a b) -> p a b", a=16),
                        vap(top[:], [[1, 16], [0, 16]], off=32 * p_), vap(top[:], [[0, 16], [1, 16]], off=32 * p_ + 16), ALU.add),
                        [t1_b[2 * p_], t1_b[2 * p_ + 1]], [cd_b[j]])
                for p_ in ps_:
                    j = p_ % GI
                    S.op("dve", lambda: nc.vector.max(out=sel[:, p_, 0:8], in_=cand[j][:]), [cd_b[j]], [sel_b[p_]])
                for p_ in ps_:
                    j = p_ % GI
                    S.op("dve", lambda: nc.vector.max_index(out=posu[:, p_, 0:8], in_max=sel[:, p_, 0:8],
                                                            in_values=cand[j][:]), [cd_b[j], sel_b[p_]], [pos_b[p_]])
                for p_ in ps_:
                    j = p_ % GI
                    S.op("dve", lambda: nc.vector.match_replace(out=cand2[j][:], in_to_replace=sel[:, p_, 0:8],
                                                                in_values=cand[j][:], imm_value=-1e30),
                         [cd_b[j], sel_b[p_]], [cd2_b[j]])
                for p_ in ps_:
                    j = p_ % GI
                    S.op("dve", lambda: nc.vector.max(out=sel[:, p_, 8:16], in_=cand2[j][:]), [cd2_b[j]], [sel_b[p_]])
                for p_ in ps_:
                    j = p_ % GI
                    S.op("dve", lambda: nc.vector.max_index(out=posu[:, p_, 8:16], in_max=sel[:, p_, 8:16],
                                                            in_values=cand2[j][:]), [cd2_b[j], sel_b[p_]], [pos_b[p_]])
                for _y in range(GI):
                    yield
            S.op("dve", lambda: nc.vector.tensor_copy(posf[:], posu[:].rearrange("p a b -> p (a b)")), pos_b, [posf_b])
            S.op("dve", lambda: nc.vector.tensor_tensor(esel[:], sel[:], vap(sel[:], [[16, 8], [0, 16]]), ALU.subtract),
                 sel_b, [es_b])
            S.op("dve", lambda: nc.vector.tensor_copy(idxf[:], idxu[:]), i1_b, [idxf_b])
            S.op("act", lambda: nc.scalar.activation(esel[:], esel[:], AF.Exp), [es_b], [es_b])
            gA = ge16[:].rearrange("p j a -> p (j a)")
            S.op("dve", lambda: nc.vector.tensor_tensor(ge16[:], vap(posf[:], [[1, P], [0, 16]]),
                                                        vap(thr16, [[0, P], [1, 16]]), ALU.is_ge),
                 [posf_b, cb] + g16_b, g16_b)
            S.op("dve", lambda: nc.vector.tensor_reduce(zs[:], esel[:], AX.X, ALU.add), [es_b], [zs_b])
            S.op("dve", lambda: nc.vector.tensor_reduce(af[:], ge16[:], AX.X, ALU.add), g16_b, [af_b])
            S.op("dve", lambda: nc.vector.reciprocal(zs[:], zs[:]), [zs_b], [zs_b])
            S.op("dve", lambda: nc.vector.tensor_scalar(af[:], af[:], -1.0, None, ALU.add), [af_b], [af_b])
            S.op("dve", lambda: nc.vector.tensor_tensor(abf[:, 2, :].rearrange("p (h k) -> p h k", h=8), esel[:],
                                                        vap(zs[:], [[1, 8], [0, 16]]), ALU.mult), [es_b, zs_b], [abf_b[2]])
            S.op("dve", lambda: nc.vector.scalar_tensor_tensor(bf_[:], af[:], -16.0, posf[:], ALU.mult, ALU.add),
                 [af_b, posf_b], [bfb_b])
            yield
            H_ = P // 2
            for which, src, srcb, o_ in ((0, af, af_b, 0), (1, bf_, bfb_b, 16)):
                for hv in range(2):
                    S.op("dve", lambda: nc.vector.tensor_tensor(
                        ge16[:, hv * H_:(hv + 1) * H_, :], vap(src[:, hv * H_:(hv + 1) * H_], [[1, H_], [0, 16]]),
                        vap(iota16, [[0, H_], [1, 16]]), ALU.is_equal), [srcb, cb, g16_b[hv]], [g16_b[hv]])
                for hv in range(2):
                    S.op("dve", lambda: nc.vector.tensor_tensor(
                        ge16[:, hv * H_:(hv + 1) * H_, :].rearrange("p (h k) a -> p h k a", h=4),
                        ge16[:, hv * H_:(hv + 1) * H_, :].rearrange("p (h k) a -> p h k a", h=4),
                        vap(idxf[:], [[32, 4], [0, 16], [1, 16]], off=o_ + hv * 128), ALU.mult),
                        [g16_b[hv], idxf_b], [g16_b[hv]])
                for hv in range(2):
                    S.op("dve", lambda: nc.vector.tensor_reduce(abf[:, which, hv * H_:(hv + 1) * H_],
                                                                ge16[:, hv * H_:(hv + 1) * H_, :], AX.X, ALU.add),
                         [g16_b[hv]], [abf_b[which]])
                yield
            S.op("dve", lambda: nc.vector.tensor_copy(ab[:], abf[:]), abf_b + [ab_b], [ab_b])

        def e1_tail(i):
            kb = i % 2
            for j in range(3):
                S.op("pe", lambda: nc.tensor.transpose(psb[kb][:, j * P:(j + 1) * P], ab[:, j, :], ident[:]),
                     [ab_b, cc], [psb_b[kb]])
            S.op("act", lambda: nc.scalar.copy(abT2[:, kb, :, :].rearrange("p a b -> p (a b)"), psb[kb][:, 0:3 * P]),
                 [psb_b[kb]], [abT2_b[kb]])

        def e2(i):
            kb = i % 2
            for sub in range(P // SUB):
                s_ = cnt["it"] % NAB
                cnt["it"] += 1
                t0 = sub * SUB
                io_bc = vap(iota128[:], [[0, SUB], [1, P]])
                S.op("dve", lambda: nc.vector.tensor_tensor(A2[s_][:], io_bc, vap(abT2[:, kb, 1, t0:t0 + SUB], [[1, SUB], [0, P]]),
                                                            ALU.is_equal), [abT2_b[kb], cc], [A2_b[s_]])
                S.op("dve", lambda: nc.vector.tensor_tensor(A1[s_][:], io_bc, vap(abT2[:, kb, 0, t0:t0 + SUB], [[1, SUB], [0, P]]),
                                                            ALU.is_equal), [abT2_b[kb], cc], [A1_b[s_]])
                S.op("pool", lambda: nc.gpsimd.tensor_tensor(A1[s_][:], A1[s_][:], vap(abT2[:, kb, 2, t0:t0 + SUB], [[1, SUB], [0, P]]),
                                                             ALU.mult), [abT2_b[kb], A1_b[s_]], [A1_b[s_]])
                for t8 in range(SUB // 8):
                    k0 = (cnt["bk"] % 3) * 2
                    cnt["bk"] += 1
                    for u8 in range(8):
                        tt = t8 * 8 + u8
                        kk_ = k0 + u8 // 4
                        S.op("pe", lambda: nc.tensor.matmul(vap(psf[kk_], [[4, P]], off=u8 % 4), A2[s_][:, tt, :], A1[s_][:, tt, :],
                                                            start=True, stop=True, skip_group_check=True),
                             [A1_b[s_], A2_b[s_]], [psf_b[kk_]])
                    tok = t0 + t8 * 8
                    S.op("act", lambda: nc.scalar.copy(vap(WT[:], [[P, P], [4, 2], [1, 4]], off=tok),
                                                       vap(psf[k0], [[4, P], [512, 2], [1, 4]])),
                         [psf_b[k0], psf_b[k0 + 1]], [WT_b])
                yield
            S.dma("sp", Wd[i], WT[:].rearrange("p a b -> p (a b)"), WT_b, reads=[WT_b], writes=[wd_b[i]])

        e1_front(0)
        for i in range(NT):
            g2 = e2(i - 1) if i >= 1 else iter(())
            kk2 = 0
            for _ in e1_chain(i):
                kk2 += 1
                if kk2 == 16 and i + 1 < NT:
                    e1_front(i + 1)
                if (kk2 * 16) // 27 > ((kk2 - 1) * 16) // 27:
                    next(g2, None)
            for _ in g2:
                pass
            e1_tail(i)
        for _ in e2(NT - 1):
            pass
        S.barrier()
    pE.close()
    if upto == "E":
        return nc

    NB = EG // P
    with Scope(mem) as st:
        ustg = sb("ustg0", [P, 8, EG], F32, st)
        vstg = sb("vstg0", [P, NB, D], F32, st)
        ustg_b, vstg_b = Buf("ustg0"), Buf("vstg0")
        ubf = [sb("ubf%d" % i, [P, 8, EG], BF, st) for i in range(2)]
        vbf = [sb("vbf%d" % i, [P, NB, D], BF, st) for i in range(2)]
        ubf_b = [Buf("ubf0"), Buf("ubf1")]
        vbf_b = [Buf("vbf0"), Buf("vbf1")]
        WTg = [sb("WTg%d" % i, [P, 4, NB * P], BF, st) for i in range(3)]
        WTg_b = [Buf("WTg%d" % i) for i in range(3)]
        ge = [sb("ge%d" % i, [P, 512], BF, st) for i in range(2)]
        ge_b = [Buf("ge0"), Buf("ge1")]
        GT = [sb("GT%d" % i, [P, NB, 512], BF, st) for i in range(2)]
        GT_b = [Buf("GT0"), Buf("GT1")]

        def load_group(g):
            S.dma("sp", ustg[:], UT_d[:, :, g * EG:(g + 1) * EG], ustg_b, writes=[ustg_b])
            S.dma("sp", vstg[:], V_d[g * EG:(g + 1) * EG, :].rearrange("(b p) d -> p b d", p=P), vstg_b,
                  writes=[vstg_b])

        def cast_group(g):
            s_ = g % 2
            S.op("pool", lambda: nc.gpsimd.tensor_tensor(ubf[s_][:], ustg[:], vap(g_ffn[:], [[1, 8], [0, EG]]), ALU.mult),
                 [ustg_b, cb], [ubf_b[s_]])
            S.op("pool", lambda: nc.gpsimd.tensor_copy(vbf[s_][:], vstg[:]), [vstg_b], [vbf_b[s_]])

        seq = [(g, q) for g in range(NG) for q in range(4)]
        NTOT = len(seq)

        def load_w(n):
            g, q = seq[n]
            S.dma("sp", WTg[n % 3][:], Wd[4 * q:4 * q + 4, :, g * EG:(g + 1) * EG].rearrange("a p f -> p a f"),
                  WTg_b[n % 3], reads=wd_b[4 * q:4 * q + 4], writes=[WTg_b[n % 3]])

        abank = [0]

        def st_AG(n):
            g, q = seq[n]
            s_, gt = g % 2, n % 2
            for b_ in range(NB):
                pa = abank[0] % 2
                abank[0] += 1
                for c in range(8):
                    S.op("pe", lambda: nc.tensor.matmul(psf[pa][:], ubf[s_][:, c, b_ * P:(b_ + 1) * P],
                                                        h2T[:, c, q * 512:(q + 1) * 512], start=(c == 0), stop=(c == 7),
                                                        skip_group_check=True), [h2T_b, ubf_b[s_]], [psf_b[pa]])
                S.op("act", lambda: nc.scalar.activation(ge[pa][:], psf[pa][:], AF.Gelu), [psf_b[pa]], [ge_b[pa]])
                S.op("dve", lambda: nc.vector.tensor_tensor(
                    GT[gt][:, b_, :].rearrange("p (a t) -> p a t", a=4), ge[pa][:].rearrange("p (a t) -> p a t", a=4),
                    vap(WTg[n % 3][:], [[NB * P, 4], [1, P]], off=b_ * P), ALU.mult),
                    [ge_b[pa], WTg_b[n % 3]], [GT_b[gt]])

        ybank = [0]

        def st_Y(n):
            g, q = seq[n]
            s_, gt = g % 2, n % 2
            for u in range(4):
                i = 4 * q + u
                for half in range(2):
                    py = 2 + ybank[0] % 4
                    ybank[0] += 1
                    for b_ in range(NB):
                        S.op("pe", lambda: nc.tensor.matmul(psf[py][:], GT[gt][:, b_, u * P:(u + 1) * P],
                                                            vbf[s_][:, b_, half * 512:(half + 1) * 512], start=(b_ == 0),
                                                            stop=(b_ == NB - 1), skip_group_check=True),
                             [GT_b[gt], vbf_b[s_]], [psf_b[py]])
                    S.op("dve", lambda: nc.vector.tensor_tensor(y_acc[:, i, half * 512:(half + 1) * 512],
                                                                psf[py][:], y_acc[:, i, half * 512:(half + 1) * 512],
                                                                ALU.add), [psf_b[py], y_b[i]], [y_b[i]])

        load_group(0)
        cast_group(0)
        if NG > 1:
            load_group(1)
        load_w(0)
        load_w(1)
        for n in range(NTOT):
            g, q = seq[n]
            if n + 2 < NTOT:
                load_w(n + 2)
            if q == 2 and g + 1 < NG:
                cast_group(g + 1)
                if g + 2 < NG:
                    load_group(g + 2)
            st_AG(n)
            if n >= 1:
                st_Y(n - 1)
        st_Y(NTOT - 1)
        ob = Buf("outst")
        for i in range(NT):
            S.dma("sp", out_d[i * P:(i + 1) * P, :], y_acc[:, i, :], ob, reads=[y_b[i]])
        S.barrier()
    pD.close()
    es.close()
    return nc


_HOST_CACHE = {}


def _prep_shared(inp):
    f = np.float32
    sh = {}
    sh["invf"] = np.ascontiguousarray(np.broadcast_to(
        (1.0 / (10000.0 ** (np.arange(0, 64, 2, dtype=f) / f(64)))).astype(f)[None, :], (P, 32)))
    sh["ident"] = np.eye(P, dtype=f)
    s = np.arange(P)[:, None]
    t = np.arange(P)[None, :]
    same = (s // 64) == (t // 64)
    sh["maskf"] = (same & (s <= t)).astype(f)
    sh["maskb"] = (same & (s >= t)).astype(f)
    rm = np.ones((P, T), f)
    rm[:, ::64] = 0.0
    sh["resetm"] = rm
    io = np.zeros((P, 160), f)
    io[:, 0:128] = np.arange(128, dtype=f)[None, :]
    io[:, 128:144] = np.arange(16, dtype=f)[None, :]
    io[:, 144:160] = 16.0 * np.arange(16, dtype=f)[None, :]
    sh["iota"] = io

    def pc(v):
        return np.ascontiguousarray(np.asarray(v, f).reshape(-1, P).T)

    def rep(v):
        v = np.asarray(v, f).reshape(1, -1)
        return np.ascontiguousarray(np.broadcast_to(v, (P, v.shape[1])))

    def kc(w):
        w = np.asarray(w, f)
        return np.ascontiguousarray(w.reshape(-1, P, w.shape[1]).transpose(1, 0, 2))

    sh["g_attn"] = pc(inp["attn_norm"][0])
    sh["g_ffn"] = pc(inp["ffn_norm"][0])
    lbl = np.asarray(inp["hg_lb_logits"], f)
    sh["lbl"] = np.ascontiguousarray(lbl.reshape(2, 2, 4, P).transpose(3, 0, 1, 2).reshape(P, 16))
    sh["g_hgo"] = np.ascontiguousarray(np.asarray(inp["hg_o_norm"][0], f).T)
    sh["g_qa"] = pc(inp["q_a_norm"][0])
    sh["g_kva"] = pc(inp["kv_a_norm"][0])
    sh["g_q"] = rep(inp["q_norm"][0])
    sh["g_k"] = rep(inp["k_norm"][0])
    sh["g_mo"] = rep(inp["mla_o_norm"][0])
    sh["w_in"] = kc(inp["w_in"][0])
    sh["w_qup"] = kc(inp["w_q_up"][0])
    sh["w_kvup"] = kc(inp["w_kv_up"][0])
    sh["w_out"] = kc(inp["w_out"][0])
    wq = np.asarray(inp["peer_w_q"][0], f)
    sh["wqT"] = np.ascontiguousarray(wq.reshape(D, 16, P).transpose(2, 1, 0))
    keys = np.asarray(inp["peer_sub_keys"][0], f)
    sh["keysT"] = np.ascontiguousarray(keys.reshape(16, P, P).transpose(2, 0, 1))
    u = np.asarray(inp["peer_u"][0], f)
    sh["UT"] = np.ascontiguousarray(u.reshape(NEXP, 8, P).transpose(2, 1, 0))
    sh["V"] = np.ascontiguousarray(np.asarray(inp["peer_v"][0], f))
    return sh


def make_in_maps(inputs, cores):
    sh = _prep_shared(inputs)
    x = np.asarray(inputs["x"], np.float32)
    pos = np.asarray(inputs["positions"], np.int32)
    maps = []
    for b in cores:
        m = dict(sh)
        m["x"] = np.ascontiguousarray(x[b])
        m["posT"] = np.ascontiguousarray(pos[b].reshape(NT, P).T)
        maps.append(m)
    return maps


def kernel(**inputs):
    nc = build_program()
    in_maps = make_in_maps(inputs, list(range(8)))
    res = run_bass_kernel_spmd(nc, in_maps, core_ids=list(range(8)))
    out = np.stack([np.asarray(r["out"], np.float32) for r in res.results], axis=0)
    return out
```

```python
import numpy as np
from contextlib import ExitStack
import concourse.bass as bass
import concourse.mybir as mybir
from concourse.bass_utils import run_bass_kernel_spmd

F32 = mybir.dt.float32
BF = mybir.dt.bfloat16
I32 = mybir.dt.int32
AF = mybir.ActivationFunctionType
ALU = mybir.AluOpType
AX = mybir.AxisListType

P = 128
T = 2048
NT = 16
D = 1024
EPS = 1e-6
NEXP = 16384
EG = 512
NG = NEXP // EG
IC = 16
NIC = 128 // IC
PI = float(np.pi)


class Buf:
    def __init__(self, name):
        self.name = name
        self.writer = None
        self.readers = []
        self.dsem = None
        self.dcnt = 0


class Sch:
    def __init__(self, nc):
        self.nc = nc
        self.eng = dict(pe=nc.tensor, dve=nc.vector, act=nc.scalar, pool=nc.gpsimd, sp=nc.sync)
        self.sem = {e: nc.alloc_semaphore("sem_" + e) for e in ("pe", "dve", "act", "pool")}
        self.cnt = {e: 0 for e in self.sem}
        self.seen = {e: {} for e in self.eng}
        self.dbufs = []

    def _wait(self, e, dep):
        key, h, v = dep
        if self.seen[e].get(key, 0) >= v:
            return
        self.eng[e].wait_ge(h, v)
        self.seen[e][key] = v

    def _deps(self, e, reads, writes):
        deps = []
        for b in reads:
            if b.writer is not None:
                deps.append(b.writer)
        for b in writes:
            if b.writer is not None:
                deps.append(b.writer)
            deps.extend(b.readers)
        for d in deps:
            if e == "pe" and d[0] == "pe":
                continue
            self._wait(e, d)

    def _mark(self, tok, reads, writes):
        for b in reads:
            b.readers.append(tok)
        for b in writes:
            b.writer = tok
            b.readers = []

    def op(self, e, fn, reads=(), writes=()):
        self._deps(e, reads, writes)
        ins = fn()
        self.cnt[e] += 1
        ins.then_inc(self.sem[e], 1)
        self.seen[e][e] = max(self.seen[e].get(e, 0), 0)
        self._mark((e, self.sem[e], self.cnt[e]), reads, writes)

    def dma(self, q, out, in_, sb, reads=(), writes=()):
        self._deps(q, reads, writes)
        if sb.dsem is None:
            sb.dsem = self.nc.alloc_semaphore("dsem_" + sb.name)
            self.dbufs.append(sb)
        ins = self.eng[q].dma_start(out=out, in_=in_)
        sb.dcnt += 16
        ins.then_inc(sb.dsem, 16)
        self._mark((("d", sb.name), sb.dsem, sb.dcnt), reads, writes)

    def barrier(self):
        for e in self.eng:
            for f in self.sem:
                if f != e and self.cnt[f] > 0:
                    self._wait(e, (f, self.sem[f], self.cnt[f]))
            for b in self.dbufs:
                if b.dcnt > 0:
                    self._wait(e, (("d", b.name), b.dsem, b.dcnt))


class Mem:
    def __init__(self, lo, hi):
        self.free = [(lo, hi)]

    def alloc(self, n):
        n = (n + 63) // 64 * 64
        for k, (a, b) in enumerate(self.free):
            if b - a >= n:
                self.free[k] = (a + n, b)
                return a, n
        raise MemoryError("SBUF arena exhausted (%d bytes) free=%s" % (n, self.free))

    def release(self, a, n):
        fl = sorted(self.free + [(a, a + n)])
        out = []
        for lo, hi in fl:
            if out and out[-1][1] >= lo:
                out[-1] = (out[-1][0], max(out[-1][1], hi))
            elif hi > lo:
                out.append((lo, hi))
        self.free = out


class Scope:
    def __init__(self, mem):
        self.mem = mem
        self.items = []

    def __enter__(self):
        return self

    def __exit__(self, *a):
        self.close()
        return False

    def close(self):
        for a, n in self.items:
            self.mem.release(a, n)
        self.items = []


DT_BYTES = {}


def vap(base, dims, off=0):
    return bass.AP(base.tensor, base.offset + off, [list(base.ap[0])] + [list(d) for d in dims])


def build_program(debug=None, upto=None):
    debug = debug or {}
    nc = bass.Bass("TRN2", target_bir_lowering=False)
    S = Sch(nc)

    def din(name, shape, dt=F32):
        return nc.dram_tensor(name, list(shape), dt, kind="ExternalInput").ap()

    x_d = din("x", [T, D])
    pos_d = din("posT", [P, NT], I32)
    invf_d = din("invf", [P, 32])
    ident_d = din("ident", [P, P])
    maskf_d = din("maskf", [P, P])
    maskb_d = din("maskb", [P, P])
    reset_d = din("resetm", [P, T])
    gattn_d = din("g_attn", [P, 8])
    gffn_d = din("g_ffn", [P, 8])
    lbl_d = din("lbl", [P, 16])
    ghgo_d = din("g_hgo", [P, 4])
    gqa_d = din("g_qa", [P, 3])
    gkva_d = din("g_kva", [P, 2])
    gq_d = din("g_q", [P, 192])
    gk_d = din("g_k", [P, 192])
    gmo_d = din("g_mo", [P, 512])
    iota_d = din("iota", [P, 160])
    win_d = din("w_in", [P, 8, 3264])
    wqup_d = din("w_qup", [P, 3, 768])
    wkvup_d = din("w_kvup", [P, 2, 1024])
    wout_d = din("w_out", [P, 8, 1024])
    wqT_d = din("wqT", [P, 16, 1024])
    keysT_d = din("keysT", [P, 16, 128])
    UT_d = din("UT", [P, 8, NEXP])
    V_d = din("V", [NEXP, D])
    out_d = nc.dram_tensor("out", [T, D], F32, kind="ExternalOutput").ap()
    Wd = nc.dram_tensor("Wd", [NT, P, NEXP], BF).ap()
    dbg_out = {}
    for k, (shp, dt_) in debug.items():
        dbg_out[k] = nc.dram_tensor("dbg_" + k, list(shp), dt_, kind="ExternalOutput").ap()

    es = ExitStack()
    mem = Mem(16512 + 64, 229344 - 64)
    root = Scope(mem)

    def sb(name, shape, dt=F32, stack=None):
        nbytes = int(np.prod(shape[1:])) * (4 if dt in (F32, I32, mybir.dt.uint32) else 2)
        a, n = mem.alloc(nbytes)
        (stack or root).items.append((a, n))
        addr_of[name] = a
        return nc.alloc_sbuf_tensor_at(name, list(shape), dt, offset=a)

    addr_of = {}

    def sb_alias(name, shape, dt, like):
        return nc.alloc_sbuf_tensor_at(name, list(shape), dt, offset=addr_of[like])

    psf_all = es.enter_context(nc.psum_tensor("psf_all", [P, 6, 512], F32))
    psf = [psf_all[:, i, :] for i in range(6)]
    psb = [es.enter_context(nc.psum_tensor("psb%d" % i, [P, 1024], BF)) for i in range(2)]
    psf_b = [Buf("psf%d" % i) for i in range(6)]
    psb_b = [Buf("psb%d" % i) for i in range(2)]

    cb = Buf("consts")
    pEarly = Scope(mem)
    ident_f = sb("ident_f", [P, P], F32, pEarly)
    ident = sb("ident", [P, P], BF)
    maskf = sb("maskf", [P, P], F32, pEarly)
    maskb = sb("maskb", [P, P], F32, pEarly)
    resetm = sb("resetm", [P, T], F32, pEarly)
    invf = sb("invf", [P, 32])
    posT = sb("posT", [P, NT], I32)
    g_attn = sb("g_attn", [P, 8])
    g_ffn = sb("g_ffn", [P, 8])
    lbl = sb("lbl", [P, 16])
    g_hgo = sb("g_hgo", [P, 4])
    g_qa = sb("g_qa", [P, 3])
    g_kva = sb("g_kva", [P, 2])
    g_q = sb("g_q", [P, 192], F32, pEarly)
    g_k = sb("g_k", [P, 192], F32, pEarly)
    g_mo = sb("g_mo", [P, 512], F32, pEarly)
    ones_bf = sb("ones_bf", [P, P], BF)
    iota_f = sb("iota_f", [P, 160])
    iota128 = sb("iota128", [P, P], BF)
    for dst, src in ((ident_f, ident_d), (maskf, maskf_d), (maskb, maskb_d), (resetm, reset_d),
                     (invf, invf_d), (posT, pos_d), (g_attn, gattn_d), (g_ffn, gffn_d), (lbl, lbl_d),
                     (g_hgo, ghgo_d), (g_qa, gqa_d), (g_kva, gkva_d), (g_q, gq_d), (g_k, gk_d),
                     (g_mo, gmo_d), (iota_f, iota_d)):
        S.dma("sp", dst[:], src, cb, writes=[cb])
    cc = Buf("consts2")
    S.op("dve", lambda: nc.vector.tensor_copy(ident[:], ident_f[:]), [cb], [cc])
    S.op("dve", lambda: nc.vector.memset(ones_bf[:], 1.0), [], [cc])
    S.op("dve", lambda: nc.vector.tensor_copy(iota128[:], iota_f[:, 0:128]), [cb], [cc])
    lb = sb("lb", [P, 8])
    oml = sb("oml", [P, 8])
    noml = sb("noml", [P, 8])
    S.op("dve", lambda: nc.vector.tensor_sub(lb[:], lbl[:, 0:8], lbl[:, 8:16]), [cb], [cc])
    S.op("act", lambda: nc.scalar.activation(lb[:], lb[:], AF.Sigmoid), [cc], [cc])
    S.op("dve", lambda: nc.vector.tensor_scalar(oml[:], lb[:], -1.0, 1.0, ALU.mult, ALU.add), [cc], [cc])
    S.op("dve", lambda: nc.vector.tensor_scalar(noml[:], oml[:], -1.0, None, ALU.mult), [cc], [cc])
    cosT = sb("cosT", [P, NT, 32], F32, pEarly)
    sinT = sb("sinT", [P, NT, 32], F32, pEarly)
    with Scope(mem) as st:
        posf = sb("posf", [P, NT], F32, st)
        ang = sb("ang", [P, NT, 32], F32, st)
        ang2 = sb("ang2", [P, NT, 32], F32, st)
        S.op("dve", lambda: nc.vector.tensor_copy(posf[:], posT[:]), [cb], [cc])
        S.op("dve", lambda: nc.vector.tensor_tensor(
            ang[:], vap(posf[:], [[1, NT], [0, 32]]), vap(invf[:], [[0, NT], [1, 32]]), ALU.mult), [cc, cb], [cc])
        ri = sb("ri", [P, NT, 32], I32, st)
        rf = sb("rf", [P, NT, 32], F32, st)
        hi = sb("hi", [P, NT, 32], F32, st)
        S.op("dve", lambda: nc.vector.tensor_scalar(ang[:], ang[:], 1.0 / (2 * PI), None, ALU.mult), [cc], [cc])
        S.op("dve", lambda: nc.vector.tensor_scalar(ang2[:], ang[:], 0.25, None, ALU.add), [cc], [cc])
        for src, dst in ((ang, sinT), (ang2, cosT)):
            S.op("dve", lambda: nc.vector.tensor_copy(ri[:], src[:]), [cc], [cc])
            S.op("dve", lambda: nc.vector.tensor_copy(rf[:], ri[:]), [cc], [cc])
            S.op("dve", lambda: nc.vector.tensor_sub(src[:], src[:], rf[:]), [cc], [cc])
            S.op("dve", lambda: nc.vector.tensor_scalar(hi[:], src[:], 0.5, None, ALU.is_gt), [cc], [cc])
            S.op("dve", lambda: nc.vector.tensor_sub(src[:], src[:], hi[:]), [cc], [cc])
            S.op("dve", lambda: nc.vector.tensor_scalar(hi[:], src[:], -0.5, None, ALU.is_lt), [cc], [cc])
            S.op("dve", lambda: nc.vector.tensor_add(src[:], src[:], hi[:]), [cc], [cc])
            S.op("act", lambda: nc.scalar.activation(dst[:], src[:], AF.Sin, scale=2 * PI), [cc], [cc])
        S.barrier()
    epsb = sb("epsb", [P, 1])
    S.op("dve", lambda: nc.vector.memset(epsb[:], EPS), [], [cc])
    if upto == "0":
        S.barrier()
        return nc

    def dump(name, src_ap, rbufs):
        if name in dbg_out:
            S.barrier()
            tb = Buf("dbg_" + name)
            S.dma("sp", dbg_out[name], src_ap, tb, reads=rbufs)
            S.barrier()

    def rstd_from_ss(dst, ss, n, bufs):
        S.op("act", lambda: nc.scalar.activation(dst, ss, AF.Sqrt, bias=epsb[:dst.shape[0]], scale=1.0 / n), bufs + [cc], bufs)
        S.op("dve", lambda: nc.vector.reciprocal(dst, dst), bufs, bufs)

    pM = Scope(mem)
    mixT = sb("mixT", [P, 8, T], BF, pM)
    mix_b = Buf("mixT")
    ph1 = Scope(mem)
    xnT = sb("xnT", [P, 8, T], BF, ph1)
    xnT_b = Buf("xnT")
    stg = [sb("stg0", [P, 8, 512], F32, ph1)] * 2
    stg_b = [Buf("stg0")] * 2
    with Scope(mem) as st:
        xt = [sb("xt%d" % i, [P, D], F32, st) for i in range(2)]
        xt_b = [Buf("xt%d" % i) for i in range(2)]
        xn = [sb("xn%d" % i, [P, D], BF, st) for i in range(2)]
        xn_b = [Buf("xn%d" % i) for i in range(2)]
        junk = sb("junkA", [P, D], BF, st)
        junk_b = Buf("junkA")
        ssA = sb("ssA", [P, NT], F32, st)
        ssA_b = [Buf("ssA%d" % i) for i in range(NT)]
        def a_front(i):
            j = i % 2
            S.dma("sp", xt[j][:], x_d[i * P:(i + 1) * P, :], xt_b[j], writes=[xt_b[j]])
            S.op("act", lambda: nc.scalar.activation(junk[:], xt[j][:], AF.Square, accum_out=ssA[:, i:i + 1]),
                 [xt_b[j]], [junk_b, ssA_b[i]])
            rstd_from_ss(ssA[:, i:i + 1], ssA[:, i:i + 1], D, [ssA_b[i]])
            S.op("dve", lambda: nc.vector.tensor_scalar(xn[j][:], xt[j][:], ssA[:, i:i + 1], None, ALU.mult),
                 [xt_b[j], ssA_b[i]], [xn_b[j]])

        def a_tail(i):
            j = i % 2
            pb = psb_b[i % 2]
            for c in range(8):
                S.op("pe", lambda: nc.tensor.transpose(psb[i % 2][:, c * P:(c + 1) * P], xn[j][:, c * P:(c + 1) * P], ident[:]),
                     [xn_b[j], cc], [pb])
            S.op("act", lambda: nc.scalar.copy(
                xnT[:, :, i * P:(i + 1) * P], vap(psb[i % 2][:], [[P, 8], [1, P]])), [pb], [xnT_b])

        a_front(0)
        for i in range(NT):
            if i + 1 < NT:
                a_front(i + 1)
            a_tail(i)
        S.barrier()

    if upto == "A":
        return nc
    def load_wslice(dst, dst_b, src_d, nchunks, cols, gain, slot):
        sg, sgb = stg[slot], stg_b[slot]
        o = 0
        for (c0, w) in cols:
            S.dma("sp", sg[:, 0:nchunks, o:o + w], src_d[:, :, c0:c0 + w], sgb, writes=[sgb])
            o += w
        for c in range(nchunks):
            if gain is not None:
                S.op("dve", lambda: nc.vector.tensor_scalar(dst[:, c, 0:o], sg[:, c, 0:o], gain[:, c:c + 1], None, ALU.mult),
                     [sgb, cb], [dst_b])
            else:
                S.op("dve", lambda: nc.vector.tensor_copy(dst[:, c, 0:o], sg[:, c, 0:o]), [sgb], [dst_b])

    def proj_fm(ps_ap, ps_b, w, w_b, col0, width, t0, n, src=None, src_b=None, nch=8):
        src = xnT if src is None else src
        src_b = xnT_b if src_b is None else src_b
        for c in range(nch):
            S.op("pe", lambda: nc.tensor.matmul(ps_ap, w[:, c, col0:col0 + width], src[:, c, t0:t0 + n],
                                                start=(c == 0), stop=(c == nch - 1), skip_group_check=True),
                 [w_b, src_b], [ps_b])

    with Scope(mem) as st:
        vtok = sb("vtok", [P, NT, 512], BF, st)
        vtok_b = Buf("vtok")
        wv = sb("wv", [P, 8, 512], BF, st)
        wv_b = Buf("wv")
        load_wslice(wv, wv_b, win_d, 8, [(1536, 512)], g_attn, 0)
        for i in range(NT):
            k = i % 2
            for c in range(8):
                S.op("pe", lambda: nc.tensor.matmul(psf[k][:], xnT[:, c, i * P:(i + 1) * P], wv[:, c, :],
                                                    start=(c == 0), stop=(c == 7), skip_group_check=True),
                     [xnT_b, wv_b], [psf_b[k]])
            S.op("act", lambda: nc.scalar.copy(vtok[:, i, :], psf[k][:]), [psf_b[k]], [vtok_b])
        wh = [wv] * 2
        wh_b = [wv_b] * 2
        if upto == "B1":
            S.barrier()
            return nc
        H = 1024
        qs = sb("qs", [P, T], F32, st)
        gsl = sb("gsl", [P, T], BF, st)
        glog = sb("glog", [P, H], F32, st)
        bcum = sb("bcum", [P, H], F32, st)
        kk = sb("kk", [P, H], F32, st)
        e1 = sb("e1", [P, H], F32, st)
        e2 = glog
        Qt = sb("Qt", [P, 2, T], BF, st)
        Kt = sb("Kt", [P, 2, T], BF, st)
        Ktok = sb("Ktok", [P, 2, NT, P], BF, st)
        Sbf = sb("Sbf", [P, 2, 32, P], BF, st)
        vm = sb("vm", [P, NT, 2, P], BF, st)
        vm_b = Buf("vm")
        Sm = [sb("Sm%d" % i, [P, 2, P], F32, st) for i in range(2)]
        dSd = sb("dSd", [P, 2, P], F32, st)
        dch = sb("dch", [P, 2, 32], F32, st)
        Pm = [sb("Pm%d" % i, [P, 2, P], BF, st) for i in range(2)]
        osq = sb("osq", [P, 512], BF, st)
        rbc = kk
        otmp = e1
        qs_b, gsl_b, e1_b, gl_b, kk_b, bc_b, dch_b = (Buf("hq"), Buf("hgs"), Buf("he1"), Buf("hgl"), Buf("hkk"), Buf("hbc"), Buf("hdch"))
        qk_b = Buf("QtKt")
        ktok_b = Buf("Ktok")
        sbf_b = Buf("Sbf")
        sm_b = Buf("Sm")
        pm_b = [Buf("Pm0"), Buf("Pm1")]
        ob = Buf("onorm")
        for h in range(4):
            w = wh[h % 2]
            wb = wh_b[h % 2]
            load_wslice(w, wb, win_d, 8, [(h * P, P), (512 + h * P, P), (1024 + h * P, P), (2048 + h * P, P)],
                        g_attn, (h + 1) % 2)
            for tb in range(4):
                k = tb % 2
                proj_fm(psf[k][:], psf_b[k], w, wb, 0, P, tb * 512, 512)
                S.op("act", lambda: nc.scalar.activation(qs[:, tb * 512:(tb + 1) * 512], psf[k][:], AF.Silu),
                     [psf_b[k]], [qs_b])
            for tb in range(4):
                k = tb % 2
                proj_fm(psf[k][:], psf_b[k], w, wb, 384, P, tb * 512, 512)
                S.op("act", lambda: nc.scalar.activation(gsl[:, tb * 512:(tb + 1) * 512], psf[k][:], AF.Silu),
                     [psf_b[k]], [gsl_b])
            for d in range(2):
                col = d * 4 + h
                for hf in range(2):
                    t0 = hf * H
                    for tb in range(2):
                        k = tb % 2
                        proj_fm(psf[k][:], psf_b[k], w, wb, (1 + d) * P, P, t0 + tb * 512, 512)
                        S.op("act", lambda: nc.scalar.activation(e1[:, tb * 512:(tb + 1) * 512], psf[k][:], AF.Sigmoid),
                             [psf_b[k]], [e1_b])
                    S.op("act", lambda: nc.scalar.activation(glog[:], e1[:], AF.Ln, bias=lb[:, col:col + 1],
                                                             scale=oml[:, col:col + 1]), [e1_b, cc], [gl_b])
                    S.op("dve", lambda: nc.vector.tensor_scalar(kk[:], e1[:], noml[:, col:col + 1], oml[:, col:col + 1],
                                                                ALU.mult, ALU.add), [e1_b, cc], [kk_b])
                    S.op("dve", lambda: nc.vector.tensor_tensor_scan(bcum[:], resetm[:, t0:t0 + H], glog[:], 0.0,
                                                                      ALU.mult, ALU.add), [gl_b, cb], [bc_b])
                    S.op("act", lambda: nc.scalar.activation(dch[:, d, hf * 16:(hf + 1) * 16],
                                                             vap(bcum[:], [[64, 16]], off=63), AF.Exp), [bc_b], [dch_b])
                    if d == 1:
                        S.op("dve", lambda: nc.vector.tensor_sub(glog[:], glog[:], bcum[:]), [gl_b, bc_b], [gl_b])
                        S.op("dve", lambda: nc.vector.tensor_tensor(
                            vap(glog[:], [[64, H // 64], [1, 64]]), vap(glog[:], [[64, H // 64], [1, 64]]),
                            vap(bcum[:], [[64, H // 64], [0, 64]], off=63), ALU.add), [gl_b, bc_b], [gl_b])
                        cur = glog
                        cur_ap = glog[:]
                    else:
                        cur_ap = bcum[:]
                    S.op("act", lambda: nc.scalar.activation(e1[:], cur_ap, AF.Exp), [gl_b, bc_b], [e1_b])
                    S.op("act", lambda: nc.scalar.activation(e2[:], cur_ap, AF.Exp, scale=-1.0), [gl_b, bc_b], [gl_b])
                    S.op("dve", lambda: nc.vector.tensor_tensor(Qt[:, d, t0:t0 + H], qs[:, t0:t0 + H], e1[:], ALU.mult),
                         [qs_b, e1_b], [qk_b])
                    S.op("dve", lambda: nc.vector.tensor_tensor(Kt[:, d, t0:t0 + H], kk[:], e2[:], ALU.mult),
                         [kk_b, gl_b], [qk_b])
            if upto == "B2":
                S.barrier()
                return nc
            for d in range(2):
                for g8 in range(2):
                    pbk = (d * 2 + g8) % 2
                    for u in range(8):
                        i = g8 * 8 + u
                        S.op("pe", lambda: nc.tensor.transpose(psb[pbk][:, u * P:(u + 1) * P], Kt[:, d, i * P:(i + 1) * P], ident[:]),
                             [qk_b, cc], [psb_b[pbk]])
                    S.op("act", lambda: nc.scalar.copy(Ktok[:, d, g8 * 8:(g8 + 1) * 8, :], vap(psb[pbk][:], [[P, 8], [1, P]])),
                         [psb_b[pbk]], [ktok_b])
            if upto == "B3":
                S.barrier()
                return nc
            for jj in range(2):
                S.op("dve", lambda: nc.vector.tensor_scalar(vm[:, :, jj, :], vtok[:, :, h * P:(h + 1) * P],
                                                            maskb[:, jj * 64:jj * 64 + 1], None, ALU.mult),
                     [vtok_b, cb], [vm_b])
            dsd_b = [Buf("dsd0"), Buf("dsd1")]
            smd_b = [[Buf("smd%d_%d" % (d_, q_)) for q_ in range(2)] for d_ in range(2)]
            S.op("pool", lambda: nc.gpsimd.memset(Sm[0][:], 0.0), [], [smd_b[0][0], smd_b[1][0]])
            S.op("pool", lambda: nc.gpsimd.memset(Sbf[:, 0, 0, :], 0.0), [], [sbf_b])
            S.op("pool", lambda: nc.gpsimd.memset(Sbf[:, 1, 31, :], 0.0), [], [sbf_b])
            for step in range(31):
                pp = step % 2
                cur, nxt = Sm[pp], Sm[1 - pp]
                cf, cbk = step, 31 - step
                for d, c in ((0, cf), (1, cbk)):
                    kd = 2 + 2 * d + (step % 2)
                    i, jj = c // 2, c % 2
                    S.op("pe", lambda: nc.tensor.matmul(psf[kd][:, 0:P], Ktok[:, d, i, :],
                                                        vm[:, i, jj, :], start=True, stop=True,
                                                        skip_group_check=True), [ktok_b, vm_b], [psf_b[kd]])
                for d, c in ((0, cf), (1, cbk)):
                    kd = 2 + 2 * d + (step % 2)
                    S.op("act", lambda: nc.scalar.mul(dSd[:, d, :], psf[kd][:, 0:P],
                                                      dch[:, d, c:c + 1]), [psf_b[kd], dch_b], [dsd_b[d]])
                for d, c in ((0, cf), (1, cbk)):
                    S.op("dve", lambda: nc.vector.scalar_tensor_tensor(nxt[:, d, :], cur[:, d, :], dch[:, d, c:c + 1],
                                                                        dSd[:, d, :], ALU.mult, ALU.add),
                         [smd_b[d][pp], dsd_b[d], dch_b], [smd_b[d][1 - pp]])
                for d, c in ((0, cf), (1, cbk)):
                    cn = c + 1 if d == 0 else c - 1
                    S.op("pool", lambda: nc.gpsimd.tensor_copy(Sbf[:, d, cn, :], nxt[:, d, :]), [smd_b[d][1 - pp]], [sbf_b])
            if upto == "B4":
                S.barrier()
                return nc
            def emit_scores(i):
                ks = i % 2
                for d in range(2):
                    S.op("pe", lambda: nc.tensor.matmul(psf[ks][:, d * P:(d + 1) * P], Kt[:, d, i * P:(i + 1) * P],
                                                        Qt[:, d, i * P:(i + 1) * P], start=True, stop=True,
                                                        skip_group_check=True), [qk_b], [psf_b[ks]])
                S.op("dve", lambda: nc.vector.tensor_tensor(Pm[ks][:, 0, :], psf[ks][:, 0:P], maskf[:], ALU.mult),
                     [psf_b[ks], cb], [pm_b[ks]])
                S.op("dve", lambda: nc.vector.tensor_tensor(Pm[ks][:, 1, :], psf[ks][:, P:2 * P], maskb[:], ALU.mult),
                     [psf_b[ks], cb], [pm_b[ks]])

            emit_scores(0)
            for i in range(NT):
                g4, u = divmod(i, 4)
                po = 4 + (g4 % 2)
                ks = i % 2
                if i + 1 < NT:
                    emit_scores(i + 1)
                oap = psf[po][:, u * P:(u + 1) * P]
                S.op("pe", lambda: nc.tensor.matmul(oap, vtok[:, i, h * P:(h + 1) * P], Pm[ks][:, 0, :], start=True,
                                                    stop=False, skip_group_check=True), [vtok_b, pm_b[ks]], [psf_b[po]])
                S.op("pe", lambda: nc.tensor.matmul(oap, vtok[:, i, h * P:(h + 1) * P], Pm[ks][:, 1, :], start=False,
                                                    stop=False, skip_group_check=True), [vtok_b, pm_b[ks]], [psf_b[po]])
                for d in range(2):
                    for jj in range(2):
                        c = 2 * i + jj
                        S.op("pe", lambda: nc.tensor.matmul(
                            psf[po][:, u * P + jj * 64:u * P + jj * 64 + 64], Sbf[:, d, c, :],
                            Qt[:, d, c * 64:(c + 1) * 64], start=False, stop=(d == 1 and jj == 1),
                            skip_group_check=True), [sbf_b, qk_b], [psf_b[po]])
                if u != 3:
                    continue
                S.op("act", lambda: nc.scalar.activation(osq[:], psf[po][:], AF.Square), [psf_b[po]], [ob])
                S.op("pe", lambda: nc.tensor.matmul(psf[3][:], ones_bf[:], osq[:], start=True, stop=True,
                                                    skip_group_check=True), [ob, cc], [psf_b[3]])
                S.op("act", lambda: nc.scalar.activation(rbc[:, 0:512], psf[3][:], AF.Sqrt, bias=epsb[:], scale=1.0 / 128),
                     [psf_b[3], cc], [ob, kk_b])
                S.op("dve", lambda: nc.vector.reciprocal(rbc[:, 0:512], rbc[:, 0:512]), [ob, kk_b], [ob, kk_b])
                S.op("dve", lambda: nc.vector.scalar_tensor_tensor(otmp[:, 0:512], psf[po][:], g_hgo[:, h:h + 1], rbc[:, 0:512],
                                                                    ALU.mult, ALU.mult), [psf_b[po], ob, cb, kk_b], [ob, e1_b])
                S.op("dve", lambda: nc.vector.tensor_tensor(mixT[:, h, g4 * 512:(g4 + 1) * 512], otmp[:, 0:512],
                                                            gsl[:, g4 * 512:(g4 + 1) * 512], ALU.mult), [ob, e1_b, gsl_b], [mix_b])
        S.barrier()
    dump("mix_hg", mixT[:, 0:4, :], [mix_b])
    if upto == "B":
        return nc

    SCALE = 192.0 ** -0.5
    pc = Scope(mem)
    cT = sb("cT", [P, 5, T], BF, pc)
    cT_b = Buf("cT")
    krraw = sb("krraw", [P, NT, 64], F32, pc)
    kr_b = Buf("krraw")
    rs2 = sb("rs2", [P, NT, 2], F32, pc)
    rs2_b = Buf("rs2")
    wqu = sb("wqu", [P, 3, 768], BF, pc)
    wkvu = sb("wkvu", [P, 2, 1024], BF, pc)
    wu_b = Buf("wup")
    with Scope(mem) as st:
        wm = sb("wm", [P, 8, 704], BF, st)
        wm_b = Buf("wm")
        load_wslice(wm[:, :, 0:512], wm_b, win_d, 8, [(2560, 512)], g_attn, 0)
        load_wslice(wm[:, :, 512:704], wm_b, win_d, 8, [(3072, 192)], g_attn, 1)
        csq = sb("csq", [P, 5, 512], BF, st)
        csq_b = Buf("csq")
        for tb in range(4):
            for j in range(5):
                k = j % 2
                proj_fm(psf[k][:], psf_b[k], wm, wm_b, j * P, P, tb * 512, 512)
                S.op("act", lambda: nc.scalar.copy(cT[:, j, tb * 512:(tb + 1) * 512], psf[k][:]), [psf_b[k]], [cT_b])
                S.op("act", lambda: nc.scalar.activation(csq[:, j, :], psf[k][:], AF.Square), [psf_b[k]], [csq_b])
            for u in range(4):
                i = tb * 4 + u
                for j in range(3):
                    S.op("pe", lambda: nc.tensor.matmul(psf[2][:, i * 2:i * 2 + 1], csq[:, j, u * P:(u + 1) * P],
                                                        ones_bf[:, 0:1], start=(j == 0), stop=(j == 2),
                                                        skip_group_check=True), [csq_b, cc], [psf_b[2]])
                for j in range(2):
                    S.op("pe", lambda: nc.tensor.matmul(psf[2][:, i * 2 + 1:i * 2 + 2], csq[:, 3 + j, u * P:(u + 1) * P],
                                                        ones_bf[:, 0:1], start=(j == 0), stop=(j == 1),
                                                        skip_group_check=True), [csq_b, cc], [psf_b[2]])
        S.op("act", lambda: nc.scalar.copy(rs2[:].rearrange("p a b -> p (a b)"), psf[2][:, 0:2 * NT]), [psf_b[2]], [rs2_b])
        rstd_from_ss(rs2[:, :, 0], rs2[:, :, 0], 384, [rs2_b])
        rstd_from_ss(rs2[:, :, 1], rs2[:, :, 1], 256, [rs2_b])
        for i in range(NT):
            k = 3 + i % 2
            for c in range(8):
                S.op("pe", lambda: nc.tensor.matmul(psf[k][:, 0:64], xnT[:, c, i * P:(i + 1) * P], wm[:, c, 640:704],
                                                    start=(c == 0), stop=(c == 7), skip_group_check=True),
                     [xnT_b, wm_b], [psf_b[k]])
            S.op("act", lambda: nc.scalar.copy(krraw[:, i, :], psf[k][:, 0:64]), [psf_b[k]], [kr_b])
        S.barrier()
    if upto == "C1":
        return nc
    sg0 = stg[0]
    S.dma("sp", sg0[:, 0:3, 0:512], wqup_d[:, :, 0:512], stg_b[0], writes=[stg_b[0]])
    for c in range(3):
        S.op("dve", lambda: nc.vector.tensor_scalar(wqu[:, c, 0:512], sg0[:, c, 0:512], g_qa[:, c:c + 1], None, ALU.mult),
             [stg_b[0], cb], [wu_b])
    S.dma("sp", sg0[:, 0:3, 0:256], wqup_d[:, :, 512:768], stg_b[0], writes=[stg_b[0]])
    for c in range(3):
        S.op("dve", lambda: nc.vector.tensor_scalar(wqu[:, c, 512:768], sg0[:, c, 0:256], g_qa[:, c:c + 1], None, ALU.mult),
             [stg_b[0], cb], [wu_b])
    for half in range(2):
        S.dma("sp", sg0[:, 0:2, 0:512], wkvup_d[:, :, half * 512:(half + 1) * 512], stg_b[0], writes=[stg_b[0]])
        for c in range(2):
            S.op("dve", lambda: nc.vector.tensor_scalar(wkvu[:, c, half * 512:(half + 1) * 512], sg0[:, c, 0:512],
                                                        g_kva[:, c:c + 1], None, ALU.mult), [stg_b[0], cb], [wu_b])
    S.barrier()
    ph1.close()
    qnT = sb("qnT", [P, 4, T], BF, pc)
    knT = sb("knT", [P, 4, T], BF, pc)
    qrT = sb("qrT", [P, 4, T], BF, pc)
    krT = sb("krT", [P, T], BF, pc)
    vaug = sb("vaug", [P, NT, 4, 132], BF, pc)
    qk2_b = Buf("qkT")
    vaug_b = Buf("vaug")
    S.op("pool", lambda: nc.gpsimd.memset(vaug[:], 1.0), [], [vaug_b])
    S.op("pool", lambda: nc.gpsimd.memset(qrT[:], 0.0), [], [qk2_b])
    S.op("pool", lambda: nc.gpsimd.memset(krT[:], 0.0), [], [qk2_b])
    with Scope(mem) as st:
        Qs2 = [sb("Qs%d" % i, [P, 768], F32, st) for i in range(2)]
        KVs2 = [sb("KVs%d" % i, [P, 1024], F32, st) for i in range(2)]
        in_b = [Buf("mla_in0"), Buf("mla_in1")]
        sq = sb("sqm", [P, 1024], F32, st)
        ssn = sb("ssn", [P, 16], F32, st)
        invn = sb("invn", [P, 16], F32, st)
        qn_s = sb("qn_s", [P, 4, P], BF, st)
        kn_s = sb("kn_s", [P, 4, P], BF, st)
        qr_f = sb("qr_f", [P, 4, 64], F32, st)
        kr_f = sb("kr_f", [P, 64], F32, st)
        qr_s = sb("qr_s", [P, 4, 64], BF, st)
        kr_s = sb("kr_s", [P, 64], BF, st)
        ra = sb("ra", [P, 4, 32], F32, st)
        rb_ = sb("rb_", [P, 4, 32], F32, st)
        dv = Buf("mla_dve")
        out_b = Buf("mla_out")
        S.op("dve", lambda: nc.vector.memset(invn[:, 0:4], 1.0 / 128), [], [dv])
        S.op("dve", lambda: nc.vector.memset(invn[:, 4:8], 1.0 / 64), [], [dv])
        S.op("dve", lambda: nc.vector.memset(invn[:, 8:12], 1.0 / 128), [], [dv])
        S.op("dve", lambda: nc.vector.memset(invn[:, 12:16], 1.0 / 64), [], [dv])

        def mla_front(i):
            Qs, KVs, ib = Qs2[i % 2], KVs2[i % 2], in_b[i % 2]
            for half in range(2):
                k = half
                for j in range(3):
                    S.op("pe", lambda: nc.tensor.matmul(psf[k][:, 0:384], cT[:, j, i * P:(i + 1) * P],
                                                        wqu[:, j, half * 384:(half + 1) * 384], start=(j == 0), stop=(j == 2),
                                                        skip_group_check=True), [cT_b, wu_b], [psf_b[k]])
                S.op("act", lambda: nc.scalar.mul(Qs[:, half * 384:(half + 1) * 384], psf[k][:, 0:384],
                                                  rs2[:, i, 0:1]), [psf_b[k], rs2_b], [ib])
            for half in range(2):
                k = 2 + half
                for j in range(2):
                    S.op("pe", lambda: nc.tensor.matmul(psf[k][:], cT[:, 3 + j, i * P:(i + 1) * P],
                                                        wkvu[:, j, half * 512:(half + 1) * 512], start=(j == 0), stop=(j == 1),
                                                        skip_group_check=True), [cT_b, wu_b], [psf_b[k]])
                S.op("act", lambda: nc.scalar.mul(KVs[:, half * 512:(half + 1) * 512], psf[k][:],
                                                  rs2[:, i, 1:2]), [psf_b[k], rs2_b], [ib])

        sqk_t = sb("sqk_t", [P, 512], F32, st)
        sqr_t = sb("sqr_t", [P, 64], F32, st)
        rak = sb("rak", [P, 32], F32, st)
        rbk = sb("rbk", [P, 32], F32, st)
        Bq, Bk, Br = Buf("m_sqq"), Buf("m_sqk"), Buf("m_sqr")
        Bs = [Buf("m_ss%d" % q_) for q_ in range(4)]
        Bqr, Bkr = Buf("m_qrf"), Buf("m_krf")
        Bra, Brb, Brak, Brbk = Buf("m_ra"), Buf("m_rb"), Buf("m_rak"), Buf("m_rbk")
        o_qn, o_kn, o_qr, o_kr = Buf("o_qn"), Buf("o_kn"), Buf("o_qr"), Buf("o_kr")

        def mla_chain(i):
            Qs, KVs, ib = Qs2[i % 2], KVs2[i % 2], in_b[i % 2]
            Q3 = Qs[:].rearrange("p (h d) -> p h d", h=4)
            KV3 = KVs[:].rearrange("p (h d) -> p h d", h=4)
            sq3q = sq[:, 0:768].rearrange("p (h d) -> p h d", h=4)
            sq3k = sqk_t[:].rearrange("p (h d) -> p h d", h=4)
            V = nc.vector
            S.op("dve", lambda: V.tensor_tensor(sq[:, 0:768], Qs[:], Qs[:], ALU.mult), [ib], [Bq])
            S.op("dve", lambda: V.tensor_tensor(sq3k, KV3[:, :, 0:128], KV3[:, :, 0:128], ALU.mult), [ib], [Bk])
            S.op("dve", lambda: V.tensor_tensor(sqr_t[:], krraw[:, i, :], krraw[:, i, :], ALU.mult), [kr_b], [Br])
            S.op("dve", lambda: V.tensor_reduce(ssn[:, 0:4], sq3q[:, :, 0:128], AX.X, ALU.add), [Bq], [Bs[0]])
            S.op("dve", lambda: V.tensor_reduce(ssn[:, 8:12], sq3k, AX.X, ALU.add), [Bk], [Bs[2]])
            S.op("dve", lambda: V.tensor_reduce(ssn[:, 12:13], sqr_t[:], AX.X, ALU.add), [Br], [Bs[3]])
            S.op("dve", lambda: V.tensor_reduce(ssn[:, 4:8], sq3q[:, :, 128:192], AX.X, ALU.add), [Bq], [Bs[1]])
            S.op("dve", lambda: V.tensor_tensor(ssn[:, 0:13], ssn[:, 0:13], invn[:, 0:13], ALU.mult), Bs + [dv], Bs)
            S.op("act", lambda: nc.scalar.activation(ssn[:, 0:13], ssn[:, 0:13], AF.Sqrt, bias=epsb[:], scale=1.0),
                 Bs + [cc], Bs)
            S.op("dve", lambda: V.reciprocal(ssn[:, 0:13], ssn[:, 0:13]), Bs, Bs)
            S.op("dve", lambda: V.tensor_tensor(sq3q[:, :, 0:128], Q3[:, :, 0:128], vap(ssn[:], [[1, 4], [0, 128]]), ALU.mult),
                 [ib] + Bs, [Bq])
            S.op("dve", lambda: V.tensor_tensor(sq3k, KV3[:, :, 0:128], vap(ssn[:], [[1, 4], [0, 128]], off=8), ALU.mult),
                 [ib] + Bs, [Bk])
            S.op("dve", lambda: V.tensor_tensor(qr_f[:], Q3[:, :, 128:192], vap(ssn[:], [[1, 4], [0, 64]], off=4), ALU.mult),
                 [ib] + Bs, [Bqr])
            S.op("dve", lambda: V.tensor_scalar(kr_f[:], krraw[:, i, :], ssn[:, 12:13], None, ALU.mult), [kr_b] + Bs, [Bkr])
            S.op("dve", lambda: V.tensor_tensor(qn_s[:], sq3q[:, :, 0:128], vap(g_q[:], [[0, 4], [1, 128]]), ALU.mult),
                 [Bq, cb], [o_qn])
            S.op("dve", lambda: V.tensor_tensor(kn_s[:], sq3k, vap(g_k[:], [[0, 4], [1, 128]]), ALU.mult), [Bk, cb], [o_kn])
            S.op("dve", lambda: V.tensor_tensor(qr_f[:], qr_f[:], vap(g_q[:], [[0, 4], [1, 64]], off=128), ALU.mult),
                 [Bqr, cb], [Bqr])
            S.op("dve", lambda: V.tensor_tensor(kr_f[:], kr_f[:], g_k[:, 128:192], ALU.mult), [Bkr, cb], [Bkr])
            cos4 = vap(cosT[:], [[0, 4], [1, 32]], off=i * 32)
            sin4 = vap(sinT[:], [[0, 4], [1, 32]], off=i * 32)
            c1 = cosT[:, i, :]
            s1 = sinT[:, i, :]
            S.op("dve", lambda: V.tensor_tensor(ra[:], qr_f[:, :, 0:32], cos4, ALU.mult), [Bqr, cc], [Bra])
            S.op("dve", lambda: V.tensor_tensor(rak[:], kr_f[:, 0:32], c1, ALU.mult), [Bkr, cc], [Brak])
            S.op("dve", lambda: V.tensor_tensor(rb_[:], qr_f[:, :, 32:64], sin4, ALU.mult), [Bqr, cc], [Brb])
            S.op("dve", lambda: V.tensor_tensor(rbk[:], kr_f[:, 32:64], s1, ALU.mult), [Bkr, cc], [Brbk])
            S.op("dve", lambda: V.tensor_sub(qr_s[:, :, 0:32], ra[:], rb_[:]), [Bra, Brb], [o_qr])
            S.op("dve", lambda: V.tensor_sub(kr_s[:, 0:32], rak[:], rbk[:]), [Brak, Brbk], [o_kr])
            S.op("dve", lambda: V.tensor_tensor(ra[:], qr_f[:, :, 32:64], cos4, ALU.mult), [Bqr, cc], [Bra])
            S.op("dve", lambda: V.tensor_tensor(rak[:], kr_f[:, 32:64], c1, ALU.mult), [Bkr, cc], [Brak])
            S.op("dve", lambda: V.tensor_tensor(rb_[:], qr_f[:, :, 0:32], sin4, ALU.mult), [Bqr, cc], [Brb])
            S.op("dve", lambda: V.tensor_tensor(rbk[:], kr_f[:, 0:32], s1, ALU.mult), [Bkr, cc], [Brbk])
            S.op("dve", lambda: V.tensor_add(qr_s[:, :, 32:64], ra[:], rb_[:]), [Bra, Brb], [o_qr])
            S.op("dve", lambda: V.tensor_add(kr_s[:, 32:64], rak[:], rbk[:]), [Brak, Brbk], [o_kr])
            S.op("pool", lambda: nc.gpsimd.tensor_copy(vaug[:, i, :, 0:128], KV3[:, :, 128:256]), [ib, vaug_b], [vaug_b])

        def mla_tail(i):
            for hh in range(4):
                S.op("pe", lambda: nc.tensor.transpose(psb[0][:, hh * P:(hh + 1) * P], qn_s[:, hh, :], ident[:]),
                     [o_qn, cc], [psb_b[0]])
                S.op("pe", lambda: nc.tensor.transpose(psb[0][:, (4 + hh) * P:(5 + hh) * P], kn_s[:, hh, :], ident[:]),
                     [o_kn, cc], [psb_b[0]])
                S.op("pe", lambda: nc.tensor.transpose(psb[1][0:64, hh * P:(hh + 1) * P], qr_s[:, hh, :], ident[:]),
                     [o_qr, cc], [psb_b[1]])
            S.op("pe", lambda: nc.tensor.transpose(psb[1][0:64, 4 * P:5 * P], kr_s[:], ident[:]), [o_kr, cc], [psb_b[1]])
            S.op("act", lambda: nc.scalar.copy(qnT[:, :, i * P:(i + 1) * P], vap(psb[0][:], [[P, 4], [1, P]])),
                 [psb_b[0]], [qk2_b])
            S.op("act", lambda: nc.scalar.copy(knT[:, :, i * P:(i + 1) * P], vap(psb[0][:], [[P, 4], [1, P]], off=4 * P)),
                 [psb_b[0]], [qk2_b])
            S.op("act", lambda: nc.scalar.copy(qrT[0:64, :, i * P:(i + 1) * P], vap(psb[1][0:64, :], [[P, 4], [1, P]])),
                 [psb_b[1]], [qk2_b])
            S.op("act", lambda: nc.scalar.copy(krT[0:64, i * P:(i + 1) * P], psb[1][0:64, 4 * P:5 * P]), [psb_b[1]], [qk2_b])

        mla_front(0)
        for i in range(NT):
            if i + 1 < NT:
                mla_front(i + 1)
            mla_chain(i)
            mla_tail(i)
        S.barrier()
    if upto == "C2":
        return nc
    with Scope(mem) as st:
        PT = [sb("PT%d" % i, [P, 512], BF, st) for i in range(2)]
        PT_b = [Buf("PT0"), Buf("PT1")]
        on4 = sb("on4", [P, 4, P], F32, st)
        onb4 = sb("onb4", [P, 4, P], BF, st)
        junk4 = sb("junk4", [P, 4, P], BF, st)
        rden = sb("rden", [P, 4], F32, st)
        ss4 = sb("ss4", [P, 4], F32, st)
        r_b = [Buf("rden%d" % q) for q in range(4)]
        on_b = [Buf("on%d" % q) for q in range(4)]
        j_b = [Buf("junk%d" % q) for q in range(4)]
        s_b = Buf("ss4")
        onb_b = [Buf("onb%d" % q) for q in range(4)]
        acc = (psf[2], psf[3], psf[4], psf[5])
        acc_b = (psf_b[2], psf_b[3], psf_b[4], psf_b[5])
        it = 0

        def tail_part1(hh, qb):
            for qt in range(4):
                S.op("dve", lambda: nc.vector.reciprocal(rden[:, qt:qt + 1], acc[qt][:, 128:129]), [acc_b[qt]], [r_b[qt]])
            for qt in range(4):
                S.op("act", lambda: nc.scalar.mul(on4[:, qt, :], acc[qt][:, 0:128], rden[:, qt:qt + 1]),
                     [acc_b[qt], r_b[qt]], [on_b[qt]])
            for qt in range(4):
                S.op("act", lambda: nc.scalar.activation(junk4[:, qt, :], on4[:, qt, :], AF.Square,
                                                         accum_out=ss4[:, qt:qt + 1]), [on_b[qt]], [j_b[qt], s_b])
            S.op("act", lambda: nc.scalar.activation(ss4[:], ss4[:], AF.Sqrt, bias=epsb[:], scale=1.0 / 128),
                 [s_b, cc], [s_b])
            S.op("dve", lambda: nc.vector.reciprocal(ss4[:], ss4[:]), [s_b], [s_b])
            for qt in range(4):
                S.op("dve", lambda: nc.vector.scalar_tensor_tensor(onb4[:, qt, :], on4[:, qt, :], ss4[:, qt:qt + 1],
                                                                    g_mo[:, hh * P:(hh + 1) * P], ALU.mult, ALU.mult),
                     [on_b[qt], s_b, cb], [onb_b[qt]])

        def tail_part2(hh, qb):
            for qt in range(4):
                S.op("pe", lambda: nc.tensor.transpose(psb[0][:, qt * P:(qt + 1) * P], onb4[:, qt, :], ident[:]),
                     [onb_b[qt], cc], [psb_b[0]])
            S.op("act", lambda: nc.scalar.copy(mixT[:, 4 + hh, qb * 512:(qb + 1) * 512], psb[0][:, 0:512]),
                 [psb_b[0]], [mix_b])

        blocks = [(hh, qb) for hh in range(4) for qb in range(4)]
        pending = None
        for (hh, qb) in blocks:
            def emit_S(kt, k):
                S.op("pe", lambda: nc.tensor.matmul(psf[k][:], knT[:, hh, kt * P:(kt + 1) * P],
                                                    qnT[:, hh, qb * 512:(qb + 1) * 512], start=True, stop=False,
                                                    skip_group_check=True), [qk2_b], [psf_b[k]])
                S.op("pe", lambda: nc.tensor.matmul(psf[k][:], krT[:, kt * P:(kt + 1) * P],
                                                    qrT[:, hh, qb * 512:(qb + 1) * 512], start=False, stop=True,
                                                    skip_group_check=True), [qk2_b], [psf_b[k]])

            emit_S(0, it % 2)
            for kt in range(NT):
                k = it % 2
                it += 1
                if kt + 1 < NT:
                    emit_S(kt + 1, it % 2)
                S.op("act", lambda: nc.scalar.activation(PT[k][:], psf[k][:], AF.Exp, scale=SCALE),
                     [psf_b[k]], [PT_b[k]])
                for qt in range(4):
                    a = acc[qt]
                    S.op("pe", lambda: nc.tensor.matmul(a[:, 0:129],
                                                        PT[k][:, qt * P:(qt + 1) * P], vaug[:, kt, hh, 0:129],
                                                        start=(kt == 0), stop=(kt == NT - 1), skip_group_check=True),
                         [PT_b[k], vaug_b], [acc_b[qt]])
                if kt == 2 and pending is not None:
                    tail_part2(*pending)
                    pending = None
            tail_part1(hh, qb)
            pending = (hh, qb)
        tail_part2(*pending)
        S.barrier()
    pc.close()
    pEarly.close()
    dump("mix_mla", mixT[:, 4:8, :], [mix_b])
    if upto == "C":
        return nc

    pD = Scope(mem)
    y_acc = sb("y_acc", [P, NT, D], F32, pD)
    y_b = [Buf("y%d" % i) for i in range(NT)]
    h2T = sb("h2T", [P, 8, T], BF, pD)
    h2T_b = Buf("h2T")
    with Scope(mem) as st:
        wo = sb("wo", [P, 8, D], BF, st)
        wo_b = Buf("wo")
        sg = [sb("sgD0", [P, 8, 512], F32, st)] * 2
        sg_b = [Buf("sgD0")] * 2
        for half in range(2):
            S.dma("sp", sg[half][:], wout_d[:, :, half * 512:(half + 1) * 512], sg_b[half], writes=[sg_b[half]])
            for c in range(8):
                S.op("dve", lambda: nc.vector.tensor_copy(wo[:, c, half * 512:(half + 1) * 512], sg[half][:, c, :]),
                     [sg_b[half]], [wo_b])
        xt = [sb("xtD%d" % i, [P, D], F32, st) for i in range(2)]
        xt_b = [Buf("xtD0"), Buf("xtD1")]
        h2 = [sb("h2_%d" % i, [P, D], BF, st) for i in range(2)]
        h2_b = [Buf("h2_0"), Buf("h2_1")]
        junk = sb("junkD", [P, D], BF, st)
        junk_b = Buf("junkD")
        ssD = sb("ssD", [P, NT], F32, st)
        ssD_b = [Buf("ssD%d" % i) for i in range(NT)]
        def d_front(i):
            j = i % 2
            S.dma("sp", xt[j][:], x_d[i * P:(i + 1) * P, :], xt_b[j], writes=[xt_b[j]])
            for half in range(2):
                k = 2 * j + half
                for c in range(8):
                    S.op("pe", lambda: nc.tensor.matmul(psf[k][:], mixT[:, c, i * P:(i + 1) * P],
                                                        wo[:, c, half * 512:(half + 1) * 512], start=(c == 0), stop=(c == 7),
                                                        skip_group_check=True), [mix_b, wo_b], [psf_b[k]])
                S.op("dve", lambda: nc.vector.tensor_tensor(y_acc[:, i, half * 512:(half + 1) * 512], psf[k][:],
                                                            xt[j][:, half * 512:(half + 1) * 512], ALU.add),
                     [psf_b[k], xt_b[j]], [y_b[i]])
            S.op("act", lambda: nc.scalar.activation(junk[:], y_acc[:, i, :], AF.Square, accum_out=ssD[:, i:i + 1]),
                 [y_b[i]], [junk_b, ssD_b[i]])
            rstd_from_ss(ssD[:, i:i + 1], ssD[:, i:i + 1], D, [ssD_b[i]])
            S.op("dve", lambda: nc.vector.tensor_scalar(h2[j][:], y_acc[:, i, :], ssD[:, i:i + 1], None, ALU.mult),
                 [y_b[i], ssD_b[i]], [h2_b[j]])

        def d_tail(i):
            j = i % 2
            for c in range(8):
                S.op("pe", lambda: nc.tensor.transpose(psb[j][:, c * P:(c + 1) * P], h2[j][:, c * P:(c + 1) * P], ident[:]),
                     [h2_b[j], cc], [psb_b[j]])
            S.op("act", lambda: nc.scalar.copy(h2T[:, :, i * P:(i + 1) * P], vap(psb[j][:], [[P, 8], [1, P]])),
                 [psb_b[j]], [h2T_b])

        d_front(0)
        for i in range(NT):
            if i + 1 < NT:
                d_front(i + 1)
            d_tail(i)
        S.barrier()
    dump("x1", y_acc[:], y_b)
    pM.close()
    if upto == "D":
        return nc

    U32 = mybir.dt.uint32
    pE = Scope(mem)
    iota16 = iota_f[:, 128:144]
    thr16 = iota_f[:, 144:160]
    with Scope(mem) as st:
        weff = sb("weff", [P, 8, 2048], BF, st)
        weff_b = Buf("weff")
        with Scope(mem) as st2:
            wqT = sb("wqT_s", [P, 8, 1024], BF, st2)
            kT = sb("kT_s", [P, 16, P], BF, st2)
            wq_b = Buf("wqT")
            sg = sb("sgE", [P, 4, 1024], F32, st2)
            sg_b = Buf("sgE")
            S.dma("sp", sg[:, 0:2, :].rearrange("p a b -> p (a b)"), keysT_d.rearrange("p a b -> p (a b)"), sg_b, writes=[sg_b])
            S.op("dve", lambda: nc.vector.tensor_copy(kT[:].rearrange("p a b -> p (a b)"),
                                                      sg[:, 0:2, :].rearrange("p a b -> p (a b)")), [sg_b], [wq_b])
            for hf in range(2):
                for q2 in range(2):
                    q4 = hf * 2 + q2
                    S.dma("sp", sg[:], wqT_d[:, q4 * 4:(q4 + 1) * 4, :], sg_b, writes=[sg_b])
                    S.op("dve", lambda: nc.vector.tensor_copy(wqT[:, q2 * 4:(q2 + 1) * 4, :], sg[:]), [sg_b], [wq_b])
                for c in range(8):
                    for q2 in range(2):
                        q4 = hf * 2 + q2
                        k = q4 % 2
                        for u in range(4):
                            pcx = q4 * 4 + u
                            S.op("pe", lambda: nc.tensor.matmul(psf[k][:, u * P:(u + 1) * P],
                                                                wqT[:, q2 * 4 + u, c * P:(c + 1) * P],
                                                                kT[:, pcx, :], start=True, stop=True, skip_group_check=True),
                                 [wq_b], [psf_b[k]])
                        S.op("act", lambda: nc.scalar.mul(weff[:, c, q4 * 512:(q4 + 1) * 512], psf[k][:],
                                                          g_ffn[:, c:c + 1]), [psf_b[k], cb], [weff_b])
            S.barrier()
        sci = sb("sc0", [P, 16, P], F32, st)
        scb = Buf("sc0")
        GI = 4
        sc2 = [sb("sc2_%d" % j, [P, P], F32, st) for j in range(GI)]
        sc2_b = [Buf("sc2_%d" % j) for j in range(GI)]
        t1_b = [Buf("t1_%d" % j) for j in range(16)]
        i1_b = [Buf("i1_%d" % j) for j in range(16)]
        top = sb("top", [P, 16, 16], F32, st)
        idxu = sb("idxu", [P, 16, 16], U32, st)
        idxf = sb("idxf", [P, 16, 16], F32, st)
        cand = [sb("cand_%d" % j, [P, 256], F32, st) for j in range(GI)]
        cand2 = [sb("cand2_%d" % j, [P, 256], F32, st) for j in range(GI)]
        cd_b = [Buf("cd%d" % j) for j in range(GI)]
        cd2_b = [Buf("cd2_%d" % j) for j in range(GI)]
        sel_b = [Buf("sel%d" % j) for j in range(8)]
        pos_b = [Buf("pos%d" % j) for j in range(8)]
        idxf_b = Buf("idxf")
        posf_b = Buf("posf")
        af_b = Buf("af")
        bfb_b = Buf("bfb")
        g16_b = [Buf("g16a"), Buf("g16b")]
        t16_b = [Buf("t16a"), Buf("t16b")]
        abf_b = [Buf("abf0"), Buf("abf1"), Buf("abf2")]
        es_b = Buf("esel")
        zs_b = Buf("zs")
        sel = sb("sel", [P, 8, 16], F32, st)
        posu = sb("posu", [P, 8, 16], U32, st)
        posf = sb("posf2", [P, P], F32, st)
        ge16 = sb("ge16", [P, P, 16], BF, st)
        af = sb("af", [P, P], F32, st)
        bf_ = sb("bf_", [P, P], F32, st)
        esel = sb("esel", [P, 8, 16], F32, st)
        zs = sb("zs", [P, 8], F32, st)
        ab = sb("ab", [P, 3, P], BF, st)
        abf = sb("abf", [P, 3, P], F32, st)
        abT2 = sb("abT2", [P, 2, 3, P], BF, st)
        abT2_b = [Buf("abT2_0"), Buf("abT2_1")]
        tk = Buf("topk")
        ab_b = Buf("ab")
        SUB = 8
        WT = sb("WT", [P, P, P], BF, st)
        WT_b = Buf("WT")
        NAB = 3
        A1 = [sb("A1_%d" % i, [P, SUB, P], BF, st) for i in range(NAB)]
        A2 = [sb("A2_%d" % i, [P, SUB, P], BF, st) for i in range(NAB)]
        A1_b = [Buf("A1_%d" % i) for i in range(NAB)]
        A2_b = [Buf("A2_%d" % i) for i in range(NAB)]
        wd_b = [Buf("Wd%d" % i) for i in range(NT)]
        cnt = {"it": 0, "bk": 0}

        def e1_front(i):
            for q4 in range(4):
                k = q4
                for c in range(8):
                    S.op("pe", lambda: nc.tensor.matmul(psf[k][:], h2T[:, c, i * P:(i + 1) * P],
                                                        weff[:, c, q4 * 512:(q4 + 1) * 512], start=(c == 0), stop=(c == 7),
                                                        skip_group_check=True), [h2T_b, weff_b], [psf_b[k]])
                S.op("act", lambda: nc.scalar.copy(sci[:, q4 * 4:(q4 + 1) * 4, :].rearrange("p a b -> p (a b)"), psf[k][:]),
                     [psf_b[k]], [scb])


        def e1_chain(i):
            for g2_ in range(16 // GI):
                pcs = tuple(GI * g2_ + q_ for q_ in range(GI))
                for pcx in pcs:
                    S.op("dve", lambda: nc.vector.max(out=top[:, pcx, 0:8], in_=sci[:, pcx, :]), [scb], [t1_b[pcx]])
                for pcx in pcs:
                    S.op("dve", lambda: nc.vector.max_index(out=idxu[:, pcx, 0:8], in_max=top[:, pcx, 0:8],
                                                            in_values=sci[:, pcx, :]), [scb, t1_b[pcx]], [i1_b[pcx]])
                for pcx in pcs:
                    j = pcx % GI
                    S.op("dve", lambda: nc.vector.match_replace(out=sc2[j][:], in_to_replace=top[:, pcx, 0:8],
                                                                in_values=sci[:, pcx, :], imm_value=-1e30),
                         [scb, t1_b[pcx]], [sc2_b[j]])
                for pcx in pcs:
                    j = pcx % GI
                    S.op("dve", lambda: nc.vector.max(out=top[:, pcx, 8:16], in_=sc2[j][:]), [sc2_b[j]], [t1_b[pcx]])
                for pcx in pcs:
                    j = pcx % GI
                    S.op("dve", lambda: nc.vector.max_index(out=idxu[:, pcx, 8:16], in_max=top[:, pcx, 8:16],
                                                            in_values=sc2[j][:]), [sc2_b[j], t1_b[pcx]], [i1_b[pcx]])
                for _y in range(GI):
                    yield
            for h2_ in range(8 // GI):
                ps_ = tuple(GI * h2_ + q_ for q_ in range(GI))
                for p_ in ps_:
                    j = p_ % GI
                    S.op("dve", lambda: nc.vector.tensor_tensor(
                        cand[j][:].rearrange("p (a b) -> p a b", a=16),
                        vap(top[:], [[1, 16], [0, 16]], off=32 * p_), vap(top[:], [[0, 16], [1, 16]], off=32 * p_ + 16), ALU.add),
                        [t1_b[2 * p_], t1_b[2 * p_ + 1]], [cd_b[j]])
                for p_ in ps_:
                    j = p_ % GI
                    S.op("dve", lambda: nc.vector.max(out=sel[:, p_, 0:8], in_=cand[j][:]), [cd_b[j]], [sel_b[p_]])
                for p_ in ps_:
                    j = p_ % GI
                    S.op("dve", lambda: nc.vector.max_index(out=posu[:, p_, 0:8], in_max=sel[:, p_, 0:8],
                                                            in_values=cand[j][:]), [cd_b[j], sel_b[p_]], [pos_b[p_]])
                for p_ in ps_:
                    j = p_ % GI
                    S.op("dve", lambda: nc.vector.match_replace(out=cand2[j][:], in_to_replace=sel[:, p_, 0:8],
                                                                in_values=cand[j][:], imm_value=-1e30),
                         [cd_b[j], sel_b[p_]], [cd2_b[j]])
                for p_ in ps_:
                    j = p_ % GI
                    S.op("dve", lambda: nc.vector.max(out=sel[:, p_, 8:16], in_=cand2[j][:]), [cd2_b[j]], [sel_b[p_]])
                for p_ in ps_:
                    j = p_ % GI
                    S.op("dve", lambda: nc.vector.max_index(out=posu[:, p_, 8:16], in_max=sel[:, p_, 8:16],
                                                            in_values=cand2[j][:]), [cd2_b[j], sel_b[p_]], [pos_b[p_]])
                for _y in range(GI):
                    yield
            S.op("dve", lambda: nc.vector.tensor_copy(posf[:], posu[:].rearrange("p a b -> p (a b)")), pos_b, [posf_b])
            S.op("dve", lambda: nc.vector.tensor_tensor(esel[:], sel[:], vap(sel[:], [[16, 8], [0, 16]]), ALU.subtract),
                 sel_b, [es_b])
            S.op("dve", lambda: nc.vector.tensor_copy(idxf[:], idxu[:]), i1_b, [idxf_b])
            S.op("act", lambda: nc.scalar.activation(esel[:], esel[:], AF.Exp), [es_b], [es_b])
            gA = ge16[:].rearrange("p j a -> p (j a)")
            S.op("dve", lambda: nc.vector.tensor_tensor(ge16[:], vap(posf[:], [[1, P], [0, 16]]),
                                                        vap(thr16, [[0, P], [1, 16]]), ALU.is_ge),
                 [posf_b, cb] + g16_b, g16_b)
            S.op("dve", lambda: nc.vector.tensor_reduce(zs[:], esel[:], AX.X, ALU.add), [es_b], [zs_b])
            S.op("dve", lambda: nc.vector.tensor_reduce(af[:], ge16[:], AX.X, ALU.add), g16_b, [af_b])
            S.op("dve", lambda: nc.vector.reciprocal(zs[:], zs[:]), [zs_b], [zs_b])
            S.op("dve", lambda: nc.vector.tensor_scalar(af[:], af[:], -1.0, None, ALU.add), [af_b], [af_b])
            S.op("dve", lambda: nc.vector.tensor_tensor(abf[:, 2, :].rearrange("p (h k) -> p h k", h=8), esel[:],
                                                        vap(zs[:], [[1, 8], [0, 16]]), ALU.mult), [es_b, zs_b], [abf_b[2]])
            S.op("dve", lambda: nc.vector.scalar_tensor_tensor(bf_[:], af[:], -16.0, posf[:], ALU.mult, ALU.add),
                 [af_b, posf_b], [bfb_b])
            yield
            H_ = P // 2
            for which, src, srcb, o_ in ((0, af, af_b, 0), (1, bf_, bfb_b, 16)):
                for hv in range(2):
                    S.op("dve", lambda: nc.vector.tensor_tensor(
                        ge16[:, hv * H_:(hv + 1) * H_, :], vap(src[:, hv * H_:(hv + 1) * H_], [[1, H_], [0, 16]]),
                        vap(iota16, [[0, H_], [1, 16]]), ALU.is_equal), [srcb, cb, g16_b[hv]], [g16_b[hv]])
                for hv in range(2):
                    S.op("dve", lambda: nc.vector.tensor_tensor(
                        ge16[:, hv * H_:(hv + 1) * H_, :].rearrange("p (h k) a -> p h k a", h=4),
                        ge16[:, hv * H_:(hv + 1) * H_, :].rearrange("p (h k) a -> p h k a", h=4),
                        vap(idxf[:], [[32, 4], [0, 16], [1, 16]], off=o_ + hv * 128), ALU.mult),
                        [g16_b[hv], idxf_b], [g16_b[hv]])
                for hv in range(2):
                    S.op("dve", lambda: nc.vector.tensor_reduce(abf[:, which, hv * H_:(hv + 1) * H_],
                                                                ge16[:, hv * H_:(hv + 1) * H_, :], AX.X, ALU.add),
                         [g16_b[hv]], [abf_b[which]])
                yield
            S.op("dve", lambda: nc.vector.tensor_copy(ab[:], abf[:]), abf_b + [ab_b], [ab_b])

        def e1_tail(i):
            kb = i % 2
            for j in range(3):
                S.op("pe", lambda: nc.tensor.transpose(psb[kb][:, j * P:(j + 1) * P], ab[:, j, :], ident[:]),
                     [ab_b, cc], [psb_b[kb]])
            S.op("act", lambda: nc.scalar.copy(abT2[:, kb, :, :].rearrange("p a b -> p (a b)"), psb[kb][:, 0:3 * P]),
                 [psb_b[kb]], [abT2_b[kb]])

        def e2(i):
            kb = i % 2
            for sub in range(P // SUB):
                s_ = cnt["it"] % NAB
                cnt["it"] += 1
                t0 = sub * SUB
                io_bc = vap(iota128[:], [[0, SUB], [1, P]])
                S.op("dve", lambda: nc.vector.tensor_tensor(A2[s_][:], io_bc, vap(abT2[:, kb, 1, t0:t0 + SUB], [[1, SUB], [0, P]]),
                                                            ALU.is_equal), [abT2_b[kb], cc], [A2_b[s_]])
                S.op("dve", lambda: nc.vector.tensor_tensor(A1[s_][:], io_bc, vap(abT2[:, kb, 0, t0:t0 + SUB], [[1, SUB], [0, P]]),
                                                            ALU.is_equal), [abT2_b[kb], cc], [A1_b[s_]])
                S.op("pool", lambda: nc.gpsimd.tensor_tensor(A1[s_][:], A1[s_][:], vap(abT2[:, kb, 2, t0:t0 + SUB], [[1, SUB], [0, P]]),
                                                             ALU.mult), [abT2_b[kb], A1_b[s_]], [A1_b[s_]])
                for t8 in range(SUB // 8):
                    k0 = (cnt["bk"] % 3) * 2
                    cnt["bk"] += 1
                    for u8 in range(8):
                        tt = t8 * 8 + u8
                        kk_ = k0 + u8 // 4
                        S.op("pe", lambda: nc.tensor.matmul(vap(psf[kk_], [[4, P]], off=u8 % 4), A2[s_][:, tt, :], A1[s_][:, tt, :],
                                                            start=True, stop=True, skip_group_check=True),
                             [A1_b[s_], A2_b[s_]], [psf_b[kk_]])
                    tok = t0 + t8 * 8
                    S.op("act", lambda: nc.scalar.copy(vap(WT[:], [[P, P], [4, 2], [1, 4]], off=tok),
                                                       vap(psf[k0], [[4, P], [512, 2], [1, 4]])),
                         [psf_b[k0], psf_b[k0 + 1]], [WT_b])
                yield
            S.dma("sp", Wd[i], WT[:].rearrange("p a b -> p (a b)"), WT_b, reads=[WT_b], writes=[wd_b[i]])

        e1_front(0)
        for i in range(NT):
            g2 = e2(i - 1) if i >= 1 else iter(())
            kk2 = 0
            for _ in e1_chain(i):
                kk2 += 1
                if kk2 == 16 and i + 1 < NT:
                    e1_front(i + 1)
                if (kk2 * 16) // 27 > ((kk2 - 1) * 16) // 27:
                    next(g2, None)
            for _ in g2:
                pass
            e1_tail(i)
        for _ in e2(NT - 1):
            pass
        S.barrier()
    pE.close()
    if upto == "E":
        return nc

    NB = EG // P
    with Scope(mem) as st:
        ustg = sb("ustg0", [P, 8, EG], F32, st)
        vstg = sb("vstg0", [P, NB, D], F32, st)
        ustg_b, vstg_b = Buf("ustg0"), Buf("vstg0")
        ubf = [sb("ubf%d" % i, [P, 8, EG], BF, st) for i in range(2)]
        vbf = [sb("vbf%d" % i, [P, NB, D], BF, st) for i in range(2)]
        ubf_b = [Buf("ubf0"), Buf("ubf1")]
        vbf_b = [Buf("vbf0"), Buf("vbf1")]
        WTg = [sb("WTg%d" % i, [P, 4, NB * P], BF, st) for i in range(3)]
        WTg_b = [Buf("WTg%d" % i) for i in range(3)]
        ge = [sb("ge%d" % i, [P, 512], BF, st) for i in range(2)]
        ge_b = [Buf("ge0"), Buf("ge1")]
        GT = [sb("GT%d" % i, [P, NB, 512], BF, st) for i in range(2)]
        GT_b = [Buf("GT0"), Buf("GT1")]

        def load_group(g):
            S.dma("sp", ustg[:], UT_d[:, :, g * EG:(g + 1) * EG], ustg_b, writes=[ustg_b])
            S.dma("sp", vstg[:], V_d[g * EG:(g + 1) * EG, :].rearrange("(b p) d -> p b d", p=P), vstg_b,
                  writes=[vstg_b])

        def cast_group(g):
            s_ = g % 2
            S.op("pool", lambda: nc.gpsimd.tensor_tensor(ubf[s_][:], ustg[:], vap(g_ffn[:], [[1, 8], [0, EG]]), ALU.mult),
                 [ustg_b, cb], [ubf_b[s_]])
            S.op("pool", lambda: nc.gpsimd.tensor_copy(vbf[s_][:], vstg[:]), [vstg_b], [vbf_b[s_]])

        seq = [(g, q) for g in range(NG) for q in range(4)]
        NTOT = len(seq)

        def load_w(n):
            g, q = seq[n]
            S.dma("sp", WTg[n % 3][:], Wd[4 * q:4 * q + 4, :, g * EG:(g + 1) * EG].rearrange("a p f -> p a f"),
                  WTg_b[n % 3], reads=wd_b[4 * q:4 * q + 4], writes=[WTg_b[n % 3]])

        abank = [0]

        def st_AG(n):
            g, q = seq[n]
            s_, gt = g % 2, n % 2
            for b_ in range(NB):
                pa = abank[0] % 2
                abank[0] += 1
                for c in range(8):
                    S.op("pe", lambda: nc.tensor.matmul(psf[pa][:], ubf[s_][:, c, b_ * P:(b_ + 1) * P],
                                                        h2T[:, c, q * 512:(q + 1) * 512], start=(c == 0), stop=(c == 7),
                                                        skip_group_check=True), [h2T_b, ubf_b[s_]], [psf_b[pa]])
                S.op("act", lambda: nc.scalar.activation(ge[pa][:], psf[pa][:], AF.Gelu), [psf_b[pa]], [ge_b[pa]])
                S.op("dve", lambda: nc.vector.tensor_tensor(
                    GT[gt][:, b_, :].rearrange("p (a t) -> p a t", a=4), ge[pa][:].rearrange("p (a t) -> p a t", a=4),
                    vap(WTg[n % 3][:], [[NB * P, 4], [1, P]], off=b_ * P), ALU.mult),
                    [ge_b[pa], WTg_b[n % 3]], [GT_b[gt]])

        ybank = [0]

        def st_Y(n):
            g, q = seq[n]
            s_, gt = g % 2, n % 2
            for u in range(4):
                i = 4 * q + u
                for half in range(2):
                    py = 2 + ybank[0] % 4
                    ybank[0] += 1
                    for b_ in range(NB):
                        S.op("pe", lambda: nc.tensor.matmul(psf[py][:], GT[gt][:, b_, u * P:(u + 1) * P],
                                                            vbf[s_][:, b_, half * 512:(half + 1) * 512], start=(b_ == 0),
                                                            stop=(b_ == NB - 1), skip_group_check=True),
                             [GT_b[gt], vbf_b[s_]], [psf_b[py]])
                    S.op("dve", lambda: nc.vector.tensor_tensor(y_acc[:, i, half * 512:(half + 1) * 512],
                                                                psf[py][:], y_acc[:, i, half * 512:(half + 1) * 512],
                                                                ALU.add), [psf_b[py], y_b[i]], [y_b[i]])

        load_group(0)
        cast_group(0)
        if NG > 1:
            load_group(1)
        load_w(0)
        load_w(1)
        for n in range(NTOT):
            g, q = seq[n]
            if n + 2 < NTOT:
                load_w(n + 2)
            if q == 2 and g + 1 < NG:
                cast_group(g + 1)
                if g + 2 < NG:
                    load_group(g + 2)
            st_AG(n)
            if n >= 1:
                st_Y(n - 1)
        st_Y(NTOT - 1)
        ob = Buf("outst")
        for i in range(NT):
            S.dma("sp", out_d[i * P:(i + 1) * P, :], y_acc[:, i, :], ob, reads=[y_b[i]])
        S.barrier()
    pD.close()
    es.close()
    return nc


_HOST_CACHE = {}


def _prep_shared(inp):
    f = np.float32
    sh = {}
    sh["invf"] = np.ascontiguousarray(np.broadcast_to(
        (1.0 / (10000.0 ** (np.arange(0, 64, 2, dtype=f) / f(64)))).astype(f)[None, :], (P, 32)))
    sh["ident"] = np.eye(P, dtype=f)
    s = np.arange(P)[:, None]
    t = np.arange(P)[None, :]
    same = (s // 64) == (t // 64)
    sh["maskf"] = (same & (s <= t)).astype(f)
    sh["maskb"] = (same & (s >= t)).astype(f)
    rm = np.ones((P, T), f)
    rm[:, ::64] = 0.0
    sh["resetm"] = rm
    io = np.zeros((P, 160), f)
    io[:, 0:128] = np.arange(128, dtype=f)[None, :]
    io[:, 128:144] = np.arange(16, dtype=f)[None, :]
    io[:, 144:160] = 16.0 * np.arange(16, dtype=f)[None, :]
    sh["iota"] = io

    def pc(v):
        return np.ascontiguousarray(np.asarray(v, f).reshape(-1, P).T)

    def rep(v):
        v = np.asarray(v, f).reshape(1, -1)
        return np.ascontiguousarray(np.broadcast_to(v, (P, v.shape[1])))

    def kc(w):
        w = np.asarray(w, f)
        return np.ascontiguousarray(w.reshape(-1, P, w.shape[1]).transpose(1, 0, 2))

    sh["g_attn"] = pc(inp["attn_norm"][0])
    sh["g_ffn"] = pc(inp["ffn_norm"][0])
    lbl = np.asarray(inp["hg_lb_logits"], f)
    sh["lbl"] = np.ascontiguousarray(lbl.reshape(2, 2, 4, P).transpose(3, 0, 1, 2).reshape(P, 16))
    sh["g_hgo"] = np.ascontiguousarray(np.asarray(inp["hg_o_norm"][0], f).T)
    sh["g_qa"] = pc(inp["q_a_norm"][0])
    sh["g_kva"] = pc(inp["kv_a_norm"][0])
    sh["g_q"] = rep(inp["q_norm"][0])
    sh["g_k"] = rep(inp["k_norm"][0])
    sh["g_mo"] = rep(inp["mla_o_norm"][0])
    sh["w_in"] = kc(inp["w_in"][0])
    sh["w_qup"] = kc(inp["w_q_up"][0])
    sh["w_kvup"] = kc(inp["w_kv_up"][0])
    sh["w_out"] = kc(inp["w_out"][0])
    wq = np.asarray(inp["peer_w_q"][0], f)
    sh["wqT"] = np.ascontiguousarray(wq.reshape(D, 16, P).transpose(2, 1, 0))
    keys = np.asarray(inp["peer_sub_keys"][0], f)
    sh["keysT"] = np.ascontiguousarray(keys.reshape(16, P, P).transpose(2, 0, 1))
    u = np.asarray(inp["peer_u"][0], f)
    sh["UT"] = np.ascontiguousarray(u.reshape(NEXP, 8, P).transpose(2, 1, 0))
    sh["V"] = np.ascontiguousarray(np.asarray(inp["peer_v"][0], f))
    return sh


def make_in_maps(inputs, cores):
    sh = _prep_shared(inputs)
    x = np.asarray(inputs["x"], np.float32)
    pos = np.asarray(inputs["positions"], np.int32)
    maps = []
    for b in cores:
        m = dict(sh)
        m["x"] = np.ascontiguousarray(x[b])
        m["posT"] = np.ascontiguousarray(pos[b].reshape(NT, P).T)
        maps.append(m)
    return maps


def kernel(**inputs):
    nc = build_program()
    in_maps = make_in_maps(inputs, list(range(8)))
    res = run_bass_kernel_spmd(nc, in_maps, core_ids=list(range(8)))
    out = np.stack([np.asarray(r["out"], np.float32) for r in res.results], axis=0)
    return out
```

```python
import numpy as np
from contextlib import ExitStack
import concourse.bass as bass
import concourse.mybir as mybir
from concourse.bass_utils import run_bass_kernel_spmd

F32 = mybir.dt.float32
BF = mybir.dt.bfloat16
I32 = mybir.dt.int32
AF = mybir.ActivationFunctionType
ALU = mybir.AluOpType
AX = mybir.AxisListType

P = 128
T = 2048
NT = 16
D = 1024
EPS = 1e-6
NEXP = 16384
EG = 512
NG = NEXP // EG
IC = 16
NIC = 128 // IC
PI = float(np.pi)


class Buf:
    def __init__(self, name):
        self.name = name
        self.writer = None
        self.readers = []
        self.dsem = None
        self.dcnt = 0


class Sch:
    def __init__(self, nc):
        self.nc = nc
        self.eng = dict(pe=nc.tensor, dve=nc.vector, act=nc.scalar, pool=nc.gpsimd, sp=nc.sync)
        self.sem = {e: nc.alloc_semaphore("sem_" + e) for e in ("pe", "dve", "act", "pool")}
        self.cnt = {e: 0 for e in self.sem}
        self.seen = {e: {} for e in self.eng}
        self.dbufs = []

    def _wait(self, e, dep):
        key, h, v = dep
        if self.seen[e].get(key, 0) >= v:
            return
        self.eng[e].wait_ge(h, v)
        self.seen[e][key] = v

    def _deps(self, e, reads, writes):
        deps = []
        for b in reads:
            if b.writer is not None:
                deps.append(b.writer)
        for b in writes:
            if b.writer is not None:
                deps.append(b.writer)
            deps.extend(b.readers)
        for d in deps:
            if e == "pe" and d[0] == "pe":
                continue
            self._wait(e, d)

    def _mark(self, tok, reads, writes):
        for b in reads:
            b.readers.append(tok)
        for b in writes:
            b.writer = tok
            b.readers = []

    def op(self, e, fn, reads=(), writes=()):
        self._deps(e, reads, writes)
        ins = fn()
        self.cnt[e] += 1
        ins.then_inc(self.sem[e], 1)
        self.seen[e][e] = max(self.seen[e].get(e, 0), 0)
        self._mark((e, self.sem[e], self.cnt[e]), reads, writes)

    def dma(self, q, out, in_, sb, reads=(), writes=()):
        self._deps(q, reads, writes)
        if sb.dsem is None:
            sb.dsem = self.nc.alloc_semaphore("dsem_" + sb.name)
            self.dbufs.append(sb)
        ins = self.eng[q].dma_start(out=out, in_=in_)
        sb.dcnt += 16
        ins.then_inc(sb.dsem, 16)
        self._mark((("d", sb.name), sb.dsem, sb.dcnt), reads, writes)

    def barrier(self):
        for e in self.eng:
            for f in self.sem:
                if f != e and self.cnt[f] > 0:
                    self._wait(e, (f, self.sem[f], self.cnt[f]))
            for b in self.dbufs:
                if b.dcnt > 0:
                    self._wait(e, (("d", b.name), b.dsem, b.dcnt))


class Mem:
    def __init__(self, lo, hi):
        self.free = [(lo, hi)]

    def alloc(self, n):
        n = (n + 63) // 64 * 64
        for k, (a, b) in enumerate(self.free):
            if b - a >= n:
                self.free[k] = (a + n, b)
                return a, n
        raise MemoryError("SBUF arena exhausted (%d bytes) free=%s" % (n, self.free))

    def release(self, a, n):
        fl = sorted(self.free + [(a, a + n)])
        out = []
        for lo, hi in fl:
            if out and out[-1][1] >= lo:
                out[-1] = (out[-1][0], max(out[-1][1], hi))
            elif hi > lo:
                out.append((lo, hi))
        self.free = out


class Scope:
    def __init__(self, mem):
        self.mem = mem
        self.items = []

    def __enter__(self):
        return self

    def __exit__(self, *a):
        self.close()
        return False

    def close(self):
        for a, n in self.items:
            self.mem.release(a, n)
        self.items = []


DT_BYTES = {}


def vap(base, dims, off=0):
    return bass.AP(base.tensor, base.offset + off, [list(base.ap[0])] + [list(d) for d in dims])


def build_program(debug=None, upto=None):
    debug = debug or {}
    nc = bass.Bass("TRN2", target_bir_lowering=False)
    S = Sch(nc)

    def din(name, shape, dt=F32):
        return nc.dram_tensor(name, list(shape), dt, kind="ExternalInput").ap()

    x_d = din("x", [T, D])
    pos_d = din("posT", [P, NT], I32)
    invf_d = din("invf", [P, 32])
    ident_d = din("ident", [P, P])
    maskf_d = din("maskf", [P, P])
    maskb_d = din("maskb", [P, P])
    reset_d = din("resetm", [P, T])
    gattn_d = din("g_attn", [P, 8])
    gffn_d = din("g_ffn", [P, 8])
    lbl_d = din("lbl", [P, 16])
    ghgo_d = din("g_hgo", [P, 4])
    gqa_d = din("g_qa", [P, 3])
    gkva_d = din("g_kva", [P, 2])
    gq_d = din("g_q", [P, 192])
    gk_d = din("g_k", [P, 192])
    gmo_d = din("g_mo", [P, 512])
    iota_d = din("iota", [P, 160])
    win_d = din("w_in", [P, 8, 3264])
    wqup_d = din("w_qup", [P, 3, 768])
    wkvup_d = din("w_kvup", [P, 2, 1024])
    wout_d = din("w_out", [P, 8, 1024])
    wqT_d = din("wqT", [P, 16, 1024])
    keysT_d = din("keysT", [P, 16, 128])
    UT_d = din("UT", [P, 8, NEXP])
    V_d = din("V", [NEXP, D])
    out_d = nc.dram_tensor("out", [T, D], F32, kind="ExternalOutput").ap()
    Wd = nc.dram_tensor("Wd", [NT, P, NEXP], BF).ap()
    dbg_out = {}
    for k, (shp, dt_) in debug.items():
        dbg_out[k] = nc.dram_tensor("dbg_" + k, list(shp), dt_, kind="ExternalOutput").ap()

    es = ExitStack()
    mem = Mem(16512 + 64, 229344 - 64)
    root = Scope(mem)

    def sb(name, shape, dt=F32, stack=None):
        nbytes = int(np.prod(shape[1:])) * (4 if dt in (F32, I32, mybir.dt.uint32) else 2)
        a, n = mem.alloc(nbytes)
        (stack or root).items.append((a, n))
        addr_of[name] = a
        return nc.alloc_sbuf_tensor_at(name, list(shape), dt, offset=a)

    addr_of = {}

    def sb_alias(name, shape, dt, like):
        return nc.alloc_sbuf_tensor_at(name, list(shape), dt, offset=addr_of[like])

    psf_all = es.enter_context(nc.psum_tensor("psf_all", [P, 6, 512], F32))
    psf = [psf_all[:, i, :] for i in range(6)]
    psb = [es.enter_context(nc.psum_tensor("psb%d" % i, [P, 1024], BF)) for i in range(2)]
    psf_b = [Buf("psf%d" % i) for i in range(6)]
    psb_b = [Buf("psb%d" % i) for i in range(2)]

    cb = Buf("consts")
    pEarly = Scope(mem)
    ident_f = sb("ident_f", [P, P], F32, pEarly)
    ident = sb("ident", [P, P], BF)
    maskf = sb("maskf", [P, P], F32, pEarly)
    maskb = sb("maskb", [P, P], F32, pEarly)
    resetm = sb("resetm", [P, T], F32, pEarly)
    invf = sb("invf", [P, 32])
    posT = sb("posT", [P, NT], I32)
    g_attn = sb("g_attn", [P, 8])
    g_ffn = sb("g_ffn", [P, 8])
    lbl = sb("lbl", [P, 16])
    g_hgo = sb("g_hgo", [P, 4])
    g_qa = sb("g_qa", [P, 3])
    g_kva = sb("g_kva", [P, 2])
    g_q = sb("g_q", [P, 192], F32, pEarly)
    g_k = sb("g_k", [P, 192], F32, pEarly)
    g_mo = sb("g_mo", [P, 512], F32, pEarly)
    ones_bf = sb("ones_bf", [P, P], BF)
    iota_f = sb("iota_f", [P, 160])
    iota128 = sb("iota128", [P, P], BF)
    for dst, src in ((ident_f, ident_d), (maskf, maskf_d), (maskb, maskb_d), (resetm, reset_d),
                     (invf, invf_d), (posT, pos_d), (g_attn, gattn_d), (g_ffn, gffn_d), (lbl, lbl_d),
                     (g_hgo, ghgo_d), (g_qa, gqa_d), (g_kva, gkva_d), (g_q, gq_d), (g_k, gk_d),
                     (g_mo, gmo_d), (iota_f, iota_d)):
        S.dma("sp", dst[:], src, cb, writes=[cb])
    cc = Buf("consts2")
    S.op("dve", lambda: nc.vector.tensor_copy(ident[:], ident_f[:]), [cb], [cc])
    S.op("dve", lambda: nc.vector.memset(ones_bf[:], 1.0), [], [cc])
    S.op("dve", lambda: nc.vector.tensor_copy(iota128[:], iota_f[:, 0:128]), [cb], [cc])
    lb = sb("lb", [P, 8])
    oml = sb("oml", [P, 8])
    noml = sb("noml", [P, 8])
    S.op("dve", lambda: nc.vector.tensor_sub(lb[:], lbl[:, 0:8], lbl[:, 8:16]), [cb], [cc])
    S.op("act", lambda: nc.scalar.activation(lb[:], lb[:], AF.Sigmoid), [cc], [cc])
    S.op("dve", lambda: nc.vector.tensor_scalar(oml[:], lb[:], -1.0, 1.0, ALU.mult, ALU.add), [cc], [cc])
    S.op("dve", lambda: nc.vector.tensor_scalar(noml[:], oml[:], -1.0, None, ALU.mult), [cc], [cc])
    cosT = sb("cosT", [P, NT, 32], F32, pEarly)
    sinT = sb("sinT", [P, NT, 32], F32, pEarly)
    with Scope(mem) as st:
        posf = sb("posf", [P, NT], F32, st)
        ang = sb("ang", [P, NT, 32], F32, st)
        ang2 = sb("ang2", [P, NT, 32], F32, st)
        S.op("dve", lambda: nc.vector.tensor_copy(posf[:], posT[:]), [cb], [cc])
        S.op("dve", lambda: nc.vector.tensor_tensor(
            ang[:], vap(posf[:], [[1, NT], [0, 32]]), vap(invf[:], [[0, NT], [1, 32]]), ALU.mult), [cc, cb], [cc])
        ri = sb("ri", [P, NT, 32], I32, st)
        rf = sb("rf", [P, NT, 32], F32, st)
        hi = sb("hi", [P, NT, 32], F32, st)
        S.op("dve", lambda: nc.vector.tensor_scalar(ang[:], ang[:], 1.0 / (2 * PI), None, ALU.mult), [cc], [cc])
        S.op("dve", lambda: nc.vector.tensor_scalar(ang2[:], ang[:], 0.25, None, ALU.add), [cc], [cc])
        for src, dst in ((ang, sinT), (ang2, cosT)):
            S.op("dve", lambda: nc.vector.tensor_copy(ri[:], src[:]), [cc], [cc])
            S.op("dve", lambda: nc.vector.tensor_copy(rf[:], ri[:]), [cc], [cc])
            S.op("dve", lambda: nc.vector.tensor_sub(src[:], src[:], rf[:]), [cc], [cc])
            S.op("dve", lambda: nc.vector.tensor_scalar(hi[:], src[:], 0.5, None, ALU.is_gt), [cc], [cc])
            S.op("dve", lambda: nc.vector.tensor_sub(src[:], src[:], hi[:]), [cc], [cc])
            S.op("dve", lambda: nc.vector.tensor_scalar(hi[:], src[:], -0.5, None, ALU.is_lt), [cc], [cc])
            S.op("dve", lambda: nc.vector.tensor_add(src[:], src[:], hi[:]), [cc], [cc])
            S.op("act", lambda: nc.scalar.activation(dst[:], src[:], AF.Sin, scale=2 * PI), [cc], [cc])
        S.barrier()
    epsb = sb("epsb", [P, 1])
    S.op("dve", lambda: nc.vector.memset(epsb[:], EPS), [], [cc])
    if upto == "0":
        S.barrier()
        return nc

    def dump(name, src_ap, rbufs):
        if name in dbg_out:
            S.barrier()
            tb = Buf("dbg_" + name)
            S.dma("sp", dbg_out[name], src_ap, tb, reads=rbufs)
            S.barrier()

    def rstd_from_ss(dst, ss, n, bufs):
        S.op("act", lambda: nc.scalar.activation(dst, ss, AF.Sqrt, bias=epsb[:dst.shape[0]], scale=1.0 / n), bufs + [cc], bufs)
        S.op("dve", lambda: nc.vector.reciprocal(dst, dst), bufs, bufs)

    pM = Scope(mem)
    mixT = sb("mixT", [P, 8, T], BF, pM)
    mix_b = Buf("mixT")
    ph1 = Scope(mem)
    xnT = sb("xnT", [P, 8, T], BF, ph1)
    xnT_b = Buf("xnT")
    stg = [sb("stg0", [P, 8, 512], F32, ph1)] * 2
    stg_b = [Buf("stg0")] * 2
    with Scope(mem) as st:
        xt = [sb("xt%d" % i, [P, D], F32, st) for i in range(2)]
        xt_b = [Buf("xt%d" % i) for i in range(2)]
        xn = [sb("xn%d" % i, [P, D], BF, st) for i in range(2)]
        xn_b = [Buf("xn%d" % i) for i in range(2)]
        junk = sb("junkA", [P, D], BF, st)
        junk_b = Buf("junkA")
        ssA = sb("ssA", [P, NT], F32, st)
        ssA_b = [Buf("ssA%d" % i) for i in range(NT)]
        def a_front(i):
            j = i % 2
            S.dma("sp", xt[j][:], x_d[i * P:(i + 1) * P, :], xt_b[j], writes=[xt_b[j]])
            S.op("act", lambda: nc.scalar.activation(junk[:], xt[j][:], AF.Square, accum_out=ssA[:, i:i + 1]),
                 [xt_b[j]], [junk_b, ssA_b[i]])
            rstd_from_ss(ssA[:, i:i + 1], ssA[:, i:i + 1], D, [ssA_b[i]])
            S.op("dve", lambda: nc.vector.tensor_scalar(xn[j][:], xt[j][:], ssA[:, i:i + 1], None, ALU.mult),
                 [xt_b[j], ssA_b[i]], [xn_b[j]])

        def a_tail(i):
            j = i % 2
            pb = psb_b[i % 2]
            for c in range(8):
                S.op("pe", lambda: nc.tensor.transpose(psb[i % 2][:, c * P:(c + 1) * P], xn[j][:, c * P:(c + 1) * P], ident[:]),
                     [xn_b[j], cc], [pb])
            S.op("act", lambda: nc.scalar.copy(
                xnT[:, :, i * P:(i + 1) * P], vap(psb[i % 2][:], [[P, 8], [1, P]])), [pb], [xnT_b])

        a_front(0)
        for i in range(NT):
            if i + 1 < NT:
                a_front(i + 1)
            a_tail(i)
        S.barrier()

    if upto == "A":
        return nc
    def load_wslice(dst, dst_b, src_d, nchunks, cols, gain, slot):
        sg, sgb = stg[slot], stg_b[slot]
        o = 0
        for (c0, w) in cols:
            S.dma("sp", sg[:, 0:nchunks, o:o + w], src_d[:, :, c0:c0 + w], sgb, writes=[sgb])
            o += w
        for c in range(nchunks):
            if gain is not None:
                S.op("dve", lambda: nc.vector.tensor_scalar(dst[:, c, 0:o], sg[:, c, 0:o], gain[:, c:c + 1], None, ALU.mult),
                     [sgb, cb], [dst_b])
            else:
                S.op("dve", lambda: nc.vector.tensor_copy(dst[:, c, 0:o], sg[:, c, 0:o]), [sgb], [dst_b])

    def proj_fm(ps_ap, ps_b, w, w_b, col0, width, t0, n, src=None, src_b=None, nch=8):
        src = xnT if src is None else src
        src_b = xnT_b if src_b is None else src_b
        for c in range(nch):
            S.op("pe", lambda: nc.tensor.matmul(ps_ap, w[:, c, col0:col0 + width], src[:, c, t0:t0 + n],
                                                start=(c == 0), stop=(c == nch - 1), skip_group_check=True),
                 [w_b, src_b], [ps_b])

    with Scope(mem) as st:
        vtok = sb("vtok", [P, NT, 512], BF, st)
        vtok_b = Buf("vtok")
        wv = sb("wv", [P, 8, 512], BF, st)
        wv_b = Buf("wv")
        load_wslice(wv, wv_b, win_d, 8, [(1536, 512)], g_attn, 0)
        for i in range(NT):
            k = i % 2
            for c in range(8):
                S.op("pe", lambda: nc.tensor.matmul(psf[k][:], xnT[:, c, i * P:(i + 1) * P], wv[:, c, :],
                                                    start=(c == 0), stop=(c == 7), skip_group_check=True),
                     [xnT_b, wv_b], [psf_b[k]])
            S.op("act", lambda: nc.scalar.copy(vtok[:, i, :], psf[k][:]), [psf_b[k]], [vtok_b])
        wh = [wv] * 2
        wh_b = [wv_b] * 2
        if upto == "B1":
            S.barrier()
            return nc
        H = 1024
        qs = sb("qs", [P, T], F32, st)
        gsl = sb("gsl", [P, T], BF, st)
        glog = sb("glog", [P, H], F32, st)
        bcum = sb("bcum", [P, H], F32, st)
        kk = sb("kk", [P, H], F32, st)
        e1 = sb("e1", [P, H], F32, st)
        e2 = glog
        Qt = sb("Qt", [P, 2, T], BF, st)
        Kt = sb("Kt", [P, 2, T], BF, st)
        Ktok = sb("Ktok", [P, 2, NT, P], BF, st)
        Sbf = sb("Sbf", [P, 2, 32, P], BF, st)
        vm = sb("vm", [P, NT, 2, P], BF, st)
        vm_b = Buf("vm")
        Sm = [sb("Sm%d" % i, [P, 2, P], F32, st) for i in range(2)]
        dSd = sb("dSd", [P, 2, P], F32, st)
        dch = sb("dch", [P, 2, 32], F32, st)
        Pm = [sb("Pm%d" % i, [P, 2, P], BF, st) for i in range(2)]
        osq = sb("osq", [P, 512], BF, st)
        rbc = kk
        otmp = e1
        qs_b, gsl_b, e1_b, gl_b, kk_b, bc_b, dch_b = (Buf("hq"), Buf("hgs"), Buf("he1"), Buf("hgl"), Buf("hkk"), Buf("hbc"), Buf("hdch"))
        qk_b = Buf("QtKt")
        ktok_b = Buf("Ktok")
        sbf_b = Buf("Sbf")
        sm_b = Buf("Sm")
        pm_b = [Buf("Pm0"), Buf("Pm1")]
        ob = Buf("onorm")
        for h in range(4):
            w = wh[h % 2]
            wb = wh_b[h % 2]
            load_wslice(w, wb, win_d, 8, [(h * P, P), (512 + h * P, P), (1024 + h * P, P), (2048 + h * P, P)],
                        g_attn, (h + 1) % 2)
            for tb in range(4):
                k = tb % 2
                proj_fm(psf[k][:], psf_b[k], w, wb, 0, P, tb * 512, 512)
                S.op("act", lambda: nc.scalar.activation(qs[:, tb * 512:(tb + 1) * 512], psf[k][:], AF.Silu),
                     [psf_b[k]], [qs_b])
            for tb in range(4):
                k = tb % 2
                proj_fm(psf[k][:], psf_b[k], w, wb, 384, P, tb * 512, 512)
                S.op("act", lambda: nc.scalar.activation(gsl[:, tb * 512:(tb + 1) * 512], psf[k][:], AF.Silu),
                     [psf_b[k]], [gsl_b])
            for d in range(2):
                col = d * 4 + h
                for hf in range(2):
                    t0 = hf * H
                    for tb in range(2):
                        k = tb % 2
                        proj_fm(psf[k][:], psf_b[k], w, wb, (1 + d) * P, P, t0 + tb * 512, 512)
                        S.op("act", lambda: nc.scalar.activation(e1[:, tb * 512:(tb + 1) * 512], psf[k][:], AF.Sigmoid),
                             [psf_b[k]], [e1_b])
                    S.op("act", lambda: nc.scalar.activation(glog[:], e1[:], AF.Ln, bias=lb[:, col:col + 1],
                                                             scale=oml[:, col:col + 1]), [e1_b, cc], [gl_b])
                    S.op("dve", lambda: nc.vector.tensor_scalar(kk[:], e1[:], noml[:, col:col + 1], oml[:, col:col + 1],
                                                                ALU.mult, ALU.add), [e1_b, cc], [kk_b])
                    S.op("dve", lambda: nc.vector.tensor_tensor_scan(bcum[:], resetm[:, t0:t0 + H], glog[:], 0.0,
                                                                      ALU.mult, ALU.add), [gl_b, cb], [bc_b])
                    S.op("act", lambda: nc.scalar.activation(dch[:, d, hf * 16:(hf + 1) * 16],
                                                             vap(bcum[:], [[64, 16]], off=63), AF.Exp), [bc_b], [dch_b])
                    if d == 1:
                        S.op("dve", lambda: nc.vector.tensor_sub(glog[:], glog[:], bcum[:]), [gl_b, bc_b], [gl_b])
                        S.op("dve", lambda: nc.vector.tensor_tensor(
                            vap(glog[:], [[64, H // 64], [1, 64]]), vap(glog[:], [[64, H // 64], [1, 64]]),
                            vap(bcum[:], [[64, H // 64], [0, 64]], off=63), ALU.add), [gl_b, bc_b], [gl_b])
                        cur = glog
                        cur_ap = glog[:]
                    else:
                        cur_ap = bcum[:]
                    S.op("act", lambda: nc.scalar.activation(e1[:], cur_ap, AF.Exp), [gl_b, bc_b], [e1_b])
                    S.op("act", lambda: nc.scalar.activation(e2[:], cur_ap, AF.Exp, scale=-1.0), [gl_b, bc_b], [gl_b])
                    S.op("dve", lambda: nc.vector.tensor_tensor(Qt[:, d, t0:t0 + H], qs[:, t0:t0 + H], e1[:], ALU.mult),
                         [qs_b, e1_b], [qk_b])
                    S.op("dve", lambda: nc.vector.tensor_tensor(Kt[:, d, t0:t0 + H], kk[:], e2[:], ALU.mult),
                         [kk_b, gl_b], [qk_b])
            if upto == "B2":
                S.barrier()
                return nc
            for d in range(2):
                for g8 in range(2):
                    pbk = (d * 2 + g8) % 2
                    for u in range(8):
                        i = g8 * 8 + u
                        S.op("pe", lambda: nc.tensor.transpose(psb[pbk][:, u * P:(u + 1) * P], Kt[:, d, i * P:(i + 1) * P], ident[:]),
                             [qk_b, cc], [psb_b[pbk]])
                    S.op("act", lambda: nc.scalar.copy(Ktok[:, d, g8 * 8:(g8 + 1) * 8, :], vap(psb[pbk][:], [[P, 8], [1, P]])),
                         [psb_b[pbk]], [ktok_b])
            if upto == "B3":
                S.barrier()
                return nc
            for jj in range(2):
                S.op("dve", lambda: nc.vector.tensor_scalar(vm[:, :, jj, :], vtok[:, :, h * P:(h + 1) * P],
                                                            maskb[:, jj * 64:jj * 64 + 1], None, ALU.mult),
                     [vtok_b, cb], [vm_b])
            dsd_b = [Buf("dsd0"), Buf("dsd1")]
            smd_b = [[Buf("smd%d_%d" % (d_, q_)) for q_ in range(2)] for d_ in range(2)]
            S.op("pool", lambda: nc.gpsimd.memset(Sm[0][:], 0.0), [], [smd_b[0][0], smd_b[1][0]])
            S.op("pool", lambda: nc.gpsimd.memset(Sbf[:, 0, 0, :], 0.0), [], [sbf_b])
            S.op("pool", lambda: nc.gpsimd.memset(Sbf[:, 1, 31, :], 0.0), [], [sbf_b])
            for step in range(31):
                pp = step % 2
                cur, nxt = Sm[pp], Sm[1 - pp]
                cf, cbk = step, 31 - step
                for d, c in ((0, cf), (1, cbk)):
                    kd = 2 + 2 * d + (step % 2)
                    i, jj = c // 2, c % 2
                    S.op("pe", lambda: nc.tensor.matmul(psf[kd][:, 0:P], Ktok[:, d, i, :],
                                                        vm[:, i, jj, :], start=True, stop=True,
                                                        skip_group_check=True), [ktok_b, vm_b], [psf_b[kd]])
                for d, c in ((0, cf), (1, cbk)):
                    kd = 2 + 2 * d + (step % 2)
                    S.op("act", lambda: nc.scalar.mul(dSd[:, d, :], psf[kd][:, 0:P],
                                                      dch[:, d, c:c + 1]), [psf_b[kd], dch_b], [dsd_b[d]])
                for d, c in ((0, cf), (1, cbk)):
                    S.op("dve", lambda: nc.vector.scalar_tensor_tensor(nxt[:, d, :], cur[:, d, :], dch[:, d, c:c + 1],
                                                                        dSd[:, d, :], ALU.mult, ALU.add),
                         [smd_b[d][pp], dsd_b[d], dch_b], [smd_b[d][1 - pp]])
                for d, c in ((0, cf), (1, cbk)):
                    cn = c + 1 if d == 0 else c - 1
                    S.op("pool", lambda: nc.gpsimd.tensor_copy(Sbf[:, d, cn, :], nxt[:, d, :]), [smd_b[d][1 - pp]], [sbf_b])
            if upto == "B4":
                S.barrier()
                return nc
            def emit_scores(i):
                ks = i % 2
                for d in range(2):
                    S.op("pe", lambda: nc.tensor.matmul(psf[ks][:, d * P:(d + 1) * P], Kt[:, d, i * P:(i + 1) * P],
                                                        Qt[:, d, i * P:(i + 1) * P], start=True, stop=True,
                                                        skip_group_check=True), [qk_b], [psf_b[ks]])
                S.op("dve", lambda: nc.vector.tensor_tensor(Pm[ks][:, 0, :], psf[ks][:, 0:P], maskf[:], ALU.mult),
                     [psf_b[ks], cb], [pm_b[ks]])
                S.op("dve", lambda: nc.vector.tensor_tensor(Pm[ks][:, 1, :], psf[ks][:, P:2 * P], maskb[:], ALU.mult),
                     [psf_b[ks], cb], [pm_b[ks]])

            emit_scores(0)
            for i in range(NT):
                g4, u = divmod(i, 4)
                po = 4 + (g4 % 2)
                ks = i % 2
                if i + 1 < NT:
                    emit_scores(i + 1)
                oap = psf[po][:, u * P:(u + 1) * P]
                S.op("pe", lambda: nc.tensor.matmul(oap, vtok[:, i, h * P:(h + 1) * P], Pm[ks][:, 0, :], start=True,
                                                    stop=False, skip_group_check=True), [vtok_b, pm_b[ks]], [psf_b[po]])
                S.op("pe", lambda: nc.tensor.matmul(oap, vtok[:, i, h * P:(h + 1) * P], Pm[ks][:, 1, :], start=False,
                                                    stop=False, skip_group_check=True), [vtok_b, pm_b[ks]], [psf_b[po]])
                for d in range(2):
                    for jj in range(2):
                        c = 2 * i + jj
                        S.op("pe", lambda: nc.tensor.matmul(
                            psf[po][:, u * P + jj * 64:u * P + jj * 64 + 64], Sbf[:, d, c, :],
                            Qt[:, d, c * 64:(c + 1) * 64], start=False, stop=(d == 1 and jj == 1),
                            skip_group_check=True), [sbf_b, qk_b], [psf_b[po]])
                if u != 3:
                    continue
                S.op("act", lambda: nc.scalar.activation(osq[:], psf[po][:], AF.Square), [psf_b[po]], [ob])
                S.op("pe", lambda: nc.tensor.matmul(psf[3][:], ones_bf[:], osq[:], start=True, stop=True,
                                                    skip_group_check=True), [ob, cc], [psf_b[3]])
                S.op("act", lambda: nc.scalar.activation(rbc[:, 0:512], psf[3][:], AF.Sqrt, bias=epsb[:], scale=1.0 / 128),
                     [psf_b[3], cc], [ob, kk_b])
                S.op("dve", lambda: nc.vector.reciprocal(rbc[:, 0:512], rbc[:, 0:512]), [ob, kk_b], [ob, kk_b])
                S.op("dve", lambda: nc.vector.scalar_tensor_tensor(otmp[:, 0:512], psf[po][:], g_hgo[:, h:h + 1], rbc[:, 0:512],
                                                                    ALU.mult, ALU.mult), [psf_b[po], ob, cb, kk_b], [ob, e1_b])
                S.op("dve", lambda: nc.vector.tensor_tensor(mixT[:, h, g4 * 512:(g4 + 1) * 512], otmp[:, 0:512],
                                                            gsl[:, g4 * 512:(g4 + 1) * 512], ALU.mult), [ob, e1_b, gsl_b], [mix_b])
        S.barrier()
    dump("mix_hg", mixT[:, 0:4, :], [mix_b])
    if upto == "B":
        return nc

    SCALE = 192.0 ** -0.5
    pc = Scope(mem)
    cT = sb("cT", [P, 5, T], BF, pc)
    cT_b = Buf("cT")
    krraw = sb("krraw", [P, NT, 64], F32, pc)
    kr_b = Buf("krraw")
    rs2 = sb("rs2", [P, NT, 2], F32, pc)
    rs2_b = Buf("rs2")
    wqu = sb("wqu", [P, 3, 768], BF, pc)
    wkvu = sb("wkvu", [P, 2, 1024], BF, pc)
    wu_b = Buf("wup")
    with Scope(mem) as st:
        wm = sb("wm", [P, 8, 704], BF, st)
        wm_b = Buf("wm")
        load_wslice(wm[:, :, 0:512], wm_b, win_d, 8, [(2560, 512)], g_attn, 0)
        load_wslice(wm[:, :, 512:704], wm_b, win_d, 8, [(3072, 192)], g_attn, 1)
        csq = sb("csq", [P, 5, 512], BF, st)
        csq_b = Buf("csq")
        for tb in range(4):
            for j in range(5):
                k = j % 2
                proj_fm(psf[k][:], psf_b[k], wm, wm_b, j * P, P, tb * 512, 512)
                S.op("act", lambda: nc.scalar.copy(cT[:, j, tb * 512:(tb + 1) * 512], psf[k][:]), [psf_b[k]], [cT_b])
                S.op("act", lambda: nc.scalar.activation(csq[:, j, :], psf[k][:], AF.Square), [psf_b[k]], [csq_b])
            for u in range(4):
                i = tb * 4 + u
                for j in range(3):
                    S.op("pe", lambda: nc.tensor.matmul(psf[2][:, i * 2:i * 2 + 1], csq[:, j, u * P:(u + 1) * P],
                                                        ones_bf[:, 0:1], start=(j == 0), stop=(j == 2),
                                                        skip_group_check=True), [csq_b, cc], [psf_b[2]])
                for j in range(2):
                    S.op("pe", lambda: nc.tensor.matmul(psf[2][:, i * 2 + 1:i * 2 + 2], csq[:, 3 + j, u * P:(u + 1) * P],
                                                        ones_bf[:, 0:1], start=(j == 0), stop=(j == 1),
                                                        skip_group_check=True), [csq_b, cc], [psf_b[2]])
        S.op("act", lambda: nc.scalar.copy(rs2[:].rearrange("p a b -> p (a b)"), psf[2][:, 0:2 * NT]), [psf_b[2]], [rs2_b])
        rstd_from_ss(rs2[:, :, 0], rs2[:, :, 0], 384, [rs2_b])
        rstd_from_ss(rs2[:, :, 1], rs2[:, :, 1], 256, [rs2_b])
        for i in range(NT):
            k = 3 + i % 2
            for c in range(8):
                S.op("pe", lambda: nc.tensor.matmul(psf[k][:, 0:64], xnT[:, c, i * P:(i + 1) * P], wm[:, c, 640:704],
                                                    start=(c == 0), stop=(c == 7), skip_group_check=True),
                     [xnT_b, wm_b], [psf_b[k]])
            S.op("act", lambda: nc.scalar.copy(krraw[:, i, :], psf[k][:, 0:64]), [psf_b[k]], [kr_b])
        S.barrier()
    if upto == "C1":
        return nc
    sg0 = stg[0]
    S.dma("sp", sg0[:, 0:3, 0:512], wqup_d[:, :, 0:512], stg_b[0], writes=[stg_b[0]])
    for c in range(3):
        S.op("dve", lambda: nc.vector.tensor_scalar(wqu[:, c, 0:512], sg0[:, c, 0:512], g_qa[:, c:c + 1], None, ALU.mult),
             [stg_b[0], cb], [wu_b])
    S.dma("sp", sg0[:, 0:3, 0:256], wqup_d[:, :, 512:768], stg_b[0], writes=[stg_b[0]])
    for c in range(3):
        S.op("dve", lambda: nc.vector.tensor_scalar(wqu[:, c, 512:768], sg0[:, c, 0:256], g_qa[:, c:c + 1], None, ALU.mult),
             [stg_b[0], cb], [wu_b])
    for half in range(2):
        S.dma("sp", sg0[:, 0:2, 0:512], wkvup_d[:, :, half * 512:(half + 1) * 512], stg_b[0], writes=[stg_b[0]])
        for c in range(2):
            S.op("dve", lambda: nc.vector.tensor_scalar(wkvu[:, c, half * 512:(half + 1) * 512], sg0[:, c, 0:512],
                                                        g_kva[:, c:c + 1], None, ALU.mult), [stg_b[0], cb], [wu_b])
    S.barrier()
    ph1.close()
    qnT = sb("qnT", [P, 4, T], BF, pc)
    knT = sb("knT", [P, 4, T], BF, pc)
    qrT = sb("qrT", [P, 4, T], BF, pc)
    krT = sb("krT", [P, T], BF, pc)
    vaug = sb("vaug", [P, NT, 4, 132], BF, pc)
    qk2_b = Buf("qkT")
    vaug_b = Buf("vaug")
    S.op("pool", lambda: nc.gpsimd.memset(vaug[:], 1.0), [], [vaug_b])
    S.op("pool", lambda: nc.gpsimd.memset(qrT[:], 0.0), [], [qk2_b])
    S.op("pool", lambda: nc.gpsimd.memset(krT[:], 0.0), [], [qk2_b])
    with Scope(mem) as st:
        Qs2 = [sb("Qs%d" % i, [P, 768], F32, st) for i in range(2)]
        KVs2 = [sb("KVs%d" % i, [P, 1024], F32, st) for i in range(2)]
        in_b = [Buf("mla_in0"), Buf("mla_in1")]
        sq = sb("sqm", [P, 1024], F32, st)
        ssn = sb("ssn", [P, 16], F32, st)
        invn = sb("invn", [P, 16], F32, st)
        qn_s = sb("qn_s", [P, 4, P], BF, st)
        kn_s = sb("kn_s", [P, 4, P], BF, st)
        qr_f = sb("qr_f", [P, 4, 64], F32, st)
        kr_f = sb("kr_f", [P, 64], F32, st)
        qr_s = sb("qr_s", [P, 4, 64], BF, st)
        kr_s = sb("kr_s", [P, 64], BF, st)
        ra = sb("ra", [P, 4, 32], F32, st)
        rb_ = sb("rb_", [P, 4, 32], F32, st)
        dv = Buf("mla_dve")
        out_b = Buf("mla_out")
        S.op("dve", lambda: nc.vector.memset(invn[:, 0:4], 1.0 / 128), [], [dv])
        S.op("dve", lambda: nc.vector.memset(invn[:, 4:8], 1.0 / 64), [], [dv])
        S.op("dve", lambda: nc.vector.memset(invn[:, 8:12], 1.0 / 128), [], [dv])
        S.op("dve", lambda: nc.vector.memset(invn[:, 12:16], 1.0 / 64), [], [dv])

        def mla_front(i):
            Qs, KVs, ib = Qs2[i % 2], KVs2[i % 2], in_b[i % 2]
            for half in range(2):
                k = half
                for j in range(3):
                    S.op("pe", lambda: nc.tensor.matmul(psf[k][:, 0:384], cT[:, j, i * P:(i + 1) * P],
                                                        wqu[:, j, half * 384:(half + 1) * 384], start=(j == 0), stop=(j == 2),
                                                        skip_group_check=True), [cT_b, wu_b], [psf_b[k]])
                S.op("act", lambda: nc.scalar.mul(Qs[:, half * 384:(half + 1) * 384], psf[k][:, 0:384],
                                                  rs2[:, i, 0:1]), [psf_b[k], rs2_b], [ib])
            for half in range(2):
                k = 2 + half
                for j in range(2):
                    S.op("pe", lambda: nc.tensor.matmul(psf[k][:], cT[:, 3 + j, i * P:(i + 1) * P],
                                                        wkvu[:, j, half * 512:(half + 1) * 512], start=(j == 0), stop=(j == 1),
                                                        skip_group_check=True), [cT_b, wu_b], [psf_b[k]])
                S.op("act", lambda: nc.scalar.mul(KVs[:, half * 512:(half + 1) * 512], psf[k][:],
                                                  rs2[:, i, 1:2]), [psf_b[k], rs2_b], [ib])

        sqk_t = sb("sqk_t", [P, 512], F32, st)
        sqr_t = sb("sqr_t", [P, 64], F32, st)
        rak = sb("rak", [P, 32], F32, st)
        rbk = sb("rbk", [P, 32], F32, st)
        Bq, Bk, Br = Buf("m_sqq"), Buf("m_sqk"), Buf("m_sqr")
        Bs = [Buf("m_ss%d" % q_) for q_ in range(4)]
        Bqr, Bkr = Buf("m_qrf"), Buf("m_krf")
        Bra, Brb, Brak, Brbk = Buf("m_ra"), Buf("m_rb"), Buf("m_rak"), Buf("m_rbk")
        o_qn, o_kn, o_qr, o_kr = Buf("o_qn"), Buf("o_kn"), Buf("o_qr"), Buf("o_kr")

        def mla_chain(i):
            Qs, KVs, ib = Qs2[i % 2], KVs2[i % 2], in_b[i % 2]
            Q3 = Qs[:].rearrange("p (h d) -> p h d", h=4)
            KV3 = KVs[:].rearrange("p (h d) -> p h d", h=4)
            sq3q = sq[:, 0:768].rearrange("p (h d) -> p h d", h=4)
            sq3k = sqk_t[:].rearrange("p (h d) -> p h d", h=4)
            V = nc.vector
            S.op("dve", lambda: V.tensor_tensor(sq[:, 0:768], Qs[:], Qs[:], ALU.mult), [ib], [Bq])
            S.op("dve", lambda: V.tensor_tensor(sq3k, KV3[:, :, 0:128], KV3[:, :, 0:128], ALU.mult), [ib], [Bk])
            S.op("dve", lambda: V.tensor_tensor(sqr_t[:], krraw[:, i, :], krraw[:, i, :], ALU.mult), [kr_b], [Br])
            S.op("dve", lambda: V.tensor_reduce(ssn[:, 0:4], sq3q[:, :, 0:128], AX.X, ALU.add), [Bq], [Bs[0]])
            S.op("dve", lambda: V.tensor_reduce(ssn[:, 8:12], sq3k, AX.X, ALU.add), [Bk], [Bs[2]])
            S.op("dve", lambda: V.tensor_reduce(ssn[:, 12:13], sqr_t[:], AX.X, ALU.add), [Br], [Bs[3]])
            S.op("dve", lambda: V.tensor_reduce(ssn[:, 4:8], sq3q[:, :, 128:192], AX.X, ALU.add), [Bq], [Bs[1]])
            S.op("dve", lambda: V.tensor_tensor(ssn[:, 0:13], ssn[:, 0:13], invn[:, 0:13], ALU.mult), Bs + [dv], Bs)
            S.op("act", lambda: nc.scalar.activation(ssn[:, 0:13], ssn[:, 0:13], AF.Sqrt, bias=epsb[:], scale=1.0),
                 Bs + [cc], Bs)
            S.op("dve", lambda: V.reciprocal(ssn[:, 0:13], ssn[:, 0:13]), Bs, Bs)
            S.op("dve", lambda: V.tensor_tensor(sq3q[:, :, 0:128], Q3[:, :, 0:128], vap(ssn[:], [[1, 4], [0, 128]]), ALU.mult),
                 [ib] + Bs, [Bq])
            S.op("dve", lambda: V.tensor_tensor(sq3k, KV3[:, :, 0:128], vap(ssn[:], [[1, 4], [0, 128]], off=8), ALU.mult),
                 [ib] + Bs, [Bk])
            S.op("dve", lambda: V.tensor_tensor(qr_f[:], Q3[:, :, 128:192], vap(ssn[:], [[1, 4], [0, 64]], off=4), ALU.mult),
                 [ib] + Bs, [Bqr])
            S.op("dve", lambda: V.tensor_scalar(kr_f[:], krraw[:, i, :], ssn[:, 12:13], None, ALU.mult), [kr_b] + Bs, [Bkr])
            S.op("dve", lambda: V.tensor_tensor(qn_s[:], sq3q[:, :, 0:128], vap(g_q[:], [[0, 4], [1, 128]]), ALU.mult),
                 [Bq, cb], [o_qn])
            S.op("dve", lambda: V.tensor_tensor(kn_s[:], sq3k, vap(g_k[:], [[0, 4], [1, 128]]), ALU.mult), [Bk, cb], [o_kn])
            S.op("dve", lambda: V.tensor_tensor(qr_f[:], qr_f[:], vap(g_q[:], [[0, 4], [1, 64]], off=128), ALU.mult),
                 [Bqr, cb], [Bqr])
            S.op("dve", lambda: V.tensor_tensor(kr_f[:], kr_f[:], g_k[:, 128:192], ALU.mult), [Bkr, cb], [Bkr])
            cos4 = vap(cosT[:], [[0, 4], [1, 32]], off=i * 32)
            sin4 = vap(sinT[:], [[0, 4], [1, 32]], off=i * 32)
            c1 = cosT[:, i, :]
            s1 = sinT[:, i, :]
            S.op("dve", lambda: V.tensor_tensor(ra[:], qr_f[:, :, 0:32], cos4, ALU.mult), [Bqr, cc], [Bra])
            S.op("dve", lambda: V.tensor_tensor(rak[:], kr_f[:, 0:32], c1, ALU.mult), [Bkr, cc], [Brak])
            S.op("dve", lambda: V.tensor_tensor(rb_[:], qr_f[:, :, 32:64], sin4, ALU.mult), [Bqr, cc], [Brb])
            S.op("dve", lambda: V.tensor_tensor(rbk[:], kr_f[:, 32:64], s1, ALU.mult), [Bkr, cc], [Brbk])
            S.op("dve", lambda: V.tensor_sub(qr_s[:, :, 0:32], ra[:], rb_[:]), [Bra, Brb], [o_qr])
            S.op("dve", lambda: V.tensor_sub(kr_s[:, 0:32], rak[:], rbk[:]), [Brak, Brbk], [o_kr])
            S.op("dve", lambda: V.tensor_tensor(ra[:], qr_f[:, :, 32:64], cos4, ALU.mult), [Bqr, cc], [Bra])
            S.op("dve", lambda: V.tensor_tensor(rak[:], kr_f[:, 32:64], c1, ALU.mult), [Bkr, cc], [Brak])
            S.op("dve", lambda: V.tensor_tensor(rb_[:], qr_f[:, :, 0:32], sin4, ALU.mult), [Bqr, cc], [Brb])
            S.op("dve", lambda: V.tensor_tensor(rbk[:], kr_f[:, 0:32], s1, ALU.mult), [Bkr, cc], [Brbk])
            S.op("dve", lambda: V.tensor_add(qr_s[:, :, 32:64], ra[:], rb_[:]), [Bra, Brb], [o_qr])
            S.op("dve", lambda: V.tensor_add(kr_s[:, 32:64], rak[:], rbk[:]), [Brak, Brbk], [o_kr])
            S.op("pool", lambda: nc.gpsimd.tensor_copy(vaug[:, i, :, 0:128], KV3[:, :, 128:256]), [ib, vaug_b], [vaug_b])

        def mla_tail(i):
            for hh in range(4):
                S.op("pe", lambda: nc.tensor.transpose(psb[0][:, hh * P:(hh + 1) * P], qn_s[:, hh, :], ident[:]),
                     [o_qn, cc], [psb_b[0]])
                S.op("pe", lambda: nc.tensor.transpose(psb[0][:, (4 + hh) * P:(5 + hh) * P], kn_s[:, hh, :], ident[:]),
                     [o_kn, cc], [psb_b[0]])
                S.op("pe", lambda: nc.tensor.transpose(psb[1][0:64, hh * P:(hh + 1) * P], qr_s[:, hh, :], ident[:]),
                     [o_qr, cc], [psb_b[1]])
            S.op("pe", lambda: nc.tensor.transpose(psb[1][0:64, 4 * P:5 * P], kr_s[:], ident[:]), [o_kr, cc], [psb_b[1]])
            S.op("act", lambda: nc.scalar.copy(qnT[:, :, i * P:(i + 1) * P], vap(psb[0][:], [[P, 4], [1, P]])),
                 [psb_b[0]], [qk2_b])
            S.op("act", lambda: nc.scalar.copy(knT[:, :, i * P:(i + 1) * P], vap(psb[0][:], [[P, 4], [1, P]], off=4 * P)),
                 [psb_b[0]], [qk2_b])
            S.op("act", lambda: nc.scalar.copy(qrT[0:64, :, i * P:(i + 1) * P], vap(psb[1][0:64, :], [[P, 4], [1, P]])),
                 [psb_b[1]], [qk2_b])
            S.op("act", lambda: nc.scalar.copy(krT[0:64, i * P:(i + 1) * P], psb[1][0:64, 4 * P:5 * P]), [psb_b[1]], [qk2_b])

        mla_front(0)
        for i in range(NT):
            if i + 1 < NT:
                mla_front(i + 1)
            mla_chain(i)
            mla_tail(i)
        S.barrier()
    if upto == "C2":
        return nc
    pW = Scope(mem)
    wo = sb("wo", [P, 8, D], BF, pW)
    wo_b = Buf("wo")
    sgW = sb("sgW", [P, 8, 512], F32, pW)
    sgW_b = Buf("sgW")
    for half in range(2):
        S.dma("sp", sgW[:], wout_d[:, :, half * 512:(half + 1) * 512], sgW_b, writes=[sgW_b])
        for c in range(8):
            S.op("dve", lambda: nc.vector.tensor_copy(wo[:, c, half * 512:(half + 1) * 512], sgW[:, c, :]),
                 [sgW_b], [wo_b])
    with Scope(mem) as st:
        PT = [sb("PT%d" % i, [P, 512], BF, st) for i in range(2)]
        PT_b = [Buf("PT0"), Buf("PT1")]
        on4 = sb("on4", [P, 4, P], F32, st)
        onb4 = sb("onb4", [P, 4, P], BF, st)
        junk4 = sb("junk4", [P, 4, P], BF, st)
        rden = sb("rden", [P, 4], F32, st)
        ss4 = sb("ss4", [P, 4], F32, st)
        r_b = [Buf("rden%d" % q) for q in range(4)]
        on_b = [Buf("on%d" % q) for q in range(4)]
        j_b = [Buf("junk%d" % q) for q in range(4)]
        s_b = Buf("ss4")
        onb_b = [Buf("onb%d" % q) for q in range(4)]
        acc = (psf[2], psf[3], psf[4], psf[5])
        acc_b = (psf_b[2], psf_b[3], psf_b[4], psf_b[5])
        it = 0

        def tail_part1(hh, qb):
            for qt in range(4):
                S.op("dve", lambda: nc.vector.reciprocal(rden[:, qt:qt + 1], acc[qt][:, 128:129]), [acc_b[qt]], [r_b[qt]])
            for qt in range(4):
                S.op("act", lambda: nc.scalar.mul(on4[:, qt, :], acc[qt][:, 0:128], rden[:, qt:qt + 1]),
                     [acc_b[qt], r_b[qt]], [on_b[qt]])
            for qt in range(4):
                S.op("act", lambda: nc.scalar.activation(junk4[:, qt, :], on4[:, qt, :], AF.Square,
                                                         accum_out=ss4[:, qt:qt + 1]), [on_b[qt]], [j_b[qt], s_b])
            S.op("act", lambda: nc.scalar.activation(ss4[:], ss4[:], AF.Sqrt, bias=epsb[:], scale=1.0 / 128),
                 [s_b, cc], [s_b])
            S.op("dve", lambda: nc.vector.reciprocal(ss4[:], ss4[:]), [s_b], [s_b])
            for qt in range(4):
                S.op("dve", lambda: nc.vector.scalar_tensor_tensor(onb4[:, qt, :], on4[:, qt, :], ss4[:, qt:qt + 1],
                                                                    g_mo[:, hh * P:(hh + 1) * P], ALU.mult, ALU.mult),
                     [on_b[qt], s_b, cb], [onb_b[qt]])

        def tail_part2(hh, qb):
            for qt in range(4):
                S.op("pe", lambda: nc.tensor.transpose(psb[0][:, qt * P:(qt + 1) * P], onb4[:, qt, :], ident[:]),
                     [onb_b[qt], cc], [psb_b[0]])
            S.op("act", lambda: nc.scalar.copy(mixT[:, 4 + hh, qb * 512:(qb + 1) * 512], psb[0][:, 0:512]),
                 [psb_b[0]], [mix_b])

        blocks = [(hh, qb) for hh in range(4) for qb in range(4)]
        pending = None
        for (hh, qb) in blocks:
            def emit_S(kt, k):
                S.op("pe", lambda: nc.tensor.matmul(psf[k][:], knT[:, hh, kt * P:(kt + 1) * P],
                                                    qnT[:, hh, qb * 512:(qb + 1) * 512], start=True, stop=False,
                                                    skip_group_check=True), [qk2_b], [psf_b[k]])
                S.op("pe", lambda: nc.tensor.matmul(psf[k][:], krT[:, kt * P:(kt + 1) * P],
                                                    qrT[:, hh, qb * 512:(qb + 1) * 512], start=False, stop=True,
                                                    skip_group_check=True), [qk2_b], [psf_b[k]])

            emit_S(0, it % 2)
            for kt in range(NT):
                k = it % 2
                it += 1
                if kt + 1 < NT:
                    emit_S(kt + 1, it % 2)
                S.op("act", lambda: nc.scalar.activation(PT[k][:], psf[k][:], AF.Exp, scale=SCALE),
                     [psf_b[k]], [PT_b[k]])
                for qt in range(4):
                    a = acc[qt]
                    S.op("pe", lambda: nc.tensor.matmul(a[:, 0:129],
                                                        PT[k][:, qt * P:(qt + 1) * P], vaug[:, kt, hh, 0:129],
                                                        start=(kt == 0), stop=(kt == NT - 1), skip_group_check=True),
                         [PT_b[k], vaug_b], [acc_b[qt]])
                if kt == 2 and pending is not None:
                    tail_part2(*pending)
                    pending = None
            tail_part1(hh, qb)
            pending = (hh, qb)
        tail_part2(*pending)
        S.barrier()
    pc.close()
    pEarly.close()
    dump("mix_mla", mixT[:, 4:8, :], [mix_b])
    if upto == "C":
        return nc

    pD = Scope(mem)
    y_acc = sb("y_acc", [P, NT, D], F32, pD)
    y_b = [Buf("y%d" % i) for i in range(NT)]
    h2T = sb("h2T", [P, 8, T], BF, pD)
    h2T_b = Buf("h2T")
    with Scope(mem) as st:
        xt = [sb("xtD%d" % i, [P, D], F32, st) for i in range(2)]
        xt_b = [Buf("xtD0"), Buf("xtD1")]
        h2 = [sb("h2_%d" % i, [P, D], BF, st) for i in range(2)]
        h2_b = [Buf("h2_0"), Buf("h2_1")]
        junk = sb("junkD", [P, D], BF, st)
        junk_b = Buf("junkD")
        ssD = sb("ssD", [P, NT], F32, st)
        ssD_b = [Buf("ssD%d" % i) for i in range(NT)]
        def d_front(i):
            j = i % 2
            S.dma("sp", xt[j][:], x_d[i * P:(i + 1) * P, :], xt_b[j], writes=[xt_b[j]])
            for half in range(2):
                k = 2 * j + half
                for c in range(8):
                    S.op("pe", lambda: nc.tensor.matmul(psf[k][:], mixT[:, c, i * P:(i + 1) * P],
                                                        wo[:, c, half * 512:(half + 1) * 512], start=(c == 0), stop=(c == 7),
                                                        skip_group_check=True), [mix_b, wo_b], [psf_b[k]])
                S.op("dve", lambda: nc.vector.tensor_tensor(y_acc[:, i, half * 512:(half + 1) * 512], psf[k][:],
                                                            xt[j][:, half * 512:(half + 1) * 512], ALU.add),
                     [psf_b[k], xt_b[j]], [y_b[i]])
            S.op("act", lambda: nc.scalar.activation(junk[:], y_acc[:, i, :], AF.Square, accum_out=ssD[:, i:i + 1]),
                 [y_b[i]], [junk_b, ssD_b[i]])
            rstd_from_ss(ssD[:, i:i + 1], ssD[:, i:i + 1], D, [ssD_b[i]])
            S.op("dve", lambda: nc.vector.tensor_scalar(h2[j][:], y_acc[:, i, :], ssD[:, i:i + 1], None, ALU.mult),
                 [y_b[i], ssD_b[i]], [h2_b[j]])

        def d_tail(i):
            j = i % 2
            for c in range(8):
                S.op("pe", lambda: nc.tensor.transpose(psb[j][:, c * P:(c + 1) * P], h2[j][:, c * P:(c + 1) * P], ident[:]),
                     [h2_b[j], cc], [psb_b[j]])
            S.op("act", lambda: nc.scalar.copy(h2T[:, :, i * P:(i + 1) * P], vap(psb[j][:], [[P, 8], [1, P]])),
                 [psb_b[j]], [h2T_b])

        d_front(0)
        for i in range(NT):
            if i + 1 < NT:
                d_front(i + 1)
            d_tail(i)
        S.barrier()
    dump("x1", y_acc[:], y_b)
    pM.close()
    pW.close()
    if upto == "D":
        return nc

    U32 = mybir.dt.uint32
    pE = Scope(mem)
    iota16 = iota_f[:, 128:144]
    thr16 = iota_f[:, 144:160]
    with Scope(mem) as st:
        weff = sb("weff", [P, 8, 2048], BF, st)
        weff_b = Buf("weff")
        with Scope(mem) as st2:
            wqT = sb("wqT_s", [P, 8, 1024], BF, st2)
            kT = sb("kT_s", [P, 16, P], BF, st2)
            wq_b = Buf("wqT")
            sg = sb("sgE", [P, 4, 1024], F32, st2)
            sg_b = Buf("sgE")
            S.dma("sp", sg[:, 0:2, :].rearrange("p a b -> p (a b)"), keysT_d.rearrange("p a b -> p (a b)"), sg_b, writes=[sg_b])
            S.op("dve", lambda: nc.vector.tensor_copy(kT[:].rearrange("p a b -> p (a b)"),
                                                      sg[:, 0:2, :].rearrange("p a b -> p (a b)")), [sg_b], [wq_b])
            for hf in range(2):
                for q2 in range(2):
                    q4 = hf * 2 + q2
                    S.dma("sp", sg[:], wqT_d[:, q4 * 4:(q4 + 1) * 4, :], sg_b, writes=[sg_b])
                    S.op("dve", lambda: nc.vector.tensor_copy(wqT[:, q2 * 4:(q2 + 1) * 4, :], sg[:]), [sg_b], [wq_b])
                for c in range(8):
                    for q2 in range(2):
                        q4 = hf * 2 + q2
                        k = q4 % 2
                        for u in range(4):
                            pcx = q4 * 4 + u
                            S.op("pe", lambda: nc.tensor.matmul(psf[k][:, u * P:(u + 1) * P],
                                                                wqT[:, q2 * 4 + u, c * P:(c + 1) * P],
                                                                kT[:, pcx, :], start=True, stop=True, skip_group_check=True),
                                 [wq_b], [psf_b[k]])
                        S.op("act", lambda: nc.scalar.mul(weff[:, c, q4 * 512:(q4 + 1) * 512], psf[k][:],
                                                          g_ffn[:, c:c + 1]), [psf_b[k], cb], [weff_b])
            S.barrier()
        sci = sb("sc0", [P, 16, P], F32, st)
        scb = Buf("sc0")
        GI = 4
        NCAND = 112
        sc2 = [sb("sc2_%d" % j, [P, P], F32, st) for j in range(GI)]
        sc2_b = [Buf("sc2_%d" % j) for j in range(GI)]
        t1_b = [Buf("t1_%d" % j) for j in range(16)]
        i1_b = [Buf("i1_%d" % j) for j in range(16)]
        top = sb("top", [P, 16, 16], F32, st)
        idxu = sb("idxu", [P, 16, 16], U32, st)
        idxf = sb("idxf", [P, 16, 16], F32, st)
        cand = [sb("cand_%d" % j, [P, 256], F32, st) for j in range(GI)]
        cand2 = [sb("cand2_%d" % j, [P, 256], F32, st) for j in range(GI)]
        cd_b = [Buf("cd%d" % j) for j in range(GI)]
        cd2_b = [Buf("cd2_%d" % j) for j in range(GI)]
        sel_b = [Buf("sel%d" % j) for j in range(8)]
        pos_b = [Buf("pos%d" % j) for j in range(8)]
        idxf_b = Buf("idxf")
        posf_b = Buf("posf")
        af_b = Buf("af")
        bfb_b = Buf("bfb")
        g16_b = [Buf("g16a"), Buf("g16b")]
        t16_b = [Buf("t16a"), Buf("t16b")]
        abf_b = [Buf("abf0"), Buf("abf1"), Buf("abf2")]
        es_b = Buf("esel")
        zs_b = Buf("zs")
        sel = sb("sel", [P, 8, 16], F32, st)
        posu = sb("posu", [P, 8, 16], U32, st)
        posf = sb("posf2", [P, P], F32, st)
        ge16 = sb("ge16", [P, P, 16], BF, st)
        af = sb("af", [P, P], F32, st)
        bf_ = sb("bf_", [P, P], F32, st)
        esel = sb("esel", [P, 8, 16], F32, st)
        zs = sb("zs", [P, 8], F32, st)
        ab = sb("ab", [P, 3, P], BF, st)
        abf = sb("abf", [P, 3, P], F32, st)
        abT2 = sb("abT2", [P, 2, 3, P], BF, st)
        abT2_b = [Buf("abT2_0"), Buf("abT2_1")]
        tk = Buf("topk")
        ab_b = Buf("ab")
        SUB = 8
        WT = sb("WT", [P, P, P], BF, st)
        WT_b = Buf("WT")
        NAB = 3
        A1 = [sb("A1_%d" % i, [P, SUB, P], BF, st) for i in range(NAB)]
        A2 = [sb("A2_%d" % i, [P, SUB, P], BF, st) for i in range(NAB)]
        A1_b = [Buf("A1_%d" % i) for i in range(NAB)]
        A2_b = [Buf("A2_%d" % i) for i in range(NAB)]
        wd_b = [Buf("Wd%d" % i) for i in range(NT)]
        cnt = {"it": 0, "bk": 0}

        def e1_front(i):
            for q4 in range(4):
                k = q4
                for c in range(8):
                    S.op("pe", lambda: nc.tensor.matmul(psf[k][:], h2T[:, c, i * P:(i + 1) * P],
                                                        weff[:, c, q4 * 512:(q4 + 1) * 512], start=(c == 0), stop=(c == 7),
                                                        skip_group_check=True), [h2T_b, weff_b], [psf_b[k]])
                S.op("act", lambda: nc.scalar.copy(sci[:, q4 * 4:(q4 + 1) * 4, :].rearrange("p a b -> p (a b)"), psf[k][:]),
                     [psf_b[k]], [scb])


        def e1_chain(i):
            for g2_ in range(16 // GI):
                pcs = tuple(GI * g2_ + q_ for q_ in range(GI))
                for pcx in pcs:
                    S.op("dve", lambda: nc.vector.max(out=top[:, pcx, 0:8], in_=sci[:, pcx, :]), [scb], [t1_b[pcx]])
                for pcx in pcs:
                    S.op("dve", lambda: nc.vector.max_index(out=idxu[:, pcx, 0:8], in_max=top[:, pcx, 0:8],
                                                            in_values=sci[:, pcx, :]), [scb, t1_b[pcx]], [i1_b[pcx]])
                for pcx in pcs:
                    j = pcx % GI
                    S.op("dve", lambda: nc.vector.match_replace(out=sc2[j][:], in_to_replace=top[:, pcx, 0:8],
                                                                in_values=sci[:, pcx, :], imm_value=-1e30),
                         [scb, t1_b[pcx]], [sc2_b[j]])
                for pcx in pcs:
                    j = pcx % GI
                    S.op("dve", lambda: nc.vector.max(out=top[:, pcx, 8:16], in_=sc2[j][:]), [sc2_b[j]], [t1_b[pcx]])
                for pcx in pcs:
                    j = pcx % GI
                    S.op("dve", lambda: nc.vector.max_index(out=idxu[:, pcx, 8:16], in_max=top[:, pcx, 8:16],
                                                            in_values=sc2[j][:]), [sc2_b[j], t1_b[pcx]], [i1_b[pcx]])
                for _y in range(GI):
                    yield
            for h2_ in range(8 // GI):
                ps_ = tuple(GI * h2_ + q_ for q_ in range(GI))
                for p_ in ps_:
                    j = p_ % GI
                    S.op("dve", lambda: nc.vector.tensor_tensor(
                        cand[j][:, 0:64].rearrange("p (a b) -> p a b", a=4),
                        vap(top[:], [[1, 4], [0, 16]], off=32 * p_), vap(top[:], [[0, 4], [1, 16]], off=32 * p_ + 16), ALU.add),
                        [t1_b[2 * p_], t1_b[2 * p_ + 1]], [cd_b[j]])
                for p_ in ps_:
                    j = p_ % GI
                    S.op("dve", lambda: nc.vector.tensor_tensor(
                        cand[j][:, 64:NCAND].rearrange("p (a b) -> p a b", a=12),
                        vap(top[:], [[1, 12], [0, 4]], off=32 * p_ + 4), vap(top[:], [[0, 12], [1, 4]], off=32 * p_ + 16), ALU.add),
                        [t1_b[2 * p_], t1_b[2 * p_ + 1], cd_b[j]], [cd_b[j]])
                for p_ in ps_:
                    j = p_ % GI
                    S.op("dve", lambda: nc.vector.max(out=sel[:, p_, 0:8], in_=cand[j][:, 0:NCAND]), [cd_b[j]], [sel_b[p_]])
                for p_ in ps_:
                    j = p_ % GI
                    S.op("dve", lambda: nc.vector.max_index(out=posu[:, p_, 0:8], in_max=sel[:, p_, 0:8],
                                                            in_values=cand[j][:, 0:NCAND]), [cd_b[j], sel_b[p_]], [pos_b[p_]])
                for p_ in ps_:
                    j = p_ % GI
                    S.op("dve", lambda: nc.vector.match_replace(out=cand2[j][:, 0:NCAND], in_to_replace=sel[:, p_, 0:8],
                                                                in_values=cand[j][:, 0:NCAND], imm_value=-1e30),
                         [cd_b[j], sel_b[p_]], [cd2_b[j]])
                for p_ in ps_:
                    j = p_ % GI
                    S.op("dve", lambda: nc.vector.max(out=sel[:, p_, 8:16], in_=cand2[j][:, 0:NCAND]), [cd2_b[j]], [sel_b[p_]])
                for p_ in ps_:
                    j = p_ % GI
                    S.op("dve", lambda: nc.vector.max_index(out=posu[:, p_, 8:16], in_max=sel[:, p_, 8:16],
                                                            in_values=cand2[j][:, 0:NCAND]), [cd2_b[j], sel_b[p_]], [pos_b[p_]])
                for _y in range(GI):
                    yield
            S.op("dve", lambda: nc.vector.tensor_copy(posf[:], posu[:].rearrange("p a b -> p (a b)")), pos_b, [posf_b])
            S.op("dve", lambda: nc.vector.tensor_tensor(esel[:], sel[:], vap(sel[:], [[16, 8], [0, 16]]), ALU.subtract),
                 sel_b, [es_b])
            S.op("dve", lambda: nc.vector.tensor_copy(idxf[:], idxu[:]), i1_b, [idxf_b])
            S.op("act", lambda: nc.scalar.activation(esel[:], esel[:], AF.Exp), [es_b], [es_b])
            gA = ge16[:].rearrange("p j a -> p (j a)")
            S.op("dve", lambda: nc.vector.tensor_tensor(ge16[:], vap(posf[:], [[1, P], [0, 16]]),
                                                        vap(thr16, [[0, P], [1, 16]]), ALU.is_ge),
                 [posf_b, cb] + g16_b, g16_b)
            S.op("dve", lambda: nc.vector.tensor_reduce(zs[:], esel[:], AX.X, ALU.add), [es_b], [zs_b])
            S.op("dve", lambda: nc.vector.tensor_reduce(af[:], ge16[:], AX.X, ALU.add), g16_b, [af_b])
            S.op("dve", lambda: nc.vector.reciprocal(zs[:], zs[:]), [zs_b], [zs_b])
            S.op("dve", lambda: nc.vector.tensor_scalar(af[:], af[:], -1.0, None, ALU.add), [af_b], [af_b])
            S.op("dve", lambda: nc.vector.tensor_tensor(abf[:, 2, :].rearrange("p (h k) -> p h k", h=8), esel[:],
                                                        vap(zs[:], [[1, 8], [0, 16]]), ALU.mult), [es_b, zs_b], [abf_b[2]])
            S.op("dve", lambda: nc.vector.scalar_tensor_tensor(bf_[:], af[:], -16.0, posf[:], ALU.mult, ALU.add),
                 [af_b, posf_b], [bfb_b])
            S.op("dve", lambda: nc.vector.tensor_scalar(posf[:], af[:], -4.0, 0.0, ALU.add, ALU.max), [af_b, posf_b, bfb_b], [posf_b])
            S.op("dve", lambda: nc.vector.scalar_tensor_tensor(bf_[:], posf[:], 12.0, bf_[:], ALU.mult, ALU.add),
                 [posf_b, bfb_b], [bfb_b])
            yield
            H_ = P // 2
            for which, src, srcb, o_ in ((0, af, af_b, 0), (1, bf_, bfb_b, 16)):
                for hv in range(2):
                    S.op("dve", lambda: nc.vector.tensor_tensor(
                        ge16[:, hv * H_:(hv + 1) * H_, :], vap(src[:, hv * H_:(hv + 1) * H_], [[1, H_], [0, 16]]),
                        vap(iota16, [[0, H_], [1, 16]]), ALU.is_equal), [srcb, cb, g16_b[hv]], [g16_b[hv]])
                for hv in range(2):
                    S.op("dve", lambda: nc.vector.tensor_tensor(
                        ge16[:, hv * H_:(hv + 1) * H_, :].rearrange("p (h k) a -> p h k a", h=4),
                        ge16[:, hv * H_:(hv + 1) * H_, :].rearrange("p (h k) a -> p h k a", h=4),
                        vap(idxf[:], [[32, 4], [0, 16], [1, 16]], off=o_ + hv * 128), ALU.mult),
                        [g16_b[hv], idxf_b], [g16_b[hv]])
                for hv in range(2):
                    S.op("dve", lambda: nc.vector.tensor_reduce(abf[:, which, hv * H_:(hv + 1) * H_],
                                                                ge16[:, hv * H_:(hv + 1) * H_, :], AX.X, ALU.add),
                         [g16_b[hv]], [abf_b[which]])
                yield
            S.op("dve", lambda: nc.vector.tensor_copy(ab[:], abf[:]), abf_b + [ab_b], [ab_b])

        def e1_tail(i):
            kb = i % 2
            for j in range(3):
                S.op("pe", lambda: nc.tensor.transpose(psb[kb][:, j * P:(j + 1) * P], ab[:, j, :], ident[:]),
                     [ab_b, cc], [psb_b[kb]])
            S.op("act", lambda: nc.scalar.copy(abT2[:, kb, :, :].rearrange("p a b -> p (a b)"), psb[kb][:, 0:3 * P]),
                 [psb_b[kb]], [abT2_b[kb]])

        def e2(i):
            kb = i % 2
            for sub in range(P // SUB):
                s_ = cnt["it"] % NAB
                cnt["it"] += 1
                t0 = sub * SUB
                io_bc = vap(iota128[:], [[0, SUB], [1, P]])
                S.op("dve", lambda: nc.vector.tensor_tensor(A2[s_][:], io_bc, vap(abT2[:, kb, 1, t0:t0 + SUB], [[1, SUB], [0, P]]),
                                                            ALU.is_equal), [abT2_b[kb], cc], [A2_b[s_]])
                S.op("dve", lambda: nc.vector.tensor_tensor(A1[s_][:], io_bc, vap(abT2[:, kb, 0, t0:t0 + SUB], [[1, SUB], [0, P]]),
                                                            ALU.is_equal), [abT2_b[kb], cc], [A1_b[s_]])
                S.op("pool", lambda: nc.gpsimd.tensor_tensor(A1[s_][:], A1[s_][:], vap(abT2[:, kb, 2, t0:t0 + SUB], [[1, SUB], [0, P]]),
                                                             ALU.mult), [abT2_b[kb], A1_b[s_]], [A1_b[s_]])
                for t8 in range(SUB // 8):
                    k0 = (cnt["bk"] % 3) * 2
                    cnt["bk"] += 1
                    for u8 in range(8):
                        tt = t8 * 8 + u8
                        kk_ = k0 + u8 // 4
                        S.op("pe", lambda: nc.tensor.matmul(vap(psf[kk_], [[4, P]], off=u8 % 4), A2[s_][:, tt, :], A1[s_][:, tt, :],
                                                            start=True, stop=True, skip_group_check=True),
                             [A1_b[s_], A2_b[s_]], [psf_b[kk_]])
                    tok = t0 + t8 * 8
                    S.op("act", lambda: nc.scalar.copy(vap(WT[:], [[P, P], [4, 2], [1, 4]], off=tok),
                                                       vap(psf[k0], [[4, P], [512, 2], [1, 4]])),
                         [psf_b[k0], psf_b[k0 + 1]], [WT_b])
                yield
            S.dma("sp", Wd[i], WT[:].rearrange("p a b -> p (a b)"), WT_b, reads=[WT_b], writes=[wd_b[i]])

        e1_front(0)
        for i in range(NT):
            g2 = e2(i - 1) if i >= 1 else iter(())
            kk2 = 0
            for _ in e1_chain(i):
                kk2 += 1
                if kk2 == 16 and i + 1 < NT:
                    e1_front(i + 1)
                if (kk2 * 16) // 27 > ((kk2 - 1) * 16) // 27:
                    next(g2, None)
            for _ in g2:
                pass
            e1_tail(i)
        for _ in e2(NT - 1):
            pass
        S.barrier()
    pE.close()
    if upto == "E":
        return nc

    NB = EG // P
    with Scope(mem) as st:
        ustg = sb("ustg0", [P, 8, EG], F32, st)
        vstg = sb("vstg0", [P, NB, D], F32, st)
        ustg_b, vstg_b = Buf("ustg0"), Buf("vstg0")
        ubf = [sb("ubf%d" % i, [P, 8, EG], BF, st) for i in range(2)]
        vbf = [sb("vbf%d" % i, [P, NB, D], BF, st) for i in range(2)]
        ubf_b = [Buf("ubf0"), Buf("ubf1")]
        vbf_b = [Buf("vbf0"), Buf("vbf1")]
        WTg = [sb("WTg%d" % i, [P, 4, NB * P], BF, st) for i in range(3)]
        WTg_b = [Buf("WTg%d" % i) for i in range(3)]
        ge = [sb("ge%d" % i, [P, 512], BF, st) for i in range(2)]
        ge_b = [Buf("ge0"), Buf("ge1")]
        GT = [sb("GT%d" % i, [P, NB, 512], BF, st) for i in range(2)]
        GT_b = [Buf("GT0"), Buf("GT1")]

        def load_group(g):
            S.dma("sp", ustg[:], UT_d[:, :, g * EG:(g + 1) * EG], ustg_b, writes=[ustg_b])
            S.dma("sp", vstg[:], V_d[g * EG:(g + 1) * EG, :].rearrange("(b p) d -> p b d", p=P), vstg_b,
                  writes=[vstg_b])

        def cast_group(g):
            s_ = g % 2
            S.op("pool", lambda: nc.gpsimd.tensor_tensor(ubf[s_][:], ustg[:], vap(g_ffn[:], [[1, 8], [0, EG]]), ALU.mult),
                 [ustg_b, cb], [ubf_b[s_]])
            S.op("pool", lambda: nc.gpsimd.tensor_copy(vbf[s_][:], vstg[:]), [vstg_b], [vbf_b[s_]])

        seq = [(g, q) for g in range(NG) for q in range(4)]
        NTOT = len(seq)

        def load_w(n):
            g, q = seq[n]
            S.dma("sp", WTg[n % 3][:], Wd[4 * q:4 * q + 4, :, g * EG:(g + 1) * EG].rearrange("a p f -> p a f"),
                  WTg_b[n % 3], reads=wd_b[4 * q:4 * q + 4], writes=[WTg_b[n % 3]])

        abank = [0]

        def st_AG(n):
            g, q = seq[n]
            s_, gt = g % 2, n % 2
            for b_ in range(NB):
                pa = abank[0] % 2
                abank[0] += 1
                for c in range(8):
                    S.op("pe", lambda: nc.tensor.matmul(psf[pa][:], ubf[s_][:, c, b_ * P:(b_ + 1) * P],
                                                        h2T[:, c, q * 512:(q + 1) * 512], start=(c == 0), stop=(c == 7),
                                                        skip_group_check=True), [h2T_b, ubf_b[s_]], [psf_b[pa]])
                S.op("act", lambda: nc.scalar.activation(ge[pa][:], psf[pa][:], AF.Gelu), [psf_b[pa]], [ge_b[pa]])
                S.op("dve", lambda: nc.vector.tensor_tensor(
                    GT[gt][:, b_, :].rearrange("p (a t) -> p a t", a=4), ge[pa][:].rearrange("p (a t) -> p a t", a=4),
                    vap(WTg[n % 3][:], [[NB * P, 4], [1, P]], off=b_ * P), ALU.mult),
                    [ge_b[pa], WTg_b[n % 3]], [GT_b[gt]])

        ybank = [0]

        def st_Y(n):
            g, q = seq[n]
            s_, gt = g % 2, n % 2
            for u in range(4):
                i = 4 * q + u
                for half in range(2):
                    py = 2 + ybank[0] % 4
                    ybank[0] += 1
                    for b_ in range(NB):
                        S.op("pe", lambda: nc.tensor.matmul(psf[py][:], GT[gt][:, b_, u * P:(u + 1) * P],
                                                            vbf[s_][:, b_, half * 512:(half + 1) * 512], start=(b_ == 0),
                                                            stop=(b_ == NB - 1), skip_group_check=True),
                             [GT_b[gt], vbf_b[s_]], [psf_b[py]])
                    S.op("dve", lambda: nc.vector.tensor_tensor(y_acc[:, i, half * 512:(half + 1) * 512],
                                                                psf[py][:], y_acc[:, i, half * 512:(half + 1) * 512],
                                                                ALU.add), [psf_b[py], y_b[i]], [y_b[i]])

        load_group(0)
        cast_group(0)
        if NG > 1:
            load_group(1)
        load_w(0)
        load_w(1)
        for n in range(NTOT):
            g, q = seq[n]
            if n + 2 < NTOT:
                load_w(n + 2)
            if q == 2 and g + 1 < NG:
                cast_group(g + 1)
                if g + 2 < NG:
                    load_group(g + 2)
            st_AG(n)
            if n >= 1:
                st_Y(n - 1)
        st_Y(NTOT - 1)
        ob = Buf("outst")
        for i in range(NT):
            S.dma("sp", out_d[i * P:(i + 1) * P, :], y_acc[:, i, :], ob, reads=[y_b[i]])
        S.barrier()
    pD.close()
    es.close()
    return nc


_HOST_CACHE = {}


def _prep_shared(inp):
    f = np.float32
    sh = {}
    sh["invf"] = np.ascontiguousarray(np.broadcast_to(
        (1.0 / (10000.0 ** (np.arange(0, 64, 2, dtype=f) / f(64)))).astype(f)[None, :], (P, 32)))
    sh["ident"] = np.eye(P, dtype=f)
    s = np.arange(P)[:, None]
    t = np.arange(P)[None, :]
    same = (s // 64) == (t // 64)
    sh["maskf"] = (same & (s <= t)).astype(f)
    sh["maskb"] = (same & (s >= t)).astype(f)
    rm = np.ones((P, T), f)
    rm[:, ::64] = 0.0
    sh["resetm"] = rm
    io = np.zeros((P, 160), f)
    io[:, 0:128] = np.arange(128, dtype=f)[None, :]
    io[:, 128:144] = np.arange(16, dtype=f)[None, :]
    io[:, 144:160] = np.array([0, 16, 32, 48, 64, 68, 72, 76, 80, 84, 88, 92, 96, 100, 104, 108], f)[None, :]
    sh["iota"] = io

    def pc(v):
        return np.ascontiguousarray(np.asarray(v, f).reshape(-1, P).T)

    def rep(v):
        v = np.asarray(v, f).reshape(1, -1)
        return np.ascontiguousarray(np.broadcast_to(v, (P, v.shape[1])))

    def kc(w):
        w = np.asarray(w, f)
        return np.ascontiguousarray(w.reshape(-1, P, w.shape[1]).transpose(1, 0, 2))

    sh["g_attn"] = pc(inp["attn_norm"][0])
    sh["g_ffn"] = pc(inp["ffn_norm"][0])
    lbl = np.asarray(inp["hg_lb_logits"], f)
    sh["lbl"] = np.ascontiguousarray(lbl.reshape(2, 2, 4, P).transpose(3, 0, 1, 2).reshape(P, 16))
    sh["g_hgo"] = np.ascontiguousarray(np.asarray(inp["hg_o_norm"][0], f).T)
    sh["g_qa"] = pc(inp["q_a_norm"][0])
    sh["g_kva"] = pc(inp["kv_a_norm"][0])
    sh["g_q"] = rep(inp["q_norm"][0])
    sh["g_k"] = rep(inp["k_norm"][0])
    sh["g_mo"] = rep(inp["mla_o_norm"][0])
    sh["w_in"] = kc(inp["w_in"][0])
    sh["w_qup"] = kc(inp["w_q_up"][0])
    sh["w_kvup"] = kc(inp["w_kv_up"][0])
    sh["w_out"] = kc(inp["w_out"][0])
    wq = np.asarray(inp["peer_w_q"][0], f)
    sh["wqT"] = np.ascontiguousarray(wq.reshape(D, 16, P).transpose(2, 1, 0))
    keys = np.asarray(inp["peer_sub_keys"][0], f)
    sh["keysT"] = np.ascontiguousarray(keys.reshape(16, P, P).transpose(2, 0, 1))
    u = np.asarray(inp["peer_u"][0], f)
    sh["UT"] = np.ascontiguousarray(u.reshape(NEXP, 8, P).transpose(2, 1, 0))
    sh["V"] = np.ascontiguousarray(np.asarray(inp["peer_v"][0], f))
    return sh


def make_in_maps(inputs, cores):
    sh = _prep_shared(inputs)
    x = np.asarray(inputs["x"], np.float32)
    pos = np.asarray(inputs["positions"], np.int32)
    maps = []
    for b in cores:
        m = dict(sh)
        m["x"] = np.ascontiguousarray(x[b])
        m["posT"] = np.ascontiguousarray(pos[b].reshape(NT, P).T)
        maps.append(m)
    return maps


def kernel(**inputs):
    nc = build_program()
    in_maps = make_in_maps(inputs, list(range(8)))
    res = run_bass_kernel_spmd(nc, in_maps, core_ids=list(range(8)))
    out = np.stack([np.asarray(r["out"], np.float32) for r in res.results], axis=0)
    return out
```

```python
import numpy as np
from contextlib import ExitStack
import concourse.bass as bass
import concourse.mybir as mybir
from concourse.bass_utils import run_bass_kernel_spmd

F32 = mybir.dt.float32
BF = mybir.dt.bfloat16
I32 = mybir.dt.int32
AF = mybir.ActivationFunctionType
ALU = mybir.AluOpType
AX = mybir.AxisListType

P = 128
T = 2048
NT = 16
D = 1024
EPS = 1e-6
NEXP = 16384
EG = 512
NG = NEXP // EG
IC = 16
NIC = 128 // IC
PI = float(np.pi)


class Buf:
    def __init__(self, name):
        self.name = name
        self.writer = None
        self.readers = []
        self.dsem = None
        self.dcnt = 0


class Sch:
    def __init__(self, nc):
        self.nc = nc
        self.eng = dict(pe=nc.tensor, dve=nc.vector, act=nc.scalar, pool=nc.gpsimd, sp=nc.sync)
        self.sem = {e: nc.alloc_semaphore("sem_" + e) for e in ("pe", "dve", "act", "pool")}
        self.cnt = {e: 0 for e in self.sem}
        self.seen = {e: {} for e in self.eng}
        self.dbufs = []

    def _wait(self, e, dep):
        key, h, v = dep
        if self.seen[e].get(key, 0) >= v:
            return
        self.eng[e].wait_ge(h, v)
        self.seen[e][key] = v

    def _deps(self, e, reads, writes):
        deps = []
        for b in reads:
            if b.writer is not None:
                deps.append(b.writer)
        for b in writes:
            if b.writer is not None:
                deps.append(b.writer)
            deps.extend(b.readers)
        for d in deps:
            if e == "pe" and d[0] == "pe":
                continue
            self._wait(e, d)

    def _mark(self, tok, reads, writes):
        for b in reads:
            b.readers.append(tok)
        for b in writes:
            b.writer = tok
            b.readers = []

    def op(self, e, fn, reads=(), writes=()):
        self._deps(e, reads, writes)
        ins = fn()
        self.cnt[e] += 1
        ins.then_inc(self.sem[e], 1)
        self.seen[e][e] = max(self.seen[e].get(e, 0), 0)
        self._mark((e, self.sem[e], self.cnt[e]), reads, writes)

    def dma(self, q, out, in_, sb, reads=(), writes=()):
        self._deps(q, reads, writes)
        if sb.dsem is None:
            sb.dsem = self.nc.alloc_semaphore("dsem_" + sb.name)
            self.dbufs.append(sb)
        ins = self.eng[q].dma_start(out=out, in_=in_)
        sb.dcnt += 16
        ins.then_inc(sb.dsem, 16)
        self._mark((("d", sb.name), sb.dsem, sb.dcnt), reads, writes)

    def barrier(self):
        for e in self.eng:
            for f in self.sem:
                if f != e and self.cnt[f] > 0:
                    self._wait(e, (f, self.sem[f], self.cnt[f]))
            for b in self.dbufs:
                if b.dcnt > 0:
                    self._wait(e, (("d", b.name), b.dsem, b.dcnt))


class Mem:
    def __init__(self, lo, hi):
        self.free = [(lo, hi)]

    def alloc(self, n):
        n = (n + 63) // 64 * 64
        for k, (a, b) in enumerate(self.free):
            if b - a >= n:
                self.free[k] = (a + n, b)
                return a, n
        raise MemoryError("SBUF arena exhausted (%d bytes) free=%s" % (n, self.free))

    def release(self, a, n):
        fl = sorted(self.free + [(a, a + n)])
        out = []
        for lo, hi in fl:
            if out and out[-1][1] >= lo:
                out[-1] = (out[-1][0], max(out[-1][1], hi))
            elif hi > lo:
                out.append((lo, hi))
        self.free = out


class Scope:
    def __init__(self, mem):
        self.mem = mem
        self.items = []

    def __enter__(self):
        return self

    def __exit__(self, *a):
        self.close()
        return False

    def close(self):
        for a, n in self.items:
            self.mem.release(a, n)
        self.items = []


DT_BYTES = {}


def vap(base, dims, off=0):
    return bass.AP(base.tensor, base.offset + off, [list(base.ap[0])] + [list(d) for d in dims])


def build_program(debug=None, upto=None):
    debug = debug or {}
    nc = bass.Bass("TRN2", target_bir_lowering=False)
    S = Sch(nc)

    def din(name, shape, dt=F32):
        return nc.dram_tensor(name, list(shape), dt, kind="ExternalInput").ap()

    x_d = din("x", [T, D])
    pos_d = din("posT", [P, NT], I32)
    invf_d = din("invf", [P, 32])
    ident_d = din("ident", [P, P])
    maskf_d = din("maskf", [P, P])
    maskb_d = din("maskb", [P, P])
    reset_d = din("resetm", [P, T])
    gattn_d = din("g_attn", [P, 8])
    gffn_d = din("g_ffn", [P, 8])
    lbl_d = din("lbl", [P, 16])
    ghgo_d = din("g_hgo", [P, 4])
    gqa_d = din("g_qa", [P, 3])
    gkva_d = din("g_kva", [P, 2])
    gq_d = din("g_q", [P, 192])
    gk_d = din("g_k", [P, 192])
    gmo_d = din("g_mo", [P, 512])
    iota_d = din("iota", [P, 160])
    win_d = din("w_in", [P, 8, 3264])
    wqup_d = din("w_qup", [P, 3, 768])
    wkvup_d = din("w_kvup", [P, 2, 1024])
    wout_d = din("w_out", [P, 8, 1024])
    wqT_d = din("wqT", [P, 16, 1024])
    keysT_d = din("keysT", [P, 16, 128])
    UT_d = din("UT", [P, 8, NEXP])
    V_d = din("V", [NEXP, D])
    out_d = nc.dram_tensor("out", [T, D], F32, kind="ExternalOutput").ap()
    Wd = nc.dram_tensor("Wd", [NT, P, NEXP], BF).ap()
    dbg_out = {}
    for k, (shp, dt_) in debug.items():
        dbg_out[k] = nc.dram_tensor("dbg_" + k, list(shp), dt_, kind="ExternalOutput").ap()

    es = ExitStack()
    mem = Mem(16512 + 64, 229344 - 64)
    root = Scope(mem)

    def sb(name, shape, dt=F32, stack=None):
        nbytes = int(np.prod(shape[1:])) * (4 if dt in (F32, I32, mybir.dt.uint32) else 2)
        a, n = mem.alloc(nbytes)
        (stack or root).items.append((a, n))
        addr_of[name] = a
        return nc.alloc_sbuf_tensor_at(name, list(shape), dt, offset=a)

    addr_of = {}

    def sb_alias(name, shape, dt, like):
        return nc.alloc_sbuf_tensor_at(name, list(shape), dt, offset=addr_of[like])

    psf_all = es.enter_context(nc.psum_tensor("psf_all", [P, 6, 512], F32))
    psf = [psf_all[:, i, :] for i in range(6)]
    psb = [es.enter_context(nc.psum_tensor("psb%d" % i, [P, 1024], BF)) for i in range(2)]
    psf_b = [Buf("psf%d" % i) for i in range(6)]
    psb_b = [Buf("psb%d" % i) for i in range(2)]

    cb = Buf("consts")
    pEarly = Scope(mem)
    ident_f = sb("ident_f", [P, P], F32, pEarly)
    ident = sb("ident", [P, P], BF)
    maskf = sb("maskf", [P, P], F32, pEarly)
    maskb = sb("maskb", [P, P], F32, pEarly)
    resetm = sb("resetm", [P, T], F32, pEarly)
    invf = sb("invf", [P, 32])
    posT = sb("posT", [P, NT], I32)
    g_attn = sb("g_attn", [P, 8])
    g_ffn = sb("g_ffn", [P, 8])
    lbl = sb("lbl", [P, 16])
    g_hgo = sb("g_hgo", [P, 4])
    g_qa = sb("g_qa", [P, 3])
    g_kva = sb("g_kva", [P, 2])
    g_q = sb("g_q", [P, 192], F32, pEarly)
    g_k = sb("g_k", [P, 192], F32, pEarly)
    g_mo = sb("g_mo", [P, 512], F32, pEarly)
    ones_bf = sb("ones_bf", [P, P], BF)
    iota_f = sb("iota_f", [P, 160])
    iota128 = sb("iota128", [P, P], BF)
    for dst, src in ((ident_f, ident_d), (maskf, maskf_d), (maskb, maskb_d), (resetm, reset_d),
                     (invf, invf_d), (posT, pos_d), (g_attn, gattn_d), (g_ffn, gffn_d), (lbl, lbl_d),
                     (g_hgo, ghgo_d), (g_qa, gqa_d), (g_kva, gkva_d), (g_q, gq_d), (g_k, gk_d),
                     (g_mo, gmo_d), (iota_f, iota_d)):
        S.dma("sp", dst[:], src, cb, writes=[cb])
    cc = Buf("consts2")
    S.op("dve", lambda: nc.vector.tensor_copy(ident[:], ident_f[:]), [cb], [cc])
    S.op("dve", lambda: nc.vector.memset(ones_bf[:], 1.0), [], [cc])
    S.op("dve", lambda: nc.vector.tensor_copy(iota128[:], iota_f[:, 0:128]), [cb], [cc])
    lb = sb("lb", [P, 8])
    oml = sb("oml", [P, 8])
    noml = sb("noml", [P, 8])
    S.op("dve", lambda: nc.vector.tensor_sub(lb[:], lbl[:, 0:8], lbl[:, 8:16]), [cb], [cc])
    S.op("act", lambda: nc.scalar.activation(lb[:], lb[:], AF.Sigmoid), [cc], [cc])
    S.op("dve", lambda: nc.vector.tensor_scalar(oml[:], lb[:], -1.0, 1.0, ALU.mult, ALU.add), [cc], [cc])
    S.op("dve", lambda: nc.vector.tensor_scalar(noml[:], oml[:], -1.0, None, ALU.mult), [cc], [cc])
    cosT = sb("cosT", [P, NT, 32], F32, pEarly)
    sinT = sb("sinT", [P, NT, 32], F32, pEarly)
    with Scope(mem) as st:
        posf = sb("posf", [P, NT], F32, st)
        ang = sb("ang", [P, NT, 32], F32, st)
        ang2 = sb("ang2", [P, NT, 32], F32, st)
        S.op("dve", lambda: nc.vector.tensor_copy(posf[:], posT[:]), [cb], [cc])
        S.op("dve", lambda: nc.vector.tensor_tensor(
            ang[:], vap(posf[:], [[1, NT], [0, 32]]), vap(invf[:], [[0, NT], [1, 32]]), ALU.mult), [cc, cb], [cc])
        ri = sb("ri", [P, NT, 32], I32, st)
        rf = sb("rf", [P, NT, 32], F32, st)
        hi = sb("hi", [P, NT, 32], F32, st)
        S.op("dve", lambda: nc.vector.tensor_scalar(ang[:], ang[:], 1.0 / (2 * PI), None, ALU.mult), [cc], [cc])
        S.op("dve", lambda: nc.vector.tensor_scalar(ang2[:], ang[:], 0.25, None, ALU.add), [cc], [cc])
        for src, dst in ((ang, sinT), (ang2, cosT)):
            S.op("dve", lambda: nc.vector.tensor_copy(ri[:], src[:]), [cc], [cc])
            S.op("dve", lambda: nc.vector.tensor_copy(rf[:], ri[:]), [cc], [cc])
            S.op("dve", lambda: nc.vector.tensor_sub(src[:], src[:], rf[:]), [cc], [cc])
            S.op("dve", lambda: nc.vector.tensor_scalar(hi[:], src[:], 0.5, None, ALU.is_gt), [cc], [cc])
            S.op("dve", lambda: nc.vector.tensor_sub(src[:], src[:], hi[:]), [cc], [cc])
            S.op("dve", lambda: nc.vector.tensor_scalar(hi[:], src[:], -0.5, None, ALU.is_lt), [cc], [cc])
            S.op("dve", lambda: nc.vector.tensor_add(src[:], src[:], hi[:]), [cc], [cc])
            S.op("act", lambda: nc.scalar.activation(dst[:], src[:], AF.Sin, scale=2 * PI), [cc], [cc])
        S.barrier()
    epsb = sb("epsb", [P, 1])
    S.op("dve", lambda: nc.vector.memset(epsb[:], EPS), [], [cc])
    if upto == "0":
        S.barrier()
        return nc

    def dump(name, src_ap, rbufs):
        if name in dbg_out:
            S.barrier()
            tb = Buf("dbg_" + name)
            S.dma("sp", dbg_out[name], src_ap, tb, reads=rbufs)
            S.barrier()

    def rstd_from_ss(dst, ss, n, bufs):
        S.op("act", lambda: nc.scalar.activation(dst, ss, AF.Sqrt, bias=epsb[:dst.shape[0]], scale=1.0 / n), bufs + [cc], bufs)
        S.op("dve", lambda: nc.vector.reciprocal(dst, dst), bufs, bufs)

    pM = Scope(mem)
    mixT = sb("mixT", [P, 8, T], BF, pM)
    mix_b = Buf("mixT")
    ph1 = Scope(mem)
    xnT = sb("xnT", [P, 8, T], BF, ph1)
    xnT_b = Buf("xnT")
    stg = [sb("stg0", [P, 8, 512], F32, ph1)] * 2
    stg_b = [Buf("stg0")] * 2
    with Scope(mem) as st:
        xt = [sb("xt%d" % i, [P, D], F32, st) for i in range(2)]
        xt_b = [Buf("xt%d" % i) for i in range(2)]
        xn = [sb("xn%d" % i, [P, D], BF, st) for i in range(2)]
        xn_b = [Buf("xn%d" % i) for i in range(2)]
        junk = sb("junkA", [P, D], BF, st)
        junk_b = Buf("junkA")
        ssA = sb("ssA", [P, NT], F32, st)
        ssA_b = [Buf("ssA%d" % i) for i in range(NT)]
        def a_front(i):
            j = i % 2
            S.dma("sp", xt[j][:], x_d[i * P:(i + 1) * P, :], xt_b[j], writes=[xt_b[j]])
            S.op("act", lambda: nc.scalar.activation(junk[:], xt[j][:], AF.Square, accum_out=ssA[:, i:i + 1]),
                 [xt_b[j]], [junk_b, ssA_b[i]])
            rstd_from_ss(ssA[:, i:i + 1], ssA[:, i:i + 1], D, [ssA_b[i]])
            S.op("dve", lambda: nc.vector.tensor_scalar(xn[j][:], xt[j][:], ssA[:, i:i + 1], None, ALU.mult),
                 [xt_b[j], ssA_b[i]], [xn_b[j]])

        def a_tail(i):
            j = i % 2
            pb = psb_b[i % 2]
            for c in range(8):
                S.op("pe", lambda: nc.tensor.transpose(psb[i % 2][:, c * P:(c + 1) * P], xn[j][:, c * P:(c + 1) * P], ident[:]),
                     [xn_b[j], cc], [pb])
            S.op("act", lambda: nc.scalar.copy(
                xnT[:, :, i * P:(i + 1) * P], vap(psb[i % 2][:], [[P, 8], [1, P]])), [pb], [xnT_b])

        a_front(0)
        for i in range(NT):
            if i + 1 < NT:
                a_front(i + 1)
            a_tail(i)
        S.barrier()

    if upto == "A":
        return nc
    def load_wslice(dst, dst_b, src_d, nchunks, cols, gain, slot):
        sg, sgb = stg[slot], stg_b[slot]
        o = 0
        for (c0, w) in cols:
            S.dma("sp", sg[:, 0:nchunks, o:o + w], src_d[:, :, c0:c0 + w], sgb, writes=[sgb])
            o += w
        for c in range(nchunks):
            if gain is not None:
                S.op("dve", lambda: nc.vector.tensor_scalar(dst[:, c, 0:o], sg[:, c, 0:o], gain[:, c:c + 1], None, ALU.mult),
                     [sgb, cb], [dst_b])
            else:
                S.op("dve", lambda: nc.vector.tensor_copy(dst[:, c, 0:o], sg[:, c, 0:o]), [sgb], [dst_b])

    def proj_fm(ps_ap, ps_b, w, w_b, col0, width, t0, n, src=None, src_b=None, nch=8):
        src = xnT if src is None else src
        src_b = xnT_b if src_b is None else src_b
        for c in range(nch):
            S.op("pe", lambda: nc.tensor.matmul(ps_ap, w[:, c, col0:col0 + width], src[:, c, t0:t0 + n],
                                                start=(c == 0), stop=(c == nch - 1), skip_group_check=True),
                 [w_b, src_b], [ps_b])

    with Scope(mem) as st:
        vtok = sb("vtok", [P, NT, 512], BF, st)
        vtok_b = Buf("vtok")
        wv = sb("wv", [P, 8, 512], BF, st)
        wv_b = Buf("wv")
        load_wslice(wv, wv_b, win_d, 8, [(1536, 512)], g_attn, 0)
        for i in range(NT):
            k = i % 2
            for c in range(8):
                S.op("pe", lambda: nc.tensor.matmul(psf[k][:], xnT[:, c, i * P:(i + 1) * P], wv[:, c, :],
                                                    start=(c == 0), stop=(c == 7), skip_group_check=True),
                     [xnT_b, wv_b], [psf_b[k]])
            S.op("act", lambda: nc.scalar.copy(vtok[:, i, :], psf[k][:]), [psf_b[k]], [vtok_b])
        wh = [wv] * 2
        wh_b = [wv_b] * 2
        if upto == "B1":
            S.barrier()
            return nc
        H = 1024
        qs = sb("qs", [P, T], F32, st)
        gsl = sb("gsl", [P, T], BF, st)
        glog = sb("glog", [P, H], F32, st)
        bcum = sb("bcum", [P, H], F32, st)
        kk = sb("kk", [P, H], F32, st)
        e1 = sb("e1", [P, H], F32, st)
        e2 = glog
        Qt = sb("Qt", [P, 2, T], BF, st)
        Kt = sb("Kt", [P, 2, T], BF, st)
        Ktok = sb("Ktok", [P, 2, NT, P], BF, st)
        Sbf = sb("Sbf", [P, 2, 32, P], BF, st)
        vm = sb("vm", [P, NT, 2, P], BF, st)
        vm_b = Buf("vm")
        Sm = [sb("Sm%d" % i, [P, 2, P], F32, st) for i in range(2)]
        dSd = sb("dSd", [P, 2, P], F32, st)
        dch = sb("dch", [P, 2, 32], F32, st)
        Pm = [sb("Pm%d" % i, [P, 2, P], BF, st) for i in range(2)]
        osq = sb("osq", [P, 512], BF, st)
        rbc = kk
        otmp = e1
        qs_b, gsl_b, e1_b, gl_b, kk_b, bc_b, dch_b = (Buf("hq"), Buf("hgs"), Buf("he1"), Buf("hgl"), Buf("hkk"), Buf("hbc"), Buf("hdch"))
        qk_b = Buf("QtKt")
        ktok_b = Buf("Ktok")
        sbf_b = Buf("Sbf")
        sm_b = Buf("Sm")
        pm_b = [Buf("Pm0"), Buf("Pm1")]
        ob = Buf("onorm")
        for h in range(4):
            w = wh[h % 2]
            wb = wh_b[h % 2]
            load_wslice(w, wb, win_d, 8, [(h * P, P), (512 + h * P, P), (1024 + h * P, P), (2048 + h * P, P)],
                        g_attn, (h + 1) % 2)
            for tb in range(4):
                k = tb % 2
                proj_fm(psf[k][:], psf_b[k], w, wb, 0, P, tb * 512, 512)
                S.op("act", lambda: nc.scalar.activation(qs[:, tb * 512:(tb + 1) * 512], psf[k][:], AF.Silu),
                     [psf_b[k]], [qs_b])
            for tb in range(4):
                k = tb % 2
                proj_fm(psf[k][:], psf_b[k], w, wb, 384, P, tb * 512, 512)
                S.op("act", lambda: nc.scalar.activation(gsl[:, tb * 512:(tb + 1) * 512], psf[k][:], AF.Silu),
                     [psf_b[k]], [gsl_b])
            for d in range(2):
                col = d * 4 + h
                for hf in range(2):
                    t0 = hf * H
                    for tb in range(2):
                        k = tb % 2
                        proj_fm(psf[k][:], psf_b[k], w, wb, (1 + d) * P, P, t0 + tb * 512, 512)
                        S.op("act", lambda: nc.scalar.activation(e1[:, tb * 512:(tb + 1) * 512], psf[k][:], AF.Sigmoid),
                             [psf_b[k]], [e1_b])
                    S.op("act", lambda: nc.scalar.activation(glog[:], e1[:], AF.Ln, bias=lb[:, col:col + 1],
                                                             scale=oml[:, col:col + 1]), [e1_b, cc], [gl_b])
                    S.op("dve", lambda: nc.vector.tensor_scalar(kk[:], e1[:], noml[:, col:col + 1], oml[:, col:col + 1],
                                                                ALU.mult, ALU.add), [e1_b, cc], [kk_b])
                    S.op("dve", lambda: nc.vector.tensor_tensor_scan(bcum[:], resetm[:, t0:t0 + H], glog[:], 0.0,
                                                                      ALU.mult, ALU.add), [gl_b, cb], [bc_b])
                    S.op("act", lambda: nc.scalar.activation(dch[:, d, hf * 16:(hf + 1) * 16],
                                                             vap(bcum[:], [[64, 16]], off=63), AF.Exp), [bc_b], [dch_b])
                    if d == 1:
                        S.op("dve", lambda: nc.vector.tensor_sub(glog[:], glog[:], bcum[:]), [gl_b, bc_b], [gl_b])
                        S.op("dve", lambda: nc.vector.tensor_tensor(
                            vap(glog[:], [[64, H // 64], [1, 64]]), vap(glog[:], [[64, H // 64], [1, 64]]),
                            vap(bcum[:], [[64, H // 64], [0, 64]], off=63), ALU.add), [gl_b, bc_b], [gl_b])
                        cur = glog
                        cur_ap = glog[:]
                    else:
                        cur_ap = bcum[:]
                    S.op("act", lambda: nc.scalar.activation(e1[:], cur_ap, AF.Exp), [gl_b, bc_b], [e1_b])
                    S.op("act", lambda: nc.scalar.activation(e2[:], cur_ap, AF.Exp, scale=-1.0), [gl_b, bc_b], [gl_b])
                    S.op("dve", lambda: nc.vector.tensor_tensor(Qt[:, d, t0:t0 + H], qs[:, t0:t0 + H], e1[:], ALU.mult),
                         [qs_b, e1_b], [qk_b])
                    S.op("dve", lambda: nc.vector.tensor_tensor(Kt[:, d, t0:t0 + H], kk[:], e2[:], ALU.mult),
                         [kk_b, gl_b], [qk_b])
            if upto == "B2":
                S.barrier()
                return nc
            for d in range(2):
                for g8 in range(2):
                    pbk = (d * 2 + g8) % 2
                    for u in range(8):
                        i = g8 * 8 + u
                        S.op("pe", lambda: nc.tensor.transpose(psb[pbk][:, u * P:(u + 1) * P], Kt[:, d, i * P:(i + 1) * P], ident[:]),
                             [qk_b, cc], [psb_b[pbk]])
                    S.op("act", lambda: nc.scalar.copy(Ktok[:, d, g8 * 8:(g8 + 1) * 8, :], vap(psb[pbk][:], [[P, 8], [1, P]])),
                         [psb_b[pbk]], [ktok_b])
            if upto == "B3":
                S.barrier()
                return nc
            for jj in range(2):
                S.op("dve", lambda: nc.vector.tensor_scalar(vm[:, :, jj, :], vtok[:, :, h * P:(h + 1) * P],
                                                            maskb[:, jj * 64:jj * 64 + 1], None, ALU.mult),
                     [vtok_b, cb], [vm_b])
            dsd_b = [Buf("dsd0"), Buf("dsd1")]
            smd_b = [[Buf("smd%d_%d" % (d_, q_)) for q_ in range(2)] for d_ in range(2)]
            S.op("pool", lambda: nc.gpsimd.memset(Sm[0][:], 0.0), [], [smd_b[0][0], smd_b[1][0]])
            S.op("pool", lambda: nc.gpsimd.memset(Sbf[:, 0, 0, :], 0.0), [], [sbf_b])
            S.op("pool", lambda: nc.gpsimd.memset(Sbf[:, 1, 31, :], 0.0), [], [sbf_b])
            for step in range(31):
                pp = step % 2
                cur, nxt = Sm[pp], Sm[1 - pp]
                cf, cbk = step, 31 - step
                for d, c in ((0, cf), (1, cbk)):
                    kd = 2 + 2 * d + (step % 2)
                    i, jj = c // 2, c % 2
                    S.op("pe", lambda: nc.tensor.matmul(psf[kd][:, 0:P], Ktok[:, d, i, :],
                                                        vm[:, i, jj, :], start=True, stop=True,
                                                        skip_group_check=True), [ktok_b, vm_b], [psf_b[kd]])
                for d, c in ((0, cf), (1, cbk)):
                    kd = 2 + 2 * d + (step % 2)
                    S.op("act", lambda: nc.scalar.mul(dSd[:, d, :], psf[kd][:, 0:P],
                                                      dch[:, d, c:c + 1]), [psf_b[kd], dch_b], [dsd_b[d]])
                for d, c in ((0, cf), (1, cbk)):
                    S.op("dve", lambda: nc.vector.scalar_tensor_tensor(nxt[:, d, :], cur[:, d, :], dch[:, d, c:c + 1],
                                                                        dSd[:, d, :], ALU.mult, ALU.add),
                         [smd_b[d][pp], dsd_b[d], dch_b], [smd_b[d][1 - pp]])
                for d, c in ((0, cf), (1, cbk)):
                    cn = c + 1 if d == 0 else c - 1
                    S.op("pool", lambda: nc.gpsimd.tensor_copy(Sbf[:, d, cn, :], nxt[:, d, :]), [smd_b[d][1 - pp]], [sbf_b])
            if upto == "B4":
                S.barrier()
                return nc
            def emit_scores(i):
                ks = i % 2
                for d in range(2):
                    S.op("pe", lambda: nc.tensor.matmul(psf[ks][:, d * P:(d + 1) * P], Kt[:, d, i * P:(i + 1) * P],
                                                        Qt[:, d, i * P:(i + 1) * P], start=True, stop=True,
                                                        skip_group_check=True), [qk_b], [psf_b[ks]])
                S.op("dve", lambda: nc.vector.tensor_tensor(Pm[ks][:, 0, :], psf[ks][:, 0:P], maskf[:], ALU.mult),
                     [psf_b[ks], cb], [pm_b[ks]])
                S.op("dve", lambda: nc.vector.tensor_tensor(Pm[ks][:, 1, :], psf[ks][:, P:2 * P], maskb[:], ALU.mult),
                     [psf_b[ks], cb], [pm_b[ks]])

            emit_scores(0)
            for i in range(NT):
                g4, u = divmod(i, 4)
                po = 4 + (g4 % 2)
                ks = i % 2
                if i + 1 < NT:
                    emit_scores(i + 1)
                oap = psf[po][:, u * P:(u + 1) * P]
                S.op("pe", lambda: nc.tensor.matmul(oap, vtok[:, i, h * P:(h + 1) * P], Pm[ks][:, 0, :], start=True,
                                                    stop=False, skip_group_check=True), [vtok_b, pm_b[ks]], [psf_b[po]])
                S.op("pe", lambda: nc.tensor.matmul(oap, vtok[:, i, h * P:(h + 1) * P], Pm[ks][:, 1, :], start=False,
                                                    stop=False, skip_group_check=True), [vtok_b, pm_b[ks]], [psf_b[po]])
                for d in range(2):
                    for jj in range(2):
                        c = 2 * i + jj
                        S.op("pe", lambda: nc.tensor.matmul(
                            psf[po][:, u * P + jj * 64:u * P + jj * 64 + 64], Sbf[:, d, c, :],
                            Qt[:, d, c * 64:(c + 1) * 64], start=False, stop=(d == 1 and jj == 1),
                            skip_group_check=True), [sbf_b, qk_b], [psf_b[po]])
                if u != 3:
                    continue
                S.op("act", lambda: nc.scalar.activation(osq[:], psf[po][:], AF.Square), [psf_b[po]], [ob])
                S.op("pe", lambda: nc.tensor.matmul(psf[3][:], ones_bf[:], osq[:], start=True, stop=True,
                                                    skip_group_check=True), [ob, cc], [psf_b[3]])
                S.op("act", lambda: nc.scalar.activation(rbc[:, 0:512], psf[3][:], AF.Sqrt, bias=epsb[:], scale=1.0 / 128),
                     [psf_b[3], cc], [ob, kk_b])
                S.op("dve", lambda: nc.vector.reciprocal(rbc[:, 0:512], rbc[:, 0:512]), [ob, kk_b], [ob, kk_b])
                S.op("dve", lambda: nc.vector.scalar_tensor_tensor(otmp[:, 0:512], psf[po][:], g_hgo[:, h:h + 1], rbc[:, 0:512],
                                                                    ALU.mult, ALU.mult), [psf_b[po], ob, cb, kk_b], [ob, e1_b])
                S.op("dve", lambda: nc.vector.tensor_tensor(mixT[:, h, g4 * 512:(g4 + 1) * 512], otmp[:, 0:512],
                                                            gsl[:, g4 * 512:(g4 + 1) * 512], ALU.mult), [ob, e1_b, gsl_b], [mix_b])
        S.barrier()
    dump("mix_hg", mixT[:, 0:4, :], [mix_b])
    if upto == "B":
        return nc

    SCALE = 192.0 ** -0.5
    pc = Scope(mem)
    cT = sb("cT", [P, 5, T], BF, pc)
    cT_b = Buf("cT")
    krraw = sb("krraw", [P, NT, 64], F32, pc)
    kr_b = Buf("krraw")
    rs2 = sb("rs2", [P, NT, 2], F32, pc)
    rs2_b = Buf("rs2")
    wqu = sb("wqu", [P, 3, 768], BF, pc)
    wkvu = sb("wkvu", [P, 2, 1024], BF, pc)
    wu_b = Buf("wup")
    with Scope(mem) as st:
        wm = sb("wm", [P, 8, 704], BF, st)
        wm_b = Buf("wm")
        load_wslice(wm[:, :, 0:512], wm_b, win_d, 8, [(2560, 512)], g_attn, 0)
        load_wslice(wm[:, :, 512:704], wm_b, win_d, 8, [(3072, 192)], g_attn, 1)
        csq = sb("csq", [P, 5, 512], BF, st)
        csq_b = Buf("csq")
        for tb in range(4):
            for j in range(5):
                k = j % 2
                proj_fm(psf[k][:], psf_b[k], wm, wm_b, j * P, P, tb * 512, 512)
                S.op("act", lambda: nc.scalar.copy(cT[:, j, tb * 512:(tb + 1) * 512], psf[k][:]), [psf_b[k]], [cT_b])
                S.op("act", lambda: nc.scalar.activation(csq[:, j, :], psf[k][:], AF.Square), [psf_b[k]], [csq_b])
            for u in range(4):
                i = tb * 4 + u
                for j in range(3):
                    S.op("pe", lambda: nc.tensor.matmul(psf[2][:, i * 2:i * 2 + 1], csq[:, j, u * P:(u + 1) * P],
                                                        ones_bf[:, 0:1], start=(j == 0), stop=(j == 2),
                                                        skip_group_check=True), [csq_b, cc], [psf_b[2]])
                for j in range(2):
                    S.op("pe", lambda: nc.tensor.matmul(psf[2][:, i * 2 + 1:i * 2 + 2], csq[:, 3 + j, u * P:(u + 1) * P],
                                                        ones_bf[:, 0:1], start=(j == 0), stop=(j == 1),
                                                        skip_group_check=True), [csq_b, cc], [psf_b[2]])
        S.op("act", lambda: nc.scalar.copy(rs2[:].rearrange("p a b -> p (a b)"), psf[2][:, 0:2 * NT]), [psf_b[2]], [rs2_b])
        rstd_from_ss(rs2[:, :, 0], rs2[:, :, 0], 384, [rs2_b])
        rstd_from_ss(rs2[:, :, 1], rs2[:, :, 1], 256, [rs2_b])
        for i in range(NT):
            k = 3 + i % 2
            for c in range(8):
                S.op("pe", lambda: nc.tensor.matmul(psf[k][:, 0:64], xnT[:, c, i * P:(i + 1) * P], wm[:, c, 640:704],
                                                    start=(c == 0), stop=(c == 7), skip_group_check=True),
                     [xnT_b, wm_b], [psf_b[k]])
            S.op("act", lambda: nc.scalar.copy(krraw[:, i, :], psf[k][:, 0:64]), [psf_b[k]], [kr_b])
        S.barrier()
    if upto == "C1":
        return nc
    sg0 = stg[0]
    S.dma("sp", sg0[:, 0:3, 0:512], wqup_d[:, :, 0:512], stg_b[0], writes=[stg_b[0]])
    for c in range(3):
        S.op("dve", lambda: nc.vector.tensor_scalar(wqu[:, c, 0:512], sg0[:, c, 0:512], g_qa[:, c:c + 1], None, ALU.mult),
             [stg_b[0], cb], [wu_b])
    S.dma("sp", sg0[:, 0:3, 0:256], wqup_d[:, :, 512:768], stg_b[0], writes=[stg_b[0]])
    for c in range(3):
        S.op("dve", lambda: nc.vector.tensor_scalar(wqu[:, c, 512:768], sg0[:, c, 0:256], g_qa[:, c:c + 1], None, ALU.mult),
             [stg_b[0], cb], [wu_b])
    for half in range(2):
        S.dma("sp", sg0[:, 0:2, 0:512], wkvup_d[:, :, half * 512:(half + 1) * 512], stg_b[0], writes=[stg_b[0]])
        for c in range(2):
            S.op("dve", lambda: nc.vector.tensor_scalar(wkvu[:, c, half * 512:(half + 1) * 512], sg0[:, c, 0:512],
                                                        g_kva[:, c:c + 1], None, ALU.mult), [stg_b[0], cb], [wu_b])
    S.barrier()
    ph1.close()
    qnT = sb("qnT", [P, 4, T], BF, pc)
    knT = sb("knT", [P, 4, T], BF, pc)
    qrT = sb("qrT", [P, 4, T], BF, pc)
    krT = sb("krT", [P, T], BF, pc)
    vaug = sb("vaug", [P, NT, 4, 132], BF, pc)
    qk2_b = Buf("qkT")
    vaug_b = Buf("vaug")
    S.op("pool", lambda: nc.gpsimd.memset(vaug[:], 1.0), [], [vaug_b])
    S.op("pool", lambda: nc.gpsimd.memset(qrT[:], 0.0), [], [qk2_b])
    S.op("pool", lambda: nc.gpsimd.memset(krT[:], 0.0), [], [qk2_b])
    with Scope(mem) as st:
        Qs2 = [sb("Qs%d" % i, [P, 768], F32, st) for i in range(2)]
        KVs2 = [sb("KVs%d" % i, [P, 1024], F32, st) for i in range(2)]
        in_b = [Buf("mla_in0"), Buf("mla_in1")]
        sq = sb("sqm", [P, 1024], F32, st)
        ssn = sb("ssn", [P, 16], F32, st)
        invn = sb("invn", [P, 16], F32, st)
        qn_s = sb("qn_s", [P, 4, P], BF, st)
        kn_s = sb("kn_s", [P, 4, P], BF, st)
        qr_f = sb("qr_f", [P, 4, 64], F32, st)
        kr_f = sb("kr_f", [P, 64], F32, st)
        qr_s = sb("qr_s", [P, 4, 64], BF, st)
        kr_s = sb("kr_s", [P, 64], BF, st)
        ra = sb("ra", [P, 4, 32], F32, st)
        rb_ = sb("rb_", [P, 4, 32], F32, st)
        dv = Buf("mla_dve")
        out_b = Buf("mla_out")
        S.op("dve", lambda: nc.vector.memset(invn[:, 0:4], 1.0 / 128), [], [dv])
        S.op("dve", lambda: nc.vector.memset(invn[:, 4:8], 1.0 / 64), [], [dv])
        S.op("dve", lambda: nc.vector.memset(invn[:, 8:12], 1.0 / 128), [], [dv])
        S.op("dve", lambda: nc.vector.memset(invn[:, 12:16], 1.0 / 64), [], [dv])

        def mla_front(i):
            Qs, KVs, ib = Qs2[i % 2], KVs2[i % 2], in_b[i % 2]
            for half in range(2):
                k = half
                for j in range(3):
                    S.op("pe", lambda: nc.tensor.matmul(psf[k][:, 0:384], cT[:, j, i * P:(i + 1) * P],
                                                        wqu[:, j, half * 384:(half + 1) * 384], start=(j == 0), stop=(j == 2),
                                                        skip_group_check=True), [cT_b, wu_b], [psf_b[k]])
                S.op("act", lambda: nc.scalar.mul(Qs[:, half * 384:(half + 1) * 384], psf[k][:, 0:384],
                                                  rs2[:, i, 0:1]), [psf_b[k], rs2_b], [ib])
            for half in range(2):
                k = 2 + half
                for j in range(2):
                    S.op("pe", lambda: nc.tensor.matmul(psf[k][:], cT[:, 3 + j, i * P:(i + 1) * P],
                                                        wkvu[:, j, half * 512:(half + 1) * 512], start=(j == 0), stop=(j == 1),
                                                        skip_group_check=True), [cT_b, wu_b], [psf_b[k]])
                S.op("act", lambda: nc.scalar.mul(KVs[:, half * 512:(half + 1) * 512], psf[k][:],
                                                  rs2[:, i, 1:2]), [psf_b[k], rs2_b], [ib])

        sqk_t = sb("sqk_t", [P, 512], F32, st)
        sqr_t = sb("sqr_t", [P, 64], F32, st)
        rak = sb("rak", [P, 32], F32, st)
        rbk = sb("rbk", [P, 32], F32, st)
        Bq, Bk, Br = Buf("m_sqq"), Buf("m_sqk"), Buf("m_sqr")
        Bs = [Buf("m_ss%d" % q_) for q_ in range(4)]
        Bqr, Bkr = Buf("m_qrf"), Buf("m_krf")
        Bra, Brb, Brak, Brbk = Buf("m_ra"), Buf("m_rb"), Buf("m_rak"), Buf("m_rbk")
        o_qn, o_kn, o_qr, o_kr = Buf("o_qn"), Buf("o_kn"), Buf("o_qr"), Buf("o_kr")

        def mla_chain(i):
            Qs, KVs, ib = Qs2[i % 2], KVs2[i % 2], in_b[i % 2]
            Q3 = Qs[:].rearrange("p (h d) -> p h d", h=4)
            KV3 = KVs[:].rearrange("p (h d) -> p h d", h=4)
            sq3q = sq[:, 0:768].rearrange("p (h d) -> p h d", h=4)
            sq3k = sqk_t[:].rearrange("p (h d) -> p h d", h=4)
            V = nc.vector
            S.op("dve", lambda: V.tensor_tensor(sq[:, 0:768], Qs[:], Qs[:], ALU.mult), [ib], [Bq])
            S.op("dve", lambda: V.tensor_tensor(sq3k, KV3[:, :, 0:128], KV3[:, :, 0:128], ALU.mult), [ib], [Bk])
            S.op("dve", lambda: V.tensor_tensor(sqr_t[:], krraw[:, i, :], krraw[:, i, :], ALU.mult), [kr_b], [Br])
            S.op("dve", lambda: V.tensor_reduce(ssn[:, 0:4], sq3q[:, :, 0:128], AX.X, ALU.add), [Bq], [Bs[0]])
            S.op("dve", lambda: V.tensor_reduce(ssn[:, 8:12], sq3k, AX.X, ALU.add), [Bk], [Bs[2]])
            S.op("dve", lambda: V.tensor_reduce(ssn[:, 12:13], sqr_t[:], AX.X, ALU.add), [Br], [Bs[3]])
            S.op("dve", lambda: V.tensor_reduce(ssn[:, 4:8], sq3q[:, :, 128:192], AX.X, ALU.add), [Bq], [Bs[1]])
            S.op("dve", lambda: V.tensor_tensor(ssn[:, 0:13], ssn[:, 0:13], invn[:, 0:13], ALU.mult), Bs + [dv], Bs)
            S.op("act", lambda: nc.scalar.activation(ssn[:, 0:13], ssn[:, 0:13], AF.Sqrt, bias=epsb[:], scale=1.0),
                 Bs + [cc], Bs)
            S.op("dve", lambda: V.reciprocal(ssn[:, 0:13], ssn[:, 0:13]), Bs, Bs)
            S.op("dve", lambda: V.tensor_tensor(sq3q[:, :, 0:128], Q3[:, :, 0:128], vap(ssn[:], [[1, 4], [0, 128]]), ALU.mult),
                 [ib] + Bs, [Bq])
            S.op("dve", lambda: V.tensor_tensor(sq3k, KV3[:, :, 0:128], vap(ssn[:], [[1, 4], [0, 128]], off=8), ALU.mult),
                 [ib] + Bs, [Bk])
            S.op("dve", lambda: V.tensor_tensor(qr_f[:], Q3[:, :, 128:192], vap(ssn[:], [[1, 4], [0, 64]], off=4), ALU.mult),
                 [ib] + Bs, [Bqr])
            S.op("dve", lambda: V.tensor_scalar(kr_f[:], krraw[:, i, :], ssn[:, 12:13], None, ALU.mult), [kr_b] + Bs, [Bkr])
            S.op("dve", lambda: V.tensor_tensor(qn_s[:], sq3q[:, :, 0:128], vap(g_q[:], [[0, 4], [1, 128]]), ALU.mult),
                 [Bq, cb], [o_qn])
            S.op("dve", lambda: V.tensor_tensor(kn_s[:], sq3k, vap(g_k[:], [[0, 4], [1, 128]]), ALU.mult), [Bk, cb], [o_kn])
            S.op("dve", lambda: V.tensor_tensor(qr_f[:], qr_f[:], vap(g_q[:], [[0, 4], [1, 64]], off=128), ALU.mult),
                 [Bqr, cb], [Bqr])
            S.op("dve", lambda: V.tensor_tensor(kr_f[:], kr_f[:], g_k[:, 128:192], ALU.mult), [Bkr, cb], [Bkr])
            cos4 = vap(cosT[:], [[0, 4], [1, 32]], off=i * 32)
            sin4 = vap(sinT[:], [[0, 4], [1, 32]], off=i * 32)
            c1 = cosT[:, i, :]
            s1 = sinT[:, i, :]
            S.op("dve", lambda: V.tensor_tensor(ra[:], qr_f[:, :, 0:32], cos4, ALU.mult), [Bqr, cc], [Bra])
            S.op("dve", lambda: V.tensor_tensor(rak[:], kr_f[:, 0:32], c1, ALU.mult), [Bkr, cc], [Brak])
            S.op("dve", lambda: V.tensor_tensor(rb_[:], qr_f[:, :, 32:64], sin4, ALU.mult), [Bqr, cc], [Brb])
            S.op("dve", lambda: V.tensor_tensor(rbk[:], kr_f[:, 32:64], s1, ALU.mult), [Bkr, cc], [Brbk])
            S.op("dve", lambda: V.tensor_sub(qr_s[:, :, 0:32], ra[:], rb_[:]), [Bra, Brb], [o_qr])
            S.op("dve", lambda: V.tensor_sub(kr_s[:, 0:32], rak[:], rbk[:]), [Brak, Brbk], [o_kr])
            S.op("dve", lambda: V.tensor_tensor(ra[:], qr_f[:, :, 32:64], cos4, ALU.mult), [Bqr, cc], [Bra])
            S.op("dve", lambda: V.tensor_tensor(rak[:], kr_f[:, 32:64], c1, ALU.mult), [Bkr, cc], [Brak])
            S.op("dve", lambda: V.tensor_tensor(rb_[:], qr_f[:, :, 0:32], sin4, ALU.mult), [Bqr, cc], [Brb])
            S.op("dve", lambda: V.tensor_tensor(rbk[:], kr_f[:, 0:32], s1, ALU.mult), [Bkr, cc], [Brbk])
            S.op("dve", lambda: V.tensor_add(qr_s[:, :, 32:64], ra[:], rb_[:]), [Bra, Brb], [o_qr])
            S.op("dve", lambda: V.tensor_add(kr_s[:, 32:64], rak[:], rbk[:]), [Brak, Brbk], [o_kr])
            S.op("pool", lambda: nc.gpsimd.tensor_copy(vaug[:, i, :, 0:128], KV3[:, :, 128:256]), [ib, vaug_b], [vaug_b])

        def mla_tail(i):
            for hh in range(4):
                S.op("pe", lambda: nc.tensor.transpose(psb[0][:, hh * P:(hh + 1) * P], qn_s[:, hh, :], ident[:]),
                     [o_qn, cc], [psb_b[0]])
                S.op("pe", lambda: nc.tensor.transpose(psb[0][:, (4 + hh) * P:(5 + hh) * P], kn_s[:, hh, :], ident[:]),
                     [o_kn, cc], [psb_b[0]])
                S.op("pe", lambda: nc.tensor.transpose(psb[1][0:64, hh * P:(hh + 1) * P], qr_s[:, hh, :], ident[:]),
                     [o_qr, cc], [psb_b[1]])
            S.op("pe", lambda: nc.tensor.transpose(psb[1][0:64, 4 * P:5 * P], kr_s[:], ident[:]), [o_kr, cc], [psb_b[1]])
            S.op("act", lambda: nc.scalar.copy(qnT[:, :, i * P:(i + 1) * P], vap(psb[0][:], [[P, 4], [1, P]])),
                 [psb_b[0]], [qk2_b])
            S.op("act", lambda: nc.scalar.copy(knT[:, :, i * P:(i + 1) * P], vap(psb[0][:], [[P, 4], [1, P]], off=4 * P)),
                 [psb_b[0]], [qk2_b])
            S.op("act", lambda: nc.scalar.copy(qrT[0:64, :, i * P:(i + 1) * P], vap(psb[1][0:64, :], [[P, 4], [1, P]])),
                 [psb_b[1]], [qk2_b])
            S.op("act", lambda: nc.scalar.copy(krT[0:64, i * P:(i + 1) * P], psb[1][0:64, 4 * P:5 * P]), [psb_b[1]], [qk2_b])

        mla_front(0)
        for i in range(NT):
            if i + 1 < NT:
                mla_front(i + 1)
            mla_chain(i)
            mla_tail(i)
        S.barrier()
    if upto == "C2":
        return nc
    pW = Scope(mem)
    wo = sb("wo", [P, 8, D], BF, pW)
    wo_b = Buf("wo")
    sgW = sb("sgW", [P, 8, 512], F32, pW)
    sgW_b = Buf("sgW")
    for half in range(2):
        S.dma("sp", sgW[:], wout_d[:, :, half * 512:(half + 1) * 512], sgW_b, writes=[sgW_b])
        for c in range(8):
            S.op("dve", lambda: nc.vector.tensor_copy(wo[:, c, half * 512:(half + 1) * 512], sgW[:, c, :]),
                 [sgW_b], [wo_b])
    with Scope(mem) as st:
        PT = [sb("PT%d" % i, [P, 512], BF, st) for i in range(2)]
        PT_b = [Buf("PT0"), Buf("PT1")]
        on4 = sb("on4", [P, 4, P], F32, st)
        onb4 = sb("onb4", [P, 4, P], BF, st)
        junk4 = sb("junk4", [P, 4, P], BF, st)
        rden = sb("rden", [P, 4], F32, st)
        ss4 = sb("ss4", [P, 4], F32, st)
        r_b = [Buf("rden%d" % q) for q in range(4)]
        on_b = [Buf("on%d" % q) for q in range(4)]
        j_b = [Buf("junk%d" % q) for q in range(4)]
        s_b = Buf("ss4")
        onb_b = [Buf("onb%d" % q) for q in range(4)]
        acc = (psf[2], psf[3], psf[4], psf[5])
        acc_b = (psf_b[2], psf_b[3], psf_b[4], psf_b[5])
        it = 0

        def tail_part1(hh, qb):
            for qt in range(4):
                S.op("dve", lambda: nc.vector.reciprocal(rden[:, qt:qt + 1], acc[qt][:, 128:129]), [acc_b[qt]], [r_b[qt]])
            for qt in range(4):
                S.op("act", lambda: nc.scalar.mul(on4[:, qt, :], acc[qt][:, 0:128], rden[:, qt:qt + 1]),
                     [acc_b[qt], r_b[qt]], [on_b[qt]])
            for qt in range(4):
                S.op("act", lambda: nc.scalar.activation(junk4[:, qt, :], on4[:, qt, :], AF.Square,
                                                         accum_out=ss4[:, qt:qt + 1]), [on_b[qt]], [j_b[qt], s_b])
            S.op("act", lambda: nc.scalar.activation(ss4[:], ss4[:], AF.Sqrt, bias=epsb[:], scale=1.0 / 128),
                 [s_b, cc], [s_b])
            S.op("dve", lambda: nc.vector.reciprocal(ss4[:], ss4[:]), [s_b], [s_b])
            for qt in range(4):
                S.op("dve", lambda: nc.vector.scalar_tensor_tensor(onb4[:, qt, :], on4[:, qt, :], ss4[:, qt:qt + 1],
                                                                    g_mo[:, hh * P:(hh + 1) * P], ALU.mult, ALU.mult),
                     [on_b[qt], s_b, cb], [onb_b[qt]])

        def tail_part2(hh, qb):
            for qt in range(4):
                S.op("pe", lambda: nc.tensor.transpose(psb[0][:, qt * P:(qt + 1) * P], onb4[:, qt, :], ident[:]),
                     [onb_b[qt], cc], [psb_b[0]])
            S.op("act", lambda: nc.scalar.copy(mixT[:, 4 + hh, qb * 512:(qb + 1) * 512], psb[0][:, 0:512]),
                 [psb_b[0]], [mix_b])

        blocks = [(hh, qb) for hh in range(4) for qb in range(4)]
        pending = None
        for (hh, qb) in blocks:
            def emit_S(kt, k):
                S.op("pe", lambda: nc.tensor.matmul(psf[k][:], knT[:, hh, kt * P:(kt + 1) * P],
                                                    qnT[:, hh, qb * 512:(qb + 1) * 512], start=True, stop=False,
                                                    skip_group_check=True), [qk2_b], [psf_b[k]])
                S.op("pe", lambda: nc.tensor.matmul(psf[k][:], krT[:, kt * P:(kt + 1) * P],
                                                    qrT[:, hh, qb * 512:(qb + 1) * 512], start=False, stop=True,
                                                    skip_group_check=True), [qk2_b], [psf_b[k]])

            emit_S(0, it % 2)
            for kt in range(NT):
                k = it % 2
                it += 1
                if kt + 1 < NT:
                    emit_S(kt + 1, it % 2)
                S.op("act", lambda: nc.scalar.activation(PT[k][:], psf[k][:], AF.Exp, scale=SCALE),
                     [psf_b[k]], [PT_b[k]])
                for qt in range(4):
                    a = acc[qt]
                    S.op("pe", lambda: nc.tensor.matmul(a[:, 0:129],
                                                        PT[k][:, qt * P:(qt + 1) * P], vaug[:, kt, hh, 0:129],
                                                        start=(kt == 0), stop=(kt == NT - 1), skip_group_check=True),
                         [PT_b[k], vaug_b], [acc_b[qt]])
                if kt == 2 and pending is not None:
                    tail_part2(*pending)
                    pending = None
            tail_part1(hh, qb)
            pending = (hh, qb)
        tail_part2(*pending)
        S.barrier()
    pc.close()
    pEarly.close()
    dump("mix_mla", mixT[:, 4:8, :], [mix_b])
    if upto == "C":
        return nc

    pD = Scope(mem)
    y_acc = sb("y_acc", [P, NT, D], F32, pD)
    y_b = [Buf("y%d" % i) for i in range(NT)]
    h2T = sb("h2T", [P, 8, T], BF, pD)
    h2T_b = Buf("h2T")
    with Scope(mem) as st:
        xt = [sb("xtD%d" % i, [P, D], F32, st) for i in range(2)]
        xt_b = [Buf("xtD0"), Buf("xtD1")]
        h2 = [sb("h2_%d" % i, [P, D], BF, st) for i in range(2)]
        h2_b = [Buf("h2_0"), Buf("h2_1")]
        junk = sb("junkD", [P, D], BF, st)
        junk_b = Buf("junkD")
        ssD = sb("ssD", [P, NT], F32, st)
        ssD_b = [Buf("ssD%d" % i) for i in range(NT)]
        def d_front(i):
            j = i % 2
            S.dma("sp", xt[j][:], x_d[i * P:(i + 1) * P, :], xt_b[j], writes=[xt_b[j]])
            for half in range(2):
                k = 2 * j + half
                for c in range(8):
                    S.op("pe", lambda: nc.tensor.matmul(psf[k][:], mixT[:, c, i * P:(i + 1) * P],
                                                        wo[:, c, half * 512:(half + 1) * 512], start=(c == 0), stop=(c == 7),
                                                        skip_group_check=True), [mix_b, wo_b], [psf_b[k]])
                S.op("dve", lambda: nc.vector.tensor_tensor(y_acc[:, i, half * 512:(half + 1) * 512], psf[k][:],
                                                            xt[j][:, half * 512:(half + 1) * 512], ALU.add),
                     [psf_b[k], xt_b[j]], [y_b[i]])
            S.op("act", lambda: nc.scalar.activation(junk[:], y_acc[:, i, :], AF.Square, accum_out=ssD[:, i:i + 1]),
                 [y_b[i]], [junk_b, ssD_b[i]])
            rstd_from_ss(ssD[:, i:i + 1], ssD[:, i:i + 1], D, [ssD_b[i]])
            S.op("dve", lambda: nc.vector.tensor_scalar(h2[j][:], y_acc[:, i, :], ssD[:, i:i + 1], None, ALU.mult),
                 [y_b[i], ssD_b[i]], [h2_b[j]])

        def d_tail(i):
            j = i % 2
            for c in range(8):
                S.op("pe", lambda: nc.tensor.transpose(psb[j][:, c * P:(c + 1) * P], h2[j][:, c * P:(c + 1) * P], ident[:]),
                     [h2_b[j], cc], [psb_b[j]])
            S.op("act", lambda: nc.scalar.copy(h2T[:, :, i * P:(i + 1) * P], vap(psb[j][:], [[P, 8], [1, P]])),
                 [psb_b[j]], [h2T_b])

        d_front(0)
        for i in range(NT):
            if i + 1 < NT:
                d_front(i + 1)
            d_tail(i)
        S.barrier()
    dump("x1", y_acc[:], y_b)
    pM.close()
    pW.close()
    if upto == "D":
        return nc

    U32 = mybir.dt.uint32
    pE = Scope(mem)
    iota16 = iota_f[:, 128:144]
    thr16 = iota_f[:, 144:160]
    with Scope(mem) as st:
        weff = sb("weff", [P, 8, 2048], BF, st)
        weff_b = Buf("weff")
        with Scope(mem) as st2:
            wqT = sb("wqT_s", [P, 8, 1024], BF, st2)
            kT = sb("kT_s", [P, 16, P], BF, st2)
            wq_b = Buf("wqT")
            sg = sb("sgE", [P, 4, 1024], F32, st2)
            sg_b = Buf("sgE")
            S.dma("sp", sg[:, 0:2, :].rearrange("p a b -> p (a b)"), keysT_d.rearrange("p a b -> p (a b)"), sg_b, writes=[sg_b])
            S.op("dve", lambda: nc.vector.tensor_copy(kT[:].rearrange("p a b -> p (a b)"),
                                                      sg[:, 0:2, :].rearrange("p a b -> p (a b)")), [sg_b], [wq_b])
            for hf in range(2):
                for q2 in range(2):
                    q4 = hf * 2 + q2
                    S.dma("sp", sg[:], wqT_d[:, q4 * 4:(q4 + 1) * 4, :], sg_b, writes=[sg_b])
                    S.op("dve", lambda: nc.vector.tensor_copy(wqT[:, q2 * 4:(q2 + 1) * 4, :], sg[:]), [sg_b], [wq_b])
                for c in range(8):
                    for q2 in range(2):
                        q4 = hf * 2 + q2
                        k = q4 % 2
                        for u in range(4):
                            pcx = q4 * 4 + u
                            S.op("pe", lambda: nc.tensor.matmul(psf[k][:, u * P:(u + 1) * P],
                                                                wqT[:, q2 * 4 + u, c * P:(c + 1) * P],
                                                                kT[:, pcx, :], start=True, stop=True, skip_group_check=True),
                                 [wq_b], [psf_b[k]])
                        S.op("act", lambda: nc.scalar.mul(weff[:, c, q4 * 512:(q4 + 1) * 512], psf[k][:],
                                                          g_ffn[:, c:c + 1]), [psf_b[k], cb], [weff_b])
            S.barrier()
        sci = sb("sc0", [P, 16, P], F32, st)
        scb = Buf("sc0")
        GI = 4
        NCAND = 112
        sc2 = [sb("sc2_%d" % j, [P, P], F32, st) for j in range(GI)]
        sc2_b = [Buf("sc2_%d" % j) for j in range(GI)]
        t1_b = [Buf("t1_%d" % j) for j in range(16)]
        i1_b = [Buf("i1_%d" % j) for j in range(16)]
        top = sb("top", [P, 16, 16], F32, st)
        idxu = sb("idxu", [P, 16, 16], U32, st)
        idxf = sb("idxf", [P, 16, 16], F32, st)
        cand = [sb("cand_%d" % j, [P, 256], F32, st) for j in range(GI)]
        cand2 = [sb("cand2_%d" % j, [P, 256], F32, st) for j in range(GI)]
        cd_b = [Buf("cd%d" % j) for j in range(GI)]
        cd2_b = [Buf("cd2_%d" % j) for j in range(GI)]
        sel_b = [Buf("sel%d" % j) for j in range(8)]
        pos_b = [Buf("pos%d" % j) for j in range(8)]
        idxf_b = Buf("idxf")
        posf_b = Buf("posf")
        af_b = Buf("af")
        bfb_b = Buf("bfb")
        g16_b = [Buf("g16a"), Buf("g16b")]
        t16_b = [Buf("t16a"), Buf("t16b")]
        abf_b = [Buf("abf0"), Buf("abf1"), Buf("abf2")]
        es_b = Buf("esel")
        zs_b = Buf("zs")
        sel = sb("sel", [P, 8, 16], F32, st)
        posu = sb("posu", [P, 8, 16], U32, st)
        posf = sb("posf2", [P, P], F32, st)
        ge16 = sb("ge16", [P, P, 16], BF, st)
        af = sb("af", [P, P], F32, st)
        bf_ = sb("bf_", [P, P], F32, st)
        esel = sb("esel", [P, 8, 16], F32, st)
        zs = sb("zs", [P, 8], F32, st)
        ab = sb("ab", [P, 3, P], BF, st)
        abf = sb("abf", [P, 3, P], F32, st)
        abT2 = sb("abT2", [P, 2, 3, P], BF, st)
        abT2_b = [Buf("abT2_0"), Buf("abT2_1")]
        tk = Buf("topk")
        ab_b = Buf("ab")
        SUB = 8
        WT = sb("WT", [P, P, P], BF, st)
        WT_b = Buf("WT")
        NAB = 3
        A12 = [sb("A12_%d" % i, [P, 2, SUB, P], BF, st) for i in range(NAB)]
        A12_b = [Buf("A12_%d" % i) for i in range(NAB)]
        wd_b = [Buf("Wd%d" % i) for i in range(NT)]
        cnt = {"it": 0, "bk": 0}

        def e1_front(i):
            for q4 in range(4):
                k = q4
                for c in range(8):
                    S.op("pe", lambda: nc.tensor.matmul(psf[k][:], h2T[:, c, i * P:(i + 1) * P],
                                                        weff[:, c, q4 * 512:(q4 + 1) * 512], start=(c == 0), stop=(c == 7),
                                                        skip_group_check=True), [h2T_b, weff_b], [psf_b[k]])
                S.op("act", lambda: nc.scalar.copy(sci[:, q4 * 4:(q4 + 1) * 4, :].rearrange("p a b -> p (a b)"), psf[k][:]),
                     [psf_b[k]], [scb])


        def e1_chain(i):
            for g2_ in range(16 // GI):
                pcs = tuple(GI * g2_ + q_ for q_ in range(GI))
                for pcx in pcs:
                    S.op("dve", lambda: nc.vector.max(out=top[:, pcx, 0:8], in_=sci[:, pcx, :]), [scb], [t1_b[pcx]])
                for pcx in pcs:
                    S.op("dve", lambda: nc.vector.max_index(out=idxu[:, pcx, 0:8], in_max=top[:, pcx, 0:8],
                                                            in_values=sci[:, pcx, :]), [scb, t1_b[pcx]], [i1_b[pcx]])
                for pcx in pcs:
                    j = pcx % GI
                    S.op("dve", lambda: nc.vector.match_replace(out=sc2[j][:], in_to_replace=top[:, pcx, 0:8],
                                                                in_values=sci[:, pcx, :], imm_value=-1e30),
                         [scb, t1_b[pcx]], [sc2_b[j]])
                for pcx in pcs:
                    j = pcx % GI
                    S.op("dve", lambda: nc.vector.max(out=top[:, pcx, 8:16], in_=sc2[j][:]), [sc2_b[j]], [t1_b[pcx]])
                for pcx in pcs:
                    j = pcx % GI
                    S.op("dve", lambda: nc.vector.max_index(out=idxu[:, pcx, 8:16], in_max=top[:, pcx, 8:16],
                                                            in_values=sc2[j][:]), [sc2_b[j], t1_b[pcx]], [i1_b[pcx]])
                for _y in range(GI):
                    yield
            for h2_ in range(8 // GI):
                ps_ = tuple(GI * h2_ + q_ for q_ in range(GI))
                for p_ in ps_:
                    j = p_ % GI
                    S.op("dve", lambda: nc.vector.tensor_tensor(
                        cand[j][:, 0:64].rearrange("p (a b) -> p a b", a=4),
                        vap(top[:], [[1, 4], [0, 16]], off=32 * p_), vap(top[:], [[0, 4], [1, 16]], off=32 * p_ + 16), ALU.add),
                        [t1_b[2 * p_], t1_b[2 * p_ + 1]], [cd_b[j]])
                for p_ in ps_:
                    j = p_ % GI
                    S.op("dve", lambda: nc.vector.tensor_tensor(
                        cand[j][:, 64:NCAND].rearrange("p (a b) -> p a b", a=12),
                        vap(top[:], [[1, 12], [0, 4]], off=32 * p_ + 4), vap(top[:], [[0, 12], [1, 4]], off=32 * p_ + 16), ALU.add),
                        [t1_b[2 * p_], t1_b[2 * p_ + 1], cd_b[j]], [cd_b[j]])
                for p_ in ps_:
                    j = p_ % GI
                    S.op("dve", lambda: nc.vector.max(out=sel[:, p_, 0:8], in_=cand[j][:, 0:NCAND]), [cd_b[j]], [sel_b[p_]])
                for p_ in ps_:
                    j = p_ % GI
                    S.op("dve", lambda: nc.vector.max_index(out=posu[:, p_, 0:8], in_max=sel[:, p_, 0:8],
                                                            in_values=cand[j][:, 0:NCAND]), [cd_b[j], sel_b[p_]], [pos_b[p_]])
                for p_ in ps_:
                    j = p_ % GI
                    S.op("dve", lambda: nc.vector.match_replace(out=cand2[j][:, 0:NCAND], in_to_replace=sel[:, p_, 0:8],
                                                                in_values=cand[j][:, 0:NCAND], imm_value=-1e30),
                         [cd_b[j], sel_b[p_]], [cd2_b[j]])
                for p_ in ps_:
                    j = p_ % GI
                    S.op("dve", lambda: nc.vector.max(out=sel[:, p_, 8:16], in_=cand2[j][:, 0:NCAND]), [cd2_b[j]], [sel_b[p_]])
                for p_ in ps_:
                    j = p_ % GI
                    S.op("dve", lambda: nc.vector.max_index(out=posu[:, p_, 8:16], in_max=sel[:, p_, 8:16],
                                                            in_values=cand2[j][:, 0:NCAND]), [cd2_b[j], sel_b[p_]], [pos_b[p_]])
                for _y in range(GI):
                    yield
            S.op("dve", lambda: nc.vector.tensor_copy(posf[:], posu[:].rearrange("p a b -> p (a b)")), pos_b, [posf_b])
            S.op("dve", lambda: nc.vector.tensor_tensor(esel[:], sel[:], vap(sel[:], [[16, 8], [0, 16]]), ALU.subtract),
                 sel_b, [es_b])
            S.op("dve", lambda: nc.vector.tensor_copy(idxf[:], idxu[:]), i1_b, [idxf_b])
            S.op("act", lambda: nc.scalar.activation(esel[:], esel[:], AF.Exp), [es_b], [es_b])
            gA = ge16[:].rearrange("p j a -> p (j a)")
            S.op("dve", lambda: nc.vector.tensor_tensor(ge16[:], vap(posf[:], [[1, P], [0, 16]]),
                                                        vap(thr16, [[0, P], [1, 16]]), ALU.is_ge),
                 [posf_b, cb] + g16_b, g16_b)
            S.op("dve", lambda: nc.vector.tensor_reduce(zs[:], esel[:], AX.X, ALU.add), [es_b], [zs_b])
            S.op("dve", lambda: nc.vector.tensor_reduce(af[:], ge16[:], AX.X, ALU.add), g16_b, [af_b])
            S.op("dve", lambda: nc.vector.reciprocal(zs[:], zs[:]), [zs_b], [zs_b])
            S.op("dve", lambda: nc.vector.tensor_scalar(af[:], af[:], -1.0, None, ALU.add), [af_b], [af_b])
            S.op("dve", lambda: nc.vector.tensor_tensor(abf[:, 2, :].rearrange("p (h k) -> p h k", h=8), esel[:],
                                                        vap(zs[:], [[1, 8], [0, 16]]), ALU.mult), [es_b, zs_b], [abf_b[2]])
            S.op("dve", lambda: nc.vector.scalar_tensor_tensor(bf_[:], af[:], -16.0, posf[:], ALU.mult, ALU.add),
                 [af_b, posf_b], [bfb_b])
            S.op("dve", lambda: nc.vector.tensor_scalar(posf[:], af[:], -4.0, 0.0, ALU.add, ALU.max), [af_b, posf_b, bfb_b], [posf_b])
            S.op("dve", lambda: nc.vector.scalar_tensor_tensor(bf_[:], posf[:], 12.0, bf_[:], ALU.mult, ALU.add),
                 [posf_b, bfb_b], [bfb_b])
            yield
            H_ = P // 2
            for which, src, srcb, o_ in ((0, af, af_b, 0), (1, bf_, bfb_b, 16)):
                for hv in range(2):
                    S.op("dve", lambda: nc.vector.tensor_tensor(
                        ge16[:, hv * H_:(hv + 1) * H_, :], vap(src[:, hv * H_:(hv + 1) * H_], [[1, H_], [0, 16]]),
                        vap(iota16, [[0, H_], [1, 16]]), ALU.is_equal), [srcb, cb, g16_b[hv]], [g16_b[hv]])
                for hv in range(2):
                    S.op("dve", lambda: nc.vector.tensor_tensor(
                        ge16[:, hv * H_:(hv + 1) * H_, :].rearrange("p (h k) a -> p h k a", h=4),
                        ge16[:, hv * H_:(hv + 1) * H_, :].rearrange("p (h k) a -> p h k a", h=4),
                        vap(idxf[:], [[32, 4], [0, 16], [1, 16]], off=o_ + hv * 128), ALU.mult),
                        [g16_b[hv], idxf_b], [g16_b[hv]])
                for hv in range(2):
                    S.op("dve", lambda: nc.vector.tensor_reduce(abf[:, which, hv * H_:(hv + 1) * H_],
                                                                ge16[:, hv * H_:(hv + 1) * H_, :], AX.X, ALU.add),
                         [g16_b[hv]], [abf_b[which]])
                yield
            S.op("dve", lambda: nc.vector.tensor_copy(ab[:], abf[:]), abf_b + [ab_b], [ab_b])

        def e1_tail(i):
            kb = i % 2
            for j in range(3):
                S.op("pe", lambda: nc.tensor.transpose(psb[kb][:, j * P:(j + 1) * P], ab[:, j, :], ident[:]),
                     [ab_b, cc], [psb_b[kb]])
            S.op("act", lambda: nc.scalar.copy(abT2[:, kb, :, :].rearrange("p a b -> p (a b)"), psb[kb][:, 0:3 * P]),
                 [psb_b[kb]], [abT2_b[kb]])

        def e2(i):
            kb = i % 2
            for sub in range(P // SUB):
                s_ = cnt["it"] % NAB
                cnt["it"] += 1
                t0 = sub * SUB
                S.op("dve", lambda: nc.vector.tensor_tensor(
                    A12[s_][:], vap(iota128[:], [[0, 2], [0, SUB], [1, P]]),
                    vap(abT2[:, kb, 0, t0:t0 + SUB], [[P, 2], [1, SUB], [0, P]]), ALU.is_equal),
                    [abT2_b[kb], cc], [A12_b[s_]])
                S.op("pool", lambda: nc.gpsimd.tensor_tensor(A12[s_][:, 0, :, :], A12[s_][:, 0, :, :],
                                                             vap(abT2[:, kb, 2, t0:t0 + SUB], [[1, SUB], [0, P]]),
                                                             ALU.mult), [abT2_b[kb], A12_b[s_]], [A12_b[s_]])
                for t8 in range(SUB // 8):
                    k0 = (cnt["bk"] % 3) * 2
                    cnt["bk"] += 1
                    for u8 in range(8):
                        tt = t8 * 8 + u8
                        kk_ = k0 + u8 // 4
                        S.op("pe", lambda: nc.tensor.matmul(vap(psf[kk_], [[4, P]], off=u8 % 4), A12[s_][:, 1, tt, :], A12[s_][:, 0, tt, :],
                                                            start=True, stop=True, skip_group_check=True),
                             [A12_b[s_]], [psf_b[kk_]])
                    tok = t0 + t8 * 8
                    S.op("act", lambda: nc.scalar.copy(vap(WT[:], [[P, P], [4, 2], [1, 4]], off=tok),
                                                       vap(psf[k0], [[4, P], [512, 2], [1, 4]])),
                         [psf_b[k0], psf_b[k0 + 1]], [WT_b])
                yield
            S.dma("sp", Wd[i], WT[:].rearrange("p a b -> p (a b)"), WT_b, reads=[WT_b], writes=[wd_b[i]])

        e1_front(0)
        for i in range(NT):
            g2 = e2(i - 1) if i >= 1 else iter(())
            kk2 = 0
            for _ in e1_chain(i):
                kk2 += 1
                if kk2 == 16 and i + 1 < NT:
                    e1_front(i + 1)
                if (kk2 * 16) // 27 > ((kk2 - 1) * 16) // 27:
                    next(g2, None)
            for _ in g2:
                pass
            e1_tail(i)
        for _ in e2(NT - 1):
            pass
        S.barrier()
    pE.close()
    if upto == "E":
        return nc

    NB = EG // P
    with Scope(mem) as st:
        ustg = sb("ustg0", [P, 8, EG], F32, st)
        vstg = sb("vstg0", [P, NB, D], F32, st)
        ustg_b, vstg_b = Buf("ustg0"), Buf("vstg0")
        ubf = [sb("ubf%d" % i, [P, 8, EG], BF, st) for i in range(2)]
        vbf = [sb("vbf%d" % i, [P, NB, D], BF, st) for i in range(2)]
        ubf_b = [Buf("ubf0"), Buf("ubf1")]
        vbf_b = [Buf("vbf0"), Buf("vbf1")]
        WTg = [sb("WTg%d" % i, [P, 4, NB * P], BF, st) for i in range(3)]
        WTg_b = [Buf("WTg%d" % i) for i in range(3)]
        ge = [sb("ge%d" % i, [P, 512], BF, st) for i in range(2)]
        ge_b = [Buf("ge0"), Buf("ge1")]
        GT = [sb("GT%d" % i, [P, NB, 512], BF, st) for i in range(2)]
        GT_b = [Buf("GT0"), Buf("GT1")]

        def load_group(g):
            S.dma("sp", ustg[:], UT_d[:, :, g * EG:(g + 1) * EG], ustg_b, writes=[ustg_b])
            S.dma("sp", vstg[:], V_d[g * EG:(g + 1) * EG, :].rearrange("(b p) d -> p b d", p=P), vstg_b,
                  writes=[vstg_b])

        def cast_group(g):
            s_ = g % 2
            S.op("pool", lambda: nc.gpsimd.tensor_tensor(ubf[s_][:], ustg[:], vap(g_ffn[:], [[1, 8], [0, EG]]), ALU.mult),
                 [ustg_b, cb], [ubf_b[s_]])
            S.op("pool", lambda: nc.gpsimd.tensor_copy(vbf[s_][:], vstg[:]), [vstg_b], [vbf_b[s_]])

        seq = [(g, q) for g in range(NG) for q in range(4)]
        NTOT = len(seq)

        def load_w(n):
            g, q = seq[n]
            S.dma("sp", WTg[n % 3][:], Wd[4 * q:4 * q + 4, :, g * EG:(g + 1) * EG].rearrange("a p f -> p a f"),
                  WTg_b[n % 3], reads=wd_b[4 * q:4 * q + 4], writes=[WTg_b[n % 3]])

        abank = [0]

        def st_AG(n):
            g, q = seq[n]
            s_, gt = g % 2, n % 2
            for b_ in range(NB):
                pa = abank[0] % 2
                abank[0] += 1
                for c in range(8):
                    S.op("pe", lambda: nc.tensor.matmul(psf[pa][:], ubf[s_][:, c, b_ * P:(b_ + 1) * P],
                                                        h2T[:, c, q * 512:(q + 1) * 512], start=(c == 0), stop=(c == 7),
                                                        skip_group_check=True), [h2T_b, ubf_b[s_]], [psf_b[pa]])
                S.op("act", lambda: nc.scalar.activation(ge[pa][:], psf[pa][:], AF.Gelu), [psf_b[pa]], [ge_b[pa]])
                S.op("dve", lambda: nc.vector.tensor_tensor(
                    GT[gt][:, b_, :].rearrange("p (a t) -> p a t", a=4), ge[pa][:].rearrange("p (a t) -> p a t", a=4),
                    vap(WTg[n % 3][:], [[NB * P, 4], [1, P]], off=b_ * P), ALU.mult),
                    [ge_b[pa], WTg_b[n % 3]], [GT_b[gt]])

        ybank = [0]

        def st_Y(n):
            g, q = seq[n]
            s_, gt = g % 2, n % 2
            for u in range(4):
                i = 4 * q + u
                for half in range(2):
                    py = 2 + ybank[0] % 4
                    ybank[0] += 1
                    for b_ in range(NB):
                        S.op("pe", lambda: nc.tensor.matmul(psf[py][:], GT[gt][:, b_, u * P:(u + 1) * P],
                                                            vbf[s_][:, b_, half * 512:(half + 1) * 512], start=(b_ == 0),
                                                            stop=(b_ == NB - 1), skip_group_check=True),
                             [GT_b[gt], vbf_b[s_]], [psf_b[py]])
                    S.op("dve", lambda: nc.vector.tensor_tensor(y_acc[:, i, half * 512:(half + 1) * 512],
                                                                psf[py][:], y_acc[:, i, half * 512:(half + 1) * 512],
                                                                ALU.add), [psf_b[py], y_b[i]], [y_b[i]])

        load_group(0)
        cast_group(0)
        if NG > 1:
            load_group(1)
        load_w(0)
        load_w(1)
        for n in range(NTOT):
            g, q = seq[n]
            if n + 2 < NTOT:
                load_w(n + 2)
            if q == 2 and g + 1 < NG:
                cast_group(g + 1)
                if g + 2 < NG:
                    load_group(g + 2)
            st_AG(n)
            if n >= 1:
                st_Y(n - 1)
        st_Y(NTOT - 1)
        ob = Buf("outst")
        for i in range(NT):
            S.dma("sp", out_d[i * P:(i + 1) * P, :], y_acc[:, i, :], ob, reads=[y_b[i]])
        S.barrier()
    pD.close()
    es.close()
    return nc


_HOST_CACHE = {}


def _prep_shared(inp):
    f = np.float32
    sh = {}
    sh["invf"] = np.ascontiguousarray(np.broadcast_to(
        (1.0 / (10000.0 ** (np.arange(0, 64, 2, dtype=f) / f(64)))).astype(f)[None, :], (P, 32)))
    sh["ident"] = np.eye(P, dtype=f)
    s = np.arange(P)[:, None]
    t = np.arange(P)[None, :]
    same = (s // 64) == (t // 64)
    sh["maskf"] = (same & (s <= t)).astype(f)
    sh["maskb"] = (same & (s >= t)).astype(f)
    rm = np.ones((P, T), f)
    rm[:, ::64] = 0.0
    sh["resetm"] = rm
    io = np.zeros((P, 160), f)
    io[:, 0:128] = np.arange(128, dtype=f)[None, :]
    io[:, 128:144] = np.arange(16, dtype=f)[None, :]
    io[:, 144:160] = np.array([0, 16, 32, 48, 64, 68, 72, 76, 80, 84, 88, 92, 96, 100, 104, 108], f)[None, :]
    sh["iota"] = io

    def pc(v):
        return np.ascontiguousarray(np.asarray(v, f).reshape(-1, P).T)

    def rep(v):
        v = np.asarray(v, f).reshape(1, -1)
        return np.ascontiguousarray(np.broadcast_to(v, (P, v.shape[1])))

    def kc(w):
        w = np.asarray(w, f)
        return np.ascontiguousarray(w.reshape(-1, P, w.shape[1]).transpose(1, 0, 2))

    sh["g_attn"] = pc(inp["attn_norm"][0])
    sh["g_ffn"] = pc(inp["ffn_norm"][0])
    lbl = np.asarray(inp["hg_lb_logits"], f)
    sh["lbl"] = np.ascontiguousarray(lbl.reshape(2, 2, 4, P).transpose(3, 0, 1, 2).reshape(P, 16))
    sh["g_hgo"] = np.ascontiguousarray(np.asarray(inp["hg_o_norm"][0], f).T)
    sh["g_qa"] = pc(inp["q_a_norm"][0])
    sh["g_kva"] = pc(inp["kv_a_norm"][0])
    sh["g_q"] = rep(inp["q_norm"][0])
    sh["g_k"] = rep(inp["k_norm"][0])
    sh["g_mo"] = rep(inp["mla_o_norm"][0])
    sh["w_in"] = kc(inp["w_in"][0])
    sh["w_qup"] = kc(inp["w_q_up"][0])
    sh["w_kvup"] = kc(inp["w_kv_up"][0])
    sh["w_out"] = kc(inp["w_out"][0])
    wq = np.asarray(inp["peer_w_q"][0], f)
    sh["wqT"] = np.ascontiguousarray(wq.reshape(D, 16, P).transpose(2, 1, 0))
    keys = np.asarray(inp["peer_sub_keys"][0], f)
    sh["keysT"] = np.ascontiguousarray(keys.reshape(16, P, P).transpose(2, 0, 1))
    u = np.asarray(inp["peer_u"][0], f)
    sh["UT"] = np.ascontiguousarray(u.reshape(NEXP, 8, P).transpose(2, 1, 0))
    sh["V"] = np.ascontiguousarray(np.asarray(inp["peer_v"][0], f))
    return sh


def make_in_maps(inputs, cores):
    sh = _prep_shared(inputs)
    x = np.asarray(inputs["x"], np.float32)
    pos = np.asarray(inputs["positions"], np.int32)
    maps = []
    for b in cores:
        m = dict(sh)
        m["x"] = np.ascontiguousarray(x[b])
        m["posT"] = np.ascontiguousarray(pos[b].reshape(NT, P).T)
        maps.append(m)
    return maps


def kernel(**inputs):
    nc = build_program()
    in_maps = make_in_maps(inputs, list(range(8)))
    res = run_bass_kernel_spmd(nc, in_maps, core_ids=list(range(8)))
    out = np.stack([np.asarray(r["out"], np.float32) for r in res.results], axis=0)
    return out
```
